# Optimizing a Trainium2 kernel written in Bass

```python
import jax
import jax.numpy as jnp
from jax import lax
import numpy as np


D_MODEL = 2048
BATCH = 4
SEQ = 4096
DEPTH = 1

EPS = 1e-6
ROPE_THETA = 500000.0
HEAD_DIM = 128
ROPE_DIM = HEAD_DIM // 4

GDN_HEADS = 8
GDN_DK = HEAD_DIM
GDN_DV = HEAD_DIM
GDN_CONV = 4
GDN_CHUNK = 64

NSA_HEADS = 16
NSA_GROUPS = 2
NSA_HPG = NSA_HEADS // NSA_GROUPS
NSA_DK = HEAD_DIM
CMP_LEN = 32
CMP_STRIDE = 16
CMP_HIDDEN = 256
SLC_LEN = 64
SLC_TOPK = 16
WIN = 512
NSA_QBLOCK = 64

PEER_HEADS = 8
PEER_NKEYS = 128
PEER_EXPERTS = PEER_NKEYS * PEER_NKEYS
PEER_QDIM = 256
PEER_TOPK = 16
PEER_TOKBLOCK = 128

GDN_QK_W = GDN_HEADS * GDN_DK
GDN_V_W = GDN_HEADS * GDN_DV
NSA_Q_W = NSA_HEADS * NSA_DK
NSA_KV_W = NSA_GROUPS * NSA_DK
IN_WIDTHS = (GDN_QK_W, GDN_QK_W, GDN_V_W, GDN_V_W, GDN_HEADS, GDN_HEADS,
             NSA_Q_W, NSA_KV_W, NSA_KV_W, NSA_KV_W, NSA_KV_W, NSA_KV_W, NSA_KV_W, 3 * NSA_HEADS,
             D_MODEL, D_MODEL)
D_IN = 2 * GDN_QK_W + 2 * GDN_V_W + 2 * GDN_HEADS + NSA_Q_W + 6 * NSA_KV_W + 3 * NSA_HEADS + 2 * D_MODEL

NEG = -1e30
BIG = 1e9

kernel_name = 'hybrid_gdn_nsa_peer_block'


def _rmsnorm(x, w):
    xf = x.astype(jnp.float32)
    y = xf * lax.rsqrt(jnp.mean(xf * xf, axis=-1, keepdims=True) + EPS)
    return (y * w.astype(jnp.float32)).astype(x.dtype)


def _l2norm(x):
    return x * lax.rsqrt(jnp.sum(x * x, axis=-1, keepdims=True) + EPS)


def _partial_rope(x, positions):
    half = ROPE_DIM // 2
    inv_freq = ROPE_THETA ** (-jnp.arange(half, dtype=jnp.float32) / half)
    ang = positions.astype(jnp.float32)[:, :, None] * inv_freq
    cos = jnp.cos(ang)[:, :, None, :]
    sin = jnp.sin(ang)[:, :, None, :]
    x1 = x[..., :half].astype(jnp.float32)
    x2 = x[..., half:ROPE_DIM].astype(jnp.float32)
    rot = jnp.concatenate([x1 * cos - x2 * sin, x2 * cos + x1 * sin], axis=-1).astype(x.dtype)
    return jnp.concatenate([rot, x[..., ROPE_DIM:]], axis=-1)


def _causal_conv(x, w):
    C = x.shape[-1]
    return lax.conv_general_dilated(x, w[:, None, :].astype(x.dtype), window_strides=(1,),
                                    padding=[(GDN_CONV - 1, 0)],
                                    dimension_numbers=('NWC', 'WIO', 'NWC'),
                                    feature_group_count=C)


def _gated_deltanet(q, k, v, z, a, b, conv_w, A_log, dt_bias, norm_w):
    f32 = jnp.float32
    Bn, S, _ = q.shape
    H, dk, dv, C = GDN_HEADS, GDN_DK, GDN_DV, GDN_CHUNK
    N = S // C
    qkv = jax.nn.silu(_causal_conv(jnp.concatenate([q, k, v], axis=-1), conv_w))
    q, k, v = jnp.split(qkv, [GDN_QK_W, 2 * GDN_QK_W], axis=-1)
    q = _l2norm(q.reshape(Bn, S, H, dk).astype(f32)) * (dk ** -0.5)
    k = _l2norm(k.reshape(Bn, S, H, dk).astype(f32))
    v = v.reshape(Bn, S, H, dv).astype(f32)
    beta = jax.nn.sigmoid(b.astype(f32))
    g = -jnp.exp(A_log.astype(f32)) * jax.nn.softplus(a.astype(f32) + dt_bias.astype(f32))

    def chunks(t):
        t = t.reshape((Bn, N, C, H) + t.shape[3:])
        return jnp.swapaxes(jnp.swapaxes(t, 0, 1), 2, 3)

    qc, kc, vc = chunks(q), chunks(k), chunks(v)
    bc = chunks(beta)
    Gc = jnp.cumsum(chunks(g), axis=-1)
    ids = jnp.arange(C)
    incl = ids[:, None] >= ids[None, :]
    strict = ids[:, None] > ids[None, :]
    decay = jnp.where(incl, jnp.exp(jnp.where(incl, Gc[..., :, None] - Gc[..., None, :], 0.0)), 0.0)
    gamma = jnp.exp(Gc)
    L = jnp.where(strict, decay * jnp.einsum('nbhid,nbhjd->nbhij', kc, kc), 0.0) * bc[..., :, None]
    A = L + jnp.eye(C, dtype=f32)
    W = lax.linalg.triangular_solve(A, (bc * gamma)[..., None] * kc, left_side=True, lower=True, unit_diagonal=True)
    U0 = lax.linalg.triangular_solve(A, bc[..., None] * vc, left_side=True, lower=True, unit_diagonal=True)
    QK = decay * jnp.einsum('nbhid,nbhjd->nbhij', qc, kc)
    Qg = gamma[..., None] * qc
    Kd = jnp.exp(Gc[..., -1:] - Gc)[..., None] * kc
    gend = gamma[..., -1]

    def step(state, inp):
        W_, U0_, QK_, Qg_, Kd_, ge_ = inp
        U = U0_ - jnp.einsum('bhck,bhkv->bhcv', W_, state)
        O = jnp.einsum('bhck,bhkv->bhcv', Qg_, state) + jnp.einsum('bhij,bhjv->bhiv', QK_, U)
        state = ge_[..., None, None] * state + jnp.einsum('bhck,bhcv->bhkv', Kd_, U)
        return state, O

    s0 = jnp.zeros((Bn, H, dk, dv), f32)
    _, O = lax.scan(step, s0, (W, U0, QK, Qg, Kd, gend))
    o = jnp.swapaxes(jnp.swapaxes(O, 2, 3), 0, 1).reshape(Bn, S, H, dv)
    o = _rmsnorm(o, norm_w) * jax.nn.silu(z.reshape(Bn, S, H, dv).astype(f32))
    return o.reshape(Bn, S, H * dv).astype(z.dtype)


def _compress(t, pos, w1, w2):
    Bn, S = t.shape[:2]
    nc = (S - CMP_LEN) // CMP_STRIDE + 1
    idx = np.arange(nc)[:, None] * CMP_STRIDE + np.arange(CMP_LEN)[None, :]
    blocks = t[:, idx] + pos[None, None, :, None, :]
    flat = jnp.swapaxes(blocks, 2, 3).reshape(Bn, nc, NSA_GROUPS, CMP_LEN * NSA_DK)
    return jax.nn.gelu(flat @ w1) @ w2


def _nsa(q, k_c, v_c, k_s, v_s, k_w, v_w, gates, positions,
         cmp_pos_k, cmp_w1_k, cmp_w2_k, cmp_pos_v, cmp_w1_v, cmp_w2_v):
    f32 = jnp.float32
    Bn, S, _ = q.shape
    G, HPG, d = NSA_GROUPS, NSA_HPG, NSA_DK
    q = _partial_rope(q.reshape(Bn, S, NSA_HEADS, d), positions) * (d ** -0.5)
    q = q.reshape(Bn, S, G, HPG, d)
    k_c = _partial_rope(k_c.reshape(Bn, S, G, d), positions)
    k_s = _partial_rope(k_s.reshape(Bn, S, G, d), positions)
    k_w = _partial_rope(k_w.reshape(Bn, S, G, d), positions)
    v_c = v_c.reshape(Bn, S, G, d)
    v_s = v_s.reshape(Bn, S, G, d)
    v_w = v_w.reshape(Bn, S, G, d)
    gates = jax.nn.sigmoid(gates.astype(f32)).reshape(Bn, S, G, HPG, 3)

    nc = (S - CMP_LEN) // CMP_STRIDE + 1
    k_cmp = _compress(k_c, cmp_pos_k, cmp_w1_k, cmp_w2_k)
    v_cmp = _compress(v_c, cmp_pos_v, cmp_w1_v, cmp_w2_v)
    cmp_start = np.arange(nc) * CMP_STRIDE
    cmp_end = jnp.asarray(cmp_start + CMP_LEN - 1)
    nsb = S // SLC_LEN
    slc_start_np = np.arange(nsb) * SLC_LEN
    overlap = jnp.asarray(((cmp_start[:, None] < slc_start_np[None, :] + SLC_LEN)
                           & (cmp_start[:, None] + CMP_LEN > slc_start_np[None, :])).astype(np.float32))
    slc_start = jnp.asarray(slc_start_np)
    n_sel = min(SLC_TOPK, nsb)
    ks_blocks = jnp.transpose(k_s.reshape(Bn, nsb, SLC_LEN, G, d), (0, 3, 1, 2, 4))
    vs_blocks = jnp.transpose(v_s.reshape(Bn, nsb, SLC_LEN, G, d), (0, 3, 1, 2, 4))
    gather = jax.vmap(jax.vmap(lambda blk, idx: blk[idx]))
    k_w_pad = jnp.pad(k_w, ((0, 0), (WIN, 0), (0, 0), (0, 0)))
    v_w_pad = jnp.pad(v_w, ((0, 0), (WIN, 0), (0, 0), (0, 0)))
    blk_ids = jnp.arange(nsb)
    offs = jnp.arange(SLC_LEN)

    def q_block(i):
        s0 = i * NSA_QBLOCK
        qi = lax.dynamic_slice_in_dim(q, s0, NSA_QBLOCK, axis=1)
        gi = lax.dynamic_slice_in_dim(gates, s0, NSA_QBLOCK, axis=1)
        tq = s0 + jnp.arange(NSA_QBLOCK)
        sc = jnp.einsum('bqghd,bngd->bghqn', qi, k_cmp).astype(f32)
        valid_c = cmp_end[None, :] <= tq[:, None]
        p_c = jax.nn.softmax(jnp.where(valid_c, sc, NEG), axis=-1) * valid_c
        o_c = jnp.einsum('bghqn,bngd->bqghd', p_c.astype(v_cmp.dtype), v_cmp)
        imp = jnp.einsum('bghqn,ns->bgqs', p_c, overlap)
        cur = tq // SLC_LEN
        forced = (blk_ids[None, :] == 0) | (blk_ids[None, :] == cur[:, None]) | (blk_ids[None, :] == cur[:, None] - 1)
        imp = jnp.where(forced, BIG, imp)
        imp = jnp.where(slc_start[None, :] <= tq[:, None], imp, NEG)
        _, sel = lax.top_k(imp, n_sel)
        k_sel = gather(ks_blocks, sel)
        v_sel = gather(vs_blocks, sel)
        key_pos = sel[..., None] * SLC_LEN + offs
        mask_s = (key_pos <= tq[:, None, None])[:, :, None]
        ss = jnp.einsum('bqghd,bgqnld->bghqnl', qi, k_sel).astype(f32)
        ss = jnp.where(mask_s, ss, NEG)
        p_s = jax.nn.softmax(ss.reshape(ss.shape[:4] + (-1,)), axis=-1).reshape(ss.shape)
        o_s = jnp.einsum('bghqnl,bgqnld->bqghd', p_s.astype(v_sel.dtype), v_sel)
        kwi = lax.dynamic_slice_in_dim(k_w_pad, s0, NSA_QBLOCK + WIN, axis=1)
        vwi = lax.dynamic_slice_in_dim(v_w_pad, s0, NSA_QBLOCK + WIN, axis=1)
        kpos = s0 - WIN + jnp.arange(NSA_QBLOCK + WIN)
        mask_w = (kpos[None, :] <= tq[:, None]) & (kpos[None, :] > tq[:, None] - WIN) & (kpos[None, :] >= 0)
        sw = jnp.einsum('bqghd,bkgd->bghqk', qi, kwi).astype(f32)
        p_w = jax.nn.softmax(jnp.where(mask_w, sw, NEG), axis=-1)
        o_w = jnp.einsum('bghqk,bkgd->bqghd', p_w.astype(vwi.dtype), vwi)
        o = gi[..., 0:1] * o_c + gi[..., 1:2] * o_s + gi[..., 2:3] * o_w
        return o.astype(q.dtype)

    out = lax.map(q_block, jnp.arange(S // NSA_QBLOCK))
    return jnp.swapaxes(out, 0, 1).reshape(Bn, S, NSA_HEADS * d)


def _peer(h, wq, keys1, keys2, u, v):
    f32 = jnp.float32
    Bn, S, D = h.shape
    ht = h.reshape(Bn * S, D)
    half = PEER_QDIM // 2
    TB = PEER_TOKBLOCK

    def tok_block(i):
        xb = lax.dynamic_slice_in_dim(ht, i * TB, TB, axis=0)
        qh = (xb @ wq).reshape(TB, PEER_HEADS, PEER_QDIM).astype(f32)
        s1 = jnp.einsum('thd,hkd->thk', qh[..., :half], keys1.astype(f32))
        s2 = jnp.einsum('thd,hkd->thk', qh[..., half:], keys2.astype(f32))
        v1, i1 = lax.top_k(s1, PEER_TOPK)
        v2, i2 = lax.top_k(s2, PEER_TOPK)
        cand = (v1[..., :, None] + v2[..., None, :]).reshape(TB, PEER_HEADS, PEER_TOPK * PEER_TOPK)
        cidx = (i1[..., :, None] * PEER_NKEYS + i2[..., None, :]).reshape(TB, PEER_HEADS, PEER_TOPK * PEER_TOPK)
        sc, pos = lax.top_k(cand, PEER_TOPK)
        eidx = jnp.take_along_axis(cidx, pos, axis=-1)
        gw = jax.nn.softmax(sc, axis=-1)
        ue = u[eidx]
        ve = v[eidx]
        act = jax.nn.gelu(jnp.einsum('td,thkd->thk', xb, ue).astype(f32))
        return jnp.einsum('thk,thkd->td', (gw * act).astype(ve.dtype), ve)

    out = lax.map(tok_block, jnp.arange(Bn * S // TB))
    return out.reshape(Bn, S, D)


def setup_inputs(seed: int = 0) -> dict:
    key = jax.random.key(seed)
    ks = jax.random.split(key, 28)
    L, D = DEPTH, D_MODEL

    def nrm(k, shape, scale):
        return jax.random.normal(k, shape, jnp.float32) * scale

    dt = jnp.exp(jax.random.uniform(ks[9], (L, GDN_HEADS), jnp.float32, float(np.log(1e-3)), float(np.log(1e-1))))
    return {
        'x': nrm(ks[0], (BATCH, SEQ, D), 1.0),
        'c': nrm(ks[1], (BATCH, D), 1.0),
        'positions': jnp.broadcast_to(jnp.arange(SEQ, dtype=jnp.int32), (BATCH, SEQ)),
        'ada_w': nrm(ks[2], (L, D, 6 * D), D ** -0.5),
        'ada_b': nrm(ks[3], (L, 6 * D), 0.01),
        'norm1_w': 1.0 + nrm(ks[4], (L, D), 0.01),
        'norm2_w': 1.0 + nrm(ks[5], (L, D), 0.01),
        'w_in': nrm(ks[6], (L, D, D_IN), D ** -0.5),
        'gdn_conv_w': nrm(ks[7], (L, GDN_CONV, 2 * GDN_QK_W + GDN_V_W), GDN_CONV ** -0.5),
        'gdn_A_log': jnp.log(jax.random.uniform(ks[8], (L, GDN_HEADS), jnp.float32, 1.0, 16.0)),
        'gdn_dt_bias': dt + jnp.log(-jnp.expm1(-dt)),
        'gdn_norm_w': 1.0 + nrm(ks[10], (L, GDN_DV), 0.01),
        'cmp_pos_k': nrm(ks[11], (L, CMP_LEN, NSA_DK), 0.1),
        'cmp_w1_k': nrm(ks[12], (L, CMP_LEN * NSA_DK, CMP_HIDDEN), (CMP_LEN * NSA_DK) ** -0.5),
        'cmp_w2_k': nrm(ks[13], (L, CMP_HIDDEN, NSA_DK), CMP_HIDDEN ** -0.5),
        'cmp_pos_v': nrm(ks[14], (L, CMP_LEN, NSA_DK), 0.1),
        'cmp_w1_v': nrm(ks[15], (L, CMP_LEN * NSA_DK, CMP_HIDDEN), (CMP_LEN * NSA_DK) ** -0.5),
        'cmp_w2_v': nrm(ks[16], (L, CMP_HIDDEN, NSA_DK), CMP_HIDDEN ** -0.5),
        'w_branch_gdn': nrm(ks[17], (L, GDN_V_W, D), GDN_V_W ** -0.5),
        'w_branch_nsa': nrm(ks[18], (L, NSA_Q_W, D), NSA_Q_W ** -0.5),
        'w_out': nrm(ks[19], (L, D, D), D ** -0.5),
        'peer_wq': nrm(ks[20], (L, D, PEER_HEADS * PEER_QDIM), D ** -0.5),
        'peer_keys1': nrm(ks[21], (L, PEER_HEADS, PEER_NKEYS, PEER_QDIM // 2), (PEER_QDIM // 2) ** -0.5),
        'peer_keys2': nrm(ks[22], (L, PEER_HEADS, PEER_NKEYS, PEER_QDIM // 2), (PEER_QDIM // 2) ** -0.5),
        'peer_u': nrm(ks[23], (L, PEER_EXPERTS, D), D ** -0.5),
        'peer_v': nrm(ks[24], (L, PEER_EXPERTS, D), PEER_HEADS ** -0.5),
        'final_norm_w': 1.0 + nrm(ks[25], (D,), 0.01),
    }


def reference(x, c, positions, ada_w, ada_b, norm1_w, norm2_w, w_in, gdn_conv_w, gdn_A_log,
              gdn_dt_bias, gdn_norm_w, cmp_pos_k, cmp_w1_k, cmp_w2_k, cmp_pos_v, cmp_w1_v,
              cmp_w2_v, w_branch_gdn, w_branch_nsa, w_out, peer_wq, peer_keys1, peer_keys2,
              peer_u, peer_v, final_norm_w):
    offsets = np.cumsum(IN_WIDTHS)[:-1].tolist()
    cs = jax.nn.silu(c)
    for l in range(DEPTH):
        mod = cs @ ada_w[l] + ada_b[l]
        sh1, sc1, g1, sh2, sc2, g2 = [m[:, None, :] for m in jnp.split(mod, 6, axis=-1)]
        h = _rmsnorm(x, norm1_w[l]) * (1.0 + sc1) + sh1
        (a_q, a_k, a_v, a_z, a_a, a_b, b_q, b_kc, b_vc, b_ks, b_vs, b_kw, b_vw, b_g,
         m_a, m_b) = jnp.split(h @ w_in[l], offsets, axis=-1)
        o_a = _gated_deltanet(a_q, a_k, a_v, a_z, a_a, a_b, gdn_conv_w[l], gdn_A_log[l],
                              gdn_dt_bias[l], gdn_norm_w[l])
        o_b = _nsa(b_q, b_kc, b_vc, b_ks, b_vs, b_kw, b_vw, b_g, positions,
                   cmp_pos_k[l], cmp_w1_k[l], cmp_w2_k[l], cmp_pos_v[l], cmp_w1_v[l], cmp_w2_v[l])
        y = jax.nn.sigmoid(m_a) * (o_a @ w_branch_gdn[l]) + jax.nn.sigmoid(m_b) * (o_b @ w_branch_nsa[l])
        x = x + g1 * (y @ w_out[l])
        h2 = _rmsnorm(x, norm2_w[l]) * (1.0 + sc2) + sh2
        x = x + g2 * _peer(h2, peer_wq[l], peer_keys1[l], peer_keys2[l], peer_u[l], peer_v[l])
    return _rmsnorm(x, final_norm_w)
```

```python
import numpy as np
from contextlib import ExitStack
import concourse.bass as bass
import concourse.mybir as mybir
from concourse.bass_utils import run_bass_kernel_spmd

F32 = mybir.dt.float32
BF16 = mybir.dt.bfloat16
I32 = mybir.dt.int32
ALU = mybir.AluOpType
AF = mybir.ActivationFunctionType
AX = mybir.AxisListType

ENGS = ['pe', 'act', 'dve', 'pool', 'sp']

D = 2048
SEQ = 4096
LOC = 4096
OWN0 = 2048
NOWN = 2048
D_IN = 11840
KC = 16
DEBUG_OUT = False
GDN_ONE_HEAD = False
FROM_REF = False
GDN_SUB = 9
PF_ROWS = 80 * 128
GDN_WAVES = None
GDN_STEPS = 10 ** 9
NSA_GROUPS_RUN = None
NSA_QT_RUN = None
SKIP_GDN = False
SKIP_NSA = False
SKIP_P5 = False
REF_IN = set()
PEER_EB = None
PEER_TG = None


class Buf:
    __slots__ = ('name', 'lw', 'rd', 'excl')

    def __init__(self, name):
        self.name = name
        self.lw = None
        self.rd = {}
        self.excl = False


class Prog:
    def __init__(self, nc, n_dma_sems=40):
        self.nc = nc
        self.ops = {e: [] for e in ENGS}
        self.cnt = {e: 0 for e in ENGS}
        self.esem = {e: nc.alloc_semaphore(name="es_" + e) for e in ENGS}
        self.dsem = [nc.alloc_semaphore(name="ds_%d" % i) for i in range(n_dma_sems)]
        self.dval = [0] * n_dma_sems
        self.dnext = 0
        self.waited = {e: {} for e in ENGS}
        self.pend = {e: [] for e in ENGS}
        self.nbuf = 0

    def barrier(self):
        snap = [(('e', o), self.cnt[o]) for o in ENGS if self.cnt[o] > 0]
        snap += [(('d', i), self.dval[i]) for i in range(len(self.dsem)) if self.dval[i] > 0]
        for e in ENGS:
            self.pend[e] = snap

    def _pending(self, eng, waits):
        w = self.waited[eng]
        for key, val in self.pend[eng]:
            if key == ('e', eng):
                continue
            if w.get(key, 0) < val:
                w[key] = val
                waits.append((self._sem(key), val))
        self.pend[eng] = []

    def buf(self, name=None):
        self.nbuf += 1
        return Buf(name or ("b%d" % self.nbuf))

    def bufs(self, n):
        return [self.buf() for _ in range(n)]

    def _sem(self, key):
        return self.esem[key[1]] if key[0] == 'e' else self.dsem[key[1]]

    def _deps(self, eng, reads, writes):
        need = {}

        def add(tok, raw):
            if tok is None:
                return
            key, val, teng = tok
            if teng == eng and eng == 'pe':
                return
            if need.get(key, 0) < val:
                need[key] = val

        for b in reads:
            add(b.lw, True)
            if b.excl:
                for t in b.rd.values():
                    if t[2] != eng:
                        add(t, True)
        for b in writes:
            add(b.lw, False)
            for t in b.rd.values():
                add(t, False)
        waits = []
        w = self.waited[eng]
        for key, val in need.items():
            if w.get(key, 0) < val:
                w[key] = val
                waits.append((self._sem(key), val))
        return waits

    def _mark(self, tok, reads, writes):
        for b in reads:
            b.rd[tok[0]] = tok
        for b in writes:
            b.lw = tok
            b.rd = {}

    def op(self, eng, fn, reads=(), writes=()):
        waits = self._deps(eng, reads, writes)
        self._pending(eng, waits)
        self.cnt[eng] += 1
        tok = (('e', eng), self.cnt[eng], eng)
        self.ops[eng].append((waits, fn, (self.esem[eng], 1)))
        self._mark(tok, reads, writes)
        return tok

    def dma(self, eng, out, in_, reads=(), writes=(), **kw):
        idx = self.dnext
        self.dnext = (self.dnext + 1) % len(self.dsem)
        waits = self._deps(eng, reads, writes)
        self._pending(eng, waits)
        key = ('d', idx)
        w = self.waited[eng]
        if w.get(key, 0) < self.dval[idx]:
            w[key] = self.dval[idx]
            waits.append((self.dsem[idx], self.dval[idx]))
        self.dval[idx] += 16
        tok = (key, self.dval[idx], None)
        self.ops[eng].append((waits, (lambda e: e.dma_start(out=out, in_=in_, **kw)), (self.dsem[idx], 16)))
        self._mark(tok, reads, writes)
        return tok

    def emit(self, block):
        fin = [(self.dsem[i], self.dval[i]) for i in range(len(self.dsem)) if self.dval[i] > 0]
        fin += [(self.esem[e], self.cnt[e]) for e in ENGS if e != 'sp' and self.cnt[e] > 0]

        def run(e, name):
            for waits, fn, inc in self.ops[name]:
                for s, v in waits:
                    e.wait_ge(s, v)
                ins = fn(e)
                ins.then_inc(inc[0], inc[1])
            if name == 'sp':
                for s, v in fin:
                    e.wait_ge(s, v)

        @block.tensor
        def _(e):
            run(e, 'pe')

        @block.scalar
        def _(e):
            run(e, 'act')

        @block.vector
        def _(e):
            run(e, 'dve')

        @block.gpsimd
        def _(e):
            run(e, 'pool')

        @block.sync
        def _(e):
            run(e, 'sp')


class K:
    def __init__(self, nc):
        self.nc = nc
        self.P = Prog(nc)
        self.es = ExitStack()
        self.n = 0

    def scope(self):
        kk = self

        class _S:
            def __enter__(s_):
                s_.old = kk.es
                kk.es = ExitStack()
                return s_

            def __exit__(s_, *a):
                kk.es.close()
                kk.es = s_.old
                kk.P.barrier()
                return False
        return _S()

    def sb(self, shape, dt=F32, name=None):
        self.n += 1
        return self.es.enter_context(self.nc.sbuf_tensor(("%s_%d" % (name, self.n)) if name else ("t%d" % self.n), list(shape), dt))

    def ps(self, shape, dt=F32, name=None):
        self.n += 1
        return self.es.enter_context(self.nc.psum_tensor(name or ("p%d" % self.n), list(shape), dt))

    def dram(self, shape, dt=F32, name=None, kind="Internal"):
        self.n += 1
        return self.nc.dram_tensor(name or ("d%d" % self.n), list(shape), dt, kind=kind).ap()

    def mm(self, out, lhsT, rhs, start, stop, r, w):
        return self.P.op('pe', lambda e: e.matmul(out, lhsT=lhsT, rhs=rhs, start=start, stop=stop), r, w)

    def tr(self, out, in_, ident, r, w):
        return self.P.op('pe', lambda e: e.transpose(out, in_, ident), r, w)

    def act(self, out, in_, func, r, w, bias=None, scale=None, accum=None, eng='act'):
        kw = {}
        if bias is not None:
            kw['bias'] = bias
        if scale is not None:
            kw['scale'] = scale
        if accum is not None:
            kw['accum_out'] = accum
        return self.P.op('act', lambda e: e.activation(out=out, in_=in_, func=func, **kw), r, w)

    def ts(self, eng, out, in0, s1, s2, op0, op1, r, w):
        if op1 is None:
            return self.P.op(eng, lambda e: e.tensor_scalar(out=out, in0=in0, scalar1=s1, scalar2=None, op0=op0), r, w)
        return self.P.op(eng, lambda e: e.tensor_scalar(out=out, in0=in0, scalar1=s1, scalar2=s2, op0=op0, op1=op1), r, w)

    def tt(self, eng, out, in0, in1, op, r, w):
        return self.P.op(eng, lambda e: e.tensor_tensor(out=out, in0=in0, in1=in1, op=op), r, w)

    def stt(self, out, in0, scalar, in1, op0, op1, r, w):
        return self.P.op('dve', lambda e: e.scalar_tensor_tensor(out=out, in0=in0, scalar=scalar, in1=in1, op0=op0, op1=op1), r, w)

    def cp(self, eng, out, in_, r, w):
        if eng == 'act':
            return self.P.op('act', lambda e: e.copy(out=out, in_=in_), r, w)
        return self.P.op(eng, lambda e: e.tensor_copy(out=out, in_=in_), r, w)

    def memset(self, eng, ap, val, w):
        return self.P.op(eng, lambda e: e.memset(ap, val), (), w)

    def recip(self, out, in_, r, w):
        return self.P.op('dve', lambda e: e.reciprocal(out=out, in_=in_), r, w)

    def dma(self, eng, out, in_, r, w, **kw):
        return self.P.dma(eng, out, in_, r, w, **kw)


def build_program(stage=99):
    nc = bass.Bass("TRN2", target_bir_lowering=False)
    k = K(nc)
    P = k.P

    def din(name, shape, dt=F32):
        return nc.dram_tensor(name, list(shape), dt, kind="ExternalInput").ap()

    xl = din("xl", [LOC, D])
    cb = din("cb", [KC, 128])
    ada_w = din("ada_w", [D, 6 * D]) if not FROM_REF else None
    ada_b = din("ada_b", [96, 128])
    norm1_w = din("norm1_w", [KC, 128])
    w_in = din("w_in", [D, D_IN]) if not FROM_REF else None
    ident_in = din("ident", [128, 128])
    pvalid_in = din("pvalid", [128, 1])
    out_d = nc.dram_tensor("out", [NOWN, D], F32, kind="ExternalOutput").ap()
    dbg_mod = nc.dram_tensor("dbg_mod", [128, 96], F32, kind="ExternalOutput").ap()
    def scratch(name, shape, dt=F32):
        if name in REF_IN:
            return din(name + "_in", shape, dt)
        return k.dram(shape, dt, name)

    if FROM_REF:
        PF = din("PF_in", [PF_ROWS, LOC])
        PT = din("PT_in", [LOC, 1600])
    else:
        PF = k.dram([80 * 128, LOC], F32, "PF")
        PT = k.dram([LOC, 1600], F32, "PT")
    OAT = scratch("OAT", [1024, NOWN], BF16)
    OBT = scratch("OBT", [2048, NOWN], BF16)
    YT = scratch("YT", [D, NOWN], BF16)
    X1 = scratch("X1", [NOWN, D], F32)

    ident = k.sb([128, 128], F32, "ident_sb")
    identb = k.sb([128, 128], BF16, "identb")
    pvalid = k.sb([128, 1], F32, "pvalid_sb")
    b_const = P.buf("const")
    k.dma('sp', ident[:], ident_in, (), [b_const])
    k.dma('sp', pvalid[:], pvalid_in, (), [b_const])
    k.cp('dve', identb[:], ident[:], [b_const], [b_const])

    banks = [k.ps([128, 512], F32, "bank%d" % i) for i in range(6)]
    bbank = [P.buf("bank%d" % i) for i in range(6)]
    for b_ in bbank:
        b_.excl = True
    pbf = [k.ps([128, 1024], BF16, "pbf%d" % i) for i in range(2)]
    bpbf = [P.buf("pbf%d" % i) for i in range(2)]
    for b_ in bpbf:
        b_.excl = True

    def load_fm(dst, src, n, bdst, bank=5):
        tmp = k.sb([96, 128], F32)
        bt = P.buf()
        k.dma('sp', tmp[0:n, :], src, (), [bt])
        k.tr(banks[bank][:, 0:n], tmp[0:n, :], ident[0:n, 0:n], [bt, b_const], [bbank[bank]])
        k.cp('dve', dst, banks[bank][:, 0:n], [bbank[bank]], [bdst])

    modT = k.sb([128, 96], F32, "modT")
    b_mod = P.buf("mod")
    A1 = k.sb([128, KC], F32, "A1")
    B1p = k.sb([128, KC], F32, "B1p")
    b_A1 = P.buf()
    if FROM_REF:
        modT_in = din("modT_in", [128, 96])
        k.dma('sp', modT[:], modT_in, (), [b_mod])
    with k.scope():
      if not FROM_REF:
        cT = k.sb([128, KC], F32, "cT")
        csT = k.sb([128, KC], F32, "csT")
        adabT = k.sb([128, 96], F32, "adabT")
        n1T = k.sb([128, KC], F32, "n1T")
        b_c, b_cs, b_adab, b_n1 = P.bufs(4)
        load_fm(cT[:], cb, KC, b_c)
        load_fm(adabT[:], ada_b, 96, b_adab)
        load_fm(n1T[:], norm1_w, KC, b_n1)
        k.act(csT[:], cT[:], AF.Silu, [b_c], [b_cs])
        NAW = 3
        awt = [k.sb([128, KC, 128], F32, "awt%d" % i) for i in range(NAW)]
        bawt = P.bufs(NAW)
        ada_v = ada_w.rearrange("(kc p) j -> p kc j", p=128)
        for m in range(96):
            s = m % NAW
            k.dma('sp', awt[s][:], ada_v[:, :, m * 128:(m + 1) * 128], (), [bawt[s]])
            for kc in range(KC):
                k.mm(banks[4][:, m:m + 1], awt[s][:, kc, :], csT[:, kc:kc + 1], kc == 0, kc == KC - 1,
                     [bawt[s], b_cs], [bbank[4]])
        k.tt('dve', modT[:], banks[4][:, 0:96], adabT[:], ALU.add, [bbank[4], b_adab], [b_mod])
        k.dma('sp', dbg_mod, modT[:], [b_mod], ())
        k.stt(A1[:], modT[:, 16:32], 1.0, n1T[:], ALU.add, ALU.mult, [b_mod, b_n1], [b_A1])
        k.ts('dve', B1p[:], modT[:, 0:16], pvalid[:, 0:1], None, ALU.mult, None, [b_mod, b_const], [b_A1])

    if stage >= 1 and not FROM_REF:
        hscope = k.scope()
        hscope.__enter__()
        hT = k.sb([128, KC, LOC], BF16, "hT")
        b_hT = [P.buf() for _ in range(LOC // 128)]
        with k.scope():
            xt = [k.sb([128, D], F32, "xt%d" % i) for i in range(2)]
            xs = [k.sb([128, D], BF16, "xs%d" % i) for i in range(2)]
            st = [k.sb([128, 4], F32, "st%d" % i) for i in range(2)]
            bxt, bxs, bst = P.bufs(2), P.bufs(2), P.bufs(2)
            xlv = xl.rearrange("(t p) d -> t p d", p=128)
            for t in range(LOC // 128):
                s = t % 2
                k.dma('sp', xt[s][:], xlv[t], (), [bxt[s]])
                k.act(xs[s][:], xt[s][:], AF.Square, [bxt[s]], [bxs[s], bst[s]], accum=st[s][:, 0:1])
                k.ts('dve', st[s][:, 1:2], st[s][:, 0:1], 1.0 / D, 1e-6, ALU.mult, ALU.add, [bst[s]], [bst[s]])
                k.act(st[s][:, 2:3], st[s][:, 1:2], AF.Sqrt, [bst[s]], [bst[s]])
                k.recip(st[s][:, 3:4], st[s][:, 2:3], [bst[s]], [bst[s]])
                k.ts('dve', xs[s][:], xt[s][:], st[s][:, 3:4], None, ALU.mult, None, [bxt[s], bst[s]], [bxs[s]])
                Bsel = B1p if t < OWN0 // 128 else modT
                for kc in range(KC):
                    pi = kc % 2
                    sl = slice((kc // 2 % 4) * 128, (kc // 2 % 4) * 128 + 128)
                    k.tr(pbf[pi][:, sl], xs[s][:, kc * 128:(kc + 1) * 128], identb[:], [bxs[s], b_const], [bpbf[pi]])
                    if kc % 2 == 0:
                        k.act(hT[:, kc, t * 128:(t + 1) * 128], pbf[pi][:, sl], AF.Identity, [bpbf[pi], b_A1, b_mod], [b_hT[t]],
                              bias=Bsel[:, kc:kc + 1], scale=A1[:, kc:kc + 1])
                    else:
                        k.ts('dve', hT[:, kc, t * 128:(t + 1) * 128], pbf[pi][:, sl], A1[:, kc:kc + 1], Bsel[:, kc:kc + 1],
                             ALU.mult, ALU.add, [bpbf[pi], b_A1, b_mod], [b_hT[t]])
        if stage == 1:
            dbg_h = nc.dram_tensor("dbg_h", [128, KC, LOC], BF16, kind="ExternalOutput").ap()
            k.dma('sp', dbg_h, hT[:], b_hT, ())

        if stage >= 2:
            FM = [(0, 3072, 0, 0), (4112, 2048, 3072, OWN0), (6160, 256, 5120, 0), (6416, 256, 5376, 0),
                  (6672, 256, 5632, 0), (7184, 256, 5888, 0), (7744, 2048, 6144, OWN0), (9792, 2048, 8192, OWN0)]
            TM = [(3072, 256, 0, OWN0), (3328, 256, 256, OWN0), (3584, 256, 512, OWN0), (3840, 256, 768, OWN0),
                  (4096, 16, 1024, 0), (6928, 256, 1040, 0), (7440, 256, 1296, 0), (7696, 48, 1552, OWN0)]
            with k.scope():
                wv = w_in.rearrange("(kc p) c -> p kc c", p=128)
                NW = 3
                wt = [k.sb([128, KC, 256], BF16, "wt%d" % i) for i in range(NW)]
                bwt = P.bufs(NW)
                ev = [k.sb([128, 512], F32, "ev%d" % i) for i in range(4)]
                bev = P.bufs(4)
                ei = 0
                wi = 0
                for (c0, ncols, r0, t0) in FM:
                    for cb_ in range(ncols // 256):
                        s = wi % NW
                        wi += 1
                        k.dma('pool', wt[s][:], wv[:, :, c0 + cb_ * 256:c0 + cb_ * 256 + 256], (), [bwt[s]])
                        for hf in range(2):
                            for tt in range(t0 // 512, LOC // 512):
                                e4 = ei % 4
                                ei += 1
                                for kc in range(KC):
                                    k.mm(banks[e4][:, :], wt[s][:, kc, hf * 128:(hf + 1) * 128],
                                         hT[:, kc, tt * 512:(tt + 1) * 512], kc == 0, kc == KC - 1,
                                         [bwt[s]] + b_hT[tt * 4:(tt + 1) * 4], [bbank[e4]])
                                k.cp('act' if e4 % 2 == 0 else 'dve', ev[e4][:], banks[e4][:, :], [bbank[e4]], [bev[e4]])
                                row = r0 + cb_ * 256 + hf * 128
                                k.dma('sp', PF[row:row + 128, tt * 512:(tt + 1) * 512], ev[e4][:], [bev[e4]], ())
                for (c0, ncols, p0, t0) in TM:
                    s = wi % NW
                    wi += 1
                    k.dma('pool', wt[s][:, :, 0:ncols], wv[:, :, c0:c0 + ncols], (), [bwt[s]])
                    for t in range(t0 // 128, LOC // 128):
                        e4 = ei % 4
                        ei += 1
                        for kc in range(KC):
                            k.mm(banks[e4][:, 0:ncols], hT[:, kc, t * 128:(t + 1) * 128], wt[s][:, kc, 0:ncols],
                                 kc == 0, kc == KC - 1, [bwt[s], b_hT[t]], [bbank[e4]])
                        k.cp('act' if e4 % 2 == 0 else 'dve', ev[e4][:, 0:ncols], banks[e4][:, 0:ncols], [bbank[e4]], [bev[e4]])
                        k.dma('sp', PT[t * 128:(t + 1) * 128, p0:p0 + ncols], ev[e4][:, 0:ncols], [bev[e4]], ())
        hscope.__exit__(None, None, None)
        if stage == 2 and DEBUG_OUT:
            dbg_pf = nc.dram_tensor("dbg_pf", [4, 128, LOC], F32, kind="ExternalOutput").ap()
            dbg_pt = nc.dram_tensor("dbg_pt", [LOC, 576], F32, kind="ExternalOutput").ap()
            for i, r in enumerate([0, 3072, 5120, 6144]):
                k.dma('sp', dbg_pf[i], PF[r:r + 128, :], (), ())
            k.dma('sp', dbg_pt, PT[:, 1024:1600], (), ())

    if stage >= 3 and not SKIP_GDN:
        gconst_in = din("gconst", [128, 5, 128])
        convw_in = din("gdn_conv_w", [96, 128])
        alog_in = din("gdn_A_log", [32, 8])
        dtb_in = din("gdn_dt_bias", [32, 8])
        gnw_in = din("gdn_norm_w", [1, 128])
        bq = [[bbank[bi]] * 4 for bi in range(6)]
        NCH = LOC // 128
        with k.scope():
            gcn = k.sb([128, 5, 128], F32, "gcn")
            ones = k.sb([128, 128], F32, "ones")
            cwT = k.sb([128, 96], F32, "cwT")
            nwb = k.sb([128, 128], F32, "nwb")
            b_gc = P.buf()
            b_cw = P.buf()
            k.dma('sp', gcn[:], gconst_in, (), [b_gc])
            k.memset('dve', ones[:], 1.0, [b_gc])
            k.dma('sp', nwb[:], gnw_in[0].partition_broadcast(128), (), [b_gc])
            load_fm(cwT[:], convw_in, 96, b_cw)
            TriU, msl, msu, miu, bmk = gcn[:, 0, :], gcn[:, 1, :], gcn[:, 2, :], gcn[:, 3, :], gcn[:, 4, :]
            ab = k.sb([128, NCH, 16], F32, "ab")
            dtb = k.sb([128, NCH, 8], F32, "dtb")
            alg = k.sb([128, NCH, 8], F32, "alg")
            vt = {nm: k.sb([128, NCH, 8], F32, "v_" + nm) for nm in
                  ["t1", "g", "beta", "lnb", "Gc", "nGc", "u", "gam", "bg", "kd", "gend", "Gl"]}
            b_v = P.buf()
            k.dma('sp', ab[:], PT[:, 1024:1040].rearrange("(n p) c -> p n c", p=128), (), [b_v])
            k.dma('sp', dtb[:], dtb_in.partition_broadcast(128), (), [b_v])
            k.dma('sp', alg[:], alog_in.partition_broadcast(128), (), [b_v])
            k.tt('dve', vt["t1"][:], ab[:, :, 0:8], dtb[:], ALU.add, [b_v], [b_v])
            k.act(vt["t1"][:], vt["t1"][:], AF.Exp, [b_v], [b_v])
            e_, ser, lnp = vt["t1"], vt["Gl"], vt["Gc"]
            k.ts('dve', ser[:], e_[:], -0.25, 1.0 / 3.0, ALU.mult, ALU.add, [b_v], [b_v])
            k.tt('dve', ser[:], ser[:], e_[:], ALU.mult, [b_v], [b_v])
            k.ts('dve', ser[:], ser[:], -0.5, None, ALU.add, None, [b_v], [b_v])
            k.tt('dve', ser[:], ser[:], e_[:], ALU.mult, [b_v], [b_v])
            k.ts('dve', ser[:], ser[:], 1.0, None, ALU.add, None, [b_v], [b_v])
            k.tt('dve', ser[:], ser[:], e_[:], ALU.mult, [b_v], [b_v])
            k.ts('dve', lnp[:], e_[:], 0.1, None, ALU.max, None, [b_v], [b_v])
            k.act(lnp[:], lnp[:], AF.Ln, [b_v], [b_v], bias=1.0)
            k.ts('dve', vt["u"][:], e_[:], 0.1, None, ALU.is_lt, None, [b_v], [b_v])
            k.tt('dve', ser[:], ser[:], lnp[:], ALU.subtract, [b_v], [b_v])
            k.tt('dve', ser[:], ser[:], vt["u"][:], ALU.mult, [b_v], [b_v])
            k.tt('dve', vt["t1"][:], lnp[:], ser[:], ALU.add, [b_v], [b_v])
            k.act(alg[:], alg[:], AF.Exp, [b_v], [b_v])
            k.stt(vt["g"][:], vt["t1"][:], -1.0, alg[:], ALU.mult, ALU.mult, [b_v], [b_v])
            k.act(vt["beta"][:], ab[:, :, 8:16], AF.Sigmoid, [b_v], [b_v])
            k.act(vt["lnb"][:], vt["beta"][:], AF.Ln, [b_v], [b_v])
            gflat = vt["g"][:].rearrange("p n h -> p (n h)")
            k.mm(banks[0][:, 0:256], TriU, gflat, True, True, [b_v, b_gc], [bq[0][0], bq[0][1]])
            k.mm(banks[0][:, 256:512], ones[:], gflat, True, True, [b_v, b_gc], [bq[0][2], bq[0][3]])
            fl = lambda nm: vt[nm][:].rearrange("p n h -> p (n h)")
            k.cp('dve', fl("Gc"), banks[0][:, 0:256], [bq[0][0], bq[0][1]], [b_v])
            k.cp('dve', fl("Gl"), banks[0][:, 256:512], [bq[0][2], bq[0][3]], [b_v])
            k.ts('dve', fl("nGc"), fl("Gc"), -1.0, None, ALU.mult, None, [b_v], [b_v])
            k.tt('dve', fl("u"), fl("Gc"), fl("lnb"), ALU.add, [b_v], [b_v])
            k.act(fl("gam"), fl("Gc"), AF.Exp, [b_v], [b_v])
            k.act(fl("bg"), fl("u"), AF.Exp, [b_v], [b_v])
            k.tt('dve', fl("kd"), fl("Gl"), fl("Gc"), ALU.subtract, [b_v], [b_v])
            k.act(fl("kd"), fl("kd"), AF.Exp, [b_v], [b_v])
            k.act(fl("gend"), fl("Gl"), AF.Exp, [b_v], [b_v])

            def col(nm, n, h):
                return vt[nm][:, n, h:h + 1]

            WV = 4
            for h in range(0 if GDN_SUB < 1 else (1 if GDN_ONE_HEAD else 8)):
                with k.scope():
                    QT = k.sb([128, LOC], F32, "QT")
                    KT = k.sb([128, LOC], F32, "KT")
                    VT = k.sb([128, LOC], F32, "VT")
                    xr = k.sb([128, LOC], F32, "xr")
                    bQ, bK, bV, bxr = P.bufs(4)
                    for (dst, bd, row0, ti) in [(QT, bQ, h * 128, h), (KT, bK, 1024 + h * 128, 8 + h), (VT, bV, 2048 + h * 128, 16 + h)]:
                        k.dma('sp', xr[:], PF[row0:row0 + 128, :], (), [bxr])
                        k.ts('dve', dst[:], xr[:], cwT[:, 3 * 24 + ti:3 * 24 + ti + 1], None, ALU.mult, None, [bxr, b_cw], [bd])
                        for sh in (1, 2, 3):
                            j = 3 - sh
                            k.stt(dst[:, sh:LOC], xr[:, 0:LOC - sh], cwT[:, j * 24 + ti:j * 24 + ti + 1], dst[:, sh:LOC],
                                  ALU.mult, ALU.add, [bxr, b_cw, bd], [bd])
                        k.act(dst[:], dst[:], AF.Silu, [bd], [bd])
                    for (dst, bd, lnc) in [(QT, bQ, float(np.log(128.0 ** -0.5))), (KT, bK, 0.0)]:
                        for tt in range(LOC // 512):
                            bk = tt % 4
                            sl = slice(tt * 512, (tt + 1) * 512)
                            k.tt('dve', xr[:, sl], dst[:, sl], dst[:, sl], ALU.mult, [bd], [bxr])
                            k.mm(banks[bk][:, :], ones[:], xr[:, sl], True, True, [bxr, b_gc], bq[bk])
                            k.act(xr[:, sl], banks[bk][:, :], AF.Ln, bq[bk], [bxr], bias=1e-6)
                            k.act(xr[:, sl], xr[:, sl], AF.Exp, [bxr], [bxr], scale=-0.5, bias=lnc)
                            k.tt('dve', dst[:, sl], dst[:, sl], xr[:, sl], ALU.mult, [bd, bxr], [bd])
                    zt = k.sb([128, NOWN // 128, 128], F32, "zt")
                    bz = P.buf()
                    k.dma('sp', zt[:], PT[OWN0:LOC, h * 128:(h + 1) * 128].rearrange("(n p) c -> p n c", p=128), (), [bz])
                    k.act(zt[:], zt[:], AF.Silu, [bz], [bz])
                    S = k.sb([128, 128], F32, "S")
                    bS = P.buf()
                    k.memset('dve', S[:], 0.0, [bS])
                    names = ["R", "Kd", "dg", "tE", "L", "LT", "QK", "Pa", "Pb", "WT", "U", "tmp", "O", "o16", "Z", "DT", "X", "Tt"]
                    T = [{nm: k.sb([128, 256 if nm in ("R", "X", "Tt") else 128], BF16 if nm == "o16" else F32, "w%d_%s" % (i, nm)) for nm in names}
                         for i in range(WV)]
                    Bf = [{nm: P.buf() for nm in names + ["st"]} for i in range(WV)]
                    stt_ = [k.sb([128, 4], F32, "w%d_st" % i) for i in range(WV)]
                    PTk = [[k.sb([128, 128], F32, "w%d_PT%d" % (i, l)) for l in range(4)] for i in range(WV)]
                    bPTk = [[P.buf() for l in range(4)] for i in range(WV)]
                    bctr = [0]

                    def nb():
                        bctr[0] = (bctr[0] + 1) % 6
                        return bctr[0]

                    for w0 in (GDN_WAVES if GDN_WAVES is not None else range(0, NCH if GDN_SUB >= 2 else 0, WV)):
                        chunks = list(range(w0, w0 + WV))
                        own = w0 >= OWN0 // 128
                        def pre_steps(i, n):
                            t, b = T[i], Bf[i]
                            cs = slice(n * 128, (n + 1) * 128)
                            q4 = slice(i * 128, (i + 1) * 128)
                            st = []
                            bkT1, bkT2, bkKK, bkKQ, bkB = 0, 1, 2, 3, 4
                            st.append(lambda: k.tr(banks[0][:, q4], KT[:, cs], ident[:], [bK, b_const], [bq[0][i]]))
                            st.append(lambda: k.act(t["R"][:, 0:128], banks[0][:, q4], AF.Identity, [bq[0][i], b_v], [b["R"]], scale=col("bg", n, h)))
                            st.append(lambda: k.ts('dve', t["Kd"][:], banks[0][:, q4], col("kd", n, h), None, ALU.mult, None, [bq[0][i], b_v], [b["Kd"]]))
                            st.append(lambda: k.tr(banks[1][:, q4], VT[:, cs], ident[:], [bV, b_const], [bq[1][i]]))
                            st.append(lambda: k.ts('dve', t["R"][:, 128:256], banks[1][:, q4], col("beta", n, h), None, ALU.mult, None, [bq[1][i], b_v], [b["R"]]))
                            st.append(lambda: k.mm(banks[2][:, q4], KT[:, cs], KT[:, cs], True, True, [bK], [bq[2][i]]))
                            st.append(lambda: k.ts('dve', t["dg"][:], ident[:], col("nGc", n, h), None, ALU.mult, None, [b_const, b_v], [b["dg"]]))
                            st.append(lambda: k.mm(banks[4][:, q4], ones[:], t["dg"][:], True, True, [b["dg"], b_gc], [bq[4][i]]))
                            st.append(lambda: k.stt(t["tE"][:], banks[4][:, q4], col("u", n, h), msl, ALU.add, ALU.add, [bq[4][i], b_v, b_gc], [b["tE"]]))
                            st.append(lambda: k.act(t["tE"][:], t["tE"][:], AF.Exp, [b["tE"]], [b["tE"]]))
                            st.append(lambda: k.tt('dve', t["L"][:], banks[2][:, q4], t["tE"][:], ALU.mult, [bq[2][i], b["tE"]], [b["L"]]))
                            st.append(lambda: k.ts('dve', t["dg"][:], ident[:], col("u", n, h), None, ALU.mult, None, [b_const, b_v], [b["dg"]]))
                            st.append(lambda: k.mm(banks[5][:, q4], ones[:], t["dg"][:], True, True, [b["dg"], b_gc], [bq[5][i]]))
                            st.append(lambda: k.stt(t["tE"][:], banks[5][:, q4], col("nGc", n, h), msu, ALU.add, ALU.add, [bq[5][i], b_v, b_gc], [b["tE"]]))
                            st.append(lambda: k.act(t["tE"][:], t["tE"][:], AF.Exp, [b["tE"]], [b["tE"]]))
                            st.append(lambda: k.tt('dve', t["LT"][:], banks[2][:, q4], t["tE"][:], ALU.mult, [bq[2][i], b["tE"]], [b["LT"]]))
                            if own:
                                st.append(lambda: k.mm(banks[3][:, q4], KT[:, cs], QT[:, cs], True, True, [bK, bQ], [bq[3][i]]))
                                st.append(lambda: k.ts('dve', t["dg"][:], ident[:], col("Gc", n, h), None, ALU.mult, None, [b_const, b_v], [b["dg"]]))
                                st.append(lambda: k.mm(banks[4][:, q4], ones[:], t["dg"][:], True, True, [b["dg"], b_gc], [bq[4][i]]))
                                st.append(lambda: k.stt(t["tE"][:], banks[4][:, q4], col("nGc", n, h), miu, ALU.add, ALU.add, [bq[4][i], b_v, b_gc], [b["tE"]]))
                                st.append(lambda: k.act(t["tE"][:], t["tE"][:], AF.Exp, [b["tE"]], [b["tE"]]))
                                st.append(lambda: k.tt('dve', t["QK"][:], banks[3][:, q4], t["tE"][:], ALU.mult, [bq[3][i], b["tE"]], [b["QK"]]))
                            st.append(lambda: k.tt('dve', t["tmp"][:], t["LT"][:], bmk, ALU.mult, [b["LT"], b_gc], [b["tmp"]]))
                            st.append(lambda: k.tt('dve', t["LT"][:], t["LT"][:], t["tmp"][:], ALU.subtract, [b["LT"], b["tmp"]], [b["LT"]]))
                            st.append(lambda: k.tt('dve', t["L"][:], t["L"][:], bmk, ALU.mult, [b["L"], b_gc], [b["L"]]))
                            cP, cbP, cPT, cbPT = t["L"], b["L"], t["tmp"], b["tmp"]
                            NLEV = 4
                            for lev in range(NLEV):
                                nP, nbP = (t["Pa"], b["Pa"]) if lev % 2 == 0 else (t["Pb"], b["Pb"])
                                bA = lev % 2
                                last = lev == NLEV - 1
                                if not last:
                                    st.append((lambda cP=cP, cbP=cbP, cPT=cPT, cbPT=cbPT, bA=bA: k.mm(banks[bA][:, q4], cPT[:], cP[:], True, True, [cbPT, cbP], [bq[bA][i]])))
                                    st.append((lambda nP=nP, nbP=nbP, bA=bA: k.cp('act', nP[:], banks[bA][:, q4], [bq[bA][i]], [nbP])))
                                st.append((lambda cP=cP, cbP=cbP, cPT=cPT, cbPT=cbPT, bA=bA: k.mm(banks[2 + bA][:, q4], cP[:], cPT[:], True, True, [cbPT, cbP], [bq[2 + bA][i]])))
                                st.append((lambda lev=lev, bA=bA: k.cp('dve', PTk[i][lev][:], banks[2 + bA][:, q4], [bq[2 + bA][i]], [bPTk[i][lev]])))
                                cP, cbP, cPT, cbPT = nP, nbP, PTk[i][lev], bPTk[i][lev]
                            st.append(lambda: k.cp('dve', t["Z"][:], ident[:], [b_const], [b["Z"]]))
                            for lev in [3, 2, 1, 0, -1]:
                                bA = 4 + (lev % 2)
                                if lev >= 0:
                                    st.append((lambda lev=lev, bA=bA: k.mm(banks[bA][:, q4], PTk[i][lev][:], t["Z"][:], True, True, [bPTk[i][lev], b["Z"]], [bq[bA][i]])))
                                    st.append((lambda bA=bA: k.tt('dve', t["Z"][:], t["Z"][:], banks[bA][:, q4], ALU.add, [bq[bA][i], b["Z"]], [b["Z"]])))
                                else:
                                    st.append((lambda bA=bA: k.mm(banks[bA][:, q4], t["tmp"][:], t["Z"][:], True, True, [b["tmp"], b["Z"]], [bq[bA][i]])))
                                    st.append((lambda bA=bA: k.tt('dve', t["Z"][:], t["Z"][:], banks[bA][:, q4], ALU.subtract, [bq[bA][i], b["Z"]], [b["Z"]])))
                            st.append(lambda: k.tr(banks[0][:, q4], t["Z"][:], ident[:], [b["Z"], b_const], [bq[0][i]]))
                            st.append(lambda: k.cp('act', t["DT"][:], banks[0][:, q4], [bq[0][i]], [b["DT"]]))
                            hq = slice((i % 2) * 256, (i % 2) * 256 + 256)
                            bkR = 4 + (i // 2)
                            bkR2 = 2 + (i // 2)
                            st.append(lambda: k.mm(banks[bkR][:, hq], t["DT"][:], t["R"][:], True, True, [b["DT"], b["R"]], [bq[bkR][0]]))
                            st.append(lambda: k.cp('dve', t["X"][:], banks[bkR][:, hq], [bq[bkR][0]], [b["X"]]))
                            for sweep in range(3):
                                st.append(lambda: k.mm(banks[bkR2][:, hq], t["LT"][:], t["X"][:], True, True, [b["LT"], b["X"]], [bq[bkR2][0]]))
                                st.append(lambda: k.tt('dve', t["Tt"][:], t["R"][:], banks[bkR2][:, hq], ALU.subtract, [b["R"], bq[bkR2][0]], [b["Tt"]]))
                                st.append(lambda: k.mm(banks[bkR][:, hq], t["DT"][:], t["Tt"][:], True, True, [b["DT"], b["Tt"]], [bq[bkR][0]]))
                                st.append(lambda: k.cp('dve', t["X"][:], banks[bkR][:, hq], [bq[bkR][0]], [b["X"]]))
                            st.append(lambda: k.cp('act', t["R"][:], t["X"][:], [b["X"]], [b["R"]]))
                            st.append(lambda: k.tr(banks[0][:, q4], t["R"][:, 0:128], ident[:], [b["R"], b_const], [bq[0][i]]))
                            st.append(lambda: k.cp('act', t["WT"][:], banks[0][:, q4], [bq[0][i]], [b["WT"]]))
                            return st

                        allst = [pre_steps(i, n) for i, n in enumerate(chunks)]
                        for si in range(min(GDN_STEPS, len(allst[0]))):
                            for i in range(WV):
                                allst[i][si]()
                        for i, n in enumerate(chunks if GDN_SUB >= 3 else []):
                            t, b = T[i], Bf[i]
                            cs = slice(n * 128, (n + 1) * 128)
                            q4 = slice(i * 128, (i + 1) * 128)
                            k.mm(banks[1][:, q4], t["WT"][:], S[:], True, True, [b["WT"], bS], [bq[1][i]])
                            k.tt('dve', t["U"][:], t["R"][:, 128:256], banks[1][:, q4], ALU.subtract, [b["R"], bq[1][i]], [b["U"]])
                            if own:
                                k.mm(banks[2][:, q4], QT[:, cs], S[:], True, True, [bQ, bS], [bq[2][i]])
                                k.mm(banks[3][:, q4], t["QK"][:], t["U"][:], True, True, [b["QK"], b["U"]], [bq[3][i]])
                                k.act(t["tmp"][:], banks[2][:, q4], AF.Identity, [bq[2][i], b_v], [b["tmp"]], scale=col("gam", n, h))
                                k.tt('dve', t["O"][:], t["tmp"][:], banks[3][:, q4], ALU.add, [b["tmp"], bq[3][i]], [b["O"]])
                            k.mm(banks[0][:, q4], t["Kd"][:], t["U"][:], True, True, [b["Kd"], b["U"]], [bq[0][i]])
                            k.stt(S[:], S[:], col("gend", n, h), banks[0][:, q4], ALU.mult, ALU.add, [bS, b_v, bq[0][i]], [bS])
                            if own:
                                no = n - OWN0 // 128
                                sti = stt_[i]
                                k.act(t["tmp"][:], t["O"][:], AF.Square, [b["O"]], [b["tmp"], b["st"]], accum=sti[:, 0:1])
                                k.act(sti[:, 1:2], sti[:, 0:1], AF.Ln, [b["st"]], [b["st"]], scale=1.0 / 128, bias=1e-6)
                                k.act(sti[:, 2:3], sti[:, 1:2], AF.Exp, [b["st"]], [b["st"]], scale=-0.5)
                                k.stt(t["O"][:], t["O"][:], sti[:, 2:3], nwb[:], ALU.mult, ALU.mult, [b["O"], b["st"], b_gc], [b["O"]])
                                k.tt('dve', t["O"][:], t["O"][:], zt[:, no, :], ALU.mult, [b["O"], bz], [b["O"]])
                                k.tr(banks[4][:, q4], t["O"][:], ident[:], [b["O"], b_const], [bq[4][i]])
                                k.cp('act', t["o16"][:], banks[4][:, q4], [bq[4][i]], [b["o16"]])
                                k.dma('sp', OAT[h * 128:(h + 1) * 128, no * 128:(no + 1) * 128], t["o16"][:], [b["o16"]], ())
        if stage == 3 and DEBUG_OUT and not SKIP_GDN:
            dbg_oa = nc.dram_tensor("dbg_oa", [1024, NOWN], BF16, kind="ExternalOutput").ap()
            k.dma('sp', dbg_oa, OAT, (), ())

    if stage >= 4 and not SKIP_NSA:
        pos_in = din("pos", [1, LOC], I32)
        nsc_in = din("nsa_c128", [128, 385])
        maskB_in = din("maskB", [128, LOC])
        eexp_in = din("eexp", [64, LOC])
        ovl_in = din("ovl", [128, 2, 65])
        fv_in = din("fbigvis", [128, 2, 16, 64])
        ncore_in = din("nsa_core", [128, 129])
        cpos_in = [din("cmp_pos_k", [32, 128]), din("cmp_pos_v", [32, 128])]
        cw1_in = [din("cmp_w1_k", [LOC, 256]), din("cmp_w1_v", [LOC, 256])]
        cw2_in = [din("cmp_w2_k", [256, 128]), din("cmp_w2_v", [256, 128])]
        NQT = NOWN // 128
        with k.scope():
            nsc = k.sb([128, 385], F32, "nsc")
            ncore = k.sb([128, 129], F32, "ncore")
            maskB = k.sb([128, LOC], BF16, "maskB")
            eexp = k.sb([64, LOC], BF16, "eexp")
            ovl = k.sb([128, 2, 65], BF16, "ovl")
            fv = k.sb([128, 2, 16, 64], F32, "fv")
            cmk = k.sb([128, 2, 128], BF16, "cmk")
            b_nc = P.buf()
            k.dma('sp', nsc[:], nsc_in, (), [b_nc])
            k.dma('sp', ncore[:], ncore_in, (), [b_nc])
            k.dma('sp', fv[:], fv_in, (), [b_nc])
            k.dma('pool', maskB[:], maskB_in, (), [b_nc])
            k.dma('pool', eexp[:], eexp_in, (), [b_nc])
            k.dma('pool', ovl[:], ovl_in, (), [b_nc])
            k.cp('dve', cmk[:, 0, :], nsc[:, 129:257], [b_nc], [b_nc])
            k.cp('dve', cmk[:, 1, :], nsc[:, 257:385], [b_nc], [b_nc])
            ropeP = nsc[:, 0:128]
            invf = nsc[:, 128:129]
            kvb = ncore[:, 128:129]
            cosT = k.sb([128, LOC], F32, "cosT")
            sinT = k.sb([128, LOC], F32, "sinT")
            cosq = k.sb([128, NOWN], F32, "cosq")
            sinq = k.sb([128, NOWN], F32, "sinq")
            b_rt = P.buf()
            with k.scope():
                posi = k.sb([128, LOC], I32, "posi")
                ang = k.sb([128, LOC], F32, "ang")
                kk_ = k.sb([128, LOC], I32, "kk")
                kf = k.sb([128, LOC], F32, "kf")
                b_a = P.buf()
                k.dma('sp', posi[:], pos_in[0].partition_broadcast(128), (), [b_a])
                k.cp('dve', ang[:], posi[:], [b_a], [b_a])
                k.ts('dve', ang[:], ang[:], invf, None, ALU.mult, None, [b_a, b_nc], [b_a])
                TWO_PI = float(2 * np.pi)
                for (dst, shift) in [(sinT, 0.0), (cosT, float(np.pi / 2))]:
                    k.ts('dve', kf[:], ang[:], shift, 1.0 / TWO_PI, ALU.add, ALU.mult, [b_a], [b_a])
                    k.cp('dve', kk_[:], kf[:], [b_a], [b_a])
                    k.cp('dve', kf[:], kk_[:], [b_a], [b_a])
                    k.stt(kf[:], kf[:], -TWO_PI, ang[:], ALU.mult, ALU.add, [b_a], [b_a])
                    k.ts('dve', kf[:], kf[:], shift, None, ALU.add, None, [b_a], [b_a])
                    k.ts('dve', dst[:], kf[:], float(np.pi), -TWO_PI, ALU.is_gt, ALU.mult, [b_a], [b_rt])
                    k.tt('dve', kf[:], kf[:], dst[:], ALU.add, [b_a, b_rt], [b_a])
                    k.ts('dve', dst[:], kf[:], -float(np.pi), TWO_PI, ALU.is_lt, ALU.mult, [b_a], [b_rt])
                    k.tt('dve', kf[:], kf[:], dst[:], ALU.add, [b_a, b_rt], [b_a])
                    k.act(dst[:], kf[:], AF.Sin, [b_a], [b_rt])
                k.ts('dve', cosq[:], cosT[:, OWN0:LOC], 128.0 ** -0.5, None, ALU.mult, None, [b_rt], [b_rt])
                k.ts('dve', sinq[:], sinT[:, OWN0:LOC], 128.0 ** -0.5, None, ALU.mult, None, [b_rt], [b_rt])

            def rope(dst_bf, X, bX, ct, st_, ntok, bdst, tmpa, tmpb, btmp):
                for tt in range(ntok // 512):
                    sl = slice(tt * 512, (tt + 1) * 512)
                    bk = tt % 3
                    k.mm(banks[bk][:, :], ropeP, X[:, sl], True, True, [bX, b_nc], [bbank[bk]])
                    k.tt('pool', tmpa[:, 0:512], X[:, sl], ct[:, sl], ALU.mult, [bX, b_rt], [btmp[0]])
                    k.tt('dve', tmpb[:, 0:512], banks[bk][:, :], st_[:, sl], ALU.mult, [bbank[bk], b_rt], [btmp[1]])
                    k.tt('dve', dst_bf[:, sl], tmpa[:, 0:512], tmpb[:, 0:512], ALU.add, [btmp[0], btmp[1]], [bdst])

            gts = k.sb([128, NQT, 48], F32, "gts")
            b_g = P.buf()
            k.dma('sp', gts[:], PT[OWN0:LOC, 1552:1600].rearrange("(n p) c -> p n c", p=128), (), [b_g])
            k.act(gts[:], gts[:], AF.Sigmoid, [b_g], [b_g])

            for g in range(2 if NSA_GROUPS_RUN is None else NSA_GROUPS_RUN):
                with k.scope():
                    KsT = k.sb([128, LOC], BF16, "KsT")
                    KwT = k.sb([128, LOC], BF16, "KwT")
                    Vs1 = k.sb([128, 32, 129], BF16, "Vs1")
                    Vw1 = k.sb([128, 32, 129], BF16, "Vw1")
                    KcmpT = k.sb([128, 256], BF16, "KcmpT")
                    RHSc = k.sb([128, 2, 193], BF16, "RHSc")
                    QTh = [k.sb([128, NOWN], BF16, "QTh%d" % i) for i in range(8)]
                    bKs, bKw, bVs, bVw, bKc, bRc = P.bufs(6)
                    bQh = P.bufs(8)
                    k.memset('dve', Vs1[:, :, 128:129], 1.0, [bVs])
                    k.memset('dve', Vw1[:, :, 128:129], 1.0, [bVw])
                    k.dma('pool', Vs1[:, :, 0:128], PT[:, 1040 + g * 128:1040 + (g + 1) * 128].rearrange("(n p) c -> p n c", p=128), (), [bVs])
                    k.dma('pool', Vw1[:, :, 0:128], PT[:, 1296 + g * 128:1296 + (g + 1) * 128].rearrange("(n p) c -> p n c", p=128), (), [bVw])
                    k.memset('dve', KcmpT[:], 0.0, [bKc])
                    k.cp('dve', RHSc[:, :, 0:65], ovl[:], [b_nc], [bRc])
                    with k.scope():
                        X = k.sb([128, LOC], F32, "ropeX")
                        tmpa = k.sb([128, 512], F32, "ropeA")
                        tmpb = k.sb([128, 512], F32, "ropeB")
                        KcT = k.sb([128, LOC], BF16, "KcT")
                        VcT = k.sb([128, LOC], BF16, "VcT")
                        bX, bKcT, bVcT = P.bufs(3)
                        btmp = P.bufs(2)
                        for (dstb, bd, row0) in [(KcT, bKcT, 5120 + g * 128), (KsT, bKs, 5632 + g * 128), (KwT, bKw, 5888 + g * 128)]:
                            k.dma('sp', X[:], PF[row0:row0 + 128, :], (), [bX])
                            rope(dstb, X, bX, cosT, sinT, LOC, bd, tmpa, tmpb, btmp)
                        k.dma('pool', VcT[:], PF[5376 + g * 128:5376 + (g + 1) * 128, :], (), [bVcT])
                        for hh in range(8):
                            row0 = 3072 + (g * 8 + hh) * 128
                            k.dma('sp', X[:, 0:NOWN], PF[row0:row0 + 128, OWN0:LOC], (), [bX])
                            rope(QTh[hh], X, bX, cosq, sinq, NOWN, bQh[hh], tmpa, tmpb, btmp)
                        w1 = k.sb([128, 32, 256], BF16, "cw1")
                        w2 = k.sb([128, 2, 128], BF16, "cw2")
                        cpT = k.sb([128, 32], F32, "cpT")
                        cpTb = k.sb([128, 32], BF16, "cpTb")
                        hidT = k.sb([128, 2, 256], BF16, "hidT")
                        hx = k.sb([128, 256], F32, "hx")
                        hy = k.sb([128, 256], F32, "hy")
                        hb = k.sb([128, 2], F32, "hb")
                        bw1, bw2, bcp, bhid, bhx, bhy, bhb = P.bufs(7)
                        for which, (srcT, bsrc) in enumerate([(KcT, bKcT), (VcT, bVcT)]):
                            k.dma('pool', w1[:], cw1_in[which].rearrange("(l d) h -> d l h", d=128), (), [bw1])
                            k.dma('pool', w2[:], cw2_in[which].rearrange("(t p) d -> p t d", p=128), (), [bw2])
                            load_fm(cpT[:], cpos_in[which], 32, bcp, bank=3)
                            k.cp('dve', cpTb[:], cpT[:], [bcp], [bcp])
                            k.memset('dve', hidT[:], 0.0, [bhid])
                            for ht in range(2):
                                for l in range(32):
                                    k.mm(banks[0][:, 0:255], w1[:, l, ht * 128:(ht + 1) * 128], srcT[:, l:l + 16 * 254 + 1:16],
                                         l == 0, l == 31, [bw1, bsrc], [bbank[0]])
                                for l in range(32):
                                    k.mm(banks[1][:, 0:1], w1[:, l, ht * 128:(ht + 1) * 128], cpTb[:, l:l + 1],
                                         l == 0, l == 31, [bw1, bcp], [bbank[1]])
                                k.cp('dve', hb[:, ht:ht + 1], banks[1][:, 0:1], [bbank[1]], [bhb])
                                k.act(hx[:, 0:255], banks[0][:, 0:255], AF.Identity, [bbank[0], bhb], [bhx], bias=hb[:, ht:ht + 1])
                                k.tt('dve', hy[:, 0:255], hx[:, 0:255], hx[:, 0:255], ALU.mult, [bhx], [bhy])
                                k.ts('dve', hy[:, 0:255], hy[:, 0:255], 0.044715, 1.0, ALU.mult, ALU.add, [bhy], [bhy])
                                k.tt('dve', hy[:, 0:255], hy[:, 0:255], hx[:, 0:255], ALU.mult, [bhy, bhx], [bhy])
                                k.act(hy[:, 0:255], hy[:, 0:255], AF.Sigmoid, [bhy], [bhy], scale=1.5957691216057308)
                                k.tt('dve', hidT[:, ht, 0:255], hy[:, 0:255], hx[:, 0:255], ALU.mult, [bhy, bhx], [bhid])
                            if which == 0:
                                for ht in range(2):
                                    k.mm(banks[2][:, 0:255], w2[:, ht, :], hidT[:, ht, 0:255], ht == 0, ht == 1, [bw2, bhid], [bbank[2]])
                                k.cp('dve', KcmpT[:, 0:255], banks[2][:, 0:255], [bbank[2]], [bKc])
                            else:
                                for j in range(2):
                                    nn = 128 if j == 0 else 127
                                    for ht in range(2):
                                        k.mm(banks[2][0:nn, 0:128], hidT[:, ht, j * 128:j * 128 + nn], w2[:, ht, :], ht == 0, ht == 1, [bw2, bhid], [bbank[2]])
                                    k.memset('dve', RHSc[:, j, 65:193], 0.0, [bRc])
                                    k.cp('dve', RHSc[0:nn, j, 65:193], banks[2][0:nn, 0:128], [bbank[2]], [bRc])
                    impacc = k.sb([128, 64], F32, "impacc")
                    v1 = k.sb([128, 64], F32, "selv1")
                    v2 = k.sb([128, 64], F32, "selv2")
                    m8 = k.sb([128, 16], F32, "m8")
                    selb = k.sb([128, 64], F32, "selb")
                    selbT = k.sb([64, 128], BF16, "selbT")
                    Oacc = k.sb([128, 8, 128], F32, "Oacc")
                    o16 = [k.sb([128, 128], BF16, "no16_%d" % i) for i in range(2)]
                    Et = [k.sb([128, 128], BF16, "Et%d" % i) for i in range(4)]
                    sc_ = k.sb([128, 8], F32, "nsc_small")
                    bimp, bsel, bselT, bO, bsc = P.bufs(5)
                    bo16 = P.bufs(2)
                    bEt = P.bufs(4)
                    ectr = [0]

                    def score_exp(lhsK, bK_, rhsQ, bQ_, extra, bias_ap):
                        e = ectr[0] % 4
                        bk = ectr[0] % 3
                        ectr[0] += 1
                        k.mm(banks[bk][:, 0:128], lhsK, rhsQ, True, len(extra) == 0, [bK_, bQ_], [bbank[bk]])
                        for xi, (l_, r_, bl_) in enumerate(extra):
                            k.mm(banks[bk][:, 0:128], l_, r_, False, xi == len(extra) - 1, bl_, [bbank[bk]])
                        if bias_ap is None:
                            k.act(Et[e][:], banks[bk][:, 0:128], AF.Exp, [bbank[bk]], [bEt[e]])
                        else:
                            k.act(Et[e][:], banks[bk][:, 0:128], AF.Exp, [bbank[bk], b_nc], [bEt[e]], bias=bias_ap)
                        return Et[e], bEt[e]

                    for qt in range(NQT if NSA_QT_RUN is None else NSA_QT_RUN):
                        qsl = slice(qt * 128, (qt + 1) * 128)
                        for hh in range(8):
                            head = g * 8 + hh
                            ets = []
                            for j in range(2):
                                u0 = (OWN0 + 128 * qt) if j == 0 else 128 * qt
                                ets.append(score_exp(KcmpT[:, j * 128:(j + 1) * 128], bKc, QTh[hh][:, qsl], bQh[hh],
                                                     [(identb[:], maskB[:, u0:u0 + 128], [b_const, b_nc])], kvb if j == 0 else None))
                            for j in range(2):
                                k.mm(banks[3][:, 0:193], ets[j][0][:], RHSc[:, j, :], j == 0, j == 1, [ets[j][1], bRc], [bbank[3]])
                            k.ts('dve', sc_[:, 0:1], banks[3][:, 64:65], 1e-30, None, ALU.max, None, [bbank[3]], [bsc])
                            k.recip(sc_[:, 1:2], sc_[:, 0:1], [bsc], [bsc])
                            if hh == 0:
                                k.ts('dve', impacc[:], banks[3][:, 0:64], sc_[:, 1:2], None, ALU.mult, None, [bbank[3], bsc], [bimp])
                            else:
                                k.stt(impacc[:], banks[3][:, 0:64], sc_[:, 1:2], impacc[:], ALU.mult, ALU.add, [bbank[3], bsc, bimp], [bimp])
                            k.ts('dve', sc_[:, 2:3], gts[:, qt, head * 3:head * 3 + 1], sc_[:, 1:2], None, ALU.mult, None, [b_g, bsc], [bsc])
                            k.ts('dve', Oacc[:, hh, :], banks[3][:, 65:193], sc_[:, 2:3], None, ALU.mult, None, [bbank[3], bsc], [bO])
                        k.tt('dve', v1[:], impacc[:], fv[:, 0, qt, :], ALU.max, [bimp, b_nc], [bsel])
                        k.tt('dve', v1[:], v1[:], ncore[:, 0:64], ALU.max, [bsel, b_nc], [bsel])
                        k.tt('dve', v1[:], v1[:], fv[:, 1, qt, :], ALU.min, [bsel, b_nc], [bsel])
                        k.tt('dve', v1[:], v1[:], ncore[:, 64:128], ALU.min, [bsel, b_nc], [bsel])
                        P.op('dve', lambda e: e.max(out=m8[:, 0:8], in_=v1[:]), [bsel], [bsel])
                        P.op('dve', lambda e: e.match_replace(out=v2[:], in_to_replace=m8[:, 0:8], in_values=v1[:], imm_value=-3.0e38), [bsel], [bsel])
                        P.op('dve', lambda e: e.max(out=m8[:, 8:16], in_=v2[:]), [bsel], [bsel])
                        k.ts('dve', v2[:], v1[:], m8[:, 15:16], None, ALU.is_ge, None, [bsel], [bsel])
                        k.ts('dve', selb[:], v1[:], -1.0e29, None, ALU.is_gt, None, [bsel], [bsel])
                        k.tt('dve', selb[:], selb[:], v2[:], ALU.mult, [bsel], [bsel])
                        k.ts('dve', selb[:], selb[:], -1.0, 30000.0, ALU.add, ALU.mult, [bsel], [bsel])
                        k.tr(banks[3][0:64, 0:128], selb[:], ident[:], [bsel, b_const], [bbank[3]])
                        k.cp('dve', selbT[:], banks[3][0:64, 0:128], [bbank[3]], [bselT])
                        for hh in range(8):
                            head = g * 8 + hh
                            kdiag = OWN0 // 128 + qt
                            for kt in range(0, kdiag + 1):
                                ex = [(eexp[:, kt * 128:(kt + 1) * 128], selbT[:], [b_nc, bselT])]
                                if kt == kdiag:
                                    ex.append((identb[:], cmk[:, 0, :], [b_const, b_nc]))
                                et, bet = score_exp(KsT[:, kt * 128:(kt + 1) * 128], bKs, QTh[hh][:, qsl], bQh[hh], ex, None)
                                k.mm(banks[4][:, 0:129], et[:], Vs1[:, kt, :], kt == 0, kt == kdiag, [bet, bVs], [bbank[4]])
                            k.ts('dve', sc_[:, 3:4], banks[4][:, 128:129], 1e-30, None, ALU.max, None, [bbank[4]], [bsc])
                            k.recip(sc_[:, 4:5], sc_[:, 3:4], [bsc], [bsc])
                            k.ts('dve', sc_[:, 4:5], sc_[:, 4:5], gts[:, qt, head * 3 + 1:head * 3 + 2], None, ALU.mult, None, [b_g, bsc], [bsc])
                            k.stt(Oacc[:, hh, :], banks[4][:, 0:128], sc_[:, 4:5], Oacc[:, hh, :], ALU.mult, ALU.add, [bbank[4], bsc, bO], [bO])
                            for kt in range(kdiag - 4, kdiag + 1):
                                ex = []
                                if kt == kdiag - 4:
                                    ex.append((identb[:], cmk[:, 1, :], [b_const, b_nc]))
                                if kt == kdiag:
                                    ex.append((identb[:], cmk[:, 0, :], [b_const, b_nc]))
                                et, bet = score_exp(KwT[:, kt * 128:(kt + 1) * 128], bKw, QTh[hh][:, qsl], bQh[hh], ex,
                                                    kvb if kt < OWN0 // 128 else None)
                                k.mm(banks[5][:, 0:129], et[:], Vw1[:, kt, :], kt == kdiag - 4, kt == kdiag, [bet, bVw], [bbank[5]])
                            k.ts('dve', sc_[:, 5:6], banks[5][:, 128:129], 1e-30, None, ALU.max, None, [bbank[5]], [bsc])
                            k.recip(sc_[:, 6:7], sc_[:, 5:6], [bsc], [bsc])
                            k.ts('dve', sc_[:, 6:7], sc_[:, 6:7], gts[:, qt, head * 3 + 2:head * 3 + 3], None, ALU.mult, None, [b_g, bsc], [bsc])
                            k.stt(Oacc[:, hh, :], banks[5][:, 0:128], sc_[:, 6:7], Oacc[:, hh, :], ALU.mult, ALU.add, [bbank[5], bsc, bO], [bO])
                            oi = hh % 2
                            k.tr(banks[3][:, 0:128], Oacc[:, hh, :], ident[:], [bO, b_const], [bbank[3]])
                            k.cp('act', o16[oi][:], banks[3][:, 0:128], [bbank[3]], [bo16[oi]])
                            k.dma('sp', OBT[head * 128:(head + 1) * 128, qsl], o16[oi][:], [bo16[oi]], ())
        if stage == 4 and DEBUG_OUT and not SKIP_NSA:
            dbg_ob = nc.dram_tensor("dbg_ob", [2048, NOWN], BF16, kind="ExternalOutput").ap()
            k.dma('sp', dbg_ob, OBT, (), ())

    def bcast_row(dst, src_fm, n, bsrc, bdst, name):
        rowd = k.dram([1, n * 128], F32, "row_" + name)
        t_ = P.dma('sp', rowd[0].rearrange("(c p) -> p c", p=128), src_fm, [bsrc], (), allow_slow_non_contiguous=True)
        brow = P.buf()
        brow.lw = t_
        k.dma('sp', dst, rowd[0].partition_broadcast(128), [brow], [bdst])

    if stage >= 5 and not SKIP_P5:
        wbg_in = din("w_branch_gdn", [1024, D])
        wbn_in = din("w_branch_nsa", [D, D])
        wout_in = din("w_out", [D, D])
        with k.scope():
            Wg = k.sb([128, 8, D], BF16, "Wg")
            Wn = k.sb([128, 16, D], BF16, "Wn")
            bWg, bWn = P.bufs(2)
            for kc in range(8):
                k.dma('pool', Wg[:, kc, :], wbg_in[kc * 128:(kc + 1) * 128, :], (), [bWg])
            for kc in range(16):
                k.dma('pool', Wn[:, kc, :], wbn_in[kc * 128:(kc + 1) * 128, :], (), [bWn])
            oat = [k.sb([128, 8, 512], BF16, "oat%d" % i) for i in range(2)]
            obt = [k.sb([128, 16, 512], BF16, "obt%d" % i) for i in range(2)]
            boat, bobt = P.bufs(2), P.bufs(2)
            ga = [k.sb([128, 512], F32, "ga%d" % i) for i in range(2)]
            gb = [k.sb([128, 512], F32, "gb%d" % i) for i in range(2)]
            y16 = [k.sb([128, 512], BF16, "y16_%d" % i) for i in range(2)]
            bga, bgb, by16 = P.bufs(2), P.bufs(2), P.bufs(2)
            OATv = OAT.rearrange("(kc p) t -> p kc t", p=128)
            OBTv = OBT.rearrange("(kc p) t -> p kc t", p=128)
            it = 0
            for tt in range(NOWN // 512):
                s2 = tt % 2
                tsl = slice(tt * 512, (tt + 1) * 512)
                k.dma('sp', oat[s2][:], OATv[:, :, tsl], (), [boat[s2]])
                k.dma('sp', obt[s2][:], OBTv[:, :, tsl], (), [bobt[s2]])
                for ct in range(16):
                    s = it % 2
                    it += 1
                    bA, bB = (0, 1) if s == 0 else (2, 3)
                    csl = slice(ct * 128, (ct + 1) * 128)
                    k.dma('sp', ga[s][:], PF[6144 + ct * 128:6144 + (ct + 1) * 128, OWN0 + tt * 512:OWN0 + (tt + 1) * 512], (), [bga[s]])
                    k.dma('sp', gb[s][:], PF[8192 + ct * 128:8192 + (ct + 1) * 128, OWN0 + tt * 512:OWN0 + (tt + 1) * 512], (), [bgb[s]])
                    k.act(ga[s][:], ga[s][:], AF.Sigmoid, [bga[s]], [bga[s]])
                    k.act(gb[s][:], gb[s][:], AF.Sigmoid, [bgb[s]], [bgb[s]])
                    for kc in range(8):
                        k.mm(banks[bA][:, :], Wg[:, kc, csl], oat[s2][:, kc, :], kc == 0, kc == 7, [bWg, boat[s2]], [bbank[bA]])
                    for kc in range(16):
                        k.mm(banks[bB][:, :], Wn[:, kc, csl], obt[s2][:, kc, :], kc == 0, kc == 15, [bWn, bobt[s2]], [bbank[bB]])
                    k.tt('dve', ga[s][:], ga[s][:], banks[bA][:, :], ALU.mult, [bga[s], bbank[bA]], [bga[s]])
                    k.tt('dve', gb[s][:], gb[s][:], banks[bB][:, :], ALU.mult, [bgb[s], bbank[bB]], [bgb[s]])
                    k.tt('pool', y16[s][:], ga[s][:], gb[s][:], ALU.add, [bga[s], bgb[s]], [by16[s]])
                    k.dma('sp', YT[csl, tsl], y16[s][:], [by16[s]], ())
        with k.scope():
            Wo = k.sb([128, 16, D], BF16, "Wo")
            g1b = k.sb([128, D], F32, "g1b")
            bWo, bg1 = P.bufs(2)
            for kc in range(16):
                k.dma('pool', Wo[:, kc, :], wout_in[kc * 128:(kc + 1) * 128, :], (), [bWo])
            bcast_row(g1b[:], modT[:, 32:48], 16, b_mod, bg1, "g1")
            yt = [k.sb([128, 16, 128], BF16, "yt%d" % i) for i in range(2)]
            xo = [k.sb([128, D], F32, "xo%d" % i) for i in range(2)]
            zt_ = [k.sb([128, 512], F32, "zt5_%d" % i) for i in range(2)]
            byt, bxo, bzt = P.bufs(2), P.bufs(2), P.bufs(2)
            YTv = YT.rearrange("(kc p) t -> p kc t", p=128)
            it = 0
            for t in range(NOWN // 128):
                s = t % 2
                k.dma('sp', yt[s][:], YTv[:, :, t * 128:(t + 1) * 128], (), [byt[s]])
                k.dma('sp', xo[s][:], xl[OWN0 + t * 128:OWN0 + (t + 1) * 128, :], (), [bxo[s]])
                for cb_ in range(4):
                    z2 = it % 2
                    bk = it % 4
                    it += 1
                    csl = slice(cb_ * 512, (cb_ + 1) * 512)
                    for kc in range(16):
                        k.mm(banks[bk][:, :], yt[s][:, kc, :], Wo[:, kc, csl], kc == 0, kc == 15, [byt[s], bWo], [bbank[bk]])
                    k.tt('dve', zt_[z2][:], banks[bk][:, :], g1b[:, csl], ALU.mult, [bbank[bk], bg1], [bzt[z2]])
                    k.tt('pool', xo[s][:, csl], xo[s][:, csl], zt_[z2][:], ALU.add, [bxo[s], bzt[z2]], [bxo[s]])
                k.dma('sp', X1[t * 128:(t + 1) * 128, :], xo[s][:], [bxo[s]], ())
        P.barrier()
        if stage == 5 and DEBUG_OUT:
            dbg_x1 = nc.dram_tensor("dbg_x1", [NOWN, D], F32, kind="ExternalOutput").ap()
            k.dma('sp', dbg_x1, X1, (), ())

    if stage == 6 and DEBUG_OUT:
        X2dbg = nc.dram_tensor("dbg_x2", [NOWN, D], F32, kind="ExternalOutput").ap()
    if stage >= 6:
        n2_in = din("norm2_w", [KC, 128])
        wq_in = din("peer_wq", [D, D])
        pk_in = [din("peer_keys1", [8, 128, 128]), din("peer_keys2", [8, 128, 128])]
        uT_in = din("peer_uT", [D, 16384])
        pv_in = din("peer_v", [16384, D])
        fnw6_in = din("final_norm_w", [1, D])
        H2T = k.dram([KC, 128, NOWN], BF16, "H2T")
        NEB = 64 if PEER_EB is None else PEER_EB
        with k.scope():
            A2 = k.sb([128, KC], F32, "A2")
            n2T = k.sb([128, KC], F32, "n2T")
            g2b = k.sb([128, D], F32, "g2b")
            fnw6 = k.sb([128, D], F32, "fnw6")
            keysT = k.sb([128, 16, 128], BF16, "keysT")
            bA2, bn2, bg2, bfn, bkT = P.bufs(5)
            load_fm(n2T[:], n2_in, KC, bn2)
            k.stt(A2[:], modT[:, 64:80], 1.0, n2T[:], ALU.add, ALU.mult, [b_mod, bn2], [bA2])
            bcast_row(g2b[:], modT[:, 80:96], 16, b_mod, bg2, "g2")
            k.dma('sp', fnw6[:], fnw6_in[0].partition_broadcast(128), (), [bfn])
            with k.scope():
                kt_ = [k.sb([128, 128], F32, "kraw%d" % i) for i in range(2)]
                bkr = P.bufs(2)
                for hh2 in range(16):
                    s = hh2 % 2
                    k.dma('sp', kt_[s][:], pk_in[hh2 % 2][hh2 // 2], (), [bkr[s]])
                    k.tr(banks[s][:, 0:128], kt_[s][:], ident[:], [bkr[s], b_const], [bbank[s]])
                    k.cp('dve', keysT[:, hh2, :], banks[s][:, 0:128], [bbank[s]], [bkT])
            with k.scope():
                xt = [k.sb([128, D], F32, "x6t%d" % i) for i in range(2)]
                xs = [k.sb([128, D], BF16, "x6s%d" % i) for i in range(2)]
                st = [k.sb([128, 4], F32, "s6t%d" % i) for i in range(2)]
                ho = [k.sb([128, KC, 128], BF16, "h6o%d" % i) for i in range(2)]
                bxt, bxs, bst, bho = P.bufs(2), P.bufs(2), P.bufs(2), P.bufs(2)
                for t in range(NOWN // 128):
                    s = t % 2
                    k.dma('sp', xt[s][:], X1[t * 128:(t + 1) * 128, :], (), [bxt[s]])
                    k.act(xs[s][:], xt[s][:], AF.Square, [bxt[s]], [bxs[s], bst[s]], accum=st[s][:, 0:1])
                    k.ts('dve', st[s][:, 1:2], st[s][:, 0:1], 1.0 / D, 1e-6, ALU.mult, ALU.add, [bst[s]], [bst[s]])
                    k.act(st[s][:, 2:3], st[s][:, 1:2], AF.Sqrt, [bst[s]], [bst[s]])
                    k.recip(st[s][:, 3:4], st[s][:, 2:3], [bst[s]], [bst[s]])
                    k.ts('dve', xs[s][:], xt[s][:], st[s][:, 3:4], None, ALU.mult, None, [bxt[s], bst[s]], [bxs[s]])
                    for kc in range(KC):
                        pi = kc % 2
                        sl = slice((kc // 2 % 4) * 128, (kc // 2 % 4) * 128 + 128)
                        k.tr(pbf[pi][:, sl], xs[s][:, kc * 128:(kc + 1) * 128], identb[:], [bxs[s], b_const], [bpbf[pi]])
                        if kc % 2 == 0:
                            k.act(ho[s][:, kc, :], pbf[pi][:, sl], AF.Identity, [bpbf[pi], bA2, b_mod], [bho[s]],
                                  bias=modT[:, 48 + kc:49 + kc], scale=A2[:, kc:kc + 1])
                        else:
                            k.ts('dve', ho[s][:, kc, :], pbf[pi][:, sl], A2[:, kc:kc + 1], modT[:, 48 + kc:49 + kc],
                                 ALU.mult, ALU.add, [bpbf[pi], bA2, b_mod], [bho[s]])
                    k.dma('sp', H2T[:, :, t * 128:(t + 1) * 128].rearrange("c p t -> p c t"), ho[s][:], [bho[s]], ())
            P.barrier()
            wqv = wq_in.rearrange("(kc p) c -> p kc c", p=128)
            uTv = uT_in.rearrange("(kc p) e -> p kc e", p=128)
            outv6 = out_d.rearrange("(t p) d -> t p d", p=128)
            for tg in range(4 if PEER_TG is None else PEER_TG):
                with k.scope():
                    h2g = k.sb([128, KC, 512], BF16, "h2g")
                    stile = k.sb([128, 4, 16, 128], F32, "stile")
                    Bt = k.sb([128, 4, 8, 128], F32, "Bt")
                    tau = k.sb([128, 4, 8], F32, "tau")
                    OUTacc = k.sb([128, 4, D], F32, "OUTacc")
                    bh2, bst_, bBt, btau, bOut = P.bufs(5)
                    k.dma('sp', h2g[:], H2T[:, :, tg * 512:(tg + 1) * 512].rearrange("c p t -> p c t"), (), [bh2])
                    k.memset('pool', OUTacc[:], 0.0, [bOut])
                    with k.scope():
                        wqb = [k.sb([128, KC, 128], BF16, "wqb%d" % i) for i in range(2)]
                        qh = [k.sb([128, 512], BF16, "qh%d" % i) for i in range(2)]
                        bwq, bqh = P.bufs(2), P.bufs(2)
                        for hh2 in range(16):
                            s = hh2 % 2
                            k.dma('pool', wqb[s][:], wqv[:, :, hh2 * 128:(hh2 + 1) * 128], (), [bwq[s]])
                            for kc in range(KC):
                                k.mm(banks[s][:, :], wqb[s][:, kc, :], h2g[:, kc, :], kc == 0, kc == KC - 1, [bwq[s], bh2], [bbank[s]])
                            k.cp('act', qh[s][:], banks[s][:, :], [bbank[s]], [bqh[s]])
                            for tl in range(4):
                                k.mm(banks[2 + s][:, tl * 128:(tl + 1) * 128], qh[s][:, tl * 128:(tl + 1) * 128], keysT[:, hh2, :], True, True,
                                     [bqh[s], bkT], [bbank[2 + s]])
                            k.cp('dve', stile[:, :, hh2, :], banks[2 + s][:, :].rearrange("p (t j) -> p t j", t=4), [bbank[2 + s]], [bst_])
                    with k.scope():
                        v16 = k.sb([128, 2, 16], F32, "v16")
                        vv = k.sb([128, 128], F32, "vv")
                        cand = k.sb([128, 16, 16], F32, "cand")
                        cv = k.sb([128, 256], F32, "cv")
                        c16 = k.sb([128, 16], F32, "c16")
                        sm = k.sb([128, 8], F32, "sm")
                        bsel6 = P.buf()
                        for tl in range(4):
                            for h in range(8):
                                for half in range(2):
                                    src = stile[:, tl, 2 * h + half, :]
                                    P.op('dve', lambda e, src=src, half=half: e.max(out=v16[:, half, 0:8], in_=src), [bst_], [bsel6])
                                    P.op('dve', lambda e, src=src, half=half: e.match_replace(out=vv[:], in_to_replace=v16[:, half, 0:8], in_values=src, imm_value=-3.0e38), [bst_, bsel6], [bsel6])
                                    P.op('dve', lambda e, half=half: e.max(out=v16[:, half, 8:16], in_=vv[:]), [bsel6], [bsel6])
                                for a in range(16):
                                    k.ts('dve', cand[:, a, :], v16[:, 1, :], v16[:, 0, a:a + 1], None, ALU.add, None, [bsel6], [bsel6])
                                cf = cand[:].rearrange("p a b -> p (a b)")
                                P.op('dve', lambda e, cf=cf: e.max(out=c16[:, 0:8], in_=cf), [bsel6], [bsel6])
                                P.op('dve', lambda e, cf=cf: e.match_replace(out=cv[:], in_to_replace=c16[:, 0:8], in_values=cf, imm_value=-3.0e38), [bsel6], [bsel6])
                                P.op('dve', lambda e: e.max(out=c16[:, 8:16], in_=cv[:]), [bsel6], [bsel6])
                                k.ts('dve', sm[:, 0:1], c16[:, 0:1], -1.0, None, ALU.mult, None, [bsel6], [bsel6])
                                k.act(cv[:, 0:16], c16[:], AF.Exp, [bsel6], [bsel6], bias=sm[:, 0:1], accum=sm[:, 1:2])
                                k.act(sm[:, 2:3], sm[:, 1:2], AF.Ln, [bsel6], [bsel6])
                                k.tt('dve', sm[:, 3:4], sm[:, 0:1], sm[:, 2:3], ALU.subtract, [bsel6], [bsel6])
                                k.ts('dve', Bt[:, tl, h, :], stile[:, tl, 2 * h, :], sm[:, 3:4], None, ALU.add, None, [bst_, bsel6], [bBt])
                                k.act(sm[:, 4:5], c16[:, 15:16], AF.Exp, [bsel6], [bsel6], bias=sm[:, 3:4])
                                k.ts('dve', tau[:, tl, h:h + 1], sm[:, 4:5], 0.99999, None, ALU.mult, None, [bsel6], [btau])
                    with k.scope():
                        ub = [k.sb([128, KC, 256], BF16, "ub%d" % i) for i in range(2)]
                        vb = [k.sb([128, 2, D], BF16, "vb%d" % i) for i in range(2)]
                        bub, bvb = P.bufs(2), P.bufs(2)
                        gx = k.sb([128, 256], F32, "gx")
                        gy = k.sb([128, 256], F32, "gy")
                        es = k.sb([128, 8, 128], F32, "es")
                        mk = k.sb([128, 8, 128], F32, "mk")
                        coef = k.sb([128, 2, 128], F32, "coef")
                        G16 = k.sb([128, 256], BF16, "G16")
                        GT = [k.sb([128, 128], BF16, "GT%d" % i) for i in range(2)]
                        bgx, bgy, bes, bmk, bcoef, bG16 = P.bufs(6)
                        bGT = P.bufs(2)
                        for eb in range(NEB):
                            s = eb % 2
                            k.dma('pool', ub[s][:], uTv[:, :, eb * 256:(eb + 1) * 256], (), [bub[s]])
                            k.dma('pool', vb[s][:], pv_in[eb * 256:(eb + 1) * 256, :].rearrange("(i p) d -> p i d", p=128), (), [bvb[s]])
                            for tl in range(4):
                                for kc in range(KC):
                                    k.mm(banks[0][:, 0:256], h2g[:, kc, tl * 128:(tl + 1) * 128], ub[s][:, kc, :], kc == 0, kc == KC - 1,
                                         [bh2, bub[s]], [bbank[0]])
                                k.cp('act', gx[:], banks[0][:, 0:256], [bbank[0]], [bgx])
                                k.tt('pool', gy[:], gx[:], gx[:], ALU.mult, [bgx], [bgy])
                                k.ts('pool', gy[:], gy[:], 0.044715, 1.0, ALU.mult, ALU.add, [bgy], [bgy])
                                k.tt('pool', gy[:], gy[:], gx[:], ALU.mult, [bgy, bgx], [bgy])
                                k.act(gy[:], gy[:], AF.Sigmoid, [bgy], [bgy], scale=1.5957691216057308)
                                k.tt('pool', gy[:], gy[:], gx[:], ALU.mult, [bgy, bgx], [bgy])
                                for il in range(2):
                                    i_ = 2 * eb + il
                                    k.tt('pool', es[:], stile[:, tl, 1:16:2, :], Bt[:, tl, :, i_:i_ + 1].to_broadcast([128, 8, 128]), ALU.add,
                                         [bst_, bBt], [bes])
                                    k.act(es[:], es[:], AF.Exp, [bes], [bes])
                                    k.tt('dve', mk[:], es[:], tau[:, tl, :].unsqueeze(2).to_broadcast([128, 8, 128]), ALU.is_ge, [bes, btau], [bmk])
                                    k.tt('dve', mk[:], mk[:], es[:], ALU.mult, [bmk, bes], [bmk])
                                    P.op('dve', lambda e, il=il: e.tensor_reduce(out=coef[:, il, :], in_=mk[:].rearrange("p h j -> p j h"),
                                                                                 axis=AX.X, op=ALU.add), [bmk], [bcoef])
                                k.tt('dve', G16[:], gy[:], coef[:].rearrange("p i j -> p (i j)"), ALU.mult, [bgy, bcoef], [bG16])
                                for il in range(2):
                                    k.tr(pbf[il][:, 0:128], G16[:, il * 128:(il + 1) * 128], identb[:], [bG16, b_const], [bpbf[il]])
                                    k.cp('act', GT[il][:], pbf[il][:, 0:128], [bpbf[il]], [bGT[il]])
                                for cb_ in range(4):
                                    bk = 2 + cb_
                                    csl = slice(cb_ * 512, (cb_ + 1) * 512)
                                    k.mm(banks[bk][:, :], GT[0][:], vb[s][:, 0, csl], True, False, [bGT[0], bvb[s]], [bbank[bk]])
                                    k.mm(banks[bk][:, :], GT[1][:], vb[s][:, 1, csl], False, True, [bGT[1], bvb[s]], [bbank[bk]])
                                    k.tt('dve', OUTacc[:, tl, csl], OUTacc[:, tl, csl], banks[bk][:, :], ALU.add, [bOut, bbank[bk]], [bOut])
                    with k.scope():
                        x1t = [k.sb([128, D], F32, "x1t%d" % i) for i in range(2)]
                        fj6 = [k.sb([128, D], BF16, "fj6%d" % i) for i in range(2)]
                        fs6 = [k.sb([128, 4], F32, "fs6%d" % i) for i in range(2)]
                        bx1t, bfj6, bfs6 = P.bufs(2), P.bufs(2), P.bufs(2)
                        for tl in range(4):
                            s = tl % 2
                            t = tg * 4 + tl
                            k.dma('sp', x1t[s][:], X1[t * 128:(t + 1) * 128, :], (), [bx1t[s]])
                            k.tt('dve', OUTacc[:, tl, :], OUTacc[:, tl, :], g2b[:], ALU.mult, [bOut, bg2], [bOut])
                            k.tt('dve', x1t[s][:], x1t[s][:], OUTacc[:, tl, :], ALU.add, [bx1t[s], bOut], [bx1t[s]])
                            if stage == 6 and DEBUG_OUT:
                                k.dma('sp', X2dbg[t * 128:(t + 1) * 128, :], x1t[s][:], [bx1t[s]], ())
                            k.act(fj6[s][:], x1t[s][:], AF.Square, [bx1t[s]], [bfj6[s], bfs6[s]], accum=fs6[s][:, 0:1])
                            k.ts('dve', fs6[s][:, 1:2], fs6[s][:, 0:1], 1.0 / D, 1e-6, ALU.mult, ALU.add, [bfs6[s]], [bfs6[s]])
                            k.act(fs6[s][:, 2:3], fs6[s][:, 1:2], AF.Sqrt, [bfs6[s]], [bfs6[s]])
                            k.recip(fs6[s][:, 3:4], fs6[s][:, 2:3], [bfs6[s]], [bfs6[s]])
                            k.stt(x1t[s][:], x1t[s][:], fs6[s][:, 3:4], fnw6[:], ALU.mult, ALU.mult, [bx1t[s], bfs6[s], bfn], [bx1t[s]])
                            k.dma('sp', outv6[t], x1t[s][:], [bx1t[s]], ())

    if stage < 6:
      pass
    fnw_in = din("final_norm_w", [1, D]) if stage < 6 else None
    fnw = k.sb([128, D], F32, "fnw")
    b_fnw = P.buf()
    if stage < 6:
        k.dma('sp', fnw[:], fnw_in[0].partition_broadcast(128), (), [b_fnw])
    X2v = xl.rearrange("(t p) d -> t p d", p=128)
    outv = out_d.rearrange("(t p) d -> t p d", p=128)
    ft = [k.sb([128, D], F32, "ft%d" % i) for i in range(2)]
    fj = [k.sb([128, D], BF16, "fj%d" % i) for i in range(2)]
    fs = [k.sb([128, 4], F32, "fs%d" % i) for i in range(2)]
    bft, bfj, bfs = P.bufs(2), P.bufs(2), P.bufs(2)
    for t in range(NOWN // 128 if stage < 6 else 0):
        s = t % 2
        k.dma('sp', ft[s][:], X2v[OWN0 // 128 + t], (), [bft[s]])
        k.act(fj[s][:], ft[s][:], AF.Square, [bft[s]], [bfj[s], bfs[s]], accum=fs[s][:, 0:1])
        k.ts('dve', fs[s][:, 1:2], fs[s][:, 0:1], 1.0 / D, 1e-6, ALU.mult, ALU.add, [bfs[s]], [bfs[s]])
        k.act(fs[s][:, 2:3], fs[s][:, 1:2], AF.Sqrt, [bfs[s]], [bfs[s]])
        k.recip(fs[s][:, 3:4], fs[s][:, 2:3], [bfs[s]], [bfs[s]])
        k.stt(ft[s][:], ft[s][:], fs[s][:, 3:4], fnw[:], ALU.mult, ALU.mult, [bft[s], bfs[s], b_fnw], [bft[s]])
        k.dma('sp', outv[t], ft[s][:], [bft[s]], ())

    with nc.Block() as block:
        P.emit(block)
    k.es.close()
    return nc


def _gconst():
    NEGM = -30000.0
    r = np.arange(128)[:, None]
    c = np.arange(128)[None, :]
    g = np.zeros((128, 5, 128), np.float32)
    g[:, 4, :] = (r // 32 == c // 32)
    g[:, 0, :] = (r <= c)
    g[:, 1, :] = np.where(r > c, 0.0, NEGM)
    g[:, 2, :] = np.where(c > r, 0.0, NEGM)
    g[:, 3, :] = np.where(c >= r, 0.0, NEGM)
    return g


GCONST = _gconst()


def _nsa_consts():
    NEGM = -30000.0
    c = {}
    n128 = np.zeros((128, 385), np.float32)
    for m in range(16):
        n128[m + 16, m] = -1.0
    for m in range(16, 32):
        n128[m - 16, m] = 1.0
    half = 16
    inv_freq = (500000.0 ** (-np.arange(half, dtype=np.float32) / half)).astype(np.float32)
    for d in range(32):
        n128[d, 128] = inv_freq[d % 16]
    r = np.arange(128)[:, None]
    q = np.arange(128)[None, :]
    n128[:, 129:257] = np.where(r <= q, 0.0, NEGM)
    n128[:, 257:385] = np.where(r > q, 0.0, NEGM)
    c["nsa_c128"] = n128
    u = np.arange(LOC)[None, :]
    c["maskB"] = np.where(16 * r + 31 <= u, 0.0, NEGM).astype(np.float32)
    c["eexp"] = (np.arange(LOC)[None, :] // 64 == np.arange(64)[:, None]).astype(np.float32)
    ovl = np.zeros((128, 2, 65), np.float32)
    for j in range(2):
        for nl in range(128):
            n = j * 128 + nl
            if n >= 255:
                continue
            for sblk in range(64):
                if 16 * n < 64 * sblk + 64 and 16 * n + 32 > 64 * sblk:
                    ovl[nl, j, sblk] = 1.0
            ovl[nl, j, 64] = 1.0
    c["ovl"] = ovl
    fvv = np.zeros((128, 2, 16, 64), np.float32)
    for qt in range(16):
        for qq in range(128):
            t = OWN0 + qt * 128 + qq
            cur = t // 64
            for sblk in range(64):
                fb = 0.0
                if sblk == cur:
                    fb = 2.0e9
                elif sblk == cur - 1:
                    fb = 3.0e9
                fvv[qq, 0, qt, sblk] = fb
                fvv[qq, 1, qt, sblk] = 3.0e38 if 64 * sblk <= t else -1.0e30
    c["fbigvis"] = fvv
    return c


NSA_CONSTS = _nsa_consts()


def _nsa_core(half):
    a = np.zeros((128, 129), np.float32)
    first = 0 if half == 1 else 32
    a[:, first] = 1.0e9
    a[:, 64:128] = 3.0e38
    if half == 0:
        a[:, 64:64 + 32] = -1.0e30
        a[:, 128] = -30000.0
    return a


def _pos_loc(inp, b, half):
    p = np.asarray(inp['positions'], np.int32)[b]
    out = np.zeros((1, LOC), np.int32)
    if half == 1:
        out[0] = p
    else:
        out[0, OWN0:] = p[:NOWN]
    return out


PEER_UT_CACHE = {}


def make_in_maps(inp):
    x = np.asarray(inp['x'], np.float32)
    c = np.asarray(inp['c'], np.float32)
    maps = []
    ident = np.eye(128, dtype=np.float32)
    for core in range(8):
        b, half = core // 2, core % 2
        xl = np.zeros((LOC, D), np.float32)
        if half == 0:
            xl[OWN0:] = x[b, :NOWN]
        else:
            xl[:] = x[b]
        m = {
            "xl": xl,
            "cb": np.ascontiguousarray(c[b].reshape(KC, 128)),
            "ada_w": np.ascontiguousarray(inp['ada_w'][0]),
            "ada_b": np.ascontiguousarray(np.asarray(inp['ada_b'][0]).reshape(96, 128)),
            "norm1_w": np.ascontiguousarray(np.asarray(inp['norm1_w'][0]).reshape(KC, 128)),
            "w_in": np.ascontiguousarray(inp['w_in'][0]),
            "ident": ident,
            "pvalid": np.full((128, 1), float(half), np.float32),
            "w_branch_gdn": np.ascontiguousarray(inp['w_branch_gdn'][0], np.float32),
            "w_branch_nsa": np.ascontiguousarray(inp['w_branch_nsa'][0], np.float32),
            "w_out": np.ascontiguousarray(inp['w_out'][0], np.float32),
            "norm2_w": np.ascontiguousarray(np.asarray(inp['norm2_w'][0], np.float32).reshape(KC, 128)),
            "peer_wq": np.ascontiguousarray(inp['peer_wq'][0], np.float32),
            "peer_keys1": np.ascontiguousarray(inp['peer_keys1'][0], np.float32),
            "peer_keys2": np.ascontiguousarray(inp['peer_keys2'][0], np.float32),
            "peer_uT": PEER_UT_CACHE.get(id(inp['peer_u'])) if id(inp['peer_u']) in PEER_UT_CACHE else PEER_UT_CACHE.setdefault(id(inp['peer_u']), np.ascontiguousarray(np.asarray(inp['peer_u'][0], np.float32).T)),
            "peer_v": np.ascontiguousarray(inp['peer_v'][0], np.float32),
            "gconst": GCONST,
            "pos": np.ascontiguousarray(_pos_loc(inp, b, half)),
            "nsa_core": _nsa_core(half),
            "cmp_pos_k": np.ascontiguousarray(inp['cmp_pos_k'][0], np.float32), "cmp_pos_v": np.ascontiguousarray(inp['cmp_pos_v'][0], np.float32),
            "cmp_w1_k": np.ascontiguousarray(inp['cmp_w1_k'][0], np.float32), "cmp_w1_v": np.ascontiguousarray(inp['cmp_w1_v'][0], np.float32),
            "cmp_w2_k": np.ascontiguousarray(inp['cmp_w2_k'][0], np.float32), "cmp_w2_v": np.ascontiguousarray(inp['cmp_w2_v'][0], np.float32),
            **NSA_CONSTS,
            "gdn_conv_w": np.ascontiguousarray(np.asarray(inp['gdn_conv_w'][0], np.float32).reshape(96, 128)),
            "gdn_A_log": np.ascontiguousarray(np.broadcast_to(np.asarray(inp['gdn_A_log'][0], np.float32)[None, :], (32, 8))),
            "gdn_dt_bias": np.ascontiguousarray(np.broadcast_to(np.asarray(inp['gdn_dt_bias'][0], np.float32)[None, :], (32, 8))),
            "gdn_norm_w": np.ascontiguousarray(np.asarray(inp['gdn_norm_w'][0], np.float32).reshape(1, 128)),
            "final_norm_w": np.ascontiguousarray(np.asarray(inp['final_norm_w'], np.float32).reshape(1, D)),
        }
        maps.append(m)
    return maps


def kernel(**inputs):
    nc = build_program(6)
    maps = make_in_maps(inputs)
    res = run_bass_kernel_spmd(nc, maps, core_ids=list(range(8)))
    out = np.zeros((4, SEQ, D), np.float32)
    for core in range(8):
        b, half = core // 2, core % 2
        out[b, half * NOWN:(half + 1) * NOWN] = res.results[core]["out"]
    return out
```

```python
import numpy as np
from contextlib import ExitStack
import concourse.bass as bass
import concourse.mybir as mybir
from concourse.bass_utils import run_bass_kernel_spmd

F32 = mybir.dt.float32
BF16 = mybir.dt.bfloat16
I32 = mybir.dt.int32
ALU = mybir.AluOpType
AF = mybir.ActivationFunctionType
AX = mybir.AxisListType

ENGS = ['pe', 'act', 'dve', 'pool', 'sp']

D = 2048
SEQ = 4096
LOC = 4096
OWN0 = 2048
NOWN = 2048
D_IN = 11840
KC = 16
DEBUG_OUT = False
GDN_ONE_HEAD = False
FROM_REF = False
GDN_SUB = 9
PF_ROWS = 80 * 128
GDN_WAVES = None
GDN_STEPS = 10 ** 9
NSA_GROUPS_RUN = None
NSA_QT_RUN = None
SKIP_GDN = False
SKIP_NSA = False
SKIP_P5 = False
REF_IN = set()
PEER_EB = None
PEER_TG = None


class Buf:
    __slots__ = ('name', 'lw', 'rd', 'excl')

    def __init__(self, name):
        self.name = name
        self.lw = None
        self.rd = {}
        self.excl = False


class Prog:
    def __init__(self, nc, n_dma_sems=40):
        self.nc = nc
        self.ops = {e: [] for e in ENGS}
        self.cnt = {e: 0 for e in ENGS}
        self.esem = {e: nc.alloc_semaphore(name="es_" + e) for e in ENGS}
        self.dsem = [nc.alloc_semaphore(name="ds_%d" % i) for i in range(n_dma_sems)]
        self.dval = [0] * n_dma_sems
        self.dnext = 0
        self.bsem = [nc.alloc_semaphore(name="bs_%d" % i) for i in range(8)]
        self.bval = [0] * 8
        self.bnext = 0
        self.waited = {e: {} for e in ENGS}
        self.pend = {e: [] for e in ENGS}
        self.nbuf = 0

    def barrier(self):
        snap = [(('e', o), self.cnt[o]) for o in ENGS if self.cnt[o] > 0]
        snap += [(('d', i), self.dval[i]) for i in range(len(self.dsem)) if self.dval[i] > 0]
        for e in ENGS:
            self.pend[e] = snap

    def _pending(self, eng, waits):
        w = self.waited[eng]
        for key, val in self.pend[eng]:
            if key == ('e', eng):
                continue
            if w.get(key, 0) < val:
                w[key] = val
                waits.append((self._sem(key), val))
        self.pend[eng] = []

    def buf(self, name=None):
        self.nbuf += 1
        return Buf(name or ("b%d" % self.nbuf))

    def bufs(self, n):
        return [self.buf() for _ in range(n)]

    def _sem(self, key):
        if key[0] == 'e':
            return self.esem[key[1]]
        return self.dsem[key[1]] if key[0] == 'd' else self.bsem[key[1]]

    def _deps(self, eng, reads, writes):
        need = {}

        def add(tok, raw):
            if tok is None:
                return
            key, val, teng = tok
            if teng == eng and eng == 'pe':
                return
            if need.get(key, 0) < val:
                need[key] = val

        for b in reads:
            add(b.lw, True)
            if b.excl:
                for t in b.rd.values():
                    if t[2] != eng:
                        add(t, True)
        for b in writes:
            add(b.lw, False)
            for t in b.rd.values():
                add(t, False)
        waits = []
        w = self.waited[eng]
        for key, val in need.items():
            if w.get(key, 0) < val:
                w[key] = val
                waits.append((self._sem(key), val))
        return waits

    def _mark(self, tok, reads, writes):
        for b in reads:
            b.rd[tok[0]] = tok
        for b in writes:
            b.lw = tok
            b.rd = {}

    def op(self, eng, fn, reads=(), writes=()):
        waits = self._deps(eng, reads, writes)
        self._pending(eng, waits)
        self.cnt[eng] += 1
        tok = (('e', eng), self.cnt[eng], eng)
        self.ops[eng].append((waits, fn, (self.esem[eng], 1)))
        self._mark(tok, reads, writes)
        return tok

    def dma(self, eng, out, in_, reads=(), writes=(), bg=False, **kw):
        if bg:
            sems, vals, idx, kk = self.bsem, self.bval, self.bnext, 'b'
            self.bnext = (self.bnext + 1) % len(self.bsem)
        else:
            sems, vals, idx, kk = self.dsem, self.dval, self.dnext, 'd'
            self.dnext = (self.dnext + 1) % len(self.dsem)
        waits = self._deps(eng, reads, writes)
        self._pending(eng, waits)
        key = (kk, idx)
        w = self.waited[eng]
        if w.get(key, 0) < vals[idx]:
            w[key] = vals[idx]
            waits.append((sems[idx], vals[idx]))
        vals[idx] += 16
        tok = (key, vals[idx], None)
        sem_ = sems[idx]
        self.ops[eng].append((waits, (lambda e: e.dma_start(out=out, in_=in_, **kw)), (sem_, 16)))
        self._mark(tok, reads, writes)
        return tok

    def emit(self, block):
        fin = [(self.dsem[i], self.dval[i]) for i in range(len(self.dsem)) if self.dval[i] > 0]
        fin += [(self.bsem[i], self.bval[i]) for i in range(len(self.bsem)) if self.bval[i] > 0]
        fin += [(self.esem[e], self.cnt[e]) for e in ENGS if e != 'sp' and self.cnt[e] > 0]

        def run(e, name):
            for waits, fn, inc in self.ops[name]:
                for s, v in waits:
                    e.wait_ge(s, v)
                ins = fn(e)
                ins.then_inc(inc[0], inc[1])
            if name == 'sp':
                for s, v in fin:
                    e.wait_ge(s, v)

        @block.tensor
        def _(e):
            run(e, 'pe')

        @block.scalar
        def _(e):
            run(e, 'act')

        @block.vector
        def _(e):
            run(e, 'dve')

        @block.gpsimd
        def _(e):
            run(e, 'pool')

        @block.sync
        def _(e):
            run(e, 'sp')


class K:
    def __init__(self, nc):
        self.nc = nc
        self.P = Prog(nc)
        self.es = ExitStack()
        self.n = 0

    def scope(self):
        kk = self

        class _S:
            def __enter__(s_):
                s_.old = kk.es
                kk.es = ExitStack()
                return s_

            def __exit__(s_, *a):
                kk.es.close()
                kk.es = s_.old
                kk.P.barrier()
                return False
        return _S()

    def sb(self, shape, dt=F32, name=None):
        self.n += 1
        return self.es.enter_context(self.nc.sbuf_tensor(("%s_%d" % (name, self.n)) if name else ("t%d" % self.n), list(shape), dt))

    def ps(self, shape, dt=F32, name=None):
        self.n += 1
        return self.es.enter_context(self.nc.psum_tensor(name or ("p%d" % self.n), list(shape), dt))

    def dram(self, shape, dt=F32, name=None, kind="Internal"):
        self.n += 1
        return self.nc.dram_tensor(name or ("d%d" % self.n), list(shape), dt, kind=kind).ap()

    def mm(self, out, lhsT, rhs, start, stop, r, w):
        return self.P.op('pe', lambda e: e.matmul(out, lhsT=lhsT, rhs=rhs, start=start, stop=stop), r, w)

    def tr(self, out, in_, ident, r, w):
        return self.P.op('pe', lambda e: e.transpose(out, in_, ident), r, w)

    def act(self, out, in_, func, r, w, bias=None, scale=None, accum=None, eng='act'):
        kw = {}
        if bias is not None:
            kw['bias'] = bias
        if scale is not None:
            kw['scale'] = scale
        if accum is not None:
            kw['accum_out'] = accum
        return self.P.op('act', lambda e: e.activation(out=out, in_=in_, func=func, **kw), r, w)

    def ts(self, eng, out, in0, s1, s2, op0, op1, r, w):
        if op1 is None:
            return self.P.op(eng, lambda e: e.tensor_scalar(out=out, in0=in0, scalar1=s1, scalar2=None, op0=op0), r, w)
        return self.P.op(eng, lambda e: e.tensor_scalar(out=out, in0=in0, scalar1=s1, scalar2=s2, op0=op0, op1=op1), r, w)

    def tt(self, eng, out, in0, in1, op, r, w):
        return self.P.op(eng, lambda e: e.tensor_tensor(out=out, in0=in0, in1=in1, op=op), r, w)

    def stt(self, out, in0, scalar, in1, op0, op1, r, w):
        return self.P.op('dve', lambda e: e.scalar_tensor_tensor(out=out, in0=in0, scalar=scalar, in1=in1, op0=op0, op1=op1), r, w)

    def cp(self, eng, out, in_, r, w):
        if eng == 'act':
            return self.P.op('act', lambda e: e.copy(out=out, in_=in_), r, w)
        return self.P.op(eng, lambda e: e.tensor_copy(out=out, in_=in_), r, w)

    def memset(self, eng, ap, val, w):
        return self.P.op(eng, lambda e: e.memset(ap, val), (), w)

    def recip(self, out, in_, r, w):
        return self.P.op('dve', lambda e: e.reciprocal(out=out, in_=in_), r, w)

    def dma(self, eng, out, in_, r, w, **kw):
        return self.P.dma(eng, out, in_, r, w, **kw)


def build_program(stage=99):
    nc = bass.Bass("TRN2", target_bir_lowering=False)
    k = K(nc)
    P = k.P

    def din(name, shape, dt=F32):
        return nc.dram_tensor(name, list(shape), dt, kind="ExternalInput").ap()

    xl = din("xl", [LOC, D])
    cb = din("cb", [KC, 128])
    ada_w = din("ada_w", [D, 6 * D]) if not FROM_REF else None
    ada_b = din("ada_b", [96, 128])
    norm1_w = din("norm1_w", [KC, 128])
    w_in = din("w_in", [D, D_IN]) if not FROM_REF else None
    ident_in = din("ident", [128, 128])
    pvalid_in = din("pvalid", [128, 1])
    out_d = nc.dram_tensor("out", [NOWN, D], F32, kind="ExternalOutput").ap()
    dbg_mod = nc.dram_tensor("dbg_mod", [128, 96], F32, kind="ExternalOutput").ap()
    def scratch(name, shape, dt=F32):
        if name in REF_IN:
            return din(name + "_in", shape, dt)
        return k.dram(shape, dt, name)

    if FROM_REF:
        PF = din("PF_in", [PF_ROWS, LOC])
        PT = din("PT_in", [LOC, 1600])
    else:
        PF = k.dram([80 * 128, LOC], F32, "PF")
        PT = k.dram([LOC, 1600], F32, "PT")
    OAT = scratch("OAT", [1024, NOWN], BF16)
    OBT = scratch("OBT", [2048, NOWN], BF16)
    YT = scratch("YT", [D, NOWN], BF16)
    X1 = scratch("X1", [NOWN, D], F32)

    ident = k.sb([128, 128], F32, "ident_sb")
    identb = k.sb([128, 128], BF16, "identb")
    pvalid = k.sb([128, 1], F32, "pvalid_sb")
    b_const = P.buf("const")
    k.dma('sp', ident[:], ident_in, (), [b_const])
    k.dma('sp', pvalid[:], pvalid_in, (), [b_const])
    k.cp('dve', identb[:], ident[:], [b_const], [b_const])

    banks = [k.ps([128, 512], F32, "bank%d" % i) for i in range(6)]
    bbank = [P.buf("bank%d" % i) for i in range(6)]
    for b_ in bbank:
        b_.excl = True
    pbf = [k.ps([128, 1024], BF16, "pbf%d" % i) for i in range(2)]
    bpbf = [P.buf("pbf%d" % i) for i in range(2)]
    for b_ in bpbf:
        b_.excl = True

    def load_fm(dst, src, n, bdst, bank=5):
        tmp = k.sb([96, 128], F32)
        bt = P.buf()
        k.dma('sp', tmp[0:n, :], src, (), [bt])
        k.tr(banks[bank][:, 0:n], tmp[0:n, :], ident[0:n, 0:n], [bt, b_const], [bbank[bank]])
        k.cp('dve', dst, banks[bank][:, 0:n], [bbank[bank]], [bdst])

    modT = k.sb([128, 96], F32, "modT")
    b_mod = P.buf("mod")
    A1 = k.sb([128, KC], F32, "A1")
    B1p = k.sb([128, KC], F32, "B1p")
    b_A1 = P.buf()
    if FROM_REF:
        modT_in = din("modT_in", [128, 96])
        k.dma('sp', modT[:], modT_in, (), [b_mod])
    with k.scope():
      if not FROM_REF:
        cT = k.sb([128, KC], F32, "cT")
        csT = k.sb([128, KC], F32, "csT")
        adabT = k.sb([128, 96], F32, "adabT")
        n1T = k.sb([128, KC], F32, "n1T")
        b_c, b_cs, b_adab, b_n1 = P.bufs(4)
        load_fm(cT[:], cb, KC, b_c)
        load_fm(adabT[:], ada_b, 96, b_adab)
        load_fm(n1T[:], norm1_w, KC, b_n1)
        k.act(csT[:], cT[:], AF.Silu, [b_c], [b_cs])
        NAW = 3
        awt = [k.sb([128, KC, 128], F32, "awt%d" % i) for i in range(NAW)]
        bawt = P.bufs(NAW)
        ada_v = ada_w.rearrange("(kc p) j -> p kc j", p=128)
        for m in range(96):
            s = m % NAW
            k.dma('sp', awt[s][:], ada_v[:, :, m * 128:(m + 1) * 128], (), [bawt[s]])
            for kc in range(KC):
                k.mm(banks[4][:, m:m + 1], awt[s][:, kc, :], csT[:, kc:kc + 1], kc == 0, kc == KC - 1,
                     [bawt[s], b_cs], [bbank[4]])
        k.tt('dve', modT[:], banks[4][:, 0:96], adabT[:], ALU.add, [bbank[4], b_adab], [b_mod])
        k.dma('sp', dbg_mod, modT[:], [b_mod], ())
        k.stt(A1[:], modT[:, 16:32], 1.0, n1T[:], ALU.add, ALU.mult, [b_mod, b_n1], [b_A1])
        k.ts('dve', B1p[:], modT[:, 0:16], pvalid[:, 0:1], None, ALU.mult, None, [b_mod, b_const], [b_A1])

    if stage >= 1 and not FROM_REF:
        hscope = k.scope()
        hscope.__enter__()
        hT = k.sb([128, KC, LOC], BF16, "hT")
        b_hT = [P.buf() for _ in range(LOC // 128)]
        with k.scope():
            xt = [k.sb([128, D], F32, "xt%d" % i) for i in range(2)]
            xs = [k.sb([128, D], BF16, "xs%d" % i) for i in range(2)]
            st = [k.sb([128, 4], F32, "st%d" % i) for i in range(2)]
            bxt, bxs, bst = P.bufs(2), P.bufs(2), P.bufs(2)
            xlv = xl.rearrange("(t p) d -> t p d", p=128)
            for t in range(LOC // 128):
                s = t % 2
                k.dma('sp', xt[s][:], xlv[t], (), [bxt[s]])
                k.act(xs[s][:], xt[s][:], AF.Square, [bxt[s]], [bxs[s], bst[s]], accum=st[s][:, 0:1])
                k.ts('dve', st[s][:, 1:2], st[s][:, 0:1], 1.0 / D, 1e-6, ALU.mult, ALU.add, [bst[s]], [bst[s]])
                k.act(st[s][:, 2:3], st[s][:, 1:2], AF.Sqrt, [bst[s]], [bst[s]])
                k.recip(st[s][:, 3:4], st[s][:, 2:3], [bst[s]], [bst[s]])
                k.ts('dve', xs[s][:], xt[s][:], st[s][:, 3:4], None, ALU.mult, None, [bxt[s], bst[s]], [bxs[s]])
                Bsel = B1p if t < OWN0 // 128 else modT
                for kc in range(KC):
                    pi = kc % 2
                    sl = slice((kc // 2 % 4) * 128, (kc // 2 % 4) * 128 + 128)
                    k.tr(pbf[pi][:, sl], xs[s][:, kc * 128:(kc + 1) * 128], identb[:], [bxs[s], b_const], [bpbf[pi]])
                    if kc % 2 == 0:
                        k.act(hT[:, kc, t * 128:(t + 1) * 128], pbf[pi][:, sl], AF.Identity, [bpbf[pi], b_A1, b_mod], [b_hT[t]],
                              bias=Bsel[:, kc:kc + 1], scale=A1[:, kc:kc + 1])
                    else:
                        k.ts('dve', hT[:, kc, t * 128:(t + 1) * 128], pbf[pi][:, sl], A1[:, kc:kc + 1], Bsel[:, kc:kc + 1],
                             ALU.mult, ALU.add, [bpbf[pi], b_A1, b_mod], [b_hT[t]])
        if stage == 1:
            dbg_h = nc.dram_tensor("dbg_h", [128, KC, LOC], BF16, kind="ExternalOutput").ap()
            k.dma('sp', dbg_h, hT[:], b_hT, ())

        if stage >= 2:
            FM = [(0, 3072, 0, 0), (4112, 2048, 3072, OWN0), (6160, 256, 5120, 0), (6416, 256, 5376, 0),
                  (6672, 256, 5632, 0), (7184, 256, 5888, 0), (7744, 2048, 6144, OWN0), (9792, 2048, 8192, OWN0)]
            TM = [(3072, 256, 0, OWN0), (3328, 256, 256, OWN0), (3584, 256, 512, OWN0), (3840, 256, 768, OWN0),
                  (4096, 16, 1024, 0), (6928, 256, 1040, 0), (7440, 256, 1296, 0), (7696, 48, 1552, OWN0)]
            with k.scope():
                wv = w_in.rearrange("(kc p) c -> p kc c", p=128)
                NW = 3
                wt = [k.sb([128, KC, 256], BF16, "wt%d" % i) for i in range(NW)]
                bwt = P.bufs(NW)
                ev = [k.sb([128, 512], F32, "ev%d" % i) for i in range(4)]
                bev = P.bufs(4)
                ei = 0
                wi = 0
                for (c0, ncols, r0, t0) in FM:
                    for cb_ in range(ncols // 256):
                        s = wi % NW
                        wi += 1
                        k.dma('pool', wt[s][:], wv[:, :, c0 + cb_ * 256:c0 + cb_ * 256 + 256], (), [bwt[s]])
                        for hf in range(2):
                            for tt in range(t0 // 512, LOC // 512):
                                e4 = ei % 4
                                ei += 1
                                for kc in range(KC):
                                    k.mm(banks[e4][:, :], wt[s][:, kc, hf * 128:(hf + 1) * 128],
                                         hT[:, kc, tt * 512:(tt + 1) * 512], kc == 0, kc == KC - 1,
                                         [bwt[s]] + b_hT[tt * 4:(tt + 1) * 4], [bbank[e4]])
                                k.cp('act' if e4 % 2 == 0 else 'dve', ev[e4][:], banks[e4][:, :], [bbank[e4]], [bev[e4]])
                                row = r0 + cb_ * 256 + hf * 128
                                k.dma('sp', PF[row:row + 128, tt * 512:(tt + 1) * 512], ev[e4][:], [bev[e4]], ())
                for (c0, ncols, p0, t0) in TM:
                    s = wi % NW
                    wi += 1
                    k.dma('pool', wt[s][:, :, 0:ncols], wv[:, :, c0:c0 + ncols], (), [bwt[s]])
                    for t in range(t0 // 128, LOC // 128):
                        e4 = ei % 4
                        ei += 1
                        for kc in range(KC):
                            k.mm(banks[e4][:, 0:ncols], hT[:, kc, t * 128:(t + 1) * 128], wt[s][:, kc, 0:ncols],
                                 kc == 0, kc == KC - 1, [bwt[s], b_hT[t]], [bbank[e4]])
                        k.cp('act' if e4 % 2 == 0 else 'dve', ev[e4][:, 0:ncols], banks[e4][:, 0:ncols], [bbank[e4]], [bev[e4]])
                        k.dma('sp', PT[t * 128:(t + 1) * 128, p0:p0 + ncols], ev[e4][:, 0:ncols], [bev[e4]], ())
        hscope.__exit__(None, None, None)
        if stage == 2 and DEBUG_OUT:
            dbg_pf = nc.dram_tensor("dbg_pf", [4, 128, LOC], F32, kind="ExternalOutput").ap()
            dbg_pt = nc.dram_tensor("dbg_pt", [LOC, 576], F32, kind="ExternalOutput").ap()
            for i, r in enumerate([0, 3072, 5120, 6144]):
                k.dma('sp', dbg_pf[i], PF[r:r + 128, :], (), ())
            k.dma('sp', dbg_pt, PT[:, 1024:1600], (), ())

    if stage >= 6:
        uT_in = din("peer_uT", [D, 16384])
        pv_in = din("peer_v", [16384, D])
        UBd = k.dram([64, 128, KC, 256], BF16, "UBd")
        VBd = k.dram([64, 128, 2, D], BF16, "VBd")
        bconvU = [P.buf() for _ in range(64)]
        bconvV = [P.buf() for _ in range(64)]
        uTv0 = uT_in.rearrange("(kc p) e -> p kc e", p=128)
        for eb in range(64):
            P.dma('pool', UBd[eb], uTv0[:, :, eb * 256:(eb + 1) * 256], (), [bconvU[eb]], bg=True)
            P.dma('pool', VBd[eb], pv_in[eb * 256:(eb + 1) * 256, :].rearrange("(i p) d -> p i d", p=128), (), [bconvV[eb]], bg=True)
    CONV_TOK = {}
    if stage >= 3 and not SKIP_GDN:
        gconst_in = din("gconst", [128, 5, 128])
        convw_in = din("gdn_conv_w", [96, 128])
        alog_in = din("gdn_A_log", [32, 8])
        dtb_in = din("gdn_dt_bias", [32, 8])
        gnw_in = din("gdn_norm_w", [1, 128])
        bq = [[bbank[bi]] * 4 for bi in range(6)]
        NCH = LOC // 128
        with k.scope():
            gcn = k.sb([128, 5, 128], F32, "gcn")
            ones = k.sb([128, 128], F32, "ones")
            cwT = k.sb([128, 96], F32, "cwT")
            nwb = k.sb([128, 128], F32, "nwb")
            b_gc = P.buf()
            b_cw = P.buf()
            k.dma('sp', gcn[:], gconst_in, (), [b_gc])
            k.memset('dve', ones[:], 1.0, [b_gc])
            k.dma('sp', nwb[:], gnw_in[0].partition_broadcast(128), (), [b_gc])
            load_fm(cwT[:], convw_in, 96, b_cw)
            TriU, msl, msu, miu, bmk = gcn[:, 0, :], gcn[:, 1, :], gcn[:, 2, :], gcn[:, 3, :], gcn[:, 4, :]
            ab = k.sb([128, NCH, 16], F32, "ab")
            dtb = k.sb([128, NCH, 8], F32, "dtb")
            alg = k.sb([128, NCH, 8], F32, "alg")
            vt = {nm: k.sb([128, NCH, 8], F32, "v_" + nm) for nm in
                  ["t1", "g", "beta", "lnb", "Gc", "nGc", "u", "gam", "bg", "kd", "gend", "Gl"]}
            b_v = P.buf()
            k.dma('sp', ab[:], PT[:, 1024:1040].rearrange("(n p) c -> p n c", p=128), (), [b_v])
            k.dma('sp', dtb[:], dtb_in.partition_broadcast(128), (), [b_v])
            k.dma('sp', alg[:], alog_in.partition_broadcast(128), (), [b_v])
            k.tt('dve', vt["t1"][:], ab[:, :, 0:8], dtb[:], ALU.add, [b_v], [b_v])
            k.act(vt["t1"][:], vt["t1"][:], AF.Exp, [b_v], [b_v])
            e_, ser, lnp = vt["t1"], vt["Gl"], vt["Gc"]
            k.ts('dve', ser[:], e_[:], -0.25, 1.0 / 3.0, ALU.mult, ALU.add, [b_v], [b_v])
            k.tt('dve', ser[:], ser[:], e_[:], ALU.mult, [b_v], [b_v])
            k.ts('dve', ser[:], ser[:], -0.5, None, ALU.add, None, [b_v], [b_v])
            k.tt('dve', ser[:], ser[:], e_[:], ALU.mult, [b_v], [b_v])
            k.ts('dve', ser[:], ser[:], 1.0, None, ALU.add, None, [b_v], [b_v])
            k.tt('dve', ser[:], ser[:], e_[:], ALU.mult, [b_v], [b_v])
            k.ts('dve', lnp[:], e_[:], 0.1, None, ALU.max, None, [b_v], [b_v])
            k.act(lnp[:], lnp[:], AF.Ln, [b_v], [b_v], bias=1.0)
            k.ts('dve', vt["u"][:], e_[:], 0.1, None, ALU.is_lt, None, [b_v], [b_v])
            k.tt('dve', ser[:], ser[:], lnp[:], ALU.subtract, [b_v], [b_v])
            k.tt('dve', ser[:], ser[:], vt["u"][:], ALU.mult, [b_v], [b_v])
            k.tt('dve', vt["t1"][:], lnp[:], ser[:], ALU.add, [b_v], [b_v])
            k.act(alg[:], alg[:], AF.Exp, [b_v], [b_v])
            k.stt(vt["g"][:], vt["t1"][:], -1.0, alg[:], ALU.mult, ALU.mult, [b_v], [b_v])
            k.act(vt["beta"][:], ab[:, :, 8:16], AF.Sigmoid, [b_v], [b_v])
            k.act(vt["lnb"][:], vt["beta"][:], AF.Ln, [b_v], [b_v])
            gflat = vt["g"][:].rearrange("p n h -> p (n h)")
            k.mm(banks[0][:, 0:256], TriU, gflat, True, True, [b_v, b_gc], [bq[0][0], bq[0][1]])
            k.mm(banks[0][:, 256:512], ones[:], gflat, True, True, [b_v, b_gc], [bq[0][2], bq[0][3]])
            fl = lambda nm: vt[nm][:].rearrange("p n h -> p (n h)")
            k.cp('dve', fl("Gc"), banks[0][:, 0:256], [bq[0][0], bq[0][1]], [b_v])
            k.cp('dve', fl("Gl"), banks[0][:, 256:512], [bq[0][2], bq[0][3]], [b_v])
            k.ts('dve', fl("nGc"), fl("Gc"), -1.0, None, ALU.mult, None, [b_v], [b_v])
            k.tt('dve', fl("u"), fl("Gc"), fl("lnb"), ALU.add, [b_v], [b_v])
            k.act(fl("gam"), fl("Gc"), AF.Exp, [b_v], [b_v])
            k.act(fl("bg"), fl("u"), AF.Exp, [b_v], [b_v])
            k.tt('dve', fl("kd"), fl("Gl"), fl("Gc"), ALU.subtract, [b_v], [b_v])
            k.act(fl("kd"), fl("kd"), AF.Exp, [b_v], [b_v])
            k.act(fl("gend"), fl("Gl"), AF.Exp, [b_v], [b_v])

            def col(nm, n, h):
                return vt[nm][:, n, h:h + 1]

            WV = 4
            for h in range(0 if GDN_SUB < 1 else (1 if GDN_ONE_HEAD else 8)):
                with k.scope():
                    QT = k.sb([128, LOC], F32, "QT")
                    KT = k.sb([128, LOC], F32, "KT")
                    VT = k.sb([128, LOC], F32, "VT")
                    xr = k.sb([128, LOC], F32, "xr")
                    bQ, bK, bV, bxr = P.bufs(4)
                    for (dst, bd, row0, ti) in [(QT, bQ, h * 128, h), (KT, bK, 1024 + h * 128, 8 + h), (VT, bV, 2048 + h * 128, 16 + h)]:
                        k.dma('sp', xr[:], PF[row0:row0 + 128, :], (), [bxr])
                        k.ts('dve', dst[:], xr[:], cwT[:, 3 * 24 + ti:3 * 24 + ti + 1], None, ALU.mult, None, [bxr, b_cw], [bd])
                        for sh in (1, 2, 3):
                            j = 3 - sh
                            k.stt(dst[:, sh:LOC], xr[:, 0:LOC - sh], cwT[:, j * 24 + ti:j * 24 + ti + 1], dst[:, sh:LOC],
                                  ALU.mult, ALU.add, [bxr, b_cw, bd], [bd])
                        k.act(dst[:], dst[:], AF.Silu, [bd], [bd])
                    for (dst, bd, lnc) in [(QT, bQ, float(np.log(128.0 ** -0.5))), (KT, bK, 0.0)]:
                        for tt in range(LOC // 512):
                            bk = tt % 4
                            sl = slice(tt * 512, (tt + 1) * 512)
                            k.tt('dve', xr[:, sl], dst[:, sl], dst[:, sl], ALU.mult, [bd], [bxr])
                            k.mm(banks[bk][:, :], ones[:], xr[:, sl], True, True, [bxr, b_gc], bq[bk])
                            k.act(xr[:, sl], banks[bk][:, :], AF.Ln, bq[bk], [bxr], bias=1e-6)
                            k.act(xr[:, sl], xr[:, sl], AF.Exp, [bxr], [bxr], scale=-0.5, bias=lnc)
                            k.tt('dve', dst[:, sl], dst[:, sl], xr[:, sl], ALU.mult, [bd, bxr], [bd])
                    zt = k.sb([128, NOWN // 128, 128], F32, "zt")
                    bz = P.buf()
                    k.dma('sp', zt[:], PT[OWN0:LOC, h * 128:(h + 1) * 128].rearrange("(n p) c -> p n c", p=128), (), [bz])
                    k.act(zt[:], zt[:], AF.Silu, [bz], [bz])
                    S = k.sb([128, 128], F32, "S")
                    bS = P.buf()
                    k.memset('dve', S[:], 0.0, [bS])
                    names = ["R", "Kd", "dg", "tE", "L", "LT", "QK", "Pa", "Pb", "WT", "U", "tmp", "O", "o16", "Z", "DT", "X", "Tt"]
                    T = [{nm: k.sb([128, 256 if nm in ("R", "X", "Tt") else 128], BF16 if nm == "o16" else F32, "w%d_%s" % (i, nm)) for nm in names}
                         for i in range(WV)]
                    Bf = [{nm: P.buf() for nm in names + ["st"]} for i in range(WV)]
                    stt_ = [k.sb([128, 4], F32, "w%d_st" % i) for i in range(WV)]
                    PTk = [[k.sb([128, 128], F32, "w%d_PT%d" % (i, l)) for l in range(4)] for i in range(WV)]
                    bPTk = [[P.buf() for l in range(4)] for i in range(WV)]
                    bctr = [0]

                    def nb():
                        bctr[0] = (bctr[0] + 1) % 6
                        return bctr[0]

                    for w0 in (GDN_WAVES if GDN_WAVES is not None else range(0, NCH if GDN_SUB >= 2 else 0, WV)):
                        chunks = list(range(w0, w0 + WV))
                        own = w0 >= OWN0 // 128
                        def pre_steps(i, n):
                            t, b = T[i], Bf[i]
                            cs = slice(n * 128, (n + 1) * 128)
                            q4 = slice(i * 128, (i + 1) * 128)
                            st = []
                            bkT1, bkT2, bkKK, bkKQ, bkB = 0, 1, 2, 3, 4
                            st.append(lambda: k.tr(banks[0][:, q4], KT[:, cs], ident[:], [bK, b_const], [bq[0][i]]))
                            st.append(lambda: k.act(t["R"][:, 0:128], banks[0][:, q4], AF.Identity, [bq[0][i], b_v], [b["R"]], scale=col("bg", n, h)))
                            st.append(lambda: k.ts('dve', t["Kd"][:], banks[0][:, q4], col("kd", n, h), None, ALU.mult, None, [bq[0][i], b_v], [b["Kd"]]))
                            st.append(lambda: k.tr(banks[1][:, q4], VT[:, cs], ident[:], [bV, b_const], [bq[1][i]]))
                            st.append(lambda: k.ts('dve', t["R"][:, 128:256], banks[1][:, q4], col("beta", n, h), None, ALU.mult, None, [bq[1][i], b_v], [b["R"]]))
                            st.append(lambda: k.mm(banks[2][:, q4], KT[:, cs], KT[:, cs], True, True, [bK], [bq[2][i]]))
                            st.append(lambda: k.ts('dve', t["dg"][:], ident[:], col("nGc", n, h), None, ALU.mult, None, [b_const, b_v], [b["dg"]]))
                            st.append(lambda: k.mm(banks[4][:, q4], ones[:], t["dg"][:], True, True, [b["dg"], b_gc], [bq[4][i]]))
                            st.append(lambda: k.stt(t["tE"][:], banks[4][:, q4], col("u", n, h), msl, ALU.add, ALU.add, [bq[4][i], b_v, b_gc], [b["tE"]]))
                            st.append(lambda: k.act(t["tE"][:], t["tE"][:], AF.Exp, [b["tE"]], [b["tE"]]))
                            st.append(lambda: k.tt('dve', t["L"][:], banks[2][:, q4], t["tE"][:], ALU.mult, [bq[2][i], b["tE"]], [b["L"]]))
                            st.append(lambda: k.ts('dve', t["dg"][:], ident[:], col("u", n, h), None, ALU.mult, None, [b_const, b_v], [b["dg"]]))
                            st.append(lambda: k.mm(banks[5][:, q4], ones[:], t["dg"][:], True, True, [b["dg"], b_gc], [bq[5][i]]))
                            st.append(lambda: k.stt(t["tE"][:], banks[5][:, q4], col("nGc", n, h), msu, ALU.add, ALU.add, [bq[5][i], b_v, b_gc], [b["tE"]]))
                            st.append(lambda: k.act(t["tE"][:], t["tE"][:], AF.Exp, [b["tE"]], [b["tE"]]))
                            st.append(lambda: k.tt('dve', t["LT"][:], banks[2][:, q4], t["tE"][:], ALU.mult, [bq[2][i], b["tE"]], [b["LT"]]))
                            if own:
                                st.append(lambda: k.mm(banks[3][:, q4], KT[:, cs], QT[:, cs], True, True, [bK, bQ], [bq[3][i]]))
                                st.append(lambda: k.ts('dve', t["dg"][:], ident[:], col("Gc", n, h), None, ALU.mult, None, [b_const, b_v], [b["dg"]]))
                                st.append(lambda: k.mm(banks[4][:, q4], ones[:], t["dg"][:], True, True, [b["dg"], b_gc], [bq[4][i]]))
                                st.append(lambda: k.stt(t["tE"][:], banks[4][:, q4], col("nGc", n, h), miu, ALU.add, ALU.add, [bq[4][i], b_v, b_gc], [b["tE"]]))
                                st.append(lambda: k.act(t["tE"][:], t["tE"][:], AF.Exp, [b["tE"]], [b["tE"]]))
                                st.append(lambda: k.tt('dve', t["QK"][:], banks[3][:, q4], t["tE"][:], ALU.mult, [bq[3][i], b["tE"]], [b["QK"]]))
                            st.append(lambda: k.tt('dve', t["tmp"][:], t["LT"][:], bmk, ALU.mult, [b["LT"], b_gc], [b["tmp"]]))
                            st.append(lambda: k.tt('dve', t["LT"][:], t["LT"][:], t["tmp"][:], ALU.subtract, [b["LT"], b["tmp"]], [b["LT"]]))
                            st.append(lambda: k.tt('dve', t["L"][:], t["L"][:], bmk, ALU.mult, [b["L"], b_gc], [b["L"]]))
                            cP, cbP, cPT, cbPT = t["L"], b["L"], t["tmp"], b["tmp"]
                            NLEV = 4
                            for lev in range(NLEV):
                                nP, nbP = (t["Pa"], b["Pa"]) if lev % 2 == 0 else (t["Pb"], b["Pb"])
                                bA = lev % 2
                                last = lev == NLEV - 1
                                if not last:
                                    st.append((lambda cP=cP, cbP=cbP, cPT=cPT, cbPT=cbPT, bA=bA: k.mm(banks[bA][:, q4], cPT[:], cP[:], True, True, [cbPT, cbP], [bq[bA][i]])))
                                    st.append((lambda nP=nP, nbP=nbP, bA=bA: k.cp('act', nP[:], banks[bA][:, q4], [bq[bA][i]], [nbP])))
                                st.append((lambda cP=cP, cbP=cbP, cPT=cPT, cbPT=cbPT, bA=bA: k.mm(banks[2 + bA][:, q4], cP[:], cPT[:], True, True, [cbPT, cbP], [bq[2 + bA][i]])))
                                st.append((lambda lev=lev, bA=bA: k.cp('dve', PTk[i][lev][:], banks[2 + bA][:, q4], [bq[2 + bA][i]], [bPTk[i][lev]])))
                                cP, cbP, cPT, cbPT = nP, nbP, PTk[i][lev], bPTk[i][lev]
                            st.append(lambda: k.cp('dve', t["Z"][:], ident[:], [b_const], [b["Z"]]))
                            for lev in [3, 2, 1, 0, -1]:
                                bA = 4 + (lev % 2)
                                if lev >= 0:
                                    st.append((lambda lev=lev, bA=bA: k.mm(banks[bA][:, q4], PTk[i][lev][:], t["Z"][:], True, True, [bPTk[i][lev], b["Z"]], [bq[bA][i]])))
                                    st.append((lambda bA=bA: k.tt('dve', t["Z"][:], t["Z"][:], banks[bA][:, q4], ALU.add, [bq[bA][i], b["Z"]], [b["Z"]])))
                                else:
                                    st.append((lambda bA=bA: k.mm(banks[bA][:, q4], t["tmp"][:], t["Z"][:], True, True, [b["tmp"], b["Z"]], [bq[bA][i]])))
                                    st.append((lambda bA=bA: k.tt('dve', t["Z"][:], t["Z"][:], banks[bA][:, q4], ALU.subtract, [bq[bA][i], b["Z"]], [b["Z"]])))
                            st.append(lambda: k.tr(banks[0][:, q4], t["Z"][:], ident[:], [b["Z"], b_const], [bq[0][i]]))
                            st.append(lambda: k.cp('act', t["DT"][:], banks[0][:, q4], [bq[0][i]], [b["DT"]]))
                            hq = slice((i % 2) * 256, (i % 2) * 256 + 256)
                            bkR = 4 + (i // 2)
                            bkR2 = 2 + (i // 2)
                            st.append(lambda: k.mm(banks[bkR][:, hq], t["DT"][:], t["R"][:], True, True, [b["DT"], b["R"]], [bq[bkR][0]]))
                            st.append(lambda: k.cp('dve', t["X"][:], banks[bkR][:, hq], [bq[bkR][0]], [b["X"]]))
                            for sweep in range(3):
                                st.append(lambda: k.mm(banks[bkR2][:, hq], t["LT"][:], t["X"][:], True, True, [b["LT"], b["X"]], [bq[bkR2][0]]))
                                st.append(lambda: k.tt('dve', t["Tt"][:], t["R"][:], banks[bkR2][:, hq], ALU.subtract, [b["R"], bq[bkR2][0]], [b["Tt"]]))
                                st.append(lambda: k.mm(banks[bkR][:, hq], t["DT"][:], t["Tt"][:], True, True, [b["DT"], b["Tt"]], [bq[bkR][0]]))
                                st.append(lambda: k.cp('dve', t["X"][:], banks[bkR][:, hq], [bq[bkR][0]], [b["X"]]))
                            st.append(lambda: k.cp('act', t["R"][:], t["X"][:], [b["X"]], [b["R"]]))
                            st.append(lambda: k.tr(banks[0][:, q4], t["R"][:, 0:128], ident[:], [b["R"], b_const], [bq[0][i]]))
                            st.append(lambda: k.cp('act', t["WT"][:], banks[0][:, q4], [bq[0][i]], [b["WT"]]))
                            return st

                        allst = [pre_steps(i, n) for i, n in enumerate(chunks)]
                        for si in range(min(GDN_STEPS, len(allst[0]))):
                            for i in range(WV):
                                allst[i][si]()
                        for i, n in enumerate(chunks if GDN_SUB >= 3 else []):
                            t, b = T[i], Bf[i]
                            cs = slice(n * 128, (n + 1) * 128)
                            q4 = slice(i * 128, (i + 1) * 128)
                            k.mm(banks[1][:, q4], t["WT"][:], S[:], True, True, [b["WT"], bS], [bq[1][i]])
                            k.tt('dve', t["U"][:], t["R"][:, 128:256], banks[1][:, q4], ALU.subtract, [b["R"], bq[1][i]], [b["U"]])
                            if own:
                                k.mm(banks[2][:, q4], QT[:, cs], S[:], True, True, [bQ, bS], [bq[2][i]])
                                k.mm(banks[3][:, q4], t["QK"][:], t["U"][:], True, True, [b["QK"], b["U"]], [bq[3][i]])
                                k.act(t["tmp"][:], banks[2][:, q4], AF.Identity, [bq[2][i], b_v], [b["tmp"]], scale=col("gam", n, h))
                                k.tt('dve', t["O"][:], t["tmp"][:], banks[3][:, q4], ALU.add, [b["tmp"], bq[3][i]], [b["O"]])
                            k.mm(banks[0][:, q4], t["Kd"][:], t["U"][:], True, True, [b["Kd"], b["U"]], [bq[0][i]])
                            k.stt(S[:], S[:], col("gend", n, h), banks[0][:, q4], ALU.mult, ALU.add, [bS, b_v, bq[0][i]], [bS])
                            if own:
                                no = n - OWN0 // 128
                                sti = stt_[i]
                                k.act(t["tmp"][:], t["O"][:], AF.Square, [b["O"]], [b["tmp"], b["st"]], accum=sti[:, 0:1])
                                k.act(sti[:, 1:2], sti[:, 0:1], AF.Ln, [b["st"]], [b["st"]], scale=1.0 / 128, bias=1e-6)
                                k.act(sti[:, 2:3], sti[:, 1:2], AF.Exp, [b["st"]], [b["st"]], scale=-0.5)
                                k.stt(t["O"][:], t["O"][:], sti[:, 2:3], nwb[:], ALU.mult, ALU.mult, [b["O"], b["st"], b_gc], [b["O"]])
                                k.tt('dve', t["O"][:], t["O"][:], zt[:, no, :], ALU.mult, [b["O"], bz], [b["O"]])
                                k.tr(banks[4][:, q4], t["O"][:], ident[:], [b["O"], b_const], [bq[4][i]])
                                k.cp('act', t["o16"][:], banks[4][:, q4], [bq[4][i]], [b["o16"]])
                                k.dma('sp', OAT[h * 128:(h + 1) * 128, no * 128:(no + 1) * 128], t["o16"][:], [b["o16"]], ())
        if stage == 3 and DEBUG_OUT and not SKIP_GDN:
            dbg_oa = nc.dram_tensor("dbg_oa", [1024, NOWN], BF16, kind="ExternalOutput").ap()
            k.dma('sp', dbg_oa, OAT, (), ())

    if stage >= 4 and not SKIP_NSA:
        pos_in = din("pos", [1, LOC], I32)
        nsc_in = din("nsa_c128", [128, 385])
        maskB_in = din("maskB", [128, LOC])
        eexp_in = din("eexp", [64, LOC])
        ovl_in = din("ovl", [128, 2, 65])
        fv_in = din("fbigvis", [128, 2, 16, 64])
        ncore_in = din("nsa_core", [128, 129])
        cpos_in = [din("cmp_pos_k", [32, 128]), din("cmp_pos_v", [32, 128])]
        cw1_in = [din("cmp_w1_k", [LOC, 256]), din("cmp_w1_v", [LOC, 256])]
        cw2_in = [din("cmp_w2_k", [256, 128]), din("cmp_w2_v", [256, 128])]
        NQT = NOWN // 128
        with k.scope():
            nsc = k.sb([128, 385], F32, "nsc")
            ncore = k.sb([128, 129], F32, "ncore")
            maskB = k.sb([128, LOC], BF16, "maskB")
            eexp = k.sb([64, LOC], BF16, "eexp")
            ovl = k.sb([128, 2, 65], BF16, "ovl")
            fv = k.sb([128, 2, 16, 64], F32, "fv")
            cmk = k.sb([128, 2, 128], BF16, "cmk")
            b_nc = P.buf()
            k.dma('sp', nsc[:], nsc_in, (), [b_nc])
            k.dma('sp', ncore[:], ncore_in, (), [b_nc])
            k.dma('sp', fv[:], fv_in, (), [b_nc])
            k.dma('pool', maskB[:], maskB_in, (), [b_nc])
            k.dma('pool', eexp[:], eexp_in, (), [b_nc])
            k.dma('pool', ovl[:], ovl_in, (), [b_nc])
            k.cp('dve', cmk[:, 0, :], nsc[:, 129:257], [b_nc], [b_nc])
            k.cp('dve', cmk[:, 1, :], nsc[:, 257:385], [b_nc], [b_nc])
            ropeP = nsc[:, 0:128]
            invf = nsc[:, 128:129]
            kvb = ncore[:, 128:129]
            cosT = k.sb([128, LOC], F32, "cosT")
            sinT = k.sb([128, LOC], F32, "sinT")
            cosq = k.sb([128, NOWN], F32, "cosq")
            sinq = k.sb([128, NOWN], F32, "sinq")
            b_rt = P.buf()
            with k.scope():
                posi = k.sb([128, LOC], I32, "posi")
                ang = k.sb([128, LOC], F32, "ang")
                kk_ = k.sb([128, LOC], I32, "kk")
                kf = k.sb([128, LOC], F32, "kf")
                b_a = P.buf()
                k.dma('sp', posi[:], pos_in[0].partition_broadcast(128), (), [b_a])
                k.cp('dve', ang[:], posi[:], [b_a], [b_a])
                k.ts('dve', ang[:], ang[:], invf, None, ALU.mult, None, [b_a, b_nc], [b_a])
                TWO_PI = float(2 * np.pi)
                for (dst, shift) in [(sinT, 0.0), (cosT, float(np.pi / 2))]:
                    k.ts('dve', kf[:], ang[:], shift, 1.0 / TWO_PI, ALU.add, ALU.mult, [b_a], [b_a])
                    k.cp('dve', kk_[:], kf[:], [b_a], [b_a])
                    k.cp('dve', kf[:], kk_[:], [b_a], [b_a])
                    k.stt(kf[:], kf[:], -TWO_PI, ang[:], ALU.mult, ALU.add, [b_a], [b_a])
                    k.ts('dve', kf[:], kf[:], shift, None, ALU.add, None, [b_a], [b_a])
                    k.ts('dve', dst[:], kf[:], float(np.pi), -TWO_PI, ALU.is_gt, ALU.mult, [b_a], [b_rt])
                    k.tt('dve', kf[:], kf[:], dst[:], ALU.add, [b_a, b_rt], [b_a])
                    k.ts('dve', dst[:], kf[:], -float(np.pi), TWO_PI, ALU.is_lt, ALU.mult, [b_a], [b_rt])
                    k.tt('dve', kf[:], kf[:], dst[:], ALU.add, [b_a, b_rt], [b_a])
                    k.act(dst[:], kf[:], AF.Sin, [b_a], [b_rt])
                k.ts('dve', cosq[:], cosT[:, OWN0:LOC], 128.0 ** -0.5, None, ALU.mult, None, [b_rt], [b_rt])
                k.ts('dve', sinq[:], sinT[:, OWN0:LOC], 128.0 ** -0.5, None, ALU.mult, None, [b_rt], [b_rt])

            def rope(dst_bf, X, bX, ct, st_, ntok, bdst, tmpa, tmpb, btmp):
                for tt in range(ntok // 512):
                    sl = slice(tt * 512, (tt + 1) * 512)
                    bk = tt % 3
                    k.mm(banks[bk][:, :], ropeP, X[:, sl], True, True, [bX, b_nc], [bbank[bk]])
                    k.tt('pool', tmpa[:, 0:512], X[:, sl], ct[:, sl], ALU.mult, [bX, b_rt], [btmp[0]])
                    k.tt('dve', tmpb[:, 0:512], banks[bk][:, :], st_[:, sl], ALU.mult, [bbank[bk], b_rt], [btmp[1]])
                    k.tt('dve', dst_bf[:, sl], tmpa[:, 0:512], tmpb[:, 0:512], ALU.add, [btmp[0], btmp[1]], [bdst])

            gts = k.sb([128, NQT, 48], F32, "gts")
            b_g = P.buf()
            k.dma('sp', gts[:], PT[OWN0:LOC, 1552:1600].rearrange("(n p) c -> p n c", p=128), (), [b_g])
            k.act(gts[:], gts[:], AF.Sigmoid, [b_g], [b_g])

            for g in range(2 if NSA_GROUPS_RUN is None else NSA_GROUPS_RUN):
                with k.scope():
                    KsT = k.sb([128, LOC], BF16, "KsT")
                    KwT = k.sb([128, LOC], BF16, "KwT")
                    Vs1 = k.sb([128, 32, 129], BF16, "Vs1")
                    Vw1 = k.sb([128, 32, 129], BF16, "Vw1")
                    KcmpT = k.sb([128, 256], BF16, "KcmpT")
                    RHSc = k.sb([128, 2, 193], BF16, "RHSc")
                    QTh = [k.sb([128, NOWN], BF16, "QTh%d" % i) for i in range(8)]
                    bKs, bKw, bVs, bVw, bKc, bRc = P.bufs(6)
                    bQh = P.bufs(8)
                    k.memset('dve', Vs1[:, :, 128:129], 1.0, [bVs])
                    k.memset('dve', Vw1[:, :, 128:129], 1.0, [bVw])
                    k.dma('pool', Vs1[:, :, 0:128], PT[:, 1040 + g * 128:1040 + (g + 1) * 128].rearrange("(n p) c -> p n c", p=128), (), [bVs])
                    k.dma('pool', Vw1[:, :, 0:128], PT[:, 1296 + g * 128:1296 + (g + 1) * 128].rearrange("(n p) c -> p n c", p=128), (), [bVw])
                    k.memset('dve', KcmpT[:], 0.0, [bKc])
                    k.cp('dve', RHSc[:, :, 0:65], ovl[:], [b_nc], [bRc])
                    with k.scope():
                        X = k.sb([128, LOC], F32, "ropeX")
                        tmpa = k.sb([128, 512], F32, "ropeA")
                        tmpb = k.sb([128, 512], F32, "ropeB")
                        KcT = k.sb([128, LOC], BF16, "KcT")
                        VcT = k.sb([128, LOC], BF16, "VcT")
                        bX, bKcT, bVcT = P.bufs(3)
                        btmp = P.bufs(2)
                        for (dstb, bd, row0) in [(KcT, bKcT, 5120 + g * 128), (KsT, bKs, 5632 + g * 128), (KwT, bKw, 5888 + g * 128)]:
                            k.dma('sp', X[:], PF[row0:row0 + 128, :], (), [bX])
                            rope(dstb, X, bX, cosT, sinT, LOC, bd, tmpa, tmpb, btmp)
                        k.dma('pool', VcT[:], PF[5376 + g * 128:5376 + (g + 1) * 128, :], (), [bVcT])
                        for hh in range(8):
                            row0 = 3072 + (g * 8 + hh) * 128
                            k.dma('sp', X[:, 0:NOWN], PF[row0:row0 + 128, OWN0:LOC], (), [bX])
                            rope(QTh[hh], X, bX, cosq, sinq, NOWN, bQh[hh], tmpa, tmpb, btmp)
                        w1 = k.sb([128, 32, 256], BF16, "cw1")
                        w2 = k.sb([128, 2, 128], BF16, "cw2")
                        cpT = k.sb([128, 32], F32, "cpT")
                        cpTb = k.sb([128, 32], BF16, "cpTb")
                        hidT = k.sb([128, 2, 256], BF16, "hidT")
                        hx = k.sb([128, 256], F32, "hx")
                        hy = k.sb([128, 256], F32, "hy")
                        hb = k.sb([128, 2], F32, "hb")
                        bw1, bw2, bcp, bhid, bhx, bhy, bhb = P.bufs(7)
                        for which, (srcT, bsrc) in enumerate([(KcT, bKcT), (VcT, bVcT)]):
                            k.dma('pool', w1[:], cw1_in[which].rearrange("(l d) h -> d l h", d=128), (), [bw1])
                            k.dma('pool', w2[:], cw2_in[which].rearrange("(t p) d -> p t d", p=128), (), [bw2])
                            load_fm(cpT[:], cpos_in[which], 32, bcp, bank=3)
                            k.cp('dve', cpTb[:], cpT[:], [bcp], [bcp])
                            k.memset('dve', hidT[:], 0.0, [bhid])
                            for ht in range(2):
                                for l in range(32):
                                    k.mm(banks[0][:, 0:255], w1[:, l, ht * 128:(ht + 1) * 128], srcT[:, l:l + 16 * 254 + 1:16],
                                         l == 0, l == 31, [bw1, bsrc], [bbank[0]])
                                for l in range(32):
                                    k.mm(banks[1][:, 0:1], w1[:, l, ht * 128:(ht + 1) * 128], cpTb[:, l:l + 1],
                                         l == 0, l == 31, [bw1, bcp], [bbank[1]])
                                k.cp('dve', hb[:, ht:ht + 1], banks[1][:, 0:1], [bbank[1]], [bhb])
                                k.act(hx[:, 0:255], banks[0][:, 0:255], AF.Identity, [bbank[0], bhb], [bhx], bias=hb[:, ht:ht + 1])
                                k.tt('dve', hy[:, 0:255], hx[:, 0:255], hx[:, 0:255], ALU.mult, [bhx], [bhy])
                                k.ts('dve', hy[:, 0:255], hy[:, 0:255], 0.044715, 1.0, ALU.mult, ALU.add, [bhy], [bhy])
                                k.tt('dve', hy[:, 0:255], hy[:, 0:255], hx[:, 0:255], ALU.mult, [bhy, bhx], [bhy])
                                k.act(hy[:, 0:255], hy[:, 0:255], AF.Sigmoid, [bhy], [bhy], scale=1.5957691216057308)
                                k.tt('dve', hidT[:, ht, 0:255], hy[:, 0:255], hx[:, 0:255], ALU.mult, [bhy, bhx], [bhid])
                            if which == 0:
                                for ht in range(2):
                                    k.mm(banks[2][:, 0:255], w2[:, ht, :], hidT[:, ht, 0:255], ht == 0, ht == 1, [bw2, bhid], [bbank[2]])
                                k.cp('dve', KcmpT[:, 0:255], banks[2][:, 0:255], [bbank[2]], [bKc])
                            else:
                                for j in range(2):
                                    nn = 128 if j == 0 else 127
                                    for ht in range(2):
                                        k.mm(banks[2][0:nn, 0:128], hidT[:, ht, j * 128:j * 128 + nn], w2[:, ht, :], ht == 0, ht == 1, [bw2, bhid], [bbank[2]])
                                    k.memset('dve', RHSc[:, j, 65:193], 0.0, [bRc])
                                    k.cp('dve', RHSc[0:nn, j, 65:193], banks[2][0:nn, 0:128], [bbank[2]], [bRc])
                    impacc = k.sb([128, 64], F32, "impacc")
                    v1 = k.sb([128, 64], F32, "selv1")
                    v2 = k.sb([128, 64], F32, "selv2")
                    m8 = k.sb([128, 16], F32, "m8")
                    selb = k.sb([128, 64], F32, "selb")
                    selbT = k.sb([64, 128], BF16, "selbT")
                    Oacc = k.sb([128, 8, 128], F32, "Oacc")
                    o16 = [k.sb([128, 128], BF16, "no16_%d" % i) for i in range(2)]
                    Et = [k.sb([128, 128], BF16, "Et%d" % i) for i in range(4)]
                    sc_ = k.sb([128, 8], F32, "nsc_small")
                    bimp, bsel, bselT, bO, bsc = P.bufs(5)
                    bo16 = P.bufs(2)
                    bEt = P.bufs(4)
                    ectr = [0]

                    def score_exp(lhsK, bK_, rhsQ, bQ_, extra, bias_ap):
                        e = ectr[0] % 4
                        bk = ectr[0] % 3
                        ectr[0] += 1
                        k.mm(banks[bk][:, 0:128], lhsK, rhsQ, True, len(extra) == 0, [bK_, bQ_], [bbank[bk]])
                        for xi, (l_, r_, bl_) in enumerate(extra):
                            k.mm(banks[bk][:, 0:128], l_, r_, False, xi == len(extra) - 1, bl_, [bbank[bk]])
                        if bias_ap is None:
                            k.act(Et[e][:], banks[bk][:, 0:128], AF.Exp, [bbank[bk]], [bEt[e]])
                        else:
                            k.act(Et[e][:], banks[bk][:, 0:128], AF.Exp, [bbank[bk], b_nc], [bEt[e]], bias=bias_ap)
                        return Et[e], bEt[e]

                    for qt in range(NQT if NSA_QT_RUN is None else NSA_QT_RUN):
                        qsl = slice(qt * 128, (qt + 1) * 128)
                        for hh in range(8):
                            head = g * 8 + hh
                            ets = []
                            for j in range(2):
                                u0 = (OWN0 + 128 * qt) if j == 0 else 128 * qt
                                ets.append(score_exp(KcmpT[:, j * 128:(j + 1) * 128], bKc, QTh[hh][:, qsl], bQh[hh],
                                                     [(identb[:], maskB[:, u0:u0 + 128], [b_const, b_nc])], kvb if j == 0 else None))
                            for j in range(2):
                                k.mm(banks[3][:, 0:193], ets[j][0][:], RHSc[:, j, :], j == 0, j == 1, [ets[j][1], bRc], [bbank[3]])
                            k.ts('dve', sc_[:, 0:1], banks[3][:, 64:65], 1e-30, None, ALU.max, None, [bbank[3]], [bsc])
                            k.recip(sc_[:, 1:2], sc_[:, 0:1], [bsc], [bsc])
                            if hh == 0:
                                k.ts('dve', impacc[:], banks[3][:, 0:64], sc_[:, 1:2], None, ALU.mult, None, [bbank[3], bsc], [bimp])
                            else:
                                k.stt(impacc[:], banks[3][:, 0:64], sc_[:, 1:2], impacc[:], ALU.mult, ALU.add, [bbank[3], bsc, bimp], [bimp])
                            k.ts('dve', sc_[:, 2:3], gts[:, qt, head * 3:head * 3 + 1], sc_[:, 1:2], None, ALU.mult, None, [b_g, bsc], [bsc])
                            k.ts('dve', Oacc[:, hh, :], banks[3][:, 65:193], sc_[:, 2:3], None, ALU.mult, None, [bbank[3], bsc], [bO])
                        k.tt('dve', v1[:], impacc[:], fv[:, 0, qt, :], ALU.max, [bimp, b_nc], [bsel])
                        k.tt('dve', v1[:], v1[:], ncore[:, 0:64], ALU.max, [bsel, b_nc], [bsel])
                        k.tt('dve', v1[:], v1[:], fv[:, 1, qt, :], ALU.min, [bsel, b_nc], [bsel])
                        k.tt('dve', v1[:], v1[:], ncore[:, 64:128], ALU.min, [bsel, b_nc], [bsel])
                        P.op('dve', lambda e: e.max(out=m8[:, 0:8], in_=v1[:]), [bsel], [bsel])
                        P.op('dve', lambda e: e.match_replace(out=v2[:], in_to_replace=m8[:, 0:8], in_values=v1[:], imm_value=-3.0e38), [bsel], [bsel])
                        P.op('dve', lambda e: e.max(out=m8[:, 8:16], in_=v2[:]), [bsel], [bsel])
                        k.ts('dve', v2[:], v1[:], m8[:, 15:16], None, ALU.is_ge, None, [bsel], [bsel])
                        k.ts('dve', selb[:], v1[:], -1.0e29, None, ALU.is_gt, None, [bsel], [bsel])
                        k.tt('dve', selb[:], selb[:], v2[:], ALU.mult, [bsel], [bsel])
                        k.ts('dve', selb[:], selb[:], -1.0, 30000.0, ALU.add, ALU.mult, [bsel], [bsel])
                        k.tr(banks[3][0:64, 0:128], selb[:], ident[:], [bsel, b_const], [bbank[3]])
                        k.cp('dve', selbT[:], banks[3][0:64, 0:128], [bbank[3]], [bselT])
                        for hh in range(8):
                            head = g * 8 + hh
                            kdiag = OWN0 // 128 + qt
                            for kt in range(0, kdiag + 1):
                                ex = [(eexp[:, kt * 128:(kt + 1) * 128], selbT[:], [b_nc, bselT])]
                                if kt == kdiag:
                                    ex.append((identb[:], cmk[:, 0, :], [b_const, b_nc]))
                                et, bet = score_exp(KsT[:, kt * 128:(kt + 1) * 128], bKs, QTh[hh][:, qsl], bQh[hh], ex, None)
                                k.mm(banks[4][:, 0:129], et[:], Vs1[:, kt, :], kt == 0, kt == kdiag, [bet, bVs], [bbank[4]])
                            k.ts('dve', sc_[:, 3:4], banks[4][:, 128:129], 1e-30, None, ALU.max, None, [bbank[4]], [bsc])
                            k.recip(sc_[:, 4:5], sc_[:, 3:4], [bsc], [bsc])
                            k.ts('dve', sc_[:, 4:5], sc_[:, 4:5], gts[:, qt, head * 3 + 1:head * 3 + 2], None, ALU.mult, None, [b_g, bsc], [bsc])
                            k.stt(Oacc[:, hh, :], banks[4][:, 0:128], sc_[:, 4:5], Oacc[:, hh, :], ALU.mult, ALU.add, [bbank[4], bsc, bO], [bO])
                            for kt in range(kdiag - 4, kdiag + 1):
                                ex = []
                                if kt == kdiag - 4:
                                    ex.append((identb[:], cmk[:, 1, :], [b_const, b_nc]))
                                if kt == kdiag:
                                    ex.append((identb[:], cmk[:, 0, :], [b_const, b_nc]))
                                et, bet = score_exp(KwT[:, kt * 128:(kt + 1) * 128], bKw, QTh[hh][:, qsl], bQh[hh], ex,
                                                    kvb if kt < OWN0 // 128 else None)
                                k.mm(banks[5][:, 0:129], et[:], Vw1[:, kt, :], kt == kdiag - 4, kt == kdiag, [bet, bVw], [bbank[5]])
                            k.ts('dve', sc_[:, 5:6], banks[5][:, 128:129], 1e-30, None, ALU.max, None, [bbank[5]], [bsc])
                            k.recip(sc_[:, 6:7], sc_[:, 5:6], [bsc], [bsc])
                            k.ts('dve', sc_[:, 6:7], sc_[:, 6:7], gts[:, qt, head * 3 + 2:head * 3 + 3], None, ALU.mult, None, [b_g, bsc], [bsc])
                            k.stt(Oacc[:, hh, :], banks[5][:, 0:128], sc_[:, 6:7], Oacc[:, hh, :], ALU.mult, ALU.add, [bbank[5], bsc, bO], [bO])
                            oi = hh % 2
                            k.tr(banks[3][:, 0:128], Oacc[:, hh, :], ident[:], [bO, b_const], [bbank[3]])
                            k.cp('act', o16[oi][:], banks[3][:, 0:128], [bbank[3]], [bo16[oi]])
                            k.dma('sp', OBT[head * 128:(head + 1) * 128, qsl], o16[oi][:], [bo16[oi]], ())
        if stage == 4 and DEBUG_OUT and not SKIP_NSA:
            dbg_ob = nc.dram_tensor("dbg_ob", [2048, NOWN], BF16, kind="ExternalOutput").ap()
            k.dma('sp', dbg_ob, OBT, (), ())

    def bcast_row(dst, src_fm, n, bsrc, bdst, name):
        rowd = k.dram([1, n * 128], F32, "row_" + name)
        t_ = P.dma('sp', rowd[0].rearrange("(c p) -> p c", p=128), src_fm, [bsrc], (), allow_slow_non_contiguous=True)
        brow = P.buf()
        brow.lw = t_
        k.dma('sp', dst, rowd[0].partition_broadcast(128), [brow], [bdst])

    if stage >= 5 and not SKIP_P5:
        wbg_in = din("w_branch_gdn", [1024, D])
        wbn_in = din("w_branch_nsa", [D, D])
        wout_in = din("w_out", [D, D])
        with k.scope():
            Wg = k.sb([128, 8, D], BF16, "Wg")
            Wn = k.sb([128, 16, D], BF16, "Wn")
            bWg, bWn = P.bufs(2)
            for kc in range(8):
                k.dma('pool', Wg[:, kc, :], wbg_in[kc * 128:(kc + 1) * 128, :], (), [bWg])
            for kc in range(16):
                k.dma('pool', Wn[:, kc, :], wbn_in[kc * 128:(kc + 1) * 128, :], (), [bWn])
            oat = [k.sb([128, 8, 512], BF16, "oat%d" % i) for i in range(2)]
            obt = [k.sb([128, 16, 512], BF16, "obt%d" % i) for i in range(2)]
            boat, bobt = P.bufs(2), P.bufs(2)
            ga = [k.sb([128, 512], F32, "ga%d" % i) for i in range(2)]
            gb = [k.sb([128, 512], F32, "gb%d" % i) for i in range(2)]
            y16 = [k.sb([128, 512], BF16, "y16_%d" % i) for i in range(2)]
            bga, bgb, by16 = P.bufs(2), P.bufs(2), P.bufs(2)
            OATv = OAT.rearrange("(kc p) t -> p kc t", p=128)
            OBTv = OBT.rearrange("(kc p) t -> p kc t", p=128)
            it = 0
            for tt in range(NOWN // 512):
                s2 = tt % 2
                tsl = slice(tt * 512, (tt + 1) * 512)
                k.dma('sp', oat[s2][:], OATv[:, :, tsl], (), [boat[s2]])
                k.dma('sp', obt[s2][:], OBTv[:, :, tsl], (), [bobt[s2]])
                for ct in range(16):
                    s = it % 2
                    it += 1
                    bA, bB = (0, 1) if s == 0 else (2, 3)
                    csl = slice(ct * 128, (ct + 1) * 128)
                    k.dma('sp', ga[s][:], PF[6144 + ct * 128:6144 + (ct + 1) * 128, OWN0 + tt * 512:OWN0 + (tt + 1) * 512], (), [bga[s]])
                    k.dma('sp', gb[s][:], PF[8192 + ct * 128:8192 + (ct + 1) * 128, OWN0 + tt * 512:OWN0 + (tt + 1) * 512], (), [bgb[s]])
                    k.act(ga[s][:], ga[s][:], AF.Sigmoid, [bga[s]], [bga[s]])
                    k.act(gb[s][:], gb[s][:], AF.Sigmoid, [bgb[s]], [bgb[s]])
                    for kc in range(8):
                        k.mm(banks[bA][:, :], Wg[:, kc, csl], oat[s2][:, kc, :], kc == 0, kc == 7, [bWg, boat[s2]], [bbank[bA]])
                    for kc in range(16):
                        k.mm(banks[bB][:, :], Wn[:, kc, csl], obt[s2][:, kc, :], kc == 0, kc == 15, [bWn, bobt[s2]], [bbank[bB]])
                    k.tt('dve', ga[s][:], ga[s][:], banks[bA][:, :], ALU.mult, [bga[s], bbank[bA]], [bga[s]])
                    k.tt('dve', gb[s][:], gb[s][:], banks[bB][:, :], ALU.mult, [bgb[s], bbank[bB]], [bgb[s]])
                    k.tt('pool', y16[s][:], ga[s][:], gb[s][:], ALU.add, [bga[s], bgb[s]], [by16[s]])
                    k.dma('sp', YT[csl, tsl], y16[s][:], [by16[s]], ())
        with k.scope():
            Wo = k.sb([128, 16, D], BF16, "Wo")
            g1b = k.sb([128, D], F32, "g1b")
            bWo, bg1 = P.bufs(2)
            for kc in range(16):
                k.dma('pool', Wo[:, kc, :], wout_in[kc * 128:(kc + 1) * 128, :], (), [bWo])
            bcast_row(g1b[:], modT[:, 32:48], 16, b_mod, bg1, "g1")
            yt = [k.sb([128, 16, 128], BF16, "yt%d" % i) for i in range(2)]
            xo = [k.sb([128, D], F32, "xo%d" % i) for i in range(2)]
            zt_ = [k.sb([128, 512], F32, "zt5_%d" % i) for i in range(2)]
            byt, bxo, bzt = P.bufs(2), P.bufs(2), P.bufs(2)
            YTv = YT.rearrange("(kc p) t -> p kc t", p=128)
            it = 0
            for t in range(NOWN // 128):
                s = t % 2
                k.dma('sp', yt[s][:], YTv[:, :, t * 128:(t + 1) * 128], (), [byt[s]])
                k.dma('sp', xo[s][:], xl[OWN0 + t * 128:OWN0 + (t + 1) * 128, :], (), [bxo[s]])
                for cb_ in range(4):
                    z2 = it % 2
                    bk = it % 4
                    it += 1
                    csl = slice(cb_ * 512, (cb_ + 1) * 512)
                    for kc in range(16):
                        k.mm(banks[bk][:, :], yt[s][:, kc, :], Wo[:, kc, csl], kc == 0, kc == 15, [byt[s], bWo], [bbank[bk]])
                    k.tt('dve', zt_[z2][:], banks[bk][:, :], g1b[:, csl], ALU.mult, [bbank[bk], bg1], [bzt[z2]])
                    k.tt('pool', xo[s][:, csl], xo[s][:, csl], zt_[z2][:], ALU.add, [bxo[s], bzt[z2]], [bxo[s]])
                k.dma('sp', X1[t * 128:(t + 1) * 128, :], xo[s][:], [bxo[s]], ())
        P.barrier()
        if stage == 5 and DEBUG_OUT:
            dbg_x1 = nc.dram_tensor("dbg_x1", [NOWN, D], F32, kind="ExternalOutput").ap()
            k.dma('sp', dbg_x1, X1, (), ())

    if stage == 6 and DEBUG_OUT:
        X2dbg = nc.dram_tensor("dbg_x2", [NOWN, D], F32, kind="ExternalOutput").ap()
    if stage >= 6:
        n2_in = din("norm2_w", [KC, 128])
        wq_in = din("peer_wq", [D, D])
        pk_in = [din("peer_keys1", [8, 128, 128]), din("peer_keys2", [8, 128, 128])]
        fnw6_in = din("final_norm_w", [1, D])
        H2T = k.dram([KC, 128, NOWN], BF16, "H2T")
        NEB = 64 if PEER_EB is None else PEER_EB
        with k.scope():
            A2 = k.sb([128, KC], F32, "A2")
            n2T = k.sb([128, KC], F32, "n2T")
            keysT = k.sb([128, 16, 128], BF16, "keysT")
            bA2, bn2, bg2, bfn, bkT = P.bufs(5)
            load_fm(n2T[:], n2_in, KC, bn2)
            k.stt(A2[:], modT[:, 64:80], 1.0, n2T[:], ALU.add, ALU.mult, [b_mod, bn2], [bA2])
            g2row = k.dram([1, D], F32, "row_g2")
            k.dma('sp', g2row[0].rearrange("(c p) -> p c", p=128), modT[:, 80:96], [b_mod], (), allow_slow_non_contiguous=True)
            with k.scope():
                kt_ = [k.sb([128, 128], F32, "kraw%d" % i) for i in range(2)]
                bkr = P.bufs(2)
                for hh2 in range(16):
                    s = hh2 % 2
                    k.dma('sp', kt_[s][:], pk_in[hh2 % 2][hh2 // 2], (), [bkr[s]])
                    k.tr(banks[s][:, 0:128], kt_[s][:], ident[:], [bkr[s], b_const], [bbank[s]])
                    k.cp('dve', keysT[:, hh2, :], banks[s][:, 0:128], [bbank[s]], [bkT])
            with k.scope():
                xt = [k.sb([128, D], F32, "x6t%d" % i) for i in range(2)]
                xs = [k.sb([128, D], BF16, "x6s%d" % i) for i in range(2)]
                st = [k.sb([128, 4], F32, "s6t%d" % i) for i in range(2)]
                ho = [k.sb([128, KC, 128], BF16, "h6o%d" % i) for i in range(2)]
                bxt, bxs, bst, bho = P.bufs(2), P.bufs(2), P.bufs(2), P.bufs(2)
                for t in range(NOWN // 128):
                    s = t % 2
                    k.dma('sp', xt[s][:], X1[t * 128:(t + 1) * 128, :], (), [bxt[s]])
                    k.act(xs[s][:], xt[s][:], AF.Square, [bxt[s]], [bxs[s], bst[s]], accum=st[s][:, 0:1])
                    k.ts('dve', st[s][:, 1:2], st[s][:, 0:1], 1.0 / D, 1e-6, ALU.mult, ALU.add, [bst[s]], [bst[s]])
                    k.act(st[s][:, 2:3], st[s][:, 1:2], AF.Sqrt, [bst[s]], [bst[s]])
                    k.recip(st[s][:, 3:4], st[s][:, 2:3], [bst[s]], [bst[s]])
                    k.ts('dve', xs[s][:], xt[s][:], st[s][:, 3:4], None, ALU.mult, None, [bxt[s], bst[s]], [bxs[s]])
                    for kc in range(KC):
                        pi = kc % 2
                        sl = slice((kc // 2 % 4) * 128, (kc // 2 % 4) * 128 + 128)
                        k.tr(pbf[pi][:, sl], xs[s][:, kc * 128:(kc + 1) * 128], identb[:], [bxs[s], b_const], [bpbf[pi]])
                        if kc % 2 == 0:
                            k.act(ho[s][:, kc, :], pbf[pi][:, sl], AF.Identity, [bpbf[pi], bA2, b_mod], [bho[s]],
                                  bias=modT[:, 48 + kc:49 + kc], scale=A2[:, kc:kc + 1])
                        else:
                            k.ts('dve', ho[s][:, kc, :], pbf[pi][:, sl], A2[:, kc:kc + 1], modT[:, 48 + kc:49 + kc],
                                 ALU.mult, ALU.add, [bpbf[pi], bA2, b_mod], [bho[s]])
                    k.dma('sp', H2T[:, :, t * 128:(t + 1) * 128].rearrange("c p t -> p c t"), ho[s][:], [bho[s]], ())
            P.barrier()
            wqv = wq_in.rearrange("(kc p) c -> p kc c", p=128)
            outv6 = out_d.rearrange("(t p) d -> t p d", p=128)
            for tg in range(4 if PEER_TG is None else PEER_TG):
                with k.scope():
                    h2g = k.sb([128, KC, 512], BF16, "h2g")
                    stile = k.sb([128, 4, 16, 128], F32, "stile")
                    Bt = k.sb([128, 4, 8, 128], F32, "Bt")
                    tau = k.sb([128, 4, 8], F32, "tau")
                    OUTacc = k.sb([128, 4, D], F32, "OUTacc")
                    bh2, bst_, bBt, btau, bOut = P.bufs(5)
                    k.dma('sp', h2g[:], H2T[:, :, tg * 512:(tg + 1) * 512].rearrange("c p t -> p c t"), (), [bh2])
                    k.memset('pool', OUTacc[:], 0.0, [bOut])
                    with k.scope():
                        wqb = [k.sb([128, KC, 128], BF16, "wqb%d" % i) for i in range(2)]
                        qh = [k.sb([128, 512], BF16, "qh%d" % i) for i in range(2)]
                        bwq, bqh = P.bufs(2), P.bufs(2)
                        for hh2 in range(16):
                            s = hh2 % 2
                            k.dma('pool', wqb[s][:], wqv[:, :, hh2 * 128:(hh2 + 1) * 128], (), [bwq[s]])
                            for kc in range(KC):
                                k.mm(banks[s][:, :], wqb[s][:, kc, :], h2g[:, kc, :], kc == 0, kc == KC - 1, [bwq[s], bh2], [bbank[s]])
                            k.cp('act', qh[s][:], banks[s][:, :], [bbank[s]], [bqh[s]])
                            for tl in range(4):
                                k.mm(banks[2 + s][:, tl * 128:(tl + 1) * 128], qh[s][:, tl * 128:(tl + 1) * 128], keysT[:, hh2, :], True, True,
                                     [bqh[s], bkT], [bbank[2 + s]])
                            k.cp('dve', stile[:, :, hh2, :], banks[2 + s][:, :].rearrange("p (t j) -> p t j", t=4), [bbank[2 + s]], [bst_])
                    with k.scope():
                        v16 = k.sb([128, 2, 16], F32, "v16")
                        vv = k.sb([128, 128], F32, "vv")
                        cand = k.sb([128, 16, 16], F32, "cand")
                        cv = k.sb([128, 256], F32, "cv")
                        c16 = k.sb([128, 16], F32, "c16")
                        sm = k.sb([128, 8], F32, "sm")
                        bsel6 = P.buf()
                        for tl in range(4):
                            for h in range(8):
                                for half in range(2):
                                    src = stile[:, tl, 2 * h + half, :]
                                    P.op('dve', lambda e, src=src, half=half: e.max(out=v16[:, half, 0:8], in_=src), [bst_], [bsel6])
                                    P.op('dve', lambda e, src=src, half=half: e.match_replace(out=vv[:], in_to_replace=v16[:, half, 0:8], in_values=src, imm_value=-3.0e38), [bst_, bsel6], [bsel6])
                                    P.op('dve', lambda e, half=half: e.max(out=v16[:, half, 8:16], in_=vv[:]), [bsel6], [bsel6])
                                for a in range(16):
                                    k.ts('dve', cand[:, a, :], v16[:, 1, :], v16[:, 0, a:a + 1], None, ALU.add, None, [bsel6], [bsel6])
                                cf = cand[:].rearrange("p a b -> p (a b)")
                                P.op('dve', lambda e, cf=cf: e.max(out=c16[:, 0:8], in_=cf), [bsel6], [bsel6])
                                P.op('dve', lambda e, cf=cf: e.match_replace(out=cv[:], in_to_replace=c16[:, 0:8], in_values=cf, imm_value=-3.0e38), [bsel6], [bsel6])
                                P.op('dve', lambda e: e.max(out=c16[:, 8:16], in_=cv[:]), [bsel6], [bsel6])
                                k.ts('dve', sm[:, 0:1], c16[:, 0:1], -1.0, None, ALU.mult, None, [bsel6], [bsel6])
                                k.act(cv[:, 0:16], c16[:], AF.Exp, [bsel6], [bsel6], bias=sm[:, 0:1], accum=sm[:, 1:2])
                                k.act(sm[:, 2:3], sm[:, 1:2], AF.Ln, [bsel6], [bsel6])
                                k.tt('dve', sm[:, 3:4], sm[:, 0:1], sm[:, 2:3], ALU.subtract, [bsel6], [bsel6])
                                k.ts('dve', Bt[:, tl, h, :], stile[:, tl, 2 * h, :], sm[:, 3:4], None, ALU.add, None, [bst_, bsel6], [bBt])
                                k.act(sm[:, 4:5], c16[:, 15:16], AF.Exp, [bsel6], [bsel6], bias=sm[:, 3:4])
                                k.ts('dve', tau[:, tl, h:h + 1], sm[:, 4:5], 0.99999, None, ALU.mult, None, [bsel6], [btau])
                    with k.scope():
                        ub = [k.sb([128, KC, 256], BF16, "ub%d" % i) for i in range(2)]
                        vb = [k.sb([128, 2, D], BF16, "vb%d" % i) for i in range(2)]
                        bub, bvb = P.bufs(2), P.bufs(2)
                        gx_ = [k.sb([128, 256], F32, "gx%d" % i) for i in range(2)]
                        gy_ = [k.sb([128, 256], F32, "gy%d" % i) for i in range(2)]
                        es_ = [k.sb([128, 8, 128], F32, "es%d" % i) for i in range(2)]
                        mk_ = [k.sb([128, 8, 128], F32, "mk%d" % i) for i in range(2)]
                        coef_ = [k.sb([128, 2, 128], F32, "coef%d" % i) for i in range(2)]
                        G16_ = [k.sb([128, 256], BF16, "G16_%d" % i) for i in range(2)]
                        GT_ = [[k.sb([128, 128], BF16, "GT%d_%d" % (p_, i)) for i in range(2)] for p_ in range(2)]
                        bgx_, bgy_, bmk_, bG16_ = P.bufs(2), P.bufs(2), P.bufs(2), P.bufs(2)
                        bes_ = [P.bufs(8) for p_ in range(2)]
                        bcoef_ = [P.bufs(2) for p_ in range(2)]
                        bOutS = [[P.buf() for c_ in range(4)] for t_ in range(4)]
                        bGT_ = [P.bufs(2) for p_ in range(2)]
                        pit = 0
                        def head(eb, tl, par, s):
                            gx, gy, coef, G16 = gx_[par], gy_[par], coef_[par], G16_[par]
                            bgx, bgy, bcoef, bG16 = bgx_[par], bgy_[par], bcoef_[par], bG16_[par]
                            for kc in range(KC):
                                k.mm(banks[par][:, 0:256], h2g[:, kc, tl * 128:(tl + 1) * 128], ub[s][:, kc, :], kc == 0, kc == KC - 1,
                                     [bh2, bub[s]], [bbank[par]])
                            k.cp('act', gx[:], banks[par][:, 0:256], [bbank[par]], [bgx])
                            k.tt('pool', gy[:], gx[:], gx[:], ALU.mult, [bgx], [bgy])
                            k.ts('pool', gy[:], gy[:], 0.044715, 1.0, ALU.mult, ALU.add, [bgy], [bgy])
                            k.tt('pool', gy[:], gy[:], gx[:], ALU.mult, [bgy, bgx], [bgy])
                            k.act(gy[:], gy[:], AF.Sigmoid, [bgy], [bgy], scale=1.5957691216057308)
                            k.tt('pool', gy[:], gy[:], gx[:], ALU.mult, [bgy, bgx], [bgy])
                            for il in range(2):
                                i_ = 2 * eb + il
                                es, mk, bes, bmk = es_[il], mk_[il], bes_[il], bmk_[il]
                                for h in range(8):
                                    k.act(es[:, h, :], stile[:, tl, 2 * h + 1, :], AF.Exp, [bst_, bBt], [bes[h]], bias=Bt[:, tl, h, i_:i_ + 1])
                                k.tt('dve', mk[:], es[:], tau[:, tl, :].unsqueeze(2).to_broadcast([128, 8, 128]), ALU.is_ge, bes + [btau], [bmk])
                                k.tt('dve', mk[:], mk[:], es[:], ALU.mult, [bmk] + bes, [bmk])
                                k.tt('dve', mk[:, 0:4, :], mk[:, 0:4, :], mk[:, 4:8, :], ALU.add, [bmk], [bmk])
                                k.tt('dve', mk[:, 0:2, :], mk[:, 0:2, :], mk[:, 2:4, :], ALU.add, [bmk], [bmk])
                                k.tt('dve', coef[:, il, :], mk[:, 0, :], mk[:, 1, :], ALU.add, [bmk], [bcoef[il]])
                            k.tt('dve', G16[:], gy[:], coef[:].rearrange("p i j -> p (i j)"), ALU.mult, [bgy] + bcoef, [bG16])

                        def tail(eb, tl, par, s):
                            G16, GT, bG16, bGT = G16_[par], GT_[par], bG16_[par], bGT_[par]
                            for il in range(2):
                                k.tr(pbf[il][:, par * 128:(par + 1) * 128], G16[:, il * 128:(il + 1) * 128], identb[:], [bG16, b_const], [bpbf[il]])
                                k.cp('act', GT[il][:], pbf[il][:, par * 128:(par + 1) * 128], [bpbf[il]], [bGT[il]])
                            for cb_ in range(4):
                                bk = 2 + cb_
                                csl = slice(cb_ * 512, (cb_ + 1) * 512)
                                k.mm(banks[bk][:, :], GT[0][:], vb[s][:, 0, csl], True, False, [bGT[0], bvb[s]], [bbank[bk]])
                                k.mm(banks[bk][:, :], GT[1][:], vb[s][:, 1, csl], False, True, [bGT[1], bvb[s]], [bbank[bk]])
                                k.tt('dve', OUTacc[:, tl, csl], OUTacc[:, tl, csl], banks[bk][:, :], ALU.add, [bOut, bOutS[tl][cb_], bbank[bk]], [bOutS[tl][cb_]])

                        prev = None
                        pit = 0
                        for eb in range(NEB):
                            s = eb % 2
                            k.dma('sp', ub[s][:], UBd[eb], [bconvU[eb]], [bub[s]])
                            k.dma('sp', vb[s][:], VBd[eb], [bconvV[eb]], [bvb[s]])
                            for tl in range(4):
                                par = pit % 2
                                pit += 1
                                head(eb, tl, par, s)
                                if prev is not None:
                                    tail(*prev)
                                prev = (eb, tl, par, s)
                        tail(*prev)
                    with k.scope():
                        g2b = k.sb([128, D], F32, "g2b")
                        fnw6 = k.sb([128, D], F32, "fnw6")
                        k.dma('sp', g2b[:], g2row[0].partition_broadcast(128), (), [bg2])
                        k.dma('sp', fnw6[:], fnw6_in[0].partition_broadcast(128), (), [bfn])
                        x1t = [k.sb([128, D], F32, "x1t%d" % i) for i in range(2)]
                        fj6 = [k.sb([128, D], BF16, "fj6%d" % i) for i in range(2)]
                        fs6 = [k.sb([128, 4], F32, "fs6%d" % i) for i in range(2)]
                        bx1t, bfj6, bfs6 = P.bufs(2), P.bufs(2), P.bufs(2)
                        for tl in range(4):
                            s = tl % 2
                            t = tg * 4 + tl
                            k.dma('sp', x1t[s][:], X1[t * 128:(t + 1) * 128, :], (), [bx1t[s]])
                            k.tt('dve', OUTacc[:, tl, :], OUTacc[:, tl, :], g2b[:], ALU.mult, [bOut, bg2] + bOutS[tl], [bOut])
                            k.tt('dve', x1t[s][:], x1t[s][:], OUTacc[:, tl, :], ALU.add, [bx1t[s], bOut], [bx1t[s]])
                            if stage == 6 and DEBUG_OUT:
                                k.dma('sp', X2dbg[t * 128:(t + 1) * 128, :], x1t[s][:], [bx1t[s]], ())
                            k.act(fj6[s][:], x1t[s][:], AF.Square, [bx1t[s]], [bfj6[s], bfs6[s]], accum=fs6[s][:, 0:1])
                            k.ts('dve', fs6[s][:, 1:2], fs6[s][:, 0:1], 1.0 / D, 1e-6, ALU.mult, ALU.add, [bfs6[s]], [bfs6[s]])
                            k.act(fs6[s][:, 2:3], fs6[s][:, 1:2], AF.Sqrt, [bfs6[s]], [bfs6[s]])
                            k.recip(fs6[s][:, 3:4], fs6[s][:, 2:3], [bfs6[s]], [bfs6[s]])
                            k.stt(x1t[s][:], x1t[s][:], fs6[s][:, 3:4], fnw6[:], ALU.mult, ALU.mult, [bx1t[s], bfs6[s], bfn], [bx1t[s]])
                            k.dma('sp', outv6[t], x1t[s][:], [bx1t[s]], ())

    if stage < 6:
      pass
    fnw_in = din("final_norm_w", [1, D]) if stage < 6 else None
    fnw = k.sb([128, D], F32, "fnw")
    b_fnw = P.buf()
    if stage < 6:
        k.dma('sp', fnw[:], fnw_in[0].partition_broadcast(128), (), [b_fnw])
    X2v = xl.rearrange("(t p) d -> t p d", p=128)
    outv = out_d.rearrange("(t p) d -> t p d", p=128)
    ft = [k.sb([128, D], F32, "ft%d" % i) for i in range(2)]
    fj = [k.sb([128, D], BF16, "fj%d" % i) for i in range(2)]
    fs = [k.sb([128, 4], F32, "fs%d" % i) for i in range(2)]
    bft, bfj, bfs = P.bufs(2), P.bufs(2), P.bufs(2)
    for t in range(NOWN // 128 if stage < 6 else 0):
        s = t % 2
        k.dma('sp', ft[s][:], X2v[OWN0 // 128 + t], (), [bft[s]])
        k.act(fj[s][:], ft[s][:], AF.Square, [bft[s]], [bfj[s], bfs[s]], accum=fs[s][:, 0:1])
        k.ts('dve', fs[s][:, 1:2], fs[s][:, 0:1], 1.0 / D, 1e-6, ALU.mult, ALU.add, [bfs[s]], [bfs[s]])
        k.act(fs[s][:, 2:3], fs[s][:, 1:2], AF.Sqrt, [bfs[s]], [bfs[s]])
        k.recip(fs[s][:, 3:4], fs[s][:, 2:3], [bfs[s]], [bfs[s]])
        k.stt(ft[s][:], ft[s][:], fs[s][:, 3:4], fnw[:], ALU.mult, ALU.mult, [bft[s], bfs[s], b_fnw], [bft[s]])
        k.dma('sp', outv[t], ft[s][:], [bft[s]], ())

    with nc.Block() as block:
        P.emit(block)
    k.es.close()
    return nc


def _gconst():
    NEGM = -30000.0
    r = np.arange(128)[:, None]
    c = np.arange(128)[None, :]
    g = np.zeros((128, 5, 128), np.float32)
    g[:, 4, :] = (r // 32 == c // 32)
    g[:, 0, :] = (r <= c)
    g[:, 1, :] = np.where(r > c, 0.0, NEGM)
    g[:, 2, :] = np.where(c > r, 0.0, NEGM)
    g[:, 3, :] = np.where(c >= r, 0.0, NEGM)
    return g


GCONST = _gconst()


def _nsa_consts():
    NEGM = -30000.0
    c = {}
    n128 = np.zeros((128, 385), np.float32)
    for m in range(16):
        n128[m + 16, m] = -1.0
    for m in range(16, 32):
        n128[m - 16, m] = 1.0
    half = 16
    inv_freq = (500000.0 ** (-np.arange(half, dtype=np.float32) / half)).astype(np.float32)
    for d in range(32):
        n128[d, 128] = inv_freq[d % 16]
    r = np.arange(128)[:, None]
    q = np.arange(128)[None, :]
    n128[:, 129:257] = np.where(r <= q, 0.0, NEGM)
    n128[:, 257:385] = np.where(r > q, 0.0, NEGM)
    c["nsa_c128"] = n128
    u = np.arange(LOC)[None, :]
    c["maskB"] = np.where(16 * r + 31 <= u, 0.0, NEGM).astype(np.float32)
    c["eexp"] = (np.arange(LOC)[None, :] // 64 == np.arange(64)[:, None]).astype(np.float32)
    ovl = np.zeros((128, 2, 65), np.float32)
    for j in range(2):
        for nl in range(128):
            n = j * 128 + nl
            if n >= 255:
                continue
            for sblk in range(64):
                if 16 * n < 64 * sblk + 64 and 16 * n + 32 > 64 * sblk:
                    ovl[nl, j, sblk] = 1.0
            ovl[nl, j, 64] = 1.0
    c["ovl"] = ovl
    fvv = np.zeros((128, 2, 16, 64), np.float32)
    for qt in range(16):
        for qq in range(128):
            t = OWN0 + qt * 128 + qq
            cur = t // 64
            for sblk in range(64):
                fb = 0.0
                if sblk == cur:
                    fb = 2.0e9
                elif sblk == cur - 1:
                    fb = 3.0e9
                fvv[qq, 0, qt, sblk] = fb
                fvv[qq, 1, qt, sblk] = 3.0e38 if 64 * sblk <= t else -1.0e30
    c["fbigvis"] = fvv
    return c


NSA_CONSTS = _nsa_consts()


def _nsa_core(half):
    a = np.zeros((128, 129), np.float32)
    first = 0 if half == 1 else 32
    a[:, first] = 1.0e9
    a[:, 64:128] = 3.0e38
    if half == 0:
        a[:, 64:64 + 32] = -1.0e30
        a[:, 128] = -30000.0
    return a


def _pos_loc(inp, b, half):
    p = np.asarray(inp['positions'], np.int32)[b]
    out = np.zeros((1, LOC), np.int32)
    if half == 1:
        out[0] = p
    else:
        out[0, OWN0:] = p[:NOWN]
    return out


PEER_UT_CACHE = {}


def make_in_maps(inp):
    x = np.asarray(inp['x'], np.float32)
    c = np.asarray(inp['c'], np.float32)
    maps = []
    ident = np.eye(128, dtype=np.float32)
    for core in range(8):
        b, half = core // 2, core % 2
        xl = np.zeros((LOC, D), np.float32)
        if half == 0:
            xl[OWN0:] = x[b, :NOWN]
        else:
            xl[:] = x[b]
        m = {
            "xl": xl,
            "cb": np.ascontiguousarray(c[b].reshape(KC, 128)),
            "ada_w": np.ascontiguousarray(inp['ada_w'][0]),
            "ada_b": np.ascontiguousarray(np.asarray(inp['ada_b'][0]).reshape(96, 128)),
            "norm1_w": np.ascontiguousarray(np.asarray(inp['norm1_w'][0]).reshape(KC, 128)),
            "w_in": np.ascontiguousarray(inp['w_in'][0]),
            "ident": ident,
            "pvalid": np.full((128, 1), float(half), np.float32),
            "w_branch_gdn": np.ascontiguousarray(inp['w_branch_gdn'][0], np.float32),
            "w_branch_nsa": np.ascontiguousarray(inp['w_branch_nsa'][0], np.float32),
            "w_out": np.ascontiguousarray(inp['w_out'][0], np.float32),
            "norm2_w": np.ascontiguousarray(np.asarray(inp['norm2_w'][0], np.float32).reshape(KC, 128)),
            "peer_wq": np.ascontiguousarray(inp['peer_wq'][0], np.float32),
            "peer_keys1": np.ascontiguousarray(inp['peer_keys1'][0], np.float32),
            "peer_keys2": np.ascontiguousarray(inp['peer_keys2'][0], np.float32),
            "peer_uT": PEER_UT_CACHE.get(id(inp['peer_u'])) if id(inp['peer_u']) in PEER_UT_CACHE else PEER_UT_CACHE.setdefault(id(inp['peer_u']), np.ascontiguousarray(np.asarray(inp['peer_u'][0], np.float32).T)),
            "peer_v": np.ascontiguousarray(inp['peer_v'][0], np.float32),
            "gconst": GCONST,
            "pos": np.ascontiguousarray(_pos_loc(inp, b, half)),
            "nsa_core": _nsa_core(half),
            "cmp_pos_k": np.ascontiguousarray(inp['cmp_pos_k'][0], np.float32), "cmp_pos_v": np.ascontiguousarray(inp['cmp_pos_v'][0], np.float32),
            "cmp_w1_k": np.ascontiguousarray(inp['cmp_w1_k'][0], np.float32), "cmp_w1_v": np.ascontiguousarray(inp['cmp_w1_v'][0], np.float32),
            "cmp_w2_k": np.ascontiguousarray(inp['cmp_w2_k'][0], np.float32), "cmp_w2_v": np.ascontiguousarray(inp['cmp_w2_v'][0], np.float32),
            **NSA_CONSTS,
            "gdn_conv_w": np.ascontiguousarray(np.asarray(inp['gdn_conv_w'][0], np.float32).reshape(96, 128)),
            "gdn_A_log": np.ascontiguousarray(np.broadcast_to(np.asarray(inp['gdn_A_log'][0], np.float32)[None, :], (32, 8))),
            "gdn_dt_bias": np.ascontiguousarray(np.broadcast_to(np.asarray(inp['gdn_dt_bias'][0], np.float32)[None, :], (32, 8))),
            "gdn_norm_w": np.ascontiguousarray(np.asarray(inp['gdn_norm_w'][0], np.float32).reshape(1, 128)),
            "final_norm_w": np.ascontiguousarray(np.asarray(inp['final_norm_w'], np.float32).reshape(1, D)),
        }
        maps.append(m)
    return maps


def kernel(**inputs):
    nc = build_program(6)
    maps = make_in_maps(inputs)
    res = run_bass_kernel_spmd(nc, maps, core_ids=list(range(8)))
    out = np.zeros((4, SEQ, D), np.float32)
    for core in range(8):
        b, half = core // 2, core % 2
        out[b, half * NOWN:(half + 1) * NOWN] = res.results[core]["out"]
    return out
```

```python
import numpy as np
from contextlib import ExitStack
import concourse.bass as bass
import concourse.mybir as mybir
from concourse.bass_utils import run_bass_kernel_spmd

F32 = mybir.dt.float32
BF16 = mybir.dt.bfloat16
I32 = mybir.dt.int32
ALU = mybir.AluOpType
AF = mybir.ActivationFunctionType
AX = mybir.AxisListType

ENGS = ['pe', 'act', 'dve', 'pool', 'sp']

D = 2048
SEQ = 4096
LOC = 4096
OWN0 = 2048
NOWN = 2048
D_IN = 11840
KC = 16
DEBUG_OUT = False
GDN_ONE_HEAD = False
FROM_REF = False
GDN_SUB = 9
PF_ROWS = 80 * 128
GDN_WAVES = None
GDN_STEPS = 10 ** 9
NSA_GROUPS_RUN = None
NSA_QT_RUN = None
SKIP_GDN = False
SKIP_NSA = False
SKIP_P5 = False
REF_IN = set()
PEER_EB = None
PEER_TG = None


class Buf:
    __slots__ = ('name', 'lw', 'rd', 'excl')

    def __init__(self, name):
        self.name = name
        self.lw = None
        self.rd = {}
        self.excl = False


class Prog:
    def __init__(self, nc, n_dma_sems=40):
        self.nc = nc
        self.ops = {e: [] for e in ENGS}
        self.cnt = {e: 0 for e in ENGS}
        self.esem = {e: nc.alloc_semaphore(name="es_" + e) for e in ENGS}
        self.dsem = [nc.alloc_semaphore(name="ds_%d" % i) for i in range(n_dma_sems)]
        self.dval = [0] * n_dma_sems
        self.dnext = 0
        self.bsem = [nc.alloc_semaphore(name="bs_%d" % i) for i in range(8)]
        self.bval = [0] * 8
        self.bnext = 0
        self.waited = {e: {} for e in ENGS}
        self.pend = {e: [] for e in ENGS}
        self.nbuf = 0

    def barrier(self):
        snap = [(('e', o), self.cnt[o]) for o in ENGS if self.cnt[o] > 0]
        snap += [(('d', i), self.dval[i]) for i in range(len(self.dsem)) if self.dval[i] > 0]
        for e in ENGS:
            self.pend[e] = snap

    def _pending(self, eng, waits):
        w = self.waited[eng]
        for key, val in self.pend[eng]:
            if key == ('e', eng):
                continue
            if w.get(key, 0) < val:
                w[key] = val
                waits.append((self._sem(key), val))
        self.pend[eng] = []

    def buf(self, name=None):
        self.nbuf += 1
        return Buf(name or ("b%d" % self.nbuf))

    def bufs(self, n):
        return [self.buf() for _ in range(n)]

    def _sem(self, key):
        if key[0] == 'e':
            return self.esem[key[1]]
        return self.dsem[key[1]] if key[0] == 'd' else self.bsem[key[1]]

    def _deps(self, eng, reads, writes):
        need = {}

        def add(tok, raw):
            if tok is None:
                return
            key, val, teng = tok
            if teng == eng and eng == 'pe':
                return
            if need.get(key, 0) < val:
                need[key] = val

        for b in reads:
            add(b.lw, True)
            if b.excl:
                for t in b.rd.values():
                    if t[2] != eng:
                        add(t, True)
        for b in writes:
            add(b.lw, False)
            for t in b.rd.values():
                add(t, False)
        waits = []
        w = self.waited[eng]
        for key, val in need.items():
            if w.get(key, 0) < val:
                w[key] = val
                waits.append((self._sem(key), val))
        return waits

    def _mark(self, tok, reads, writes):
        for b in reads:
            b.rd[tok[0]] = tok
        for b in writes:
            b.lw = tok
            b.rd = {}

    def op(self, eng, fn, reads=(), writes=()):
        waits = self._deps(eng, reads, writes)
        self._pending(eng, waits)
        self.cnt[eng] += 1
        tok = (('e', eng), self.cnt[eng], eng)
        self.ops[eng].append((waits, fn, (self.esem[eng], 1)))
        self._mark(tok, reads, writes)
        return tok

    def dma(self, eng, out, in_, reads=(), writes=(), bg=False, **kw):
        if bg:
            sems, vals, idx, kk = self.bsem, self.bval, self.bnext, 'b'
            self.bnext = (self.bnext + 1) % len(self.bsem)
        else:
            sems, vals, idx, kk = self.dsem, self.dval, self.dnext, 'd'
            self.dnext = (self.dnext + 1) % len(self.dsem)
        waits = self._deps(eng, reads, writes)
        self._pending(eng, waits)
        key = (kk, idx)
        w = self.waited[eng]
        if w.get(key, 0) < vals[idx]:
            w[key] = vals[idx]
            waits.append((sems[idx], vals[idx]))
        vals[idx] += 16
        tok = (key, vals[idx], None)
        sem_ = sems[idx]
        self.ops[eng].append((waits, (lambda e: e.dma_start(out=out, in_=in_, **kw)), (sem_, 16)))
        self._mark(tok, reads, writes)
        return tok

    def emit(self, block):
        fin = [(self.dsem[i], self.dval[i]) for i in range(len(self.dsem)) if self.dval[i] > 0]
        fin += [(self.bsem[i], self.bval[i]) for i in range(len(self.bsem)) if self.bval[i] > 0]
        fin += [(self.esem[e], self.cnt[e]) for e in ENGS if e != 'sp' and self.cnt[e] > 0]

        def run(e, name):
            for waits, fn, inc in self.ops[name]:
                for s, v in waits:
                    e.wait_ge(s, v)
                ins = fn(e)
                ins.then_inc(inc[0], inc[1])
            if name == 'sp':
                for s, v in fin:
                    e.wait_ge(s, v)

        @block.tensor
        def _(e):
            run(e, 'pe')

        @block.scalar
        def _(e):
            run(e, 'act')

        @block.vector
        def _(e):
            run(e, 'dve')

        @block.gpsimd
        def _(e):
            run(e, 'pool')

        @block.sync
        def _(e):
            run(e, 'sp')


class K:
    def __init__(self, nc):
        self.nc = nc
        self.P = Prog(nc)
        self.es = ExitStack()
        self.n = 0

    def scope(self):
        kk = self

        class _S:
            def __enter__(s_):
                s_.old = kk.es
                kk.es = ExitStack()
                return s_

            def __exit__(s_, *a):
                kk.es.close()
                kk.es = s_.old
                kk.P.barrier()
                return False
        return _S()

    def sb(self, shape, dt=F32, name=None):
        self.n += 1
        return self.es.enter_context(self.nc.sbuf_tensor(("%s_%d" % (name, self.n)) if name else ("t%d" % self.n), list(shape), dt))

    def ps(self, shape, dt=F32, name=None):
        self.n += 1
        return self.es.enter_context(self.nc.psum_tensor(name or ("p%d" % self.n), list(shape), dt))

    def dram(self, shape, dt=F32, name=None, kind="Internal"):
        self.n += 1
        return self.nc.dram_tensor(name or ("d%d" % self.n), list(shape), dt, kind=kind).ap()

    def mm(self, out, lhsT, rhs, start, stop, r, w):
        return self.P.op('pe', lambda e: e.matmul(out, lhsT=lhsT, rhs=rhs, start=start, stop=stop), r, w)

    def tr(self, out, in_, ident, r, w):
        return self.P.op('pe', lambda e: e.transpose(out, in_, ident), r, w)

    def act(self, out, in_, func, r, w, bias=None, scale=None, accum=None, eng='act'):
        kw = {}
        if bias is not None:
            kw['bias'] = bias
        if scale is not None:
            kw['scale'] = scale
        if accum is not None:
            kw['accum_out'] = accum
        return self.P.op('act', lambda e: e.activation(out=out, in_=in_, func=func, **kw), r, w)

    def ts(self, eng, out, in0, s1, s2, op0, op1, r, w):
        if op1 is None:
            return self.P.op(eng, lambda e: e.tensor_scalar(out=out, in0=in0, scalar1=s1, scalar2=None, op0=op0), r, w)
        return self.P.op(eng, lambda e: e.tensor_scalar(out=out, in0=in0, scalar1=s1, scalar2=s2, op0=op0, op1=op1), r, w)

    def tt(self, eng, out, in0, in1, op, r, w):
        return self.P.op(eng, lambda e: e.tensor_tensor(out=out, in0=in0, in1=in1, op=op), r, w)

    def stt(self, out, in0, scalar, in1, op0, op1, r, w):
        return self.P.op('dve', lambda e: e.scalar_tensor_tensor(out=out, in0=in0, scalar=scalar, in1=in1, op0=op0, op1=op1), r, w)

    def cp(self, eng, out, in_, r, w):
        if eng == 'act':
            return self.P.op('act', lambda e: e.copy(out=out, in_=in_), r, w)
        return self.P.op(eng, lambda e: e.tensor_copy(out=out, in_=in_), r, w)

    def memset(self, eng, ap, val, w):
        return self.P.op(eng, lambda e: e.memset(ap, val), (), w)

    def recip(self, out, in_, r, w):
        return self.P.op('dve', lambda e: e.reciprocal(out=out, in_=in_), r, w)

    def dma(self, eng, out, in_, r, w, **kw):
        return self.P.dma(eng, out, in_, r, w, **kw)


def build_program(stage=99):
    nc = bass.Bass("TRN2", target_bir_lowering=False)
    k = K(nc)
    P = k.P

    def din(name, shape, dt=F32):
        return nc.dram_tensor(name, list(shape), dt, kind="ExternalInput").ap()

    xl = din("xl", [LOC, D])
    cb = din("cb", [KC, 128])
    ada_w = din("ada_w", [D, 6 * D]) if not FROM_REF else None
    ada_b = din("ada_b", [96, 128])
    norm1_w = din("norm1_w", [KC, 128])
    w_in = din("w_in", [D, D_IN]) if not FROM_REF else None
    ident_in = din("ident", [128, 128])
    pvalid_in = din("pvalid", [128, 1])
    out_d = nc.dram_tensor("out", [NOWN, D], F32, kind="ExternalOutput").ap()
    dbg_mod = nc.dram_tensor("dbg_mod", [128, 96], F32, kind="ExternalOutput").ap()
    def scratch(name, shape, dt=F32):
        if name in REF_IN:
            return din(name + "_in", shape, dt)
        return k.dram(shape, dt, name)

    if FROM_REF:
        PF = din("PF_in", [PF_ROWS, LOC])
        PT = din("PT_in", [LOC, 1600])
    else:
        PF = k.dram([80 * 128, LOC], F32, "PF")
        PT = k.dram([LOC, 1600], F32, "PT")
    OAT = scratch("OAT", [1024, NOWN], BF16)
    OBT = scratch("OBT", [2048, NOWN], BF16)
    YT = scratch("YT", [D, NOWN], BF16)
    X1 = scratch("X1", [NOWN, D], F32)

    ident = k.sb([128, 128], F32, "ident_sb")
    identb = k.sb([128, 128], BF16, "identb")
    pvalid = k.sb([128, 1], F32, "pvalid_sb")
    b_const = P.buf("const")
    k.dma('sp', ident[:], ident_in, (), [b_const])
    k.dma('sp', pvalid[:], pvalid_in, (), [b_const])
    k.cp('dve', identb[:], ident[:], [b_const], [b_const])

    banks = [k.ps([128, 512], F32, "bank%d" % i) for i in range(6)]
    bbank = [P.buf("bank%d" % i) for i in range(6)]
    for b_ in bbank:
        b_.excl = True
    pbf = [k.ps([128, 1024], BF16, "pbf%d" % i) for i in range(2)]
    bpbf = [P.buf("pbf%d" % i) for i in range(2)]
    for b_ in bpbf:
        b_.excl = True

    def load_fm(dst, src, n, bdst, bank=5):
        tmp = k.sb([96, 128], F32)
        bt = P.buf()
        k.dma('sp', tmp[0:n, :], src, (), [bt])
        k.tr(banks[bank][:, 0:n], tmp[0:n, :], ident[0:n, 0:n], [bt, b_const], [bbank[bank]])
        k.cp('dve', dst, banks[bank][:, 0:n], [bbank[bank]], [bdst])

    modT = k.sb([128, 96], F32, "modT")
    b_mod = P.buf("mod")
    A1 = k.sb([128, KC], F32, "A1")
    B1p = k.sb([128, KC], F32, "B1p")
    b_A1 = P.buf()
    if FROM_REF:
        modT_in = din("modT_in", [128, 96])
        k.dma('sp', modT[:], modT_in, (), [b_mod])
    with k.scope():
      if not FROM_REF:
        cT = k.sb([128, KC], F32, "cT")
        csT = k.sb([128, KC], F32, "csT")
        adabT = k.sb([128, 96], F32, "adabT")
        n1T = k.sb([128, KC], F32, "n1T")
        b_c, b_cs, b_adab, b_n1 = P.bufs(4)
        load_fm(cT[:], cb, KC, b_c)
        load_fm(adabT[:], ada_b, 96, b_adab)
        load_fm(n1T[:], norm1_w, KC, b_n1)
        k.act(csT[:], cT[:], AF.Silu, [b_c], [b_cs])
        NAW = 3
        awt = [k.sb([128, KC, 128], F32, "awt%d" % i) for i in range(NAW)]
        bawt = P.bufs(NAW)
        ada_v = ada_w.rearrange("(kc p) j -> p kc j", p=128)
        for m in range(96):
            s = m % NAW
            k.dma('sp', awt[s][:], ada_v[:, :, m * 128:(m + 1) * 128], (), [bawt[s]])
            for kc in range(KC):
                k.mm(banks[4][:, m:m + 1], awt[s][:, kc, :], csT[:, kc:kc + 1], kc == 0, kc == KC - 1,
                     [bawt[s], b_cs], [bbank[4]])
        k.tt('dve', modT[:], banks[4][:, 0:96], adabT[:], ALU.add, [bbank[4], b_adab], [b_mod])
        k.dma('sp', dbg_mod, modT[:], [b_mod], ())
        k.stt(A1[:], modT[:, 16:32], 1.0, n1T[:], ALU.add, ALU.mult, [b_mod, b_n1], [b_A1])
        k.ts('dve', B1p[:], modT[:, 0:16], pvalid[:, 0:1], None, ALU.mult, None, [b_mod, b_const], [b_A1])

    if stage >= 1 and not FROM_REF:
        hscope = k.scope()
        hscope.__enter__()
        hT = k.sb([128, KC, LOC], BF16, "hT")
        b_hT = [P.buf() for _ in range(LOC // 128)]
        with k.scope():
            xt = [k.sb([128, D], F32, "xt%d" % i) for i in range(2)]
            xs = [k.sb([128, D], BF16, "xs%d" % i) for i in range(2)]
            st = [k.sb([128, 4], F32, "st%d" % i) for i in range(2)]
            bxt, bxs, bst = P.bufs(2), P.bufs(2), P.bufs(2)
            xlv = xl.rearrange("(t p) d -> t p d", p=128)
            for t in range(LOC // 128):
                s = t % 2
                k.dma('sp', xt[s][:], xlv[t], (), [bxt[s]])
                k.act(xs[s][:], xt[s][:], AF.Square, [bxt[s]], [bxs[s], bst[s]], accum=st[s][:, 0:1])
                k.ts('dve', st[s][:, 1:2], st[s][:, 0:1], 1.0 / D, 1e-6, ALU.mult, ALU.add, [bst[s]], [bst[s]])
                k.act(st[s][:, 2:3], st[s][:, 1:2], AF.Sqrt, [bst[s]], [bst[s]])
                k.recip(st[s][:, 3:4], st[s][:, 2:3], [bst[s]], [bst[s]])
                k.ts('dve', xs[s][:], xt[s][:], st[s][:, 3:4], None, ALU.mult, None, [bxt[s], bst[s]], [bxs[s]])
                Bsel = B1p if t < OWN0 // 128 else modT
                for kc in range(KC):
                    pi = kc % 2
                    sl = slice((kc // 2 % 4) * 128, (kc // 2 % 4) * 128 + 128)
                    k.tr(pbf[pi][:, sl], xs[s][:, kc * 128:(kc + 1) * 128], identb[:], [bxs[s], b_const], [bpbf[pi]])
                    if kc % 2 == 0:
                        k.act(hT[:, kc, t * 128:(t + 1) * 128], pbf[pi][:, sl], AF.Identity, [bpbf[pi], b_A1, b_mod], [b_hT[t]],
                              bias=Bsel[:, kc:kc + 1], scale=A1[:, kc:kc + 1])
                    else:
                        k.ts('dve', hT[:, kc, t * 128:(t + 1) * 128], pbf[pi][:, sl], A1[:, kc:kc + 1], Bsel[:, kc:kc + 1],
                             ALU.mult, ALU.add, [bpbf[pi], b_A1, b_mod], [b_hT[t]])
        if stage == 1:
            dbg_h = nc.dram_tensor("dbg_h", [128, KC, LOC], BF16, kind="ExternalOutput").ap()
            k.dma('sp', dbg_h, hT[:], b_hT, ())

        if stage >= 2:
            FM = [(0, 3072, 0, 0), (4112, 2048, 3072, OWN0), (6160, 256, 5120, 0), (6416, 256, 5376, 0),
                  (6672, 256, 5632, 0), (7184, 256, 5888, 0), (7744, 2048, 6144, OWN0), (9792, 2048, 8192, OWN0)]
            TM = [(3072, 256, 0, OWN0), (3328, 256, 256, OWN0), (3584, 256, 512, OWN0), (3840, 256, 768, OWN0),
                  (4096, 16, 1024, 0), (6928, 256, 1040, 0), (7440, 256, 1296, 0), (7696, 48, 1552, OWN0)]
            with k.scope():
                wv = w_in.rearrange("(kc p) c -> p kc c", p=128)
                NW = 3
                wt = [k.sb([128, KC, 256], BF16, "wt%d" % i) for i in range(NW)]
                bwt = P.bufs(NW)
                ev = [k.sb([128, 512], F32, "ev%d" % i) for i in range(4)]
                bev = P.bufs(4)
                ei = 0
                wi = 0
                for (c0, ncols, r0, t0) in FM:
                    for cb_ in range(ncols // 256):
                        s = wi % NW
                        wi += 1
                        k.dma('pool', wt[s][:], wv[:, :, c0 + cb_ * 256:c0 + cb_ * 256 + 256], (), [bwt[s]])
                        for hf in range(2):
                            for tt in range(t0 // 512, LOC // 512):
                                e4 = ei % 4
                                ei += 1
                                for kc in range(KC):
                                    k.mm(banks[e4][:, :], wt[s][:, kc, hf * 128:(hf + 1) * 128],
                                         hT[:, kc, tt * 512:(tt + 1) * 512], kc == 0, kc == KC - 1,
                                         [bwt[s]] + b_hT[tt * 4:(tt + 1) * 4], [bbank[e4]])
                                k.cp('act' if e4 % 2 == 0 else 'dve', ev[e4][:], banks[e4][:, :], [bbank[e4]], [bev[e4]])
                                row = r0 + cb_ * 256 + hf * 128
                                k.dma('sp', PF[row:row + 128, tt * 512:(tt + 1) * 512], ev[e4][:], [bev[e4]], ())
                for (c0, ncols, p0, t0) in TM:
                    s = wi % NW
                    wi += 1
                    k.dma('pool', wt[s][:, :, 0:ncols], wv[:, :, c0:c0 + ncols], (), [bwt[s]])
                    for t in range(t0 // 128, LOC // 128):
                        e4 = ei % 4
                        ei += 1
                        for kc in range(KC):
                            k.mm(banks[e4][:, 0:ncols], hT[:, kc, t * 128:(t + 1) * 128], wt[s][:, kc, 0:ncols],
                                 kc == 0, kc == KC - 1, [bwt[s], b_hT[t]], [bbank[e4]])
                        k.cp('act' if e4 % 2 == 0 else 'dve', ev[e4][:, 0:ncols], banks[e4][:, 0:ncols], [bbank[e4]], [bev[e4]])
                        k.dma('sp', PT[t * 128:(t + 1) * 128, p0:p0 + ncols], ev[e4][:, 0:ncols], [bev[e4]], ())
        hscope.__exit__(None, None, None)
        if stage == 2 and DEBUG_OUT:
            dbg_pf = nc.dram_tensor("dbg_pf", [4, 128, LOC], F32, kind="ExternalOutput").ap()
            dbg_pt = nc.dram_tensor("dbg_pt", [LOC, 576], F32, kind="ExternalOutput").ap()
            for i, r in enumerate([0, 3072, 5120, 6144]):
                k.dma('sp', dbg_pf[i], PF[r:r + 128, :], (), ())
            k.dma('sp', dbg_pt, PT[:, 1024:1600], (), ())

    if stage >= 6:
        uT_in = din("peer_uT", [D, 16384])
        pv_in = din("peer_v", [16384, D])
        UBd = k.dram([64, 128, KC, 256], BF16, "UBd")
        VBd = k.dram([64, 128, 2, D], BF16, "VBd")
        bconvU = [P.buf() for _ in range(64)]
        bconvV = [P.buf() for _ in range(64)]
        uTv0 = uT_in.rearrange("(kc p) e -> p kc e", p=128)
        for eb in range(64):
            P.dma('pool', UBd[eb], uTv0[:, :, eb * 256:(eb + 1) * 256], (), [bconvU[eb]], bg=True)
            P.dma('pool', VBd[eb], pv_in[eb * 256:(eb + 1) * 256, :].rearrange("(i p) d -> p i d", p=128), (), [bconvV[eb]], bg=True)
    CONV_TOK = {}
    if stage >= 3 and not SKIP_GDN:
        gconst_in = din("gconst", [128, 5, 128])
        convw_in = din("gdn_conv_w", [96, 128])
        alog_in = din("gdn_A_log", [32, 8])
        dtb_in = din("gdn_dt_bias", [32, 8])
        gnw_in = din("gdn_norm_w", [1, 128])
        bq = [[bbank[bi]] * 4 for bi in range(6)]
        NCH = LOC // 128
        with k.scope():
            gcn = k.sb([128, 5, 128], F32, "gcn")
            ones = k.sb([128, 128], F32, "ones")
            cwT = k.sb([128, 96], F32, "cwT")
            nwb = k.sb([128, 128], F32, "nwb")
            b_gc = P.buf()
            b_cw = P.buf()
            k.dma('sp', gcn[:], gconst_in, (), [b_gc])
            k.memset('dve', ones[:], 1.0, [b_gc])
            k.dma('sp', nwb[:], gnw_in[0].partition_broadcast(128), (), [b_gc])
            load_fm(cwT[:], convw_in, 96, b_cw)
            TriU, msl, msu, miu, bmk = gcn[:, 0, :], gcn[:, 1, :], gcn[:, 2, :], gcn[:, 3, :], gcn[:, 4, :]
            ab = k.sb([128, NCH, 16], F32, "ab")
            dtb = k.sb([128, NCH, 8], F32, "dtb")
            alg = k.sb([128, NCH, 8], F32, "alg")
            vt = {nm: k.sb([128, NCH, 8], F32, "v_" + nm) for nm in
                  ["t1", "g", "beta", "lnb", "Gc", "nGc", "u", "gam", "bg", "kd", "gend", "Gl"]}
            b_v = P.buf()
            k.dma('sp', ab[:], PT[:, 1024:1040].rearrange("(n p) c -> p n c", p=128), (), [b_v])
            k.dma('sp', dtb[:], dtb_in.partition_broadcast(128), (), [b_v])
            k.dma('sp', alg[:], alog_in.partition_broadcast(128), (), [b_v])
            k.tt('dve', vt["t1"][:], ab[:, :, 0:8], dtb[:], ALU.add, [b_v], [b_v])
            k.act(vt["t1"][:], vt["t1"][:], AF.Exp, [b_v], [b_v])
            e_, ser, lnp = vt["t1"], vt["Gl"], vt["Gc"]
            k.ts('dve', ser[:], e_[:], -0.25, 1.0 / 3.0, ALU.mult, ALU.add, [b_v], [b_v])
            k.tt('dve', ser[:], ser[:], e_[:], ALU.mult, [b_v], [b_v])
            k.ts('dve', ser[:], ser[:], -0.5, None, ALU.add, None, [b_v], [b_v])
            k.tt('dve', ser[:], ser[:], e_[:], ALU.mult, [b_v], [b_v])
            k.ts('dve', ser[:], ser[:], 1.0, None, ALU.add, None, [b_v], [b_v])
            k.tt('dve', ser[:], ser[:], e_[:], ALU.mult, [b_v], [b_v])
            k.ts('dve', lnp[:], e_[:], 0.1, None, ALU.max, None, [b_v], [b_v])
            k.act(lnp[:], lnp[:], AF.Ln, [b_v], [b_v], bias=1.0)
            k.ts('dve', vt["u"][:], e_[:], 0.1, None, ALU.is_lt, None, [b_v], [b_v])
            k.tt('dve', ser[:], ser[:], lnp[:], ALU.subtract, [b_v], [b_v])
            k.tt('dve', ser[:], ser[:], vt["u"][:], ALU.mult, [b_v], [b_v])
            k.tt('dve', vt["t1"][:], lnp[:], ser[:], ALU.add, [b_v], [b_v])
            k.act(alg[:], alg[:], AF.Exp, [b_v], [b_v])
            k.stt(vt["g"][:], vt["t1"][:], -1.0, alg[:], ALU.mult, ALU.mult, [b_v], [b_v])
            k.act(vt["beta"][:], ab[:, :, 8:16], AF.Sigmoid, [b_v], [b_v])
            k.act(vt["lnb"][:], vt["beta"][:], AF.Ln, [b_v], [b_v])
            gflat = vt["g"][:].rearrange("p n h -> p (n h)")
            k.mm(banks[0][:, 0:256], TriU, gflat, True, True, [b_v, b_gc], [bq[0][0], bq[0][1]])
            k.mm(banks[0][:, 256:512], ones[:], gflat, True, True, [b_v, b_gc], [bq[0][2], bq[0][3]])
            fl = lambda nm: vt[nm][:].rearrange("p n h -> p (n h)")
            k.cp('dve', fl("Gc"), banks[0][:, 0:256], [bq[0][0], bq[0][1]], [b_v])
            k.cp('dve', fl("Gl"), banks[0][:, 256:512], [bq[0][2], bq[0][3]], [b_v])
            k.ts('dve', fl("nGc"), fl("Gc"), -1.0, None, ALU.mult, None, [b_v], [b_v])
            k.tt('dve', fl("u"), fl("Gc"), fl("lnb"), ALU.add, [b_v], [b_v])
            k.act(fl("gam"), fl("Gc"), AF.Exp, [b_v], [b_v])
            k.act(fl("bg"), fl("u"), AF.Exp, [b_v], [b_v])
            k.tt('dve', fl("kd"), fl("Gl"), fl("Gc"), ALU.subtract, [b_v], [b_v])
            k.act(fl("kd"), fl("kd"), AF.Exp, [b_v], [b_v])
            k.act(fl("gend"), fl("Gl"), AF.Exp, [b_v], [b_v])

            def col(nm, n, h):
                return vt[nm][:, n, h:h + 1]

            WV = 4
            for h in range(0 if GDN_SUB < 1 else (1 if GDN_ONE_HEAD else 8)):
                with k.scope():
                    QT = k.sb([128, LOC], F32, "QT")
                    KT = k.sb([128, LOC], F32, "KT")
                    VT = k.sb([128, LOC], F32, "VT")
                    xr = k.sb([128, LOC], F32, "xr")
                    bQ, bK, bV, bxr = P.bufs(4)
                    for (dst, bd, row0, ti) in [(QT, bQ, h * 128, h), (KT, bK, 1024 + h * 128, 8 + h), (VT, bV, 2048 + h * 128, 16 + h)]:
                        k.dma('sp', xr[:], PF[row0:row0 + 128, :], (), [bxr])
                        k.ts('dve', dst[:], xr[:], cwT[:, 3 * 24 + ti:3 * 24 + ti + 1], None, ALU.mult, None, [bxr, b_cw], [bd])
                        for sh in (1, 2, 3):
                            j = 3 - sh
                            k.stt(dst[:, sh:LOC], xr[:, 0:LOC - sh], cwT[:, j * 24 + ti:j * 24 + ti + 1], dst[:, sh:LOC],
                                  ALU.mult, ALU.add, [bxr, b_cw, bd], [bd])
                        k.act(dst[:], dst[:], AF.Silu, [bd], [bd])
                    for (dst, bd, lnc) in [(QT, bQ, float(np.log(128.0 ** -0.5))), (KT, bK, 0.0)]:
                        for tt in range(LOC // 512):
                            bk = tt % 4
                            sl = slice(tt * 512, (tt + 1) * 512)
                            k.tt('dve', xr[:, sl], dst[:, sl], dst[:, sl], ALU.mult, [bd], [bxr])
                            k.mm(banks[bk][:, :], ones[:], xr[:, sl], True, True, [bxr, b_gc], bq[bk])
                            k.act(xr[:, sl], banks[bk][:, :], AF.Ln, bq[bk], [bxr], bias=1e-6)
                            k.act(xr[:, sl], xr[:, sl], AF.Exp, [bxr], [bxr], scale=-0.5, bias=lnc)
                            k.tt('dve', dst[:, sl], dst[:, sl], xr[:, sl], ALU.mult, [bd, bxr], [bd])
                    zt = k.sb([128, NOWN // 128, 128], F32, "zt")
                    bz = P.buf()
                    k.dma('sp', zt[:], PT[OWN0:LOC, h * 128:(h + 1) * 128].rearrange("(n p) c -> p n c", p=128), (), [bz])
                    k.act(zt[:], zt[:], AF.Silu, [bz], [bz])
                    S = k.sb([128, 128], F32, "S")
                    bS = P.buf()
                    k.memset('dve', S[:], 0.0, [bS])
                    names = ["R", "Kd", "dg", "tE", "L", "LT", "QK", "Pa", "Pb", "WT", "U", "tmp", "O", "o16", "Z", "DT", "X", "Tt"]
                    T = [{nm: k.sb([128, 256 if nm in ("R", "X", "Tt") else 128], BF16 if nm == "o16" else F32, "w%d_%s" % (i, nm)) for nm in names}
                         for i in range(WV)]
                    Bf = [{nm: P.buf() for nm in names + ["st"]} for i in range(WV)]
                    stt_ = [k.sb([128, 4], F32, "w%d_st" % i) for i in range(WV)]
                    PTk = [[k.sb([128, 128], F32, "w%d_PT%d" % (i, l)) for l in range(4)] for i in range(WV)]
                    bPTk = [[P.buf() for l in range(4)] for i in range(WV)]
                    bctr = [0]

                    def nb():
                        bctr[0] = (bctr[0] + 1) % 6
                        return bctr[0]

                    for w0 in (GDN_WAVES if GDN_WAVES is not None else range(0, NCH if GDN_SUB >= 2 else 0, WV)):
                        chunks = list(range(w0, w0 + WV))
                        own = w0 >= OWN0 // 128
                        def pre_steps(i, n):
                            t, b = T[i], Bf[i]
                            cs = slice(n * 128, (n + 1) * 128)
                            q4 = slice(i * 128, (i + 1) * 128)
                            st = []
                            bkT1, bkT2, bkKK, bkKQ, bkB = 0, 1, 2, 3, 4
                            st.append(lambda: k.tr(banks[0][:, q4], KT[:, cs], ident[:], [bK, b_const], [bq[0][i]]))
                            st.append(lambda: k.act(t["R"][:, 0:128], banks[0][:, q4], AF.Identity, [bq[0][i], b_v], [b["R"]], scale=col("bg", n, h)))
                            st.append(lambda: k.ts('dve', t["Kd"][:], banks[0][:, q4], col("kd", n, h), None, ALU.mult, None, [bq[0][i], b_v], [b["Kd"]]))
                            st.append(lambda: k.tr(banks[1][:, q4], VT[:, cs], ident[:], [bV, b_const], [bq[1][i]]))
                            st.append(lambda: k.ts('dve', t["R"][:, 128:256], banks[1][:, q4], col("beta", n, h), None, ALU.mult, None, [bq[1][i], b_v], [b["R"]]))
                            st.append(lambda: k.mm(banks[2][:, q4], KT[:, cs], KT[:, cs], True, True, [bK], [bq[2][i]]))
                            st.append(lambda: k.ts('dve', t["dg"][:], ident[:], col("nGc", n, h), None, ALU.mult, None, [b_const, b_v], [b["dg"]]))
                            st.append(lambda: k.mm(banks[4][:, q4], ones[:], t["dg"][:], True, True, [b["dg"], b_gc], [bq[4][i]]))
                            st.append(lambda: k.stt(t["tE"][:], banks[4][:, q4], col("u", n, h), msl, ALU.add, ALU.add, [bq[4][i], b_v, b_gc], [b["tE"]]))
                            st.append(lambda: k.act(t["tE"][:], t["tE"][:], AF.Exp, [b["tE"]], [b["tE"]]))
                            st.append(lambda: k.tt('dve', t["L"][:], banks[2][:, q4], t["tE"][:], ALU.mult, [bq[2][i], b["tE"]], [b["L"]]))
                            st.append(lambda: k.ts('dve', t["dg"][:], ident[:], col("u", n, h), None, ALU.mult, None, [b_const, b_v], [b["dg"]]))
                            st.append(lambda: k.mm(banks[5][:, q4], ones[:], t["dg"][:], True, True, [b["dg"], b_gc], [bq[5][i]]))
                            st.append(lambda: k.stt(t["tE"][:], banks[5][:, q4], col("nGc", n, h), msu, ALU.add, ALU.add, [bq[5][i], b_v, b_gc], [b["tE"]]))
                            st.append(lambda: k.act(t["tE"][:], t["tE"][:], AF.Exp, [b["tE"]], [b["tE"]]))
                            st.append(lambda: k.tt('dve', t["LT"][:], banks[2][:, q4], t["tE"][:], ALU.mult, [bq[2][i], b["tE"]], [b["LT"]]))
                            if own:
                                st.append(lambda: k.mm(banks[3][:, q4], KT[:, cs], QT[:, cs], True, True, [bK, bQ], [bq[3][i]]))
                                st.append(lambda: k.ts('dve', t["dg"][:], ident[:], col("Gc", n, h), None, ALU.mult, None, [b_const, b_v], [b["dg"]]))
                                st.append(lambda: k.mm(banks[4][:, q4], ones[:], t["dg"][:], True, True, [b["dg"], b_gc], [bq[4][i]]))
                                st.append(lambda: k.stt(t["tE"][:], banks[4][:, q4], col("nGc", n, h), miu, ALU.add, ALU.add, [bq[4][i], b_v, b_gc], [b["tE"]]))
                                st.append(lambda: k.act(t["tE"][:], t["tE"][:], AF.Exp, [b["tE"]], [b["tE"]]))
                                st.append(lambda: k.tt('dve', t["QK"][:], banks[3][:, q4], t["tE"][:], ALU.mult, [bq[3][i], b["tE"]], [b["QK"]]))
                            st.append(lambda: k.tt('dve', t["tmp"][:], t["LT"][:], bmk, ALU.mult, [b["LT"], b_gc], [b["tmp"]]))
                            st.append(lambda: k.tt('dve', t["LT"][:], t["LT"][:], t["tmp"][:], ALU.subtract, [b["LT"], b["tmp"]], [b["LT"]]))
                            st.append(lambda: k.tt('dve', t["L"][:], t["L"][:], bmk, ALU.mult, [b["L"], b_gc], [b["L"]]))
                            cP, cbP, cPT, cbPT = t["L"], b["L"], t["tmp"], b["tmp"]
                            NLEV = 4
                            for lev in range(NLEV):
                                nP, nbP = (t["Pa"], b["Pa"]) if lev % 2 == 0 else (t["Pb"], b["Pb"])
                                bA = lev % 2
                                last = lev == NLEV - 1
                                if not last:
                                    st.append((lambda cP=cP, cbP=cbP, cPT=cPT, cbPT=cbPT, bA=bA: k.mm(banks[bA][:, q4], cPT[:], cP[:], True, True, [cbPT, cbP], [bq[bA][i]])))
                                    st.append((lambda nP=nP, nbP=nbP, bA=bA: k.cp('act', nP[:], banks[bA][:, q4], [bq[bA][i]], [nbP])))
                                st.append((lambda cP=cP, cbP=cbP, cPT=cPT, cbPT=cbPT, bA=bA: k.mm(banks[2 + bA][:, q4], cP[:], cPT[:], True, True, [cbPT, cbP], [bq[2 + bA][i]])))
                                st.append((lambda lev=lev, bA=bA: k.cp('dve', PTk[i][lev][:], banks[2 + bA][:, q4], [bq[2 + bA][i]], [bPTk[i][lev]])))
                                cP, cbP, cPT, cbPT = nP, nbP, PTk[i][lev], bPTk[i][lev]
                            st.append(lambda: k.cp('dve', t["Z"][:], ident[:], [b_const], [b["Z"]]))
                            for lev in [3, 2, 1, 0, -1]:
                                bA = 4 + (lev % 2)
                                if lev >= 0:
                                    st.append((lambda lev=lev, bA=bA: k.mm(banks[bA][:, q4], PTk[i][lev][:], t["Z"][:], True, True, [bPTk[i][lev], b["Z"]], [bq[bA][i]])))
                                    st.append((lambda bA=bA: k.tt('dve', t["Z"][:], t["Z"][:], banks[bA][:, q4], ALU.add, [bq[bA][i], b["Z"]], [b["Z"]])))
                                else:
                                    st.append((lambda bA=bA: k.mm(banks[bA][:, q4], t["tmp"][:], t["Z"][:], True, True, [b["tmp"], b["Z"]], [bq[bA][i]])))
                                    st.append((lambda bA=bA: k.tt('dve', t["Z"][:], t["Z"][:], banks[bA][:, q4], ALU.subtract, [bq[bA][i], b["Z"]], [b["Z"]])))
                            st.append(lambda: k.tr(banks[0][:, q4], t["Z"][:], ident[:], [b["Z"], b_const], [bq[0][i]]))
                            st.append(lambda: k.cp('act', t["DT"][:], banks[0][:, q4], [bq[0][i]], [b["DT"]]))
                            hq = slice((i % 2) * 256, (i % 2) * 256 + 256)
                            bkR = 4 + (i // 2)
                            bkR2 = 2 + (i // 2)
                            st.append(lambda: k.mm(banks[bkR][:, hq], t["DT"][:], t["R"][:], True, True, [b["DT"], b["R"]], [bq[bkR][0]]))
                            st.append(lambda: k.cp('dve', t["X"][:], banks[bkR][:, hq], [bq[bkR][0]], [b["X"]]))
                            for sweep in range(3):
                                st.append(lambda: k.mm(banks[bkR2][:, hq], t["LT"][:], t["X"][:], True, True, [b["LT"], b["X"]], [bq[bkR2][0]]))
                                st.append(lambda: k.tt('dve', t["Tt"][:], t["R"][:], banks[bkR2][:, hq], ALU.subtract, [b["R"], bq[bkR2][0]], [b["Tt"]]))
                                st.append(lambda: k.mm(banks[bkR][:, hq], t["DT"][:], t["Tt"][:], True, True, [b["DT"], b["Tt"]], [bq[bkR][0]]))
                                st.append(lambda: k.cp('dve', t["X"][:], banks[bkR][:, hq], [bq[bkR][0]], [b["X"]]))
                            st.append(lambda: k.cp('act', t["R"][:], t["X"][:], [b["X"]], [b["R"]]))
                            st.append(lambda: k.tr(banks[0][:, q4], t["R"][:, 0:128], ident[:], [b["R"], b_const], [bq[0][i]]))
                            st.append(lambda: k.cp('act', t["WT"][:], banks[0][:, q4], [bq[0][i]], [b["WT"]]))
                            return st

                        allst = [pre_steps(i, n) for i, n in enumerate(chunks)]
                        for si in range(min(GDN_STEPS, len(allst[0]))):
                            for i in range(WV):
                                allst[i][si]()
                        for i, n in enumerate(chunks if GDN_SUB >= 3 else []):
                            t, b = T[i], Bf[i]
                            cs = slice(n * 128, (n + 1) * 128)
                            q4 = slice(i * 128, (i + 1) * 128)
                            k.mm(banks[1][:, q4], t["WT"][:], S[:], True, True, [b["WT"], bS], [bq[1][i]])
                            k.tt('dve', t["U"][:], t["R"][:, 128:256], banks[1][:, q4], ALU.subtract, [b["R"], bq[1][i]], [b["U"]])
                            if own:
                                k.mm(banks[2][:, q4], QT[:, cs], S[:], True, True, [bQ, bS], [bq[2][i]])
                                k.mm(banks[3][:, q4], t["QK"][:], t["U"][:], True, True, [b["QK"], b["U"]], [bq[3][i]])
                                k.act(t["tmp"][:], banks[2][:, q4], AF.Identity, [bq[2][i], b_v], [b["tmp"]], scale=col("gam", n, h))
                                k.tt('dve', t["O"][:], t["tmp"][:], banks[3][:, q4], ALU.add, [b["tmp"], bq[3][i]], [b["O"]])
                            k.mm(banks[0][:, q4], t["Kd"][:], t["U"][:], True, True, [b["Kd"], b["U"]], [bq[0][i]])
                            k.stt(S[:], S[:], col("gend", n, h), banks[0][:, q4], ALU.mult, ALU.add, [bS, b_v, bq[0][i]], [bS])
                            if own:
                                no = n - OWN0 // 128
                                sti = stt_[i]
                                k.act(t["tmp"][:], t["O"][:], AF.Square, [b["O"]], [b["tmp"], b["st"]], accum=sti[:, 0:1])
                                k.act(sti[:, 1:2], sti[:, 0:1], AF.Ln, [b["st"]], [b["st"]], scale=1.0 / 128, bias=1e-6)
                                k.act(sti[:, 2:3], sti[:, 1:2], AF.Exp, [b["st"]], [b["st"]], scale=-0.5)
                                k.stt(t["O"][:], t["O"][:], sti[:, 2:3], nwb[:], ALU.mult, ALU.mult, [b["O"], b["st"], b_gc], [b["O"]])
                                k.tt('dve', t["O"][:], t["O"][:], zt[:, no, :], ALU.mult, [b["O"], bz], [b["O"]])
                                k.tr(banks[4][:, q4], t["O"][:], ident[:], [b["O"], b_const], [bq[4][i]])
                                k.cp('act', t["o16"][:], banks[4][:, q4], [bq[4][i]], [b["o16"]])
                                k.dma('sp', OAT[h * 128:(h + 1) * 128, no * 128:(no + 1) * 128], t["o16"][:], [b["o16"]], ())
        if stage == 3 and DEBUG_OUT and not SKIP_GDN:
            dbg_oa = nc.dram_tensor("dbg_oa", [1024, NOWN], BF16, kind="ExternalOutput").ap()
            k.dma('sp', dbg_oa, OAT, (), ())

    if stage >= 4 and not SKIP_NSA:
        pos_in = din("pos", [1, LOC], I32)
        nsc_in = din("nsa_c128", [128, 385])
        maskB_in = din("maskB", [128, LOC])
        eexp_in = din("eexp", [64, LOC])
        ovl_in = din("ovl", [128, 2, 65])
        fv_in = din("fbigvis", [128, 2, 16, 64])
        ncore_in = din("nsa_core", [128, 129])
        cpos_in = [din("cmp_pos_k", [32, 128]), din("cmp_pos_v", [32, 128])]
        cw1_in = [din("cmp_w1_k", [LOC, 256]), din("cmp_w1_v", [LOC, 256])]
        cw2_in = [din("cmp_w2_k", [256, 128]), din("cmp_w2_v", [256, 128])]
        NQT = NOWN // 128
        with k.scope():
            nsc = k.sb([128, 385], F32, "nsc")
            ncore = k.sb([128, 129], F32, "ncore")
            maskB = k.sb([128, LOC], BF16, "maskB")
            eexp = k.sb([64, LOC], BF16, "eexp")
            ovl = k.sb([128, 2, 65], BF16, "ovl")
            fv = k.sb([128, 2, 16, 64], F32, "fv")
            cmk = k.sb([128, 2, 128], BF16, "cmk")
            b_nc = P.buf()
            k.dma('sp', nsc[:], nsc_in, (), [b_nc])
            k.dma('sp', ncore[:], ncore_in, (), [b_nc])
            k.dma('sp', fv[:], fv_in, (), [b_nc])
            k.dma('pool', maskB[:], maskB_in, (), [b_nc])
            k.dma('pool', eexp[:], eexp_in, (), [b_nc])
            k.dma('pool', ovl[:], ovl_in, (), [b_nc])
            k.cp('dve', cmk[:, 0, :], nsc[:, 129:257], [b_nc], [b_nc])
            k.cp('dve', cmk[:, 1, :], nsc[:, 257:385], [b_nc], [b_nc])
            ropeP = nsc[:, 0:128]
            invf = nsc[:, 128:129]
            kvb = ncore[:, 128:129]
            cosT = k.sb([128, LOC], F32, "cosT")
            sinT = k.sb([128, LOC], F32, "sinT")
            cosq = k.sb([128, NOWN], F32, "cosq")
            sinq = k.sb([128, NOWN], F32, "sinq")
            b_rt = P.buf()
            with k.scope():
                posi = k.sb([128, LOC], I32, "posi")
                ang = k.sb([128, LOC], F32, "ang")
                kk_ = k.sb([128, LOC], I32, "kk")
                kf = k.sb([128, LOC], F32, "kf")
                b_a = P.buf()
                k.dma('sp', posi[:], pos_in[0].partition_broadcast(128), (), [b_a])
                k.cp('dve', ang[:], posi[:], [b_a], [b_a])
                k.ts('dve', ang[:], ang[:], invf, None, ALU.mult, None, [b_a, b_nc], [b_a])
                TWO_PI = float(2 * np.pi)
                for (dst, shift) in [(sinT, 0.0), (cosT, float(np.pi / 2))]:
                    k.ts('dve', kf[:], ang[:], shift, 1.0 / TWO_PI, ALU.add, ALU.mult, [b_a], [b_a])
                    k.cp('dve', kk_[:], kf[:], [b_a], [b_a])
                    k.cp('dve', kf[:], kk_[:], [b_a], [b_a])
                    k.stt(kf[:], kf[:], -TWO_PI, ang[:], ALU.mult, ALU.add, [b_a], [b_a])
                    k.ts('dve', kf[:], kf[:], shift, None, ALU.add, None, [b_a], [b_a])
                    k.ts('dve', dst[:], kf[:], float(np.pi), -TWO_PI, ALU.is_gt, ALU.mult, [b_a], [b_rt])
                    k.tt('dve', kf[:], kf[:], dst[:], ALU.add, [b_a, b_rt], [b_a])
                    k.ts('dve', dst[:], kf[:], -float(np.pi), TWO_PI, ALU.is_lt, ALU.mult, [b_a], [b_rt])
                    k.tt('dve', kf[:], kf[:], dst[:], ALU.add, [b_a, b_rt], [b_a])
                    k.act(dst[:], kf[:], AF.Sin, [b_a], [b_rt])
                k.ts('dve', cosq[:], cosT[:, OWN0:LOC], 128.0 ** -0.5, None, ALU.mult, None, [b_rt], [b_rt])
                k.ts('dve', sinq[:], sinT[:, OWN0:LOC], 128.0 ** -0.5, None, ALU.mult, None, [b_rt], [b_rt])

            def rope(dst_bf, X, bX, ct, st_, ntok, bdst, tmpa, tmpb, btmp):
                for tt in range(ntok // 512):
                    sl = slice(tt * 512, (tt + 1) * 512)
                    bk = tt % 3
                    k.mm(banks[bk][:, :], ropeP, X[:, sl], True, True, [bX, b_nc], [bbank[bk]])
                    k.tt('pool', tmpa[:, 0:512], X[:, sl], ct[:, sl], ALU.mult, [bX, b_rt], [btmp[0]])
                    k.tt('dve', tmpb[:, 0:512], banks[bk][:, :], st_[:, sl], ALU.mult, [bbank[bk], b_rt], [btmp[1]])
                    k.tt('dve', dst_bf[:, sl], tmpa[:, 0:512], tmpb[:, 0:512], ALU.add, [btmp[0], btmp[1]], [bdst])

            gts = k.sb([128, NQT, 48], F32, "gts")
            b_g = P.buf()
            k.dma('sp', gts[:], PT[OWN0:LOC, 1552:1600].rearrange("(n p) c -> p n c", p=128), (), [b_g])
            k.act(gts[:], gts[:], AF.Sigmoid, [b_g], [b_g])

            for g in range(2 if NSA_GROUPS_RUN is None else NSA_GROUPS_RUN):
                with k.scope():
                    KsT = k.sb([128, LOC], BF16, "KsT")
                    KwT = k.sb([128, LOC], BF16, "KwT")
                    Vs1 = k.sb([128, 32, 129], BF16, "Vs1")
                    Vw1 = k.sb([128, 32, 129], BF16, "Vw1")
                    KcmpT = k.sb([128, 256], BF16, "KcmpT")
                    RHSc = k.sb([128, 2, 193], BF16, "RHSc")
                    QTh = [k.sb([128, NOWN], BF16, "QTh%d" % i) for i in range(8)]
                    bKs, bKw, bVs, bVw, bKc, bRc = P.bufs(6)
                    bQh = P.bufs(8)
                    k.memset('dve', Vs1[:, :, 128:129], 1.0, [bVs])
                    k.memset('dve', Vw1[:, :, 128:129], 1.0, [bVw])
                    k.dma('pool', Vs1[:, :, 0:128], PT[:, 1040 + g * 128:1040 + (g + 1) * 128].rearrange("(n p) c -> p n c", p=128), (), [bVs])
                    k.dma('pool', Vw1[:, :, 0:128], PT[:, 1296 + g * 128:1296 + (g + 1) * 128].rearrange("(n p) c -> p n c", p=128), (), [bVw])
                    k.memset('dve', KcmpT[:], 0.0, [bKc])
                    k.cp('dve', RHSc[:, :, 0:65], ovl[:], [b_nc], [bRc])
                    with k.scope():
                        X = k.sb([128, LOC], F32, "ropeX")
                        tmpa = k.sb([128, 512], F32, "ropeA")
                        tmpb = k.sb([128, 512], F32, "ropeB")
                        KcT = k.sb([128, LOC], BF16, "KcT")
                        VcT = k.sb([128, LOC], BF16, "VcT")
                        bX, bKcT, bVcT = P.bufs(3)
                        btmp = P.bufs(2)
                        for (dstb, bd, row0) in [(KcT, bKcT, 5120 + g * 128), (KsT, bKs, 5632 + g * 128), (KwT, bKw, 5888 + g * 128)]:
                            k.dma('sp', X[:], PF[row0:row0 + 128, :], (), [bX])
                            rope(dstb, X, bX, cosT, sinT, LOC, bd, tmpa, tmpb, btmp)
                        k.dma('pool', VcT[:], PF[5376 + g * 128:5376 + (g + 1) * 128, :], (), [bVcT])
                        for hh in range(8):
                            row0 = 3072 + (g * 8 + hh) * 128
                            k.dma('sp', X[:, 0:NOWN], PF[row0:row0 + 128, OWN0:LOC], (), [bX])
                            rope(QTh[hh], X, bX, cosq, sinq, NOWN, bQh[hh], tmpa, tmpb, btmp)
                        w1 = k.sb([128, 32, 256], BF16, "cw1")
                        w2 = k.sb([128, 2, 128], BF16, "cw2")
                        cpT = k.sb([128, 32], F32, "cpT")
                        cpTb = k.sb([128, 32], BF16, "cpTb")
                        hidT = k.sb([128, 2, 256], BF16, "hidT")
                        hx = k.sb([128, 256], F32, "hx")
                        hy = k.sb([128, 256], F32, "hy")
                        hb = k.sb([128, 2], F32, "hb")
                        bw1, bw2, bcp, bhid, bhx, bhy, bhb = P.bufs(7)
                        for which, (srcT, bsrc) in enumerate([(KcT, bKcT), (VcT, bVcT)]):
                            k.dma('pool', w1[:], cw1_in[which].rearrange("(l d) h -> d l h", d=128), (), [bw1])
                            k.dma('pool', w2[:], cw2_in[which].rearrange("(t p) d -> p t d", p=128), (), [bw2])
                            load_fm(cpT[:], cpos_in[which], 32, bcp, bank=3)
                            k.cp('dve', cpTb[:], cpT[:], [bcp], [bcp])
                            k.memset('dve', hidT[:], 0.0, [bhid])
                            for ht in range(2):
                                for l in range(32):
                                    k.mm(banks[0][:, 0:255], w1[:, l, ht * 128:(ht + 1) * 128], srcT[:, l:l + 16 * 254 + 1:16],
                                         l == 0, l == 31, [bw1, bsrc], [bbank[0]])
                                for l in range(32):
                                    k.mm(banks[1][:, 0:1], w1[:, l, ht * 128:(ht + 1) * 128], cpTb[:, l:l + 1],
                                         l == 0, l == 31, [bw1, bcp], [bbank[1]])
                                k.cp('dve', hb[:, ht:ht + 1], banks[1][:, 0:1], [bbank[1]], [bhb])
                                k.act(hx[:, 0:255], banks[0][:, 0:255], AF.Identity, [bbank[0], bhb], [bhx], bias=hb[:, ht:ht + 1])
                                k.tt('dve', hy[:, 0:255], hx[:, 0:255], hx[:, 0:255], ALU.mult, [bhx], [bhy])
                                k.ts('dve', hy[:, 0:255], hy[:, 0:255], 0.044715, 1.0, ALU.mult, ALU.add, [bhy], [bhy])
                                k.tt('dve', hy[:, 0:255], hy[:, 0:255], hx[:, 0:255], ALU.mult, [bhy, bhx], [bhy])
                                k.act(hy[:, 0:255], hy[:, 0:255], AF.Sigmoid, [bhy], [bhy], scale=1.5957691216057308)
                                k.tt('dve', hidT[:, ht, 0:255], hy[:, 0:255], hx[:, 0:255], ALU.mult, [bhy, bhx], [bhid])
                            if which == 0:
                                for ht in range(2):
                                    k.mm(banks[2][:, 0:255], w2[:, ht, :], hidT[:, ht, 0:255], ht == 0, ht == 1, [bw2, bhid], [bbank[2]])
                                k.cp('dve', KcmpT[:, 0:255], banks[2][:, 0:255], [bbank[2]], [bKc])
                            else:
                                for j in range(2):
                                    nn = 128 if j == 0 else 127
                                    for ht in range(2):
                                        k.mm(banks[2][0:nn, 0:128], hidT[:, ht, j * 128:j * 128 + nn], w2[:, ht, :], ht == 0, ht == 1, [bw2, bhid], [bbank[2]])
                                    k.memset('dve', RHSc[:, j, 65:193], 0.0, [bRc])
                                    k.cp('dve', RHSc[0:nn, j, 65:193], banks[2][0:nn, 0:128], [bbank[2]], [bRc])
                    impacc = k.sb([128, 64], F32, "impacc")
                    v1 = k.sb([128, 64], F32, "selv1")
                    v2 = k.sb([128, 64], F32, "selv2")
                    m8 = k.sb([128, 16], F32, "m8")
                    selb = k.sb([128, 64], F32, "selb")
                    selbT = k.sb([64, 128], BF16, "selbT")
                    Oacc = k.sb([128, 8, 128], F32, "Oacc")
                    o16 = [k.sb([128, 128], BF16, "no16_%d" % i) for i in range(2)]
                    Et = [k.sb([128, 128], BF16, "Et%d" % i) for i in range(4)]
                    sc_ = k.sb([128, 8, 8], F32, "nsc_small")
                    bimp, bsel, bselT = P.bufs(3)
                    bOh = P.bufs(8)
                    bsch = P.bufs(8)
                    bo16 = P.bufs(2)
                    bEt = P.bufs(4)
                    ectr = [0]

                    def score_exp(lhsK, bK_, rhsQ, bQ_, extra, bias_ap):
                        e = ectr[0] % 4
                        bk = ectr[0] % 2
                        ectr[0] += 1
                        k.mm(banks[bk][:, 0:128], lhsK, rhsQ, True, len(extra) == 0, [bK_, bQ_], [bbank[bk]])
                        for xi, (l_, r_, bl_) in enumerate(extra):
                            k.mm(banks[bk][:, 0:128], l_, r_, False, xi == len(extra) - 1, bl_, [bbank[bk]])
                        if bias_ap is None:
                            k.act(Et[e][:], banks[bk][:, 0:128], AF.Exp, [bbank[bk]], [bEt[e]])
                        else:
                            k.act(Et[e][:], banks[bk][:, 0:128], AF.Exp, [bbank[bk], b_nc], [bEt[e]], bias=bias_ap)
                        return Et[e], bEt[e]

                    def tbank():
                        bk = ectr[0] % 2
                        ectr[0] += 1
                        return bk

                    for qt in range(NQT if NSA_QT_RUN is None else NSA_QT_RUN):
                        qsl = slice(qt * 128, (qt + 1) * 128)
                        for hh in range(8):
                            head = g * 8 + hh
                            sch, bsc = sc_[:, hh, :], bsch[hh]
                            cbk = 3 + (hh % 3)
                            ets = []
                            for j in range(2):
                                u0 = (OWN0 + 128 * qt) if j == 0 else 128 * qt
                                ets.append(score_exp(KcmpT[:, j * 128:(j + 1) * 128], bKc, QTh[hh][:, qsl], bQh[hh],
                                                     [(identb[:], maskB[:, u0:u0 + 128], [b_const, b_nc])], kvb if j == 0 else None))
                            for j in range(2):
                                k.mm(banks[cbk][:, 0:193], ets[j][0][:], RHSc[:, j, :], j == 0, j == 1, [ets[j][1], bRc], [bbank[cbk]])
                            k.ts('dve', sch[:, 0:1], banks[cbk][:, 64:65], 1e-30, None, ALU.max, None, [bbank[cbk]], [bsc])
                            k.recip(sch[:, 1:2], sch[:, 0:1], [bsc], [bsc])
                            if hh == 0:
                                k.ts('dve', impacc[:], banks[cbk][:, 0:64], sch[:, 1:2], None, ALU.mult, None, [bbank[cbk], bsc], [bimp])
                            else:
                                k.stt(impacc[:], banks[cbk][:, 0:64], sch[:, 1:2], impacc[:], ALU.mult, ALU.add, [bbank[cbk], bsc, bimp], [bimp])
                            k.ts('dve', sch[:, 2:3], gts[:, qt, head * 3:head * 3 + 1], sch[:, 1:2], None, ALU.mult, None, [b_g, bsc], [bsc])
                            k.ts('dve', Oacc[:, hh, :], banks[cbk][:, 65:193], sch[:, 2:3], None, ALU.mult, None, [bbank[cbk], bsc], [bOh[hh]])
                        k.tt('dve', v1[:], impacc[:], fv[:, 0, qt, :], ALU.max, [bimp, b_nc], [bsel])
                        k.tt('dve', v1[:], v1[:], ncore[:, 0:64], ALU.max, [bsel, b_nc], [bsel])
                        k.tt('dve', v1[:], v1[:], fv[:, 1, qt, :], ALU.min, [bsel, b_nc], [bsel])
                        k.tt('dve', v1[:], v1[:], ncore[:, 64:128], ALU.min, [bsel, b_nc], [bsel])
                        P.op('dve', lambda e: e.max(out=m8[:, 0:8], in_=v1[:]), [bsel], [bsel])
                        P.op('dve', lambda e: e.match_replace(out=v2[:], in_to_replace=m8[:, 0:8], in_values=v1[:], imm_value=-3.0e38), [bsel], [bsel])
                        P.op('dve', lambda e: e.max(out=m8[:, 8:16], in_=v2[:]), [bsel], [bsel])
                        k.ts('dve', v2[:], v1[:], m8[:, 15:16], None, ALU.is_ge, None, [bsel], [bsel])
                        k.ts('dve', selb[:], v1[:], -1.0e29, None, ALU.is_gt, None, [bsel], [bsel])
                        k.tt('dve', selb[:], selb[:], v2[:], ALU.mult, [bsel], [bsel])
                        k.ts('dve', selb[:], selb[:], -1.0, 30000.0, ALU.add, ALU.mult, [bsel], [bsel])
                        tb = tbank()
                        k.tr(banks[tb][0:64, 0:128], selb[:], ident[:], [bsel, b_const], [bbank[tb]])
                        k.cp('dve', selbT[:], banks[tb][0:64, 0:128], [bbank[tb]], [bselT])
                        for hh in range(8):
                            head = g * 8 + hh
                            sch, bsc, bO = sc_[:, hh, :], bsch[hh], bOh[hh]
                            sbk = 4 if hh % 2 == 0 else 2
                            wbk = 5 if hh % 2 == 0 else 3
                            kdiag = OWN0 // 128 + qt
                            tiles = []
                            for kt in range(0, kdiag + 1):
                                ex = [(eexp[:, kt * 128:(kt + 1) * 128], selbT[:], [b_nc, bselT])]
                                if kt == kdiag:
                                    ex.append((identb[:], cmk[:, 0, :], [b_const, b_nc]))
                                tiles.append((KsT[:, kt * 128:(kt + 1) * 128], bKs, ex, None, sbk, Vs1[:, kt, :], bVs, kt == 0, kt == kdiag))
                            for kt in range(kdiag - 4, kdiag + 1):
                                ex = []
                                if kt == kdiag - 4:
                                    ex.append((identb[:], cmk[:, 1, :], [b_const, b_nc]))
                                if kt == kdiag:
                                    ex.append((identb[:], cmk[:, 0, :], [b_const, b_nc]))
                                tiles.append((KwT[:, kt * 128:(kt + 1) * 128], bKw, ex, kvb if kt < OWN0 // 128 else None, wbk, Vw1[:, kt, :], bVw,
                                              kt == kdiag - 4, kt == kdiag))
                            pend = None
                            for (lk, blk_, ex, bias_, abk, vap, bv_, st_f, sp_f) in tiles:
                                et, bet = score_exp(lk, blk_, QTh[hh][:, qsl], bQh[hh], ex, bias_)
                                if pend is not None:
                                    pe_, pb_, pa_, pv_, pbv_, ps_, pp_ = pend
                                    k.mm(banks[pa_][:, 0:129], pe_[:], pv_, ps_, pp_, [pb_, pbv_], [bbank[pa_]])
                                pend = (et, bet, abk, vap, bv_, st_f, sp_f)
                            pe_, pb_, pa_, pv_, pbv_, ps_, pp_ = pend
                            k.mm(banks[pa_][:, 0:129], pe_[:], pv_, ps_, pp_, [pb_, pbv_], [bbank[pa_]])
                            k.ts('dve', sch[:, 3:4], banks[sbk][:, 128:129], 1e-30, None, ALU.max, None, [bbank[sbk]], [bsc])
                            k.recip(sch[:, 4:5], sch[:, 3:4], [bsc], [bsc])
                            k.ts('dve', sch[:, 4:5], sch[:, 4:5], gts[:, qt, head * 3 + 1:head * 3 + 2], None, ALU.mult, None, [b_g, bsc], [bsc])
                            k.stt(Oacc[:, hh, :], banks[sbk][:, 0:128], sch[:, 4:5], Oacc[:, hh, :], ALU.mult, ALU.add, [bbank[sbk], bsc, bO], [bO])
                            k.ts('dve', sch[:, 5:6], banks[wbk][:, 128:129], 1e-30, None, ALU.max, None, [bbank[wbk]], [bsc])
                            k.recip(sch[:, 6:7], sch[:, 5:6], [bsc], [bsc])
                            k.ts('dve', sch[:, 6:7], sch[:, 6:7], gts[:, qt, head * 3 + 2:head * 3 + 3], None, ALU.mult, None, [b_g, bsc], [bsc])
                            k.stt(Oacc[:, hh, :], banks[wbk][:, 0:128], sch[:, 6:7], Oacc[:, hh, :], ALU.mult, ALU.add, [bbank[wbk], bsc, bO], [bO])
                            oi = hh % 2
                            tb = tbank()
                            k.tr(banks[tb][:, 0:128], Oacc[:, hh, :], ident[:], [bO, b_const], [bbank[tb]])
                            k.cp('dve', o16[oi][:], banks[tb][:, 0:128], [bbank[tb]], [bo16[oi]])
                            k.dma('sp', OBT[head * 128:(head + 1) * 128, qsl], o16[oi][:], [bo16[oi]], ())
        if stage == 4 and DEBUG_OUT and not SKIP_NSA:
            dbg_ob = nc.dram_tensor("dbg_ob", [2048, NOWN], BF16, kind="ExternalOutput").ap()
            k.dma('sp', dbg_ob, OBT, (), ())

    def bcast_row(dst, src_fm, n, bsrc, bdst, name):
        rowd = k.dram([1, n * 128], F32, "row_" + name)
        t_ = P.dma('sp', rowd[0].rearrange("(c p) -> p c", p=128), src_fm, [bsrc], (), allow_slow_non_contiguous=True)
        brow = P.buf()
        brow.lw = t_
        k.dma('sp', dst, rowd[0].partition_broadcast(128), [brow], [bdst])

    if stage >= 5 and not SKIP_P5:
        wbg_in = din("w_branch_gdn", [1024, D])
        wbn_in = din("w_branch_nsa", [D, D])
        wout_in = din("w_out", [D, D])
        with k.scope():
            Wg = k.sb([128, 8, D], BF16, "Wg")
            Wn = k.sb([128, 16, D], BF16, "Wn")
            bWg, bWn = P.bufs(2)
            for kc in range(8):
                k.dma('pool', Wg[:, kc, :], wbg_in[kc * 128:(kc + 1) * 128, :], (), [bWg])
            for kc in range(16):
                k.dma('pool', Wn[:, kc, :], wbn_in[kc * 128:(kc + 1) * 128, :], (), [bWn])
            oat = [k.sb([128, 8, 512], BF16, "oat%d" % i) for i in range(2)]
            obt = [k.sb([128, 16, 512], BF16, "obt%d" % i) for i in range(2)]
            boat, bobt = P.bufs(2), P.bufs(2)
            ga = [k.sb([128, 512], F32, "ga%d" % i) for i in range(2)]
            gb = [k.sb([128, 512], F32, "gb%d" % i) for i in range(2)]
            y16 = [k.sb([128, 512], BF16, "y16_%d" % i) for i in range(2)]
            bga, bgb, by16 = P.bufs(2), P.bufs(2), P.bufs(2)
            OATv = OAT.rearrange("(kc p) t -> p kc t", p=128)
            OBTv = OBT.rearrange("(kc p) t -> p kc t", p=128)
            it = 0
            for tt in range(NOWN // 512):
                s2 = tt % 2
                tsl = slice(tt * 512, (tt + 1) * 512)
                k.dma('sp', oat[s2][:], OATv[:, :, tsl], (), [boat[s2]])
                k.dma('sp', obt[s2][:], OBTv[:, :, tsl], (), [bobt[s2]])
                for ct in range(16):
                    s = it % 2
                    it += 1
                    bA, bB = (0, 1) if s == 0 else (2, 3)
                    csl = slice(ct * 128, (ct + 1) * 128)
                    k.dma('sp', ga[s][:], PF[6144 + ct * 128:6144 + (ct + 1) * 128, OWN0 + tt * 512:OWN0 + (tt + 1) * 512], (), [bga[s]])
                    k.dma('sp', gb[s][:], PF[8192 + ct * 128:8192 + (ct + 1) * 128, OWN0 + tt * 512:OWN0 + (tt + 1) * 512], (), [bgb[s]])
                    k.act(ga[s][:], ga[s][:], AF.Sigmoid, [bga[s]], [bga[s]])
                    k.act(gb[s][:], gb[s][:], AF.Sigmoid, [bgb[s]], [bgb[s]])
                    for kc in range(8):
                        k.mm(banks[bA][:, :], Wg[:, kc, csl], oat[s2][:, kc, :], kc == 0, kc == 7, [bWg, boat[s2]], [bbank[bA]])
                    for kc in range(16):
                        k.mm(banks[bB][:, :], Wn[:, kc, csl], obt[s2][:, kc, :], kc == 0, kc == 15, [bWn, bobt[s2]], [bbank[bB]])
                    k.tt('dve', ga[s][:], ga[s][:], banks[bA][:, :], ALU.mult, [bga[s], bbank[bA]], [bga[s]])
                    k.tt('dve', gb[s][:], gb[s][:], banks[bB][:, :], ALU.mult, [bgb[s], bbank[bB]], [bgb[s]])
                    k.tt('pool', y16[s][:], ga[s][:], gb[s][:], ALU.add, [bga[s], bgb[s]], [by16[s]])
                    k.dma('sp', YT[csl, tsl], y16[s][:], [by16[s]], ())
        with k.scope():
            Wo = k.sb([128, 16, D], BF16, "Wo")
            g1b = k.sb([128, D], F32, "g1b")
            bWo, bg1 = P.bufs(2)
            for kc in range(16):
                k.dma('pool', Wo[:, kc, :], wout_in[kc * 128:(kc + 1) * 128, :], (), [bWo])
            bcast_row(g1b[:], modT[:, 32:48], 16, b_mod, bg1, "g1")
            yt = [k.sb([128, 16, 128], BF16, "yt%d" % i) for i in range(2)]
            xo = [k.sb([128, D], F32, "xo%d" % i) for i in range(2)]
            zt_ = [k.sb([128, 512], F32, "zt5_%d" % i) for i in range(2)]
            byt, bxo, bzt = P.bufs(2), P.bufs(2), P.bufs(2)
            YTv = YT.rearrange("(kc p) t -> p kc t", p=128)
            it = 0
            for t in range(NOWN // 128):
                s = t % 2
                k.dma('sp', yt[s][:], YTv[:, :, t * 128:(t + 1) * 128], (), [byt[s]])
                k.dma('sp', xo[s][:], xl[OWN0 + t * 128:OWN0 + (t + 1) * 128, :], (), [bxo[s]])
                for cb_ in range(4):
                    z2 = it % 2
                    bk = it % 4
                    it += 1
                    csl = slice(cb_ * 512, (cb_ + 1) * 512)
                    for kc in range(16):
                        k.mm(banks[bk][:, :], yt[s][:, kc, :], Wo[:, kc, csl], kc == 0, kc == 15, [byt[s], bWo], [bbank[bk]])
                    k.tt('dve', zt_[z2][:], banks[bk][:, :], g1b[:, csl], ALU.mult, [bbank[bk], bg1], [bzt[z2]])
                    k.tt('pool', xo[s][:, csl], xo[s][:, csl], zt_[z2][:], ALU.add, [bxo[s], bzt[z2]], [bxo[s]])
                k.dma('sp', X1[t * 128:(t + 1) * 128, :], xo[s][:], [bxo[s]], ())
        P.barrier()
        if stage == 5 and DEBUG_OUT:
            dbg_x1 = nc.dram_tensor("dbg_x1", [NOWN, D], F32, kind="ExternalOutput").ap()
            k.dma('sp', dbg_x1, X1, (), ())

    if stage == 6 and DEBUG_OUT:
        X2dbg = nc.dram_tensor("dbg_x2", [NOWN, D], F32, kind="ExternalOutput").ap()
    if stage >= 6:
        n2_in = din("norm2_w", [KC, 128])
        wq_in = din("peer_wq", [D, D])
        pk_in = [din("peer_keys1", [8, 128, 128]), din("peer_keys2", [8, 128, 128])]
        fnw6_in = din("final_norm_w", [1, D])
        H2T = k.dram([KC, 128, NOWN], BF16, "H2T")
        NEB = 64 if PEER_EB is None else PEER_EB
        with k.scope():
            A2 = k.sb([128, KC], F32, "A2")
            n2T = k.sb([128, KC], F32, "n2T")
            keysT = k.sb([128, 16, 128], BF16, "keysT")
            bA2, bn2, bg2, bfn, bkT = P.bufs(5)
            load_fm(n2T[:], n2_in, KC, bn2)
            k.stt(A2[:], modT[:, 64:80], 1.0, n2T[:], ALU.add, ALU.mult, [b_mod, bn2], [bA2])
            g2row = k.dram([1, D], F32, "row_g2")
            k.dma('sp', g2row[0].rearrange("(c p) -> p c", p=128), modT[:, 80:96], [b_mod], (), allow_slow_non_contiguous=True)
            with k.scope():
                kt_ = [k.sb([128, 128], F32, "kraw%d" % i) for i in range(2)]
                bkr = P.bufs(2)
                for hh2 in range(16):
                    s = hh2 % 2
                    k.dma('sp', kt_[s][:], pk_in[hh2 % 2][hh2 // 2], (), [bkr[s]])
                    k.tr(banks[s][:, 0:128], kt_[s][:], ident[:], [bkr[s], b_const], [bbank[s]])
                    k.cp('dve', keysT[:, hh2, :], banks[s][:, 0:128], [bbank[s]], [bkT])
            with k.scope():
                xt = [k.sb([128, D], F32, "x6t%d" % i) for i in range(2)]
                xs = [k.sb([128, D], BF16, "x6s%d" % i) for i in range(2)]
                st = [k.sb([128, 4], F32, "s6t%d" % i) for i in range(2)]
                ho = [k.sb([128, KC, 128], BF16, "h6o%d" % i) for i in range(2)]
                bxt, bxs, bst, bho = P.bufs(2), P.bufs(2), P.bufs(2), P.bufs(2)
                for t in range(NOWN // 128):
                    s = t % 2
                    k.dma('sp', xt[s][:], X1[t * 128:(t + 1) * 128, :], (), [bxt[s]])
                    k.act(xs[s][:], xt[s][:], AF.Square, [bxt[s]], [bxs[s], bst[s]], accum=st[s][:, 0:1])
                    k.ts('dve', st[s][:, 1:2], st[s][:, 0:1], 1.0 / D, 1e-6, ALU.mult, ALU.add, [bst[s]], [bst[s]])
                    k.act(st[s][:, 2:3], st[s][:, 1:2], AF.Sqrt, [bst[s]], [bst[s]])
                    k.recip(st[s][:, 3:4], st[s][:, 2:3], [bst[s]], [bst[s]])
                    k.ts('dve', xs[s][:], xt[s][:], st[s][:, 3:4], None, ALU.mult, None, [bxt[s], bst[s]], [bxs[s]])
                    for kc in range(KC):
                        pi = kc % 2
                        sl = slice((kc // 2 % 4) * 128, (kc // 2 % 4) * 128 + 128)
                        k.tr(pbf[pi][:, sl], xs[s][:, kc * 128:(kc + 1) * 128], identb[:], [bxs[s], b_const], [bpbf[pi]])
                        if kc % 2 == 0:
                            k.act(ho[s][:, kc, :], pbf[pi][:, sl], AF.Identity, [bpbf[pi], bA2, b_mod], [bho[s]],
                                  bias=modT[:, 48 + kc:49 + kc], scale=A2[:, kc:kc + 1])
                        else:
                            k.ts('dve', ho[s][:, kc, :], pbf[pi][:, sl], A2[:, kc:kc + 1], modT[:, 48 + kc:49 + kc],
                                 ALU.mult, ALU.add, [bpbf[pi], bA2, b_mod], [bho[s]])
                    k.dma('sp', H2T[:, :, t * 128:(t + 1) * 128].rearrange("c p t -> p c t"), ho[s][:], [bho[s]], ())
            P.barrier()
            wqv = wq_in.rearrange("(kc p) c -> p kc c", p=128)
            outv6 = out_d.rearrange("(t p) d -> t p d", p=128)
            for tg in range(4 if PEER_TG is None else PEER_TG):
                with k.scope():
                    h2g = k.sb([128, KC, 512], BF16, "h2g")
                    stile = k.sb([128, 4, 16, 128], F32, "stile")
                    Bt = k.sb([128, 4, 8, 128], F32, "Bt")
                    tau = k.sb([128, 4, 8], F32, "tau")
                    OUTacc = k.sb([128, 4, D], F32, "OUTacc")
                    bh2, bst_, bBt, btau, bOut = P.bufs(5)
                    k.dma('sp', h2g[:], H2T[:, :, tg * 512:(tg + 1) * 512].rearrange("c p t -> p c t"), (), [bh2])
                    k.memset('pool', OUTacc[:], 0.0, [bOut])
                    with k.scope():
                        wqb = [k.sb([128, KC, 128], BF16, "wqb%d" % i) for i in range(2)]
                        qh = [k.sb([128, 512], BF16, "qh%d" % i) for i in range(2)]
                        bwq, bqh = P.bufs(2), P.bufs(2)
                        for hh2 in range(16):
                            s = hh2 % 2
                            k.dma('pool', wqb[s][:], wqv[:, :, hh2 * 128:(hh2 + 1) * 128], (), [bwq[s]])
                            for kc in range(KC):
                                k.mm(banks[s][:, :], wqb[s][:, kc, :], h2g[:, kc, :], kc == 0, kc == KC - 1, [bwq[s], bh2], [bbank[s]])
                            k.cp('act', qh[s][:], banks[s][:, :], [bbank[s]], [bqh[s]])
                            for tl in range(4):
                                k.mm(banks[2 + s][:, tl * 128:(tl + 1) * 128], qh[s][:, tl * 128:(tl + 1) * 128], keysT[:, hh2, :], True, True,
                                     [bqh[s], bkT], [bbank[2 + s]])
                            k.cp('dve', stile[:, :, hh2, :], banks[2 + s][:, :].rearrange("p (t j) -> p t j", t=4), [bbank[2 + s]], [bst_])
                    with k.scope():
                        v16 = k.sb([128, 2, 16], F32, "v16")
                        vv = k.sb([128, 128], F32, "vv")
                        cand = k.sb([128, 16, 16], F32, "cand")
                        cv = k.sb([128, 256], F32, "cv")
                        c16 = k.sb([128, 16], F32, "c16")
                        sm = k.sb([128, 8], F32, "sm")
                        bsel6 = P.buf()
                        for tl in range(4):
                            for h in range(8):
                                for half in range(2):
                                    src = stile[:, tl, 2 * h + half, :]
                                    P.op('dve', lambda e, src=src, half=half: e.max(out=v16[:, half, 0:8], in_=src), [bst_], [bsel6])
                                    P.op('dve', lambda e, src=src, half=half: e.match_replace(out=vv[:], in_to_replace=v16[:, half, 0:8], in_values=src, imm_value=-3.0e38), [bst_, bsel6], [bsel6])
                                    P.op('dve', lambda e, half=half: e.max(out=v16[:, half, 8:16], in_=vv[:]), [bsel6], [bsel6])
                                for a in range(16):
                                    k.ts('dve', cand[:, a, :], v16[:, 1, :], v16[:, 0, a:a + 1], None, ALU.add, None, [bsel6], [bsel6])
                                cf = cand[:].rearrange("p a b -> p (a b)")
                                P.op('dve', lambda e, cf=cf: e.max(out=c16[:, 0:8], in_=cf), [bsel6], [bsel6])
                                P.op('dve', lambda e, cf=cf: e.match_replace(out=cv[:], in_to_replace=c16[:, 0:8], in_values=cf, imm_value=-3.0e38), [bsel6], [bsel6])
                                P.op('dve', lambda e: e.max(out=c16[:, 8:16], in_=cv[:]), [bsel6], [bsel6])
                                k.ts('dve', sm[:, 0:1], c16[:, 0:1], -1.0, None, ALU.mult, None, [bsel6], [bsel6])
                                k.act(cv[:, 0:16], c16[:], AF.Exp, [bsel6], [bsel6], bias=sm[:, 0:1], accum=sm[:, 1:2])
                                k.act(sm[:, 2:3], sm[:, 1:2], AF.Ln, [bsel6], [bsel6])
                                k.tt('dve', sm[:, 3:4], sm[:, 0:1], sm[:, 2:3], ALU.subtract, [bsel6], [bsel6])
                                k.ts('dve', Bt[:, tl, h, :], stile[:, tl, 2 * h, :], sm[:, 3:4], None, ALU.add, None, [bst_, bsel6], [bBt])
                                k.act(sm[:, 4:5], c16[:, 15:16], AF.Exp, [bsel6], [bsel6], bias=sm[:, 3:4])
                                k.ts('dve', tau[:, tl, h:h + 1], sm[:, 4:5], 0.99999, None, ALU.mult, None, [bsel6], [btau])
                    with k.scope():
                        ub = [k.sb([128, KC, 256], BF16, "ub%d" % i) for i in range(2)]
                        vb = [k.sb([128, 2, D], BF16, "vb%d" % i) for i in range(2)]
                        bub, bvb = P.bufs(2), P.bufs(2)
                        gx_ = [k.sb([128, 256], F32, "gx%d" % i) for i in range(2)]
                        gy_ = [k.sb([128, 256], F32, "gy%d" % i) for i in range(2)]
                        es_ = [k.sb([128, 8, 128], F32, "es%d" % i) for i in range(2)]
                        mk_ = [k.sb([128, 8, 128], F32, "mk%d" % i) for i in range(2)]
                        coef_ = [k.sb([128, 2, 128], F32, "coef%d" % i) for i in range(2)]
                        G16_ = [k.sb([128, 256], BF16, "G16_%d" % i) for i in range(2)]
                        GT_ = [[k.sb([128, 128], BF16, "GT%d_%d" % (p_, i)) for i in range(2)] for p_ in range(2)]
                        bgx_, bgy_, bmk_, bG16_ = P.bufs(2), P.bufs(2), P.bufs(2), P.bufs(2)
                        bes_ = [P.bufs(8) for p_ in range(2)]
                        bcoef_ = [P.bufs(2) for p_ in range(2)]
                        bOutS = [[P.buf() for c_ in range(4)] for t_ in range(4)]
                        bGT_ = [P.bufs(2) for p_ in range(2)]
                        pit = 0
                        def head(eb, tl, par, s):
                            gx, gy, coef, G16 = gx_[par], gy_[par], coef_[par], G16_[par]
                            bgx, bgy, bcoef, bG16 = bgx_[par], bgy_[par], bcoef_[par], bG16_[par]
                            for kc in range(KC):
                                k.mm(banks[par][:, 0:256], h2g[:, kc, tl * 128:(tl + 1) * 128], ub[s][:, kc, :], kc == 0, kc == KC - 1,
                                     [bh2, bub[s]], [bbank[par]])
                            k.cp('act', gx[:], banks[par][:, 0:256], [bbank[par]], [bgx])
                            k.tt('pool', gy[:], gx[:], gx[:], ALU.mult, [bgx], [bgy])
                            k.ts('pool', gy[:], gy[:], 0.044715, 1.0, ALU.mult, ALU.add, [bgy], [bgy])
                            k.tt('pool', gy[:], gy[:], gx[:], ALU.mult, [bgy, bgx], [bgy])
                            k.act(gy[:], gy[:], AF.Sigmoid, [bgy], [bgy], scale=1.5957691216057308)
                            k.tt('pool', gy[:], gy[:], gx[:], ALU.mult, [bgy, bgx], [bgy])
                            for il in range(2):
                                i_ = 2 * eb + il
                                es, mk, bes, bmk = es_[il], mk_[il], bes_[il], bmk_[il]
                                for h in range(8):
                                    k.act(es[:, h, :], stile[:, tl, 2 * h + 1, :], AF.Exp, [bst_, bBt], [bes[h]], bias=Bt[:, tl, h, i_:i_ + 1])
                                k.tt('dve', mk[:], es[:], tau[:, tl, :].unsqueeze(2).to_broadcast([128, 8, 128]), ALU.is_ge, bes + [btau], [bmk])
                                k.tt('dve', mk[:], mk[:], es[:], ALU.mult, [bmk] + bes, [bmk])
                                k.tt('dve', mk[:, 0:4, :], mk[:, 0:4, :], mk[:, 4:8, :], ALU.add, [bmk], [bmk])
                                k.tt('dve', mk[:, 0:2, :], mk[:, 0:2, :], mk[:, 2:4, :], ALU.add, [bmk], [bmk])
                                k.tt('dve', coef[:, il, :], mk[:, 0, :], mk[:, 1, :], ALU.add, [bmk], [bcoef[il]])
                            k.tt('dve', G16[:], gy[:], coef[:].rearrange("p i j -> p (i j)"), ALU.mult, [bgy] + bcoef, [bG16])

                        def tail(eb, tl, par, s):
                            G16, GT, bG16, bGT = G16_[par], GT_[par], bG16_[par], bGT_[par]
                            for il in range(2):
                                k.tr(pbf[il][:, par * 128:(par + 1) * 128], G16[:, il * 128:(il + 1) * 128], identb[:], [bG16, b_const], [bpbf[il]])
                                k.cp('act', GT[il][:], pbf[il][:, par * 128:(par + 1) * 128], [bpbf[il]], [bGT[il]])
                            for cb_ in range(4):
                                bk = 2 + cb_
                                csl = slice(cb_ * 512, (cb_ + 1) * 512)
                                k.mm(banks[bk][:, :], GT[0][:], vb[s][:, 0, csl], True, False, [bGT[0], bvb[s]], [bbank[bk]])
                                k.mm(banks[bk][:, :], GT[1][:], vb[s][:, 1, csl], False, True, [bGT[1], bvb[s]], [bbank[bk]])
                                k.tt('dve', OUTacc[:, tl, csl], OUTacc[:, tl, csl], banks[bk][:, :], ALU.add, [bOut, bOutS[tl][cb_], bbank[bk]], [bOutS[tl][cb_]])

                        prev = None
                        pit = 0
                        for eb in range(NEB):
                            s = eb % 2
                            k.dma('sp', ub[s][:], UBd[eb], [bconvU[eb]], [bub[s]])
                            k.dma('sp', vb[s][:], VBd[eb], [bconvV[eb]], [bvb[s]])
                            for tl in range(4):
                                par = pit % 2
                                pit += 1
                                head(eb, tl, par, s)
                                if prev is not None:
                                    tail(*prev)
                                prev = (eb, tl, par, s)
                        tail(*prev)
                    with k.scope():
                        g2b = k.sb([128, D], F32, "g2b")
                        fnw6 = k.sb([128, D], F32, "fnw6")
                        k.dma('sp', g2b[:], g2row[0].partition_broadcast(128), (), [bg2])
                        k.dma('sp', fnw6[:], fnw6_in[0].partition_broadcast(128), (), [bfn])
                        x1t = [k.sb([128, D], F32, "x1t%d" % i) for i in range(2)]
                        fj6 = [k.sb([128, D], BF16, "fj6%d" % i) for i in range(2)]
                        fs6 = [k.sb([128, 4], F32, "fs6%d" % i) for i in range(2)]
                        bx1t, bfj6, bfs6 = P.bufs(2), P.bufs(2), P.bufs(2)
                        for tl in range(4):
                            s = tl % 2
                            t = tg * 4 + tl
                            k.dma('sp', x1t[s][:], X1[t * 128:(t + 1) * 128, :], (), [bx1t[s]])
                            k.tt('dve', OUTacc[:, tl, :], OUTacc[:, tl, :], g2b[:], ALU.mult, [bOut, bg2] + bOutS[tl], [bOut])
                            k.tt('dve', x1t[s][:], x1t[s][:], OUTacc[:, tl, :], ALU.add, [bx1t[s], bOut], [bx1t[s]])
                            if stage == 6 and DEBUG_OUT:
                                k.dma('sp', X2dbg[t * 128:(t + 1) * 128, :], x1t[s][:], [bx1t[s]], ())
                            k.act(fj6[s][:], x1t[s][:], AF.Square, [bx1t[s]], [bfj6[s], bfs6[s]], accum=fs6[s][:, 0:1])
                            k.ts('dve', fs6[s][:, 1:2], fs6[s][:, 0:1], 1.0 / D, 1e-6, ALU.mult, ALU.add, [bfs6[s]], [bfs6[s]])
                            k.act(fs6[s][:, 2:3], fs6[s][:, 1:2], AF.Sqrt, [bfs6[s]], [bfs6[s]])
                            k.recip(fs6[s][:, 3:4], fs6[s][:, 2:3], [bfs6[s]], [bfs6[s]])
                            k.stt(x1t[s][:], x1t[s][:], fs6[s][:, 3:4], fnw6[:], ALU.mult, ALU.mult, [bx1t[s], bfs6[s], bfn], [bx1t[s]])
                            k.dma('sp', outv6[t], x1t[s][:], [bx1t[s]], ())

    if stage < 6:
      pass
    fnw_in = din("final_norm_w", [1, D]) if stage < 6 else None
    fnw = k.sb([128, D], F32, "fnw")
    b_fnw = P.buf()
    if stage < 6:
        k.dma('sp', fnw[:], fnw_in[0].partition_broadcast(128), (), [b_fnw])
    X2v = xl.rearrange("(t p) d -> t p d", p=128)
    outv = out_d.rearrange("(t p) d -> t p d", p=128)
    ft = [k.sb([128, D], F32, "ft%d" % i) for i in range(2)]
    fj = [k.sb([128, D], BF16, "fj%d" % i) for i in range(2)]
    fs = [k.sb([128, 4], F32, "fs%d" % i) for i in range(2)]
    bft, bfj, bfs = P.bufs(2), P.bufs(2), P.bufs(2)
    for t in range(NOWN // 128 if stage < 6 else 0):
        s = t % 2
        k.dma('sp', ft[s][:], X2v[OWN0 // 128 + t], (), [bft[s]])
        k.act(fj[s][:], ft[s][:], AF.Square, [bft[s]], [bfj[s], bfs[s]], accum=fs[s][:, 0:1])
        k.ts('dve', fs[s][:, 1:2], fs[s][:, 0:1], 1.0 / D, 1e-6, ALU.mult, ALU.add, [bfs[s]], [bfs[s]])
        k.act(fs[s][:, 2:3], fs[s][:, 1:2], AF.Sqrt, [bfs[s]], [bfs[s]])
        k.recip(fs[s][:, 3:4], fs[s][:, 2:3], [bfs[s]], [bfs[s]])
        k.stt(ft[s][:], ft[s][:], fs[s][:, 3:4], fnw[:], ALU.mult, ALU.mult, [bft[s], bfs[s], b_fnw], [bft[s]])
        k.dma('sp', outv[t], ft[s][:], [bft[s]], ())

    with nc.Block() as block:
        P.emit(block)
    k.es.close()
    return nc


def _gconst():
    NEGM = -30000.0
    r = np.arange(128)[:, None]
    c = np.arange(128)[None, :]
    g = np.zeros((128, 5, 128), np.float32)
    g[:, 4, :] = (r // 32 == c // 32)
    g[:, 0, :] = (r <= c)
    g[:, 1, :] = np.where(r > c, 0.0, NEGM)
    g[:, 2, :] = np.where(c > r, 0.0, NEGM)
    g[:, 3, :] = np.where(c >= r, 0.0, NEGM)
    return g


GCONST = _gconst()


def _nsa_consts():
    NEGM = -30000.0
    c = {}
    n128 = np.zeros((128, 385), np.float32)
    for m in range(16):
        n128[m + 16, m] = -1.0
    for m in range(16, 32):
        n128[m - 16, m] = 1.0
    half = 16
    inv_freq = (500000.0 ** (-np.arange(half, dtype=np.float32) / half)).astype(np.float32)
    for d in range(32):
        n128[d, 128] = inv_freq[d % 16]
    r = np.arange(128)[:, None]
    q = np.arange(128)[None, :]
    n128[:, 129:257] = np.where(r <= q, 0.0, NEGM)
    n128[:, 257:385] = np.where(r > q, 0.0, NEGM)
    c["nsa_c128"] = n128
    u = np.arange(LOC)[None, :]
    c["maskB"] = np.where(16 * r + 31 <= u, 0.0, NEGM).astype(np.float32)
    c["eexp"] = (np.arange(LOC)[None, :] // 64 == np.arange(64)[:, None]).astype(np.float32)
    ovl = np.zeros((128, 2, 65), np.float32)
    for j in range(2):
        for nl in range(128):
            n = j * 128 + nl
            if n >= 255:
                continue
            for sblk in range(64):
                if 16 * n < 64 * sblk + 64 and 16 * n + 32 > 64 * sblk:
                    ovl[nl, j, sblk] = 1.0
            ovl[nl, j, 64] = 1.0
    c["ovl"] = ovl
    fvv = np.zeros((128, 2, 16, 64), np.float32)
    for qt in range(16):
        for qq in range(128):
            t = OWN0 + qt * 128 + qq
            cur = t // 64
            for sblk in range(64):
                fb = 0.0
                if sblk == cur:
                    fb = 2.0e9
                elif sblk == cur - 1:
                    fb = 3.0e9
                fvv[qq, 0, qt, sblk] = fb
                fvv[qq, 1, qt, sblk] = 3.0e38 if 64 * sblk <= t else -1.0e30
    c["fbigvis"] = fvv
    return c


NSA_CONSTS = _nsa_consts()


def _nsa_core(half):
    a = np.zeros((128, 129), np.float32)
    first = 0 if half == 1 else 32
    a[:, first] = 1.0e9
    a[:, 64:128] = 3.0e38
    if half == 0:
        a[:, 64:64 + 32] = -1.0e30
        a[:, 128] = -30000.0
    return a


def _pos_loc(inp, b, half):
    p = np.asarray(inp['positions'], np.int32)[b]
    out = np.zeros((1, LOC), np.int32)
    if half == 1:
        out[0] = p
    else:
        out[0, OWN0:] = p[:NOWN]
    return out


PEER_UT_CACHE = {}


def make_in_maps(inp):
    x = np.asarray(inp['x'], np.float32)
    c = np.asarray(inp['c'], np.float32)
    maps = []
    ident = np.eye(128, dtype=np.float32)
    for core in range(8):
        b, half = core // 2, core % 2
        xl = np.zeros((LOC, D), np.float32)
        if half == 0:
            xl[OWN0:] = x[b, :NOWN]
        else:
            xl[:] = x[b]
        m = {
            "xl": xl,
            "cb": np.ascontiguousarray(c[b].reshape(KC, 128)),
            "ada_w": np.ascontiguousarray(inp['ada_w'][0]),
            "ada_b": np.ascontiguousarray(np.asarray(inp['ada_b'][0]).reshape(96, 128)),
            "norm1_w": np.ascontiguousarray(np.asarray(inp['norm1_w'][0]).reshape(KC, 128)),
            "w_in": np.ascontiguousarray(inp['w_in'][0]),
            "ident": ident,
            "pvalid": np.full((128, 1), float(half), np.float32),
            "w_branch_gdn": np.ascontiguousarray(inp['w_branch_gdn'][0], np.float32),
            "w_branch_nsa": np.ascontiguousarray(inp['w_branch_nsa'][0], np.float32),
            "w_out": np.ascontiguousarray(inp['w_out'][0], np.float32),
            "norm2_w": np.ascontiguousarray(np.asarray(inp['norm2_w'][0], np.float32).reshape(KC, 128)),
            "peer_wq": np.ascontiguousarray(inp['peer_wq'][0], np.float32),
            "peer_keys1": np.ascontiguousarray(inp['peer_keys1'][0], np.float32),
            "peer_keys2": np.ascontiguousarray(inp['peer_keys2'][0], np.float32),
            "peer_uT": PEER_UT_CACHE.get(id(inp['peer_u'])) if id(inp['peer_u']) in PEER_UT_CACHE else PEER_UT_CACHE.setdefault(id(inp['peer_u']), np.ascontiguousarray(np.asarray(inp['peer_u'][0], np.float32).T)),
            "peer_v": np.ascontiguousarray(inp['peer_v'][0], np.float32),
            "gconst": GCONST,
            "pos": np.ascontiguousarray(_pos_loc(inp, b, half)),
            "nsa_core": _nsa_core(half),
            "cmp_pos_k": np.ascontiguousarray(inp['cmp_pos_k'][0], np.float32), "cmp_pos_v": np.ascontiguousarray(inp['cmp_pos_v'][0], np.float32),
            "cmp_w1_k": np.ascontiguousarray(inp['cmp_w1_k'][0], np.float32), "cmp_w1_v": np.ascontiguousarray(inp['cmp_w1_v'][0], np.float32),
            "cmp_w2_k": np.ascontiguousarray(inp['cmp_w2_k'][0], np.float32), "cmp_w2_v": np.ascontiguousarray(inp['cmp_w2_v'][0], np.float32),
            **NSA_CONSTS,
            "gdn_conv_w": np.ascontiguousarray(np.asarray(inp['gdn_conv_w'][0], np.float32).reshape(96, 128)),
            "gdn_A_log": np.ascontiguousarray(np.broadcast_to(np.asarray(inp['gdn_A_log'][0], np.float32)[None, :], (32, 8))),
            "gdn_dt_bias": np.ascontiguousarray(np.broadcast_to(np.asarray(inp['gdn_dt_bias'][0], np.float32)[None, :], (32, 8))),
            "gdn_norm_w": np.ascontiguousarray(np.asarray(inp['gdn_norm_w'][0], np.float32).reshape(1, 128)),
            "final_norm_w": np.ascontiguousarray(np.asarray(inp['final_norm_w'], np.float32).reshape(1, D)),
        }
        maps.append(m)
    return maps


def kernel(**inputs):
    nc = build_program(6)
    maps = make_in_maps(inputs)
    res = run_bass_kernel_spmd(nc, maps, core_ids=list(range(8)))
    out = np.zeros((4, SEQ, D), np.float32)
    for core in range(8):
        b, half = core // 2, core % 2
        out[b, half * NOWN:(half + 1) * NOWN] = res.results[core]["out"]
    return out
```

```python
import numpy as np
from contextlib import ExitStack
import concourse.bass as bass
import concourse.mybir as mybir
from concourse.bass_utils import run_bass_kernel_spmd

F32 = mybir.dt.float32
BF16 = mybir.dt.bfloat16
I32 = mybir.dt.int32
ALU = mybir.AluOpType
AF = mybir.ActivationFunctionType
AX = mybir.AxisListType

ENGS = ['pe', 'act', 'dve', 'pool', 'sp']

D = 2048
SEQ = 4096
LOC = 4096
OWN0 = 2048
NOWN = 2048
D_IN = 11840
KC = 16
DEBUG_OUT = False
GDN_ONE_HEAD = False
FROM_REF = False
GDN_SUB = 9
PF_ROWS = 80 * 128
GDN_WAVES = None
GDN_STEPS = 10 ** 9
NSA_GROUPS_RUN = None
NSA_QT_RUN = None
SKIP_GDN = False
SKIP_NSA = False
SKIP_P5 = False
REF_IN = set()
PEER_EB = None
PEER_TG = None


class Buf:
    __slots__ = ('name', 'lw', 'rd', 'excl')

    def __init__(self, name):
        self.name = name
        self.lw = None
        self.rd = {}
        self.excl = False


class Prog:
    def __init__(self, nc, n_dma_sems=40):
        self.nc = nc
        self.ops = {e: [] for e in ENGS}
        self.cnt = {e: 0 for e in ENGS}
        self.esem = {e: nc.alloc_semaphore(name="es_" + e) for e in ENGS}
        self.dsem = [nc.alloc_semaphore(name="ds_%d" % i) for i in range(n_dma_sems)]
        self.dval = [0] * n_dma_sems
        self.dnext = 0
        self.bsem = [nc.alloc_semaphore(name="bs_%d" % i) for i in range(8)]
        self.bval = [0] * 8
        self.bnext = 0
        self.waited = {e: {} for e in ENGS}
        self.pend = {e: [] for e in ENGS}
        self.nbuf = 0

    def barrier(self):
        snap = [(('e', o), self.cnt[o]) for o in ENGS if self.cnt[o] > 0]
        snap += [(('d', i), self.dval[i]) for i in range(len(self.dsem)) if self.dval[i] > 0]
        for e in ENGS:
            self.pend[e] = snap

    def _pending(self, eng, waits):
        w = self.waited[eng]
        for key, val in self.pend[eng]:
            if key == ('e', eng):
                continue
            if w.get(key, 0) < val:
                w[key] = val
                waits.append((self._sem(key), val))
        self.pend[eng] = []

    def buf(self, name=None):
        self.nbuf += 1
        return Buf(name or ("b%d" % self.nbuf))

    def bufs(self, n):
        return [self.buf() for _ in range(n)]

    def _sem(self, key):
        if key[0] == 'e':
            return self.esem[key[1]]
        return self.dsem[key[1]] if key[0] == 'd' else self.bsem[key[1]]

    def _deps(self, eng, reads, writes):
        need = {}

        def add(tok, raw):
            if tok is None:
                return
            key, val, teng = tok
            if teng == eng and eng == 'pe':
                return
            if need.get(key, 0) < val:
                need[key] = val

        for b in reads:
            add(b.lw, True)
            if b.excl:
                for t in b.rd.values():
                    if t[2] != eng:
                        add(t, True)
        for b in writes:
            add(b.lw, False)
            for t in b.rd.values():
                add(t, False)
        waits = []
        w = self.waited[eng]
        for key, val in need.items():
            if w.get(key, 0) < val:
                w[key] = val
                waits.append((self._sem(key), val))
        return waits

    def _mark(self, tok, reads, writes):
        for b in reads:
            b.rd[tok[0]] = tok
        for b in writes:
            b.lw = tok
            b.rd = {}

    def op(self, eng, fn, reads=(), writes=()):
        waits = self._deps(eng, reads, writes)
        self._pending(eng, waits)
        self.cnt[eng] += 1
        tok = (('e', eng), self.cnt[eng], eng)
        self.ops[eng].append((waits, fn, (self.esem[eng], 1)))
        self._mark(tok, reads, writes)
        return tok

    def dma(self, eng, out, in_, reads=(), writes=(), bg=False, **kw):
        if bg:
            sems, vals, idx, kk = self.bsem, self.bval, self.bnext, 'b'
            self.bnext = (self.bnext + 1) % len(self.bsem)
        else:
            sems, vals, idx, kk = self.dsem, self.dval, self.dnext, 'd'
            self.dnext = (self.dnext + 1) % len(self.dsem)
        waits = self._deps(eng, reads, writes)
        self._pending(eng, waits)
        key = (kk, idx)
        w = self.waited[eng]
        if w.get(key, 0) < vals[idx]:
            w[key] = vals[idx]
            waits.append((sems[idx], vals[idx]))
        vals[idx] += 16
        tok = (key, vals[idx], None)
        sem_ = sems[idx]
        self.ops[eng].append((waits, (lambda e: e.dma_start(out=out, in_=in_, **kw)), (sem_, 16)))
        self._mark(tok, reads, writes)
        return tok

    def emit(self, block):
        fin = [(self.dsem[i], self.dval[i]) for i in range(len(self.dsem)) if self.dval[i] > 0]
        fin += [(self.bsem[i], self.bval[i]) for i in range(len(self.bsem)) if self.bval[i] > 0]
        fin += [(self.esem[e], self.cnt[e]) for e in ENGS if e != 'sp' and self.cnt[e] > 0]

        def run(e, name):
            for waits, fn, inc in self.ops[name]:
                for s, v in waits:
                    e.wait_ge(s, v)
                ins = fn(e)
                ins.then_inc(inc[0], inc[1])
            if name == 'sp':
                for s, v in fin:
                    e.wait_ge(s, v)

        @block.tensor
        def _(e):
            run(e, 'pe')

        @block.scalar
        def _(e):
            run(e, 'act')

        @block.vector
        def _(e):
            run(e, 'dve')

        @block.gpsimd
        def _(e):
            run(e, 'pool')

        @block.sync
        def _(e):
            run(e, 'sp')


class K:
    def __init__(self, nc):
        self.nc = nc
        self.P = Prog(nc)
        self.es = ExitStack()
        self.n = 0

    def scope(self):
        kk = self

        class _S:
            def __enter__(s_):
                s_.old = kk.es
                kk.es = ExitStack()
                return s_

            def __exit__(s_, *a):
                kk.es.close()
                kk.es = s_.old
                kk.P.barrier()
                return False
        return _S()

    def sb(self, shape, dt=F32, name=None):
        self.n += 1
        return self.es.enter_context(self.nc.sbuf_tensor(("%s_%d" % (name, self.n)) if name else ("t%d" % self.n), list(shape), dt))

    def ps(self, shape, dt=F32, name=None):
        self.n += 1
        return self.es.enter_context(self.nc.psum_tensor(name or ("p%d" % self.n), list(shape), dt))

    def dram(self, shape, dt=F32, name=None, kind="Internal"):
        self.n += 1
        return self.nc.dram_tensor(name or ("d%d" % self.n), list(shape), dt, kind=kind).ap()

    def mm(self, out, lhsT, rhs, start, stop, r, w):
        return self.P.op('pe', lambda e: e.matmul(out, lhsT=lhsT, rhs=rhs, start=start, stop=stop), r, w)

    def tr(self, out, in_, ident, r, w):
        return self.P.op('pe', lambda e: e.transpose(out, in_, ident), r, w)

    def act(self, out, in_, func, r, w, bias=None, scale=None, accum=None, eng='act'):
        kw = {}
        if bias is not None:
            kw['bias'] = bias
        if scale is not None:
            kw['scale'] = scale
        if accum is not None:
            kw['accum_out'] = accum
        return self.P.op('act', lambda e: e.activation(out=out, in_=in_, func=func, **kw), r, w)

    def ts(self, eng, out, in0, s1, s2, op0, op1, r, w):
        if op1 is None:
            return self.P.op(eng, lambda e: e.tensor_scalar(out=out, in0=in0, scalar1=s1, scalar2=None, op0=op0), r, w)
        return self.P.op(eng, lambda e: e.tensor_scalar(out=out, in0=in0, scalar1=s1, scalar2=s2, op0=op0, op1=op1), r, w)

    def tt(self, eng, out, in0, in1, op, r, w):
        return self.P.op(eng, lambda e: e.tensor_tensor(out=out, in0=in0, in1=in1, op=op), r, w)

    def stt(self, out, in0, scalar, in1, op0, op1, r, w):
        return self.P.op('dve', lambda e: e.scalar_tensor_tensor(out=out, in0=in0, scalar=scalar, in1=in1, op0=op0, op1=op1), r, w)

    def cp(self, eng, out, in_, r, w):
        if eng == 'act':
            return self.P.op('act', lambda e: e.copy(out=out, in_=in_), r, w)
        return self.P.op(eng, lambda e: e.tensor_copy(out=out, in_=in_), r, w)

    def memset(self, eng, ap, val, w):
        return self.P.op(eng, lambda e: e.memset(ap, val), (), w)

    def recip(self, out, in_, r, w):
        return self.P.op('dve', lambda e: e.reciprocal(out=out, in_=in_), r, w)

    def dma(self, eng, out, in_, r, w, **kw):
        return self.P.dma(eng, out, in_, r, w, **kw)


def build_program(stage=99):
    nc = bass.Bass("TRN2", target_bir_lowering=False)
    k = K(nc)
    P = k.P

    def din(name, shape, dt=F32):
        return nc.dram_tensor(name, list(shape), dt, kind="ExternalInput").ap()

    xl = din("xl", [LOC, D])
    cb = din("cb", [KC, 128])
    ada_w = din("ada_w", [D, 6 * D]) if not FROM_REF else None
    ada_b = din("ada_b", [96, 128])
    norm1_w = din("norm1_w", [KC, 128])
    w_in = din("w_in", [D, D_IN]) if not FROM_REF else None
    ident_in = din("ident", [128, 128])
    pvalid_in = din("pvalid", [128, 1])
    out_d = nc.dram_tensor("out", [NOWN, D], F32, kind="ExternalOutput").ap()
    dbg_mod = nc.dram_tensor("dbg_mod", [128, 96], F32, kind="ExternalOutput").ap()
    def scratch(name, shape, dt=F32):
        if name in REF_IN:
            return din(name + "_in", shape, dt)
        return k.dram(shape, dt, name)

    if FROM_REF:
        PF = din("PF_in", [PF_ROWS, LOC])
        PT = din("PT_in", [LOC, 1600])
    else:
        PF = k.dram([80 * 128, LOC], F32, "PF")
        PT = k.dram([LOC, 1600], F32, "PT")
    OAT = scratch("OAT", [1024, NOWN], BF16)
    OBT = scratch("OBT", [2048, NOWN], BF16)
    YT = scratch("YT", [D, NOWN], BF16)
    X1 = scratch("X1", [NOWN, D], F32)

    ident = k.sb([128, 128], F32, "ident_sb")
    identb = k.sb([128, 128], BF16, "identb")
    pvalid = k.sb([128, 1], F32, "pvalid_sb")
    b_const = P.buf("const")
    k.dma('sp', ident[:], ident_in, (), [b_const])
    k.dma('sp', pvalid[:], pvalid_in, (), [b_const])
    k.cp('dve', identb[:], ident[:], [b_const], [b_const])

    banks = [k.ps([128, 512], F32, "bank%d" % i) for i in range(6)]
    bbank = [P.buf("bank%d" % i) for i in range(6)]
    for b_ in bbank:
        b_.excl = True
    pbf = [k.ps([128, 1024], BF16, "pbf%d" % i) for i in range(2)]
    bpbf = [P.buf("pbf%d" % i) for i in range(2)]
    for b_ in bpbf:
        b_.excl = True

    def load_fm(dst, src, n, bdst, bank=5):
        tmp = k.sb([96, 128], F32)
        bt = P.buf()
        k.dma('sp', tmp[0:n, :], src, (), [bt])
        k.tr(banks[bank][:, 0:n], tmp[0:n, :], ident[0:n, 0:n], [bt, b_const], [bbank[bank]])
        k.cp('dve', dst, banks[bank][:, 0:n], [bbank[bank]], [bdst])

    modT = k.sb([128, 96], F32, "modT")
    b_mod = P.buf("mod")
    A1 = k.sb([128, KC], F32, "A1")
    B1p = k.sb([128, KC], F32, "B1p")
    b_A1 = P.buf()
    if FROM_REF:
        modT_in = din("modT_in", [128, 96])
        k.dma('sp', modT[:], modT_in, (), [b_mod])
    with k.scope():
      if not FROM_REF:
        cT = k.sb([128, KC], F32, "cT")
        csT = k.sb([128, KC], F32, "csT")
        adabT = k.sb([128, 96], F32, "adabT")
        n1T = k.sb([128, KC], F32, "n1T")
        b_c, b_cs, b_adab, b_n1 = P.bufs(4)
        load_fm(cT[:], cb, KC, b_c)
        load_fm(adabT[:], ada_b, 96, b_adab)
        load_fm(n1T[:], norm1_w, KC, b_n1)
        k.act(csT[:], cT[:], AF.Silu, [b_c], [b_cs])
        NAW = 3
        awt = [k.sb([128, KC, 128], F32, "awt%d" % i) for i in range(NAW)]
        bawt = P.bufs(NAW)
        ada_v = ada_w.rearrange("(kc p) j -> p kc j", p=128)
        for m in range(96):
            s = m % NAW
            k.dma('sp', awt[s][:], ada_v[:, :, m * 128:(m + 1) * 128], (), [bawt[s]])
            for kc in range(KC):
                k.mm(banks[4][:, m:m + 1], awt[s][:, kc, :], csT[:, kc:kc + 1], kc == 0, kc == KC - 1,
                     [bawt[s], b_cs], [bbank[4]])
        k.tt('dve', modT[:], banks[4][:, 0:96], adabT[:], ALU.add, [bbank[4], b_adab], [b_mod])
        k.dma('sp', dbg_mod, modT[:], [b_mod], ())
        k.stt(A1[:], modT[:, 16:32], 1.0, n1T[:], ALU.add, ALU.mult, [b_mod, b_n1], [b_A1])
        k.ts('dve', B1p[:], modT[:, 0:16], pvalid[:, 0:1], None, ALU.mult, None, [b_mod, b_const], [b_A1])

    if stage >= 1 and not FROM_REF:
        hscope = k.scope()
        hscope.__enter__()
        hT = k.sb([128, KC, LOC], BF16, "hT")
        b_hT = [P.buf() for _ in range(LOC // 128)]
        with k.scope():
            xt = [k.sb([128, D], F32, "xt%d" % i) for i in range(2)]
            xs = [k.sb([128, D], BF16, "xs%d" % i) for i in range(2)]
            st = [k.sb([128, 4], F32, "st%d" % i) for i in range(2)]
            bxt, bxs, bst = P.bufs(2), P.bufs(2), P.bufs(2)
            xlv = xl.rearrange("(t p) d -> t p d", p=128)
            for t in range(LOC // 128):
                s = t % 2
                k.dma('sp', xt[s][:], xlv[t], (), [bxt[s]])
                k.act(xs[s][:], xt[s][:], AF.Square, [bxt[s]], [bxs[s], bst[s]], accum=st[s][:, 0:1])
                k.ts('dve', st[s][:, 1:2], st[s][:, 0:1], 1.0 / D, 1e-6, ALU.mult, ALU.add, [bst[s]], [bst[s]])
                k.act(st[s][:, 2:3], st[s][:, 1:2], AF.Sqrt, [bst[s]], [bst[s]])
                k.recip(st[s][:, 3:4], st[s][:, 2:3], [bst[s]], [bst[s]])
                k.ts('dve', xs[s][:], xt[s][:], st[s][:, 3:4], None, ALU.mult, None, [bxt[s], bst[s]], [bxs[s]])
                Bsel = B1p if t < OWN0 // 128 else modT
                for kc in range(KC):
                    pi = kc % 2
                    sl = slice((kc // 2 % 4) * 128, (kc // 2 % 4) * 128 + 128)
                    k.tr(pbf[pi][:, sl], xs[s][:, kc * 128:(kc + 1) * 128], identb[:], [bxs[s], b_const], [bpbf[pi]])
                    if kc % 2 == 0:
                        k.act(hT[:, kc, t * 128:(t + 1) * 128], pbf[pi][:, sl], AF.Identity, [bpbf[pi], b_A1, b_mod], [b_hT[t]],
                              bias=Bsel[:, kc:kc + 1], scale=A1[:, kc:kc + 1])
                    else:
                        k.ts('dve', hT[:, kc, t * 128:(t + 1) * 128], pbf[pi][:, sl], A1[:, kc:kc + 1], Bsel[:, kc:kc + 1],
                             ALU.mult, ALU.add, [bpbf[pi], b_A1, b_mod], [b_hT[t]])
        if stage == 1:
            dbg_h = nc.dram_tensor("dbg_h", [128, KC, LOC], BF16, kind="ExternalOutput").ap()
            k.dma('sp', dbg_h, hT[:], b_hT, ())

        if stage >= 2:
            FM = [(0, 3072, 0, 0), (4112, 2048, 3072, OWN0), (6160, 256, 5120, 0), (6416, 256, 5376, 0),
                  (6672, 256, 5632, 0), (7184, 256, 5888, 0), (7744, 2048, 6144, OWN0), (9792, 2048, 8192, OWN0)]
            TM = [(3072, 256, 0, OWN0), (3328, 256, 256, OWN0), (3584, 256, 512, OWN0), (3840, 256, 768, OWN0),
                  (4096, 16, 1024, 0), (6928, 256, 1040, 0), (7440, 256, 1296, 0), (7696, 48, 1552, OWN0)]
            with k.scope():
                wv = w_in.rearrange("(kc p) c -> p kc c", p=128)
                NW = 3
                wt = [k.sb([128, KC, 256], BF16, "wt%d" % i) for i in range(NW)]
                bwt = P.bufs(NW)
                ev = [k.sb([128, 512], F32, "ev%d" % i) for i in range(4)]
                bev = P.bufs(4)
                ei = 0
                wi = 0
                for (c0, ncols, r0, t0) in FM:
                    for cb_ in range(ncols // 256):
                        s = wi % NW
                        wi += 1
                        k.dma('pool', wt[s][:], wv[:, :, c0 + cb_ * 256:c0 + cb_ * 256 + 256], (), [bwt[s]])
                        for hf in range(2):
                            for tt in range(t0 // 512, LOC // 512):
                                e4 = ei % 4
                                ei += 1
                                for kc in range(KC):
                                    k.mm(banks[e4][:, :], wt[s][:, kc, hf * 128:(hf + 1) * 128],
                                         hT[:, kc, tt * 512:(tt + 1) * 512], kc == 0, kc == KC - 1,
                                         [bwt[s]] + b_hT[tt * 4:(tt + 1) * 4], [bbank[e4]])
                                k.cp('act' if e4 % 2 == 0 else 'dve', ev[e4][:], banks[e4][:, :], [bbank[e4]], [bev[e4]])
                                row = r0 + cb_ * 256 + hf * 128
                                k.dma('sp', PF[row:row + 128, tt * 512:(tt + 1) * 512], ev[e4][:], [bev[e4]], ())
                for (c0, ncols, p0, t0) in TM:
                    s = wi % NW
                    wi += 1
                    k.dma('pool', wt[s][:, :, 0:ncols], wv[:, :, c0:c0 + ncols], (), [bwt[s]])
                    for t in range(t0 // 128, LOC // 128):
                        e4 = ei % 4
                        ei += 1
                        for kc in range(KC):
                            k.mm(banks[e4][:, 0:ncols], hT[:, kc, t * 128:(t + 1) * 128], wt[s][:, kc, 0:ncols],
                                 kc == 0, kc == KC - 1, [bwt[s], b_hT[t]], [bbank[e4]])
                        k.cp('act' if e4 % 2 == 0 else 'dve', ev[e4][:, 0:ncols], banks[e4][:, 0:ncols], [bbank[e4]], [bev[e4]])
                        k.dma('sp', PT[t * 128:(t + 1) * 128, p0:p0 + ncols], ev[e4][:, 0:ncols], [bev[e4]], ())
        hscope.__exit__(None, None, None)
        if stage == 2 and DEBUG_OUT:
            dbg_pf = nc.dram_tensor("dbg_pf", [4, 128, LOC], F32, kind="ExternalOutput").ap()
            dbg_pt = nc.dram_tensor("dbg_pt", [LOC, 576], F32, kind="ExternalOutput").ap()
            for i, r in enumerate([0, 3072, 5120, 6144]):
                k.dma('sp', dbg_pf[i], PF[r:r + 128, :], (), ())
            k.dma('sp', dbg_pt, PT[:, 1024:1600], (), ())

    if stage >= 6:
        uT_in = din("peer_uT", [D, 16384])
        pv_in = din("peer_v", [16384, D])
        UBd = k.dram([64, 128, KC, 256], BF16, "UBd")
        VBd = k.dram([64, 128, 2, D], BF16, "VBd")
        bconvU = [P.buf() for _ in range(64)]
        bconvV = [P.buf() for _ in range(64)]
        uTv0 = uT_in.rearrange("(kc p) e -> p kc e", p=128)
        for eb in range(64):
            P.dma('pool', UBd[eb], uTv0[:, :, eb * 256:(eb + 1) * 256], (), [bconvU[eb]], bg=True)
            P.dma('pool', VBd[eb], pv_in[eb * 256:(eb + 1) * 256, :].rearrange("(i p) d -> p i d", p=128), (), [bconvV[eb]], bg=True)
    CONV_TOK = {}
    if stage >= 3 and not SKIP_GDN:
        gconst_in = din("gconst", [128, 5, 128])
        convw_in = din("gdn_conv_w", [96, 128])
        alog_in = din("gdn_A_log", [32, 8])
        dtb_in = din("gdn_dt_bias", [32, 8])
        gnw_in = din("gdn_norm_w", [1, 128])
        bq = [[bbank[bi]] * 4 for bi in range(6)]
        NCH = LOC // 128
        with k.scope():
            gcn = k.sb([128, 5, 128], F32, "gcn")
            ones = k.sb([128, 128], F32, "ones")
            cwT = k.sb([128, 96], F32, "cwT")
            nwb = k.sb([128, 128], F32, "nwb")
            b_gc = P.buf()
            b_cw = P.buf()
            k.dma('sp', gcn[:], gconst_in, (), [b_gc])
            k.memset('dve', ones[:], 1.0, [b_gc])
            k.dma('sp', nwb[:], gnw_in[0].partition_broadcast(128), (), [b_gc])
            load_fm(cwT[:], convw_in, 96, b_cw)
            TriU, msl, msu, miu, bmk = gcn[:, 0, :], gcn[:, 1, :], gcn[:, 2, :], gcn[:, 3, :], gcn[:, 4, :]
            ab = k.sb([128, NCH, 16], F32, "ab")
            dtb = k.sb([128, NCH, 8], F32, "dtb")
            alg = k.sb([128, NCH, 8], F32, "alg")
            vt = {nm: k.sb([128, NCH, 8], F32, "v_" + nm) for nm in
                  ["t1", "g", "beta", "lnb", "Gc", "nGc", "u", "gam", "bg", "kd", "gend", "Gl"]}
            b_v = P.buf()
            k.dma('sp', ab[:], PT[:, 1024:1040].rearrange("(n p) c -> p n c", p=128), (), [b_v])
            k.dma('sp', dtb[:], dtb_in.partition_broadcast(128), (), [b_v])
            k.dma('sp', alg[:], alog_in.partition_broadcast(128), (), [b_v])
            k.tt('dve', vt["t1"][:], ab[:, :, 0:8], dtb[:], ALU.add, [b_v], [b_v])
            k.act(vt["t1"][:], vt["t1"][:], AF.Exp, [b_v], [b_v])
            e_, ser, lnp = vt["t1"], vt["Gl"], vt["Gc"]
            k.ts('dve', ser[:], e_[:], -0.25, 1.0 / 3.0, ALU.mult, ALU.add, [b_v], [b_v])
            k.tt('dve', ser[:], ser[:], e_[:], ALU.mult, [b_v], [b_v])
            k.ts('dve', ser[:], ser[:], -0.5, None, ALU.add, None, [b_v], [b_v])
            k.tt('dve', ser[:], ser[:], e_[:], ALU.mult, [b_v], [b_v])
            k.ts('dve', ser[:], ser[:], 1.0, None, ALU.add, None, [b_v], [b_v])
            k.tt('dve', ser[:], ser[:], e_[:], ALU.mult, [b_v], [b_v])
            k.ts('dve', lnp[:], e_[:], 0.1, None, ALU.max, None, [b_v], [b_v])
            k.act(lnp[:], lnp[:], AF.Ln, [b_v], [b_v], bias=1.0)
            k.ts('dve', vt["u"][:], e_[:], 0.1, None, ALU.is_lt, None, [b_v], [b_v])
            k.tt('dve', ser[:], ser[:], lnp[:], ALU.subtract, [b_v], [b_v])
            k.tt('dve', ser[:], ser[:], vt["u"][:], ALU.mult, [b_v], [b_v])
            k.tt('dve', vt["t1"][:], lnp[:], ser[:], ALU.add, [b_v], [b_v])
            k.act(alg[:], alg[:], AF.Exp, [b_v], [b_v])
            k.stt(vt["g"][:], vt["t1"][:], -1.0, alg[:], ALU.mult, ALU.mult, [b_v], [b_v])
            k.act(vt["beta"][:], ab[:, :, 8:16], AF.Sigmoid, [b_v], [b_v])
            k.act(vt["lnb"][:], vt["beta"][:], AF.Ln, [b_v], [b_v])
            gflat = vt["g"][:].rearrange("p n h -> p (n h)")
            k.mm(banks[0][:, 0:256], TriU, gflat, True, True, [b_v, b_gc], [bq[0][0], bq[0][1]])
            k.mm(banks[0][:, 256:512], ones[:], gflat, True, True, [b_v, b_gc], [bq[0][2], bq[0][3]])
            fl = lambda nm: vt[nm][:].rearrange("p n h -> p (n h)")
            k.cp('dve', fl("Gc"), banks[0][:, 0:256], [bq[0][0], bq[0][1]], [b_v])
            k.cp('dve', fl("Gl"), banks[0][:, 256:512], [bq[0][2], bq[0][3]], [b_v])
            k.ts('dve', fl("nGc"), fl("Gc"), -1.0, None, ALU.mult, None, [b_v], [b_v])
            k.tt('dve', fl("u"), fl("Gc"), fl("lnb"), ALU.add, [b_v], [b_v])
            k.act(fl("gam"), fl("Gc"), AF.Exp, [b_v], [b_v])
            k.act(fl("bg"), fl("u"), AF.Exp, [b_v], [b_v])
            k.tt('dve', fl("kd"), fl("Gl"), fl("Gc"), ALU.subtract, [b_v], [b_v])
            k.act(fl("kd"), fl("kd"), AF.Exp, [b_v], [b_v])
            k.act(fl("gend"), fl("Gl"), AF.Exp, [b_v], [b_v])

            def col(nm, n, h):
                return vt[nm][:, n, h:h + 1]

            WV = 4
            for h in range(0 if GDN_SUB < 1 else (1 if GDN_ONE_HEAD else 8)):
                with k.scope():
                    QT = k.sb([128, LOC], F32, "QT")
                    KT = k.sb([128, LOC], F32, "KT")
                    VT = k.sb([128, LOC], F32, "VT")
                    xr = k.sb([128, LOC], F32, "xr")
                    bQ, bK, bV, bxr = P.bufs(4)
                    for (dst, bd, row0, ti) in [(QT, bQ, h * 128, h), (KT, bK, 1024 + h * 128, 8 + h), (VT, bV, 2048 + h * 128, 16 + h)]:
                        k.dma('sp', xr[:], PF[row0:row0 + 128, :], (), [bxr])
                        k.ts('dve', dst[:], xr[:], cwT[:, 3 * 24 + ti:3 * 24 + ti + 1], None, ALU.mult, None, [bxr, b_cw], [bd])
                        for sh in (1, 2, 3):
                            j = 3 - sh
                            k.stt(dst[:, sh:LOC], xr[:, 0:LOC - sh], cwT[:, j * 24 + ti:j * 24 + ti + 1], dst[:, sh:LOC],
                                  ALU.mult, ALU.add, [bxr, b_cw, bd], [bd])
                        k.act(dst[:], dst[:], AF.Silu, [bd], [bd])
                    for (dst, bd, lnc) in [(QT, bQ, float(np.log(128.0 ** -0.5))), (KT, bK, 0.0)]:
                        for tt in range(LOC // 512):
                            bk = tt % 4
                            sl = slice(tt * 512, (tt + 1) * 512)
                            k.tt('dve', xr[:, sl], dst[:, sl], dst[:, sl], ALU.mult, [bd], [bxr])
                            k.mm(banks[bk][:, :], ones[:], xr[:, sl], True, True, [bxr, b_gc], bq[bk])
                            k.act(xr[:, sl], banks[bk][:, :], AF.Ln, bq[bk], [bxr], bias=1e-6)
                            k.act(xr[:, sl], xr[:, sl], AF.Exp, [bxr], [bxr], scale=-0.5, bias=lnc)
                            k.tt('dve', dst[:, sl], dst[:, sl], xr[:, sl], ALU.mult, [bd, bxr], [bd])
                    zt = k.sb([128, NOWN // 128, 128], F32, "zt")
                    bz = P.buf()
                    k.dma('sp', zt[:], PT[OWN0:LOC, h * 128:(h + 1) * 128].rearrange("(n p) c -> p n c", p=128), (), [bz])
                    k.act(zt[:], zt[:], AF.Silu, [bz], [bz])
                    S = k.sb([128, 128], F32, "S")
                    bS = P.buf()
                    k.memset('dve', S[:], 0.0, [bS])
                    names = ["R", "Kd", "dg", "tE", "L", "LT", "QK", "Pa", "Pb", "WT", "U", "tmp", "O", "o16", "Z", "DT", "X", "Tt"]
                    T = [{nm: k.sb([128, 256 if nm in ("R", "X", "Tt") else 128], BF16 if nm == "o16" else F32, "w%d_%s" % (i, nm)) for nm in names}
                         for i in range(WV)]
                    Bf = [{nm: P.buf() for nm in names + ["st"]} for i in range(WV)]
                    stt_ = [k.sb([128, 4], F32, "w%d_st" % i) for i in range(WV)]
                    PTk = [[k.sb([128, 128], F32, "w%d_PT%d" % (i, l)) for l in range(4)] for i in range(WV)]
                    bPTk = [[P.buf() for l in range(4)] for i in range(WV)]
                    bctr = [0]

                    def nb():
                        bctr[0] = (bctr[0] + 1) % 6
                        return bctr[0]

                    for w0 in (GDN_WAVES if GDN_WAVES is not None else range(0, NCH if GDN_SUB >= 2 else 0, WV)):
                        chunks = list(range(w0, w0 + WV))
                        own = w0 >= OWN0 // 128
                        def pre_steps(i, n):
                            t, b = T[i], Bf[i]
                            cs = slice(n * 128, (n + 1) * 128)
                            q4 = slice(i * 128, (i + 1) * 128)
                            st = []
                            bkT1, bkT2, bkKK, bkKQ, bkB = 0, 1, 2, 3, 4
                            st.append(lambda: k.tr(banks[0][:, q4], KT[:, cs], ident[:], [bK, b_const], [bq[0][i]]))
                            st.append(lambda: k.act(t["R"][:, 0:128], banks[0][:, q4], AF.Identity, [bq[0][i], b_v], [b["R"]], scale=col("bg", n, h)))
                            st.append(lambda: k.ts('dve', t["Kd"][:], banks[0][:, q4], col("kd", n, h), None, ALU.mult, None, [bq[0][i], b_v], [b["Kd"]]))
                            st.append(lambda: k.tr(banks[1][:, q4], VT[:, cs], ident[:], [bV, b_const], [bq[1][i]]))
                            st.append(lambda: k.ts('dve', t["R"][:, 128:256], banks[1][:, q4], col("beta", n, h), None, ALU.mult, None, [bq[1][i], b_v], [b["R"]]))
                            st.append(lambda: k.mm(banks[2][:, q4], KT[:, cs], KT[:, cs], True, True, [bK], [bq[2][i]]))
                            st.append(lambda: k.ts('dve', t["dg"][:], ident[:], col("nGc", n, h), None, ALU.mult, None, [b_const, b_v], [b["dg"]]))
                            st.append(lambda: k.mm(banks[4][:, q4], ones[:], t["dg"][:], True, True, [b["dg"], b_gc], [bq[4][i]]))
                            st.append(lambda: k.stt(t["tE"][:], banks[4][:, q4], col("u", n, h), msl, ALU.add, ALU.add, [bq[4][i], b_v, b_gc], [b["tE"]]))
                            st.append(lambda: k.act(t["tE"][:], t["tE"][:], AF.Exp, [b["tE"]], [b["tE"]]))
                            st.append(lambda: k.tt('dve', t["L"][:], banks[2][:, q4], t["tE"][:], ALU.mult, [bq[2][i], b["tE"]], [b["L"]]))
                            st.append(lambda: k.ts('dve', t["dg"][:], ident[:], col("u", n, h), None, ALU.mult, None, [b_const, b_v], [b["dg"]]))
                            st.append(lambda: k.mm(banks[5][:, q4], ones[:], t["dg"][:], True, True, [b["dg"], b_gc], [bq[5][i]]))
                            st.append(lambda: k.stt(t["tE"][:], banks[5][:, q4], col("nGc", n, h), msu, ALU.add, ALU.add, [bq[5][i], b_v, b_gc], [b["tE"]]))
                            st.append(lambda: k.act(t["tE"][:], t["tE"][:], AF.Exp, [b["tE"]], [b["tE"]]))
                            st.append(lambda: k.tt('dve', t["LT"][:], banks[2][:, q4], t["tE"][:], ALU.mult, [bq[2][i], b["tE"]], [b["LT"]]))
                            if own:
                                st.append(lambda: k.mm(banks[3][:, q4], KT[:, cs], QT[:, cs], True, True, [bK, bQ], [bq[3][i]]))
                                st.append(lambda: k.ts('dve', t["dg"][:], ident[:], col("Gc", n, h), None, ALU.mult, None, [b_const, b_v], [b["dg"]]))
                                st.append(lambda: k.mm(banks[4][:, q4], ones[:], t["dg"][:], True, True, [b["dg"], b_gc], [bq[4][i]]))
                                st.append(lambda: k.stt(t["tE"][:], banks[4][:, q4], col("nGc", n, h), miu, ALU.add, ALU.add, [bq[4][i], b_v, b_gc], [b["tE"]]))
                                st.append(lambda: k.act(t["tE"][:], t["tE"][:], AF.Exp, [b["tE"]], [b["tE"]]))
                                st.append(lambda: k.tt('dve', t["QK"][:], banks[3][:, q4], t["tE"][:], ALU.mult, [bq[3][i], b["tE"]], [b["QK"]]))
                            st.append(lambda: k.tt('dve', t["tmp"][:], t["LT"][:], bmk, ALU.mult, [b["LT"], b_gc], [b["tmp"]]))
                            st.append(lambda: k.tt('dve', t["LT"][:], t["LT"][:], t["tmp"][:], ALU.subtract, [b["LT"], b["tmp"]], [b["LT"]]))
                            st.append(lambda: k.tt('dve', t["L"][:], t["L"][:], bmk, ALU.mult, [b["L"], b_gc], [b["L"]]))
                            cP, cbP, cPT, cbPT = t["L"], b["L"], t["tmp"], b["tmp"]
                            NLEV = 4
                            for lev in range(NLEV):
                                nP, nbP = (t["Pa"], b["Pa"]) if lev % 2 == 0 else (t["Pb"], b["Pb"])
                                bA = lev % 2
                                last = lev == NLEV - 1
                                if not last:
                                    st.append((lambda cP=cP, cbP=cbP, cPT=cPT, cbPT=cbPT, bA=bA: k.mm(banks[bA][:, q4], cPT[:], cP[:], True, True, [cbPT, cbP], [bq[bA][i]])))
                                    st.append((lambda nP=nP, nbP=nbP, bA=bA: k.cp('act', nP[:], banks[bA][:, q4], [bq[bA][i]], [nbP])))
                                st.append((lambda cP=cP, cbP=cbP, cPT=cPT, cbPT=cbPT, bA=bA: k.mm(banks[2 + bA][:, q4], cP[:], cPT[:], True, True, [cbPT, cbP], [bq[2 + bA][i]])))
                                st.append((lambda lev=lev, bA=bA: k.cp('dve', PTk[i][lev][:], banks[2 + bA][:, q4], [bq[2 + bA][i]], [bPTk[i][lev]])))
                                cP, cbP, cPT, cbPT = nP, nbP, PTk[i][lev], bPTk[i][lev]
                            st.append(lambda: k.cp('dve', t["Z"][:], ident[:], [b_const], [b["Z"]]))
                            for lev in [3, 2, 1, 0, -1]:
                                bA = 4 + (lev % 2)
                                if lev >= 0:
                                    st.append((lambda lev=lev, bA=bA: k.mm(banks[bA][:, q4], PTk[i][lev][:], t["Z"][:], True, True, [bPTk[i][lev], b["Z"]], [bq[bA][i]])))
                                    st.append((lambda bA=bA: k.tt('dve', t["Z"][:], t["Z"][:], banks[bA][:, q4], ALU.add, [bq[bA][i], b["Z"]], [b["Z"]])))
                                else:
                                    st.append((lambda bA=bA: k.mm(banks[bA][:, q4], t["tmp"][:], t["Z"][:], True, True, [b["tmp"], b["Z"]], [bq[bA][i]])))
                                    st.append((lambda bA=bA: k.tt('dve', t["Z"][:], t["Z"][:], banks[bA][:, q4], ALU.subtract, [bq[bA][i], b["Z"]], [b["Z"]])))
                            st.append(lambda: k.tr(banks[0][:, q4], t["Z"][:], ident[:], [b["Z"], b_const], [bq[0][i]]))
                            st.append(lambda: k.cp('act', t["DT"][:], banks[0][:, q4], [bq[0][i]], [b["DT"]]))
                            hq = slice((i % 2) * 256, (i % 2) * 256 + 256)
                            bkR = 4 + (i // 2)
                            bkR2 = 2 + (i // 2)
                            st.append(lambda: k.mm(banks[bkR][:, hq], t["DT"][:], t["R"][:], True, True, [b["DT"], b["R"]], [bq[bkR][0]]))
                            st.append(lambda: k.cp('dve', t["X"][:], banks[bkR][:, hq], [bq[bkR][0]], [b["X"]]))
                            for sweep in range(3):
                                st.append(lambda: k.mm(banks[bkR2][:, hq], t["LT"][:], t["X"][:], True, True, [b["LT"], b["X"]], [bq[bkR2][0]]))
                                st.append(lambda: k.tt('dve', t["Tt"][:], t["R"][:], banks[bkR2][:, hq], ALU.subtract, [b["R"], bq[bkR2][0]], [b["Tt"]]))
                                st.append(lambda: k.mm(banks[bkR][:, hq], t["DT"][:], t["Tt"][:], True, True, [b["DT"], b["Tt"]], [bq[bkR][0]]))
                                st.append(lambda: k.cp('dve', t["X"][:], banks[bkR][:, hq], [bq[bkR][0]], [b["X"]]))
                            st.append(lambda: k.cp('act', t["R"][:], t["X"][:], [b["X"]], [b["R"]]))
                            st.append(lambda: k.tr(banks[0][:, q4], t["R"][:, 0:128], ident[:], [b["R"], b_const], [bq[0][i]]))
                            st.append(lambda: k.cp('act', t["WT"][:], banks[0][:, q4], [bq[0][i]], [b["WT"]]))
                            return st

                        allst = [pre_steps(i, n) for i, n in enumerate(chunks)]
                        for si in range(min(GDN_STEPS, len(allst[0]))):
                            for i in range(WV):
                                allst[i][si]()
                        for i, n in enumerate(chunks if GDN_SUB >= 3 else []):
                            t, b = T[i], Bf[i]
                            cs = slice(n * 128, (n + 1) * 128)
                            q4 = slice(i * 128, (i + 1) * 128)
                            k.mm(banks[1][:, q4], t["WT"][:], S[:], True, True, [b["WT"], bS], [bq[1][i]])
                            k.tt('dve', t["U"][:], t["R"][:, 128:256], banks[1][:, q4], ALU.subtract, [b["R"], bq[1][i]], [b["U"]])
                            if own:
                                k.mm(banks[2][:, q4], QT[:, cs], S[:], True, True, [bQ, bS], [bq[2][i]])
                                k.mm(banks[3][:, q4], t["QK"][:], t["U"][:], True, True, [b["QK"], b["U"]], [bq[3][i]])
                                k.act(t["tmp"][:], banks[2][:, q4], AF.Identity, [bq[2][i], b_v], [b["tmp"]], scale=col("gam", n, h))
                                k.tt('dve', t["O"][:], t["tmp"][:], banks[3][:, q4], ALU.add, [b["tmp"], bq[3][i]], [b["O"]])
                            k.mm(banks[0][:, q4], t["Kd"][:], t["U"][:], True, True, [b["Kd"], b["U"]], [bq[0][i]])
                            k.stt(S[:], S[:], col("gend", n, h), banks[0][:, q4], ALU.mult, ALU.add, [bS, b_v, bq[0][i]], [bS])
                            if own:
                                no = n - OWN0 // 128
                                sti = stt_[i]
                                k.act(t["tmp"][:], t["O"][:], AF.Square, [b["O"]], [b["tmp"], b["st"]], accum=sti[:, 0:1])
                                k.act(sti[:, 1:2], sti[:, 0:1], AF.Ln, [b["st"]], [b["st"]], scale=1.0 / 128, bias=1e-6)
                                k.act(sti[:, 2:3], sti[:, 1:2], AF.Exp, [b["st"]], [b["st"]], scale=-0.5)
                                k.stt(t["O"][:], t["O"][:], sti[:, 2:3], nwb[:], ALU.mult, ALU.mult, [b["O"], b["st"], b_gc], [b["O"]])
                                k.tt('dve', t["O"][:], t["O"][:], zt[:, no, :], ALU.mult, [b["O"], bz], [b["O"]])
                                k.tr(banks[4][:, q4], t["O"][:], ident[:], [b["O"], b_const], [bq[4][i]])
                                k.cp('act', t["o16"][:], banks[4][:, q4], [bq[4][i]], [b["o16"]])
                                k.dma('sp', OAT[h * 128:(h + 1) * 128, no * 128:(no + 1) * 128], t["o16"][:], [b["o16"]], ())
        if stage == 3 and DEBUG_OUT and not SKIP_GDN:
            dbg_oa = nc.dram_tensor("dbg_oa", [1024, NOWN], BF16, kind="ExternalOutput").ap()
            k.dma('sp', dbg_oa, OAT, (), ())

    if stage >= 4 and not SKIP_NSA:
        pos_in = din("pos", [1, LOC], I32)
        nsc_in = din("nsa_c128", [128, 385])
        maskB_in = din("maskB", [128, LOC])
        eexp_in = din("eexp", [64, LOC])
        ovl_in = din("ovl", [128, 2, 65])
        fv_in = din("fbigvis", [128, 2, 16, 64])
        ncore_in = din("nsa_core", [128, 129])
        cpos_in = [din("cmp_pos_k", [32, 128]), din("cmp_pos_v", [32, 128])]
        cw1_in = [din("cmp_w1_k", [LOC, 256]), din("cmp_w1_v", [LOC, 256])]
        cw2_in = [din("cmp_w2_k", [256, 128]), din("cmp_w2_v", [256, 128])]
        NQT = NOWN // 128
        with k.scope():
            nsc = k.sb([128, 385], F32, "nsc")
            ncore = k.sb([128, 129], F32, "ncore")
            maskB = k.sb([128, LOC], BF16, "maskB")
            eexp = k.sb([64, LOC], BF16, "eexp")
            ovl = k.sb([128, 2, 65], BF16, "ovl")
            fv = k.sb([128, 2, 16, 64], F32, "fv")
            cmk = k.sb([128, 2, 128], BF16, "cmk")
            b_nc = P.buf()
            k.dma('sp', nsc[:], nsc_in, (), [b_nc])
            k.dma('sp', ncore[:], ncore_in, (), [b_nc])
            k.dma('sp', fv[:], fv_in, (), [b_nc])
            k.dma('pool', maskB[:], maskB_in, (), [b_nc])
            k.dma('pool', eexp[:], eexp_in, (), [b_nc])
            k.dma('pool', ovl[:], ovl_in, (), [b_nc])
            k.cp('dve', cmk[:, 0, :], nsc[:, 129:257], [b_nc], [b_nc])
            k.cp('dve', cmk[:, 1, :], nsc[:, 257:385], [b_nc], [b_nc])
            ropeP = nsc[:, 0:128]
            invf = nsc[:, 128:129]
            kvb = ncore[:, 128:129]
            cosT = k.sb([128, LOC], F32, "cosT")
            sinT = k.sb([128, LOC], F32, "sinT")
            cosq = k.sb([128, NOWN], F32, "cosq")
            sinq = k.sb([128, NOWN], F32, "sinq")
            b_rt = P.buf()
            with k.scope():
                posi = k.sb([128, LOC], I32, "posi")
                ang = k.sb([128, LOC], F32, "ang")
                kk_ = k.sb([128, LOC], I32, "kk")
                kf = k.sb([128, LOC], F32, "kf")
                b_a = P.buf()
                k.dma('sp', posi[:], pos_in[0].partition_broadcast(128), (), [b_a])
                k.cp('dve', ang[:], posi[:], [b_a], [b_a])
                k.ts('dve', ang[:], ang[:], invf, None, ALU.mult, None, [b_a, b_nc], [b_a])
                TWO_PI = float(2 * np.pi)
                for (dst, shift) in [(sinT, 0.0), (cosT, float(np.pi / 2))]:
                    k.ts('dve', kf[:], ang[:], shift, 1.0 / TWO_PI, ALU.add, ALU.mult, [b_a], [b_a])
                    k.cp('dve', kk_[:], kf[:], [b_a], [b_a])
                    k.cp('dve', kf[:], kk_[:], [b_a], [b_a])
                    k.stt(kf[:], kf[:], -TWO_PI, ang[:], ALU.mult, ALU.add, [b_a], [b_a])
                    k.ts('dve', kf[:], kf[:], shift, None, ALU.add, None, [b_a], [b_a])
                    k.ts('dve', dst[:], kf[:], float(np.pi), -TWO_PI, ALU.is_gt, ALU.mult, [b_a], [b_rt])
                    k.tt('dve', kf[:], kf[:], dst[:], ALU.add, [b_a, b_rt], [b_a])
                    k.ts('dve', dst[:], kf[:], -float(np.pi), TWO_PI, ALU.is_lt, ALU.mult, [b_a], [b_rt])
                    k.tt('dve', kf[:], kf[:], dst[:], ALU.add, [b_a, b_rt], [b_a])
                    k.act(dst[:], kf[:], AF.Sin, [b_a], [b_rt])
                k.ts('dve', cosq[:], cosT[:, OWN0:LOC], 128.0 ** -0.5, None, ALU.mult, None, [b_rt], [b_rt])
                k.ts('dve', sinq[:], sinT[:, OWN0:LOC], 128.0 ** -0.5, None, ALU.mult, None, [b_rt], [b_rt])

            def rope(dst_bf, X, bX, ct, st_, ntok, bdst, tmpa, tmpb, btmp):
                for tt in range(ntok // 512):
                    sl = slice(tt * 512, (tt + 1) * 512)
                    bk = tt % 3
                    k.mm(banks[bk][:, :], ropeP, X[:, sl], True, True, [bX, b_nc], [bbank[bk]])
                    k.tt('pool', tmpa[:, 0:512], X[:, sl], ct[:, sl], ALU.mult, [bX, b_rt], [btmp[0]])
                    k.tt('dve', tmpb[:, 0:512], banks[bk][:, :], st_[:, sl], ALU.mult, [bbank[bk], b_rt], [btmp[1]])
                    k.tt('dve', dst_bf[:, sl], tmpa[:, 0:512], tmpb[:, 0:512], ALU.add, [btmp[0], btmp[1]], [bdst])

            gts = k.sb([128, NQT, 48], F32, "gts")
            b_g = P.buf()
            k.dma('sp', gts[:], PT[OWN0:LOC, 1552:1600].rearrange("(n p) c -> p n c", p=128), (), [b_g])
            k.act(gts[:], gts[:], AF.Sigmoid, [b_g], [b_g])

            for g in range(2 if NSA_GROUPS_RUN is None else NSA_GROUPS_RUN):
                with k.scope():
                    KsT = k.sb([128, LOC], BF16, "KsT")
                    KwT = k.sb([128, LOC], BF16, "KwT")
                    Vs1 = k.sb([128, 32, 129], BF16, "Vs1")
                    Vw1 = k.sb([128, 32, 129], BF16, "Vw1")
                    KcmpT = k.sb([128, 256], BF16, "KcmpT")
                    RHSc = k.sb([128, 2, 193], BF16, "RHSc")
                    QTh = [k.sb([128, NOWN], BF16, "QTh%d" % i) for i in range(8)]
                    bKs, bKw, bVs, bVw, bKc, bRc = P.bufs(6)
                    bQh = P.bufs(8)
                    k.memset('dve', Vs1[:, :, 128:129], 1.0, [bVs])
                    k.memset('dve', Vw1[:, :, 128:129], 1.0, [bVw])
                    k.dma('pool', Vs1[:, :, 0:128], PT[:, 1040 + g * 128:1040 + (g + 1) * 128].rearrange("(n p) c -> p n c", p=128), (), [bVs])
                    k.dma('pool', Vw1[:, :, 0:128], PT[:, 1296 + g * 128:1296 + (g + 1) * 128].rearrange("(n p) c -> p n c", p=128), (), [bVw])
                    k.memset('dve', KcmpT[:], 0.0, [bKc])
                    k.cp('dve', RHSc[:, :, 0:65], ovl[:], [b_nc], [bRc])
                    with k.scope():
                        X = k.sb([128, LOC], F32, "ropeX")
                        tmpa = k.sb([128, 512], F32, "ropeA")
                        tmpb = k.sb([128, 512], F32, "ropeB")
                        KcT = k.sb([128, LOC], BF16, "KcT")
                        VcT = k.sb([128, LOC], BF16, "VcT")
                        bX, bKcT, bVcT = P.bufs(3)
                        btmp = P.bufs(2)
                        for (dstb, bd, row0) in [(KcT, bKcT, 5120 + g * 128), (KsT, bKs, 5632 + g * 128), (KwT, bKw, 5888 + g * 128)]:
                            k.dma('sp', X[:], PF[row0:row0 + 128, :], (), [bX])
                            rope(dstb, X, bX, cosT, sinT, LOC, bd, tmpa, tmpb, btmp)
                        k.dma('pool', VcT[:], PF[5376 + g * 128:5376 + (g + 1) * 128, :], (), [bVcT])
                        for hh in range(8):
                            row0 = 3072 + (g * 8 + hh) * 128
                            k.dma('sp', X[:, 0:NOWN], PF[row0:row0 + 128, OWN0:LOC], (), [bX])
                            rope(QTh[hh], X, bX, cosq, sinq, NOWN, bQh[hh], tmpa, tmpb, btmp)
                        w1 = k.sb([128, 32, 256], BF16, "cw1")
                        w2 = k.sb([128, 2, 128], BF16, "cw2")
                        cpT = k.sb([128, 32], F32, "cpT")
                        cpTb = k.sb([128, 32], BF16, "cpTb")
                        hidT = k.sb([128, 2, 256], BF16, "hidT")
                        hx = k.sb([128, 256], F32, "hx")
                        hy = k.sb([128, 256], F32, "hy")
                        hb = k.sb([128, 2], F32, "hb")
                        bw1, bw2, bcp, bhid, bhx, bhy, bhb = P.bufs(7)
                        for which, (srcT, bsrc) in enumerate([(KcT, bKcT), (VcT, bVcT)]):
                            k.dma('pool', w1[:], cw1_in[which].rearrange("(l d) h -> d l h", d=128), (), [bw1])
                            k.dma('pool', w2[:], cw2_in[which].rearrange("(t p) d -> p t d", p=128), (), [bw2])
                            load_fm(cpT[:], cpos_in[which], 32, bcp, bank=3)
                            k.cp('dve', cpTb[:], cpT[:], [bcp], [bcp])
                            k.memset('dve', hidT[:], 0.0, [bhid])
                            for ht in range(2):
                                for l in range(32):
                                    k.mm(banks[0][:, 0:255], w1[:, l, ht * 128:(ht + 1) * 128], srcT[:, l:l + 16 * 254 + 1:16],
                                         l == 0, l == 31, [bw1, bsrc], [bbank[0]])
                                for l in range(32):
                                    k.mm(banks[1][:, 0:1], w1[:, l, ht * 128:(ht + 1) * 128], cpTb[:, l:l + 1],
                                         l == 0, l == 31, [bw1, bcp], [bbank[1]])
                                k.cp('dve', hb[:, ht:ht + 1], banks[1][:, 0:1], [bbank[1]], [bhb])
                                k.act(hx[:, 0:255], banks[0][:, 0:255], AF.Identity, [bbank[0], bhb], [bhx], bias=hb[:, ht:ht + 1])
                                k.tt('dve', hy[:, 0:255], hx[:, 0:255], hx[:, 0:255], ALU.mult, [bhx], [bhy])
                                k.ts('dve', hy[:, 0:255], hy[:, 0:255], 0.044715, 1.0, ALU.mult, ALU.add, [bhy], [bhy])
                                k.tt('dve', hy[:, 0:255], hy[:, 0:255], hx[:, 0:255], ALU.mult, [bhy, bhx], [bhy])
                                k.act(hy[:, 0:255], hy[:, 0:255], AF.Sigmoid, [bhy], [bhy], scale=1.5957691216057308)
                                k.tt('dve', hidT[:, ht, 0:255], hy[:, 0:255], hx[:, 0:255], ALU.mult, [bhy, bhx], [bhid])
                            if which == 0:
                                for ht in range(2):
                                    k.mm(banks[2][:, 0:255], w2[:, ht, :], hidT[:, ht, 0:255], ht == 0, ht == 1, [bw2, bhid], [bbank[2]])
                                k.cp('dve', KcmpT[:, 0:255], banks[2][:, 0:255], [bbank[2]], [bKc])
                            else:
                                for j in range(2):
                                    nn = 128 if j == 0 else 127
                                    for ht in range(2):
                                        k.mm(banks[2][0:nn, 0:128], hidT[:, ht, j * 128:j * 128 + nn], w2[:, ht, :], ht == 0, ht == 1, [bw2, bhid], [bbank[2]])
                                    k.memset('dve', RHSc[:, j, 65:193], 0.0, [bRc])
                                    k.cp('dve', RHSc[0:nn, j, 65:193], banks[2][0:nn, 0:128], [bbank[2]], [bRc])
                    impacc = k.sb([128, 64], F32, "impacc")
                    v1 = k.sb([128, 64], F32, "selv1")
                    v2 = k.sb([128, 64], F32, "selv2")
                    m8 = k.sb([128, 16], F32, "m8")
                    selb = k.sb([128, 64], F32, "selb")
                    selbT = k.sb([64, 128], BF16, "selbT")
                    Oacc = k.sb([128, 8, 128], F32, "Oacc")
                    o16 = [k.sb([128, 128], BF16, "no16_%d" % i) for i in range(2)]
                    Et = [k.sb([128, 128], BF16, "Et%d" % i) for i in range(4)]
                    sc_ = k.sb([128, 8, 8], F32, "nsc_small")
                    bimp, bsel, bselT = P.bufs(3)
                    bOh = P.bufs(8)
                    bsch = P.bufs(8)
                    bo16 = P.bufs(2)
                    bEt = P.bufs(4)
                    ectr = [0]

                    def score_exp(lhsK, bK_, rhsQ, bQ_, extra, bias_ap):
                        e = ectr[0] % 4
                        bk = ectr[0] % 2
                        ectr[0] += 1
                        k.mm(banks[bk][:, 0:128], lhsK, rhsQ, True, len(extra) == 0, [bK_, bQ_], [bbank[bk]])
                        for xi, (l_, r_, bl_) in enumerate(extra):
                            k.mm(banks[bk][:, 0:128], l_, r_, False, xi == len(extra) - 1, bl_, [bbank[bk]])
                        if bias_ap is None:
                            k.act(Et[e][:], banks[bk][:, 0:128], AF.Exp, [bbank[bk]], [bEt[e]])
                        else:
                            k.act(Et[e][:], banks[bk][:, 0:128], AF.Exp, [bbank[bk], b_nc], [bEt[e]], bias=bias_ap)
                        return Et[e], bEt[e]

                    def tbank():
                        bk = ectr[0] % 2
                        ectr[0] += 1
                        return bk

                    for qt in range(NQT if NSA_QT_RUN is None else NSA_QT_RUN):
                        qsl = slice(qt * 128, (qt + 1) * 128)
                        for hh in range(8):
                            head = g * 8 + hh
                            sch, bsc = sc_[:, hh, :], bsch[hh]
                            cbk = 3 + (hh % 3)
                            ets = []
                            for j in range(2):
                                u0 = (OWN0 + 128 * qt) if j == 0 else 128 * qt
                                ets.append(score_exp(KcmpT[:, j * 128:(j + 1) * 128], bKc, QTh[hh][:, qsl], bQh[hh],
                                                     [(identb[:], maskB[:, u0:u0 + 128], [b_const, b_nc])], kvb if j == 0 else None))
                            for j in range(2):
                                k.mm(banks[cbk][:, 0:193], ets[j][0][:], RHSc[:, j, :], j == 0, j == 1, [ets[j][1], bRc], [bbank[cbk]])
                            k.ts('dve', sch[:, 0:1], banks[cbk][:, 64:65], 1e-30, None, ALU.max, None, [bbank[cbk]], [bsc])
                            k.recip(sch[:, 1:2], sch[:, 0:1], [bsc], [bsc])
                            if hh == 0:
                                k.ts('dve', impacc[:], banks[cbk][:, 0:64], sch[:, 1:2], None, ALU.mult, None, [bbank[cbk], bsc], [bimp])
                            else:
                                k.stt(impacc[:], banks[cbk][:, 0:64], sch[:, 1:2], impacc[:], ALU.mult, ALU.add, [bbank[cbk], bsc, bimp], [bimp])
                            k.ts('dve', sch[:, 2:3], gts[:, qt, head * 3:head * 3 + 1], sch[:, 1:2], None, ALU.mult, None, [b_g, bsc], [bsc])
                            k.ts('dve', Oacc[:, hh, :], banks[cbk][:, 65:193], sch[:, 2:3], None, ALU.mult, None, [bbank[cbk], bsc], [bOh[hh]])
                        k.tt('dve', v1[:], impacc[:], fv[:, 0, qt, :], ALU.max, [bimp, b_nc], [bsel])
                        k.tt('dve', v1[:], v1[:], ncore[:, 0:64], ALU.max, [bsel, b_nc], [bsel])
                        k.tt('dve', v1[:], v1[:], fv[:, 1, qt, :], ALU.min, [bsel, b_nc], [bsel])
                        k.tt('dve', v1[:], v1[:], ncore[:, 64:128], ALU.min, [bsel, b_nc], [bsel])
                        P.op('dve', lambda e: e.max(out=m8[:, 0:8], in_=v1[:]), [bsel], [bsel])
                        P.op('dve', lambda e: e.match_replace(out=v2[:], in_to_replace=m8[:, 0:8], in_values=v1[:], imm_value=-3.0e38), [bsel], [bsel])
                        P.op('dve', lambda e: e.max(out=m8[:, 8:16], in_=v2[:]), [bsel], [bsel])
                        k.ts('dve', v2[:], v1[:], m8[:, 15:16], None, ALU.is_ge, None, [bsel], [bsel])
                        k.ts('dve', selb[:], v1[:], -1.0e29, None, ALU.is_gt, None, [bsel], [bsel])
                        k.tt('dve', selb[:], selb[:], v2[:], ALU.mult, [bsel], [bsel])
                        k.ts('dve', selb[:], selb[:], -1.0, 30000.0, ALU.add, ALU.mult, [bsel], [bsel])
                        tb = tbank()
                        k.tr(banks[tb][0:64, 0:128], selb[:], ident[:], [bsel, b_const], [bbank[tb]])
                        k.cp('dve', selbT[:], banks[tb][0:64, 0:128], [bbank[tb]], [bselT])
                        for hh in range(8):
                            head = g * 8 + hh
                            sch, bsc, bO = sc_[:, hh, :], bsch[hh], bOh[hh]
                            sbk = 4 if hh % 2 == 0 else 2
                            wbk = 5 if hh % 2 == 0 else 3
                            kdiag = OWN0 // 128 + qt
                            tiles = []
                            for kt in range(0, kdiag + 1):
                                ex = [(eexp[:, kt * 128:(kt + 1) * 128], selbT[:], [b_nc, bselT])]
                                if kt == kdiag:
                                    ex.append((identb[:], cmk[:, 0, :], [b_const, b_nc]))
                                tiles.append((KsT[:, kt * 128:(kt + 1) * 128], bKs, ex, None, sbk, Vs1[:, kt, :], bVs, kt == 0, kt == kdiag))
                            for kt in range(kdiag - 4, kdiag + 1):
                                ex = []
                                if kt == kdiag - 4:
                                    ex.append((identb[:], cmk[:, 1, :], [b_const, b_nc]))
                                if kt == kdiag:
                                    ex.append((identb[:], cmk[:, 0, :], [b_const, b_nc]))
                                tiles.append((KwT[:, kt * 128:(kt + 1) * 128], bKw, ex, kvb if kt < OWN0 // 128 else None, wbk, Vw1[:, kt, :], bVw,
                                              kt == kdiag - 4, kt == kdiag))
                            pend = None
                            for (lk, blk_, ex, bias_, abk, vap, bv_, st_f, sp_f) in tiles:
                                et, bet = score_exp(lk, blk_, QTh[hh][:, qsl], bQh[hh], ex, bias_)
                                if pend is not None:
                                    pe_, pb_, pa_, pv_, pbv_, ps_, pp_ = pend
                                    k.mm(banks[pa_][:, 0:129], pe_[:], pv_, ps_, pp_, [pb_, pbv_], [bbank[pa_]])
                                pend = (et, bet, abk, vap, bv_, st_f, sp_f)
                            pe_, pb_, pa_, pv_, pbv_, ps_, pp_ = pend
                            k.mm(banks[pa_][:, 0:129], pe_[:], pv_, ps_, pp_, [pb_, pbv_], [bbank[pa_]])
                            k.ts('dve', sch[:, 3:4], banks[sbk][:, 128:129], 1e-30, None, ALU.max, None, [bbank[sbk]], [bsc])
                            k.recip(sch[:, 4:5], sch[:, 3:4], [bsc], [bsc])
                            k.ts('dve', sch[:, 4:5], sch[:, 4:5], gts[:, qt, head * 3 + 1:head * 3 + 2], None, ALU.mult, None, [b_g, bsc], [bsc])
                            k.stt(Oacc[:, hh, :], banks[sbk][:, 0:128], sch[:, 4:5], Oacc[:, hh, :], ALU.mult, ALU.add, [bbank[sbk], bsc, bO], [bO])
                            k.ts('dve', sch[:, 5:6], banks[wbk][:, 128:129], 1e-30, None, ALU.max, None, [bbank[wbk]], [bsc])
                            k.recip(sch[:, 6:7], sch[:, 5:6], [bsc], [bsc])
                            k.ts('dve', sch[:, 6:7], sch[:, 6:7], gts[:, qt, head * 3 + 2:head * 3 + 3], None, ALU.mult, None, [b_g, bsc], [bsc])
                            k.stt(Oacc[:, hh, :], banks[wbk][:, 0:128], sch[:, 6:7], Oacc[:, hh, :], ALU.mult, ALU.add, [bbank[wbk], bsc, bO], [bO])
                            oi = hh % 2
                            tb = tbank()
                            k.tr(banks[tb][:, 0:128], Oacc[:, hh, :], ident[:], [bO, b_const], [bbank[tb]])
                            k.cp('dve', o16[oi][:], banks[tb][:, 0:128], [bbank[tb]], [bo16[oi]])
                            k.dma('sp', OBT[head * 128:(head + 1) * 128, qsl], o16[oi][:], [bo16[oi]], ())
        if stage == 4 and DEBUG_OUT and not SKIP_NSA:
            dbg_ob = nc.dram_tensor("dbg_ob", [2048, NOWN], BF16, kind="ExternalOutput").ap()
            k.dma('sp', dbg_ob, OBT, (), ())

    def bcast_row(dst, src_fm, n, bsrc, bdst, name):
        rowd = k.dram([1, n * 128], F32, "row_" + name)
        t_ = P.dma('sp', rowd[0].rearrange("(c p) -> p c", p=128), src_fm, [bsrc], (), allow_slow_non_contiguous=True)
        brow = P.buf()
        brow.lw = t_
        k.dma('sp', dst, rowd[0].partition_broadcast(128), [brow], [bdst])

    if stage >= 5 and not SKIP_P5:
        wbg_in = din("w_branch_gdn", [1024, D])
        wbn_in = din("w_branch_nsa", [D, D])
        wout_in = din("w_out", [D, D])
        with k.scope():
            Wg = k.sb([128, 8, D], BF16, "Wg")
            Wn = k.sb([128, 16, D], BF16, "Wn")
            bWg, bWn = P.bufs(2)
            for kc in range(8):
                k.dma('pool', Wg[:, kc, :], wbg_in[kc * 128:(kc + 1) * 128, :], (), [bWg])
            for kc in range(16):
                k.dma('pool', Wn[:, kc, :], wbn_in[kc * 128:(kc + 1) * 128, :], (), [bWn])
            oat = [k.sb([128, 8, 512], BF16, "oat%d" % i) for i in range(2)]
            obt = [k.sb([128, 16, 512], BF16, "obt%d" % i) for i in range(2)]
            boat, bobt = P.bufs(2), P.bufs(2)
            ga = [k.sb([128, 512], F32, "ga%d" % i) for i in range(2)]
            gb = [k.sb([128, 512], F32, "gb%d" % i) for i in range(2)]
            y16 = [k.sb([128, 512], BF16, "y16_%d" % i) for i in range(2)]
            bga, bgb, by16 = P.bufs(2), P.bufs(2), P.bufs(2)
            OATv = OAT.rearrange("(kc p) t -> p kc t", p=128)
            OBTv = OBT.rearrange("(kc p) t -> p kc t", p=128)
            it = 0
            for tt in range(NOWN // 512):
                s2 = tt % 2
                tsl = slice(tt * 512, (tt + 1) * 512)
                k.dma('sp', oat[s2][:], OATv[:, :, tsl], (), [boat[s2]])
                k.dma('sp', obt[s2][:], OBTv[:, :, tsl], (), [bobt[s2]])
                for ct in range(16):
                    s = it % 2
                    it += 1
                    bA, bB = (0, 1) if s == 0 else (2, 3)
                    csl = slice(ct * 128, (ct + 1) * 128)
                    k.dma('sp', ga[s][:], PF[6144 + ct * 128:6144 + (ct + 1) * 128, OWN0 + tt * 512:OWN0 + (tt + 1) * 512], (), [bga[s]])
                    k.dma('sp', gb[s][:], PF[8192 + ct * 128:8192 + (ct + 1) * 128, OWN0 + tt * 512:OWN0 + (tt + 1) * 512], (), [bgb[s]])
                    k.act(ga[s][:], ga[s][:], AF.Sigmoid, [bga[s]], [bga[s]])
                    k.act(gb[s][:], gb[s][:], AF.Sigmoid, [bgb[s]], [bgb[s]])
                    for kc in range(8):
                        k.mm(banks[bA][:, :], Wg[:, kc, csl], oat[s2][:, kc, :], kc == 0, kc == 7, [bWg, boat[s2]], [bbank[bA]])
                    for kc in range(16):
                        k.mm(banks[bB][:, :], Wn[:, kc, csl], obt[s2][:, kc, :], kc == 0, kc == 15, [bWn, bobt[s2]], [bbank[bB]])
                    k.tt('dve', ga[s][:], ga[s][:], banks[bA][:, :], ALU.mult, [bga[s], bbank[bA]], [bga[s]])
                    k.tt('dve', gb[s][:], gb[s][:], banks[bB][:, :], ALU.mult, [bgb[s], bbank[bB]], [bgb[s]])
                    k.tt('pool', y16[s][:], ga[s][:], gb[s][:], ALU.add, [bga[s], bgb[s]], [by16[s]])
                    k.dma('sp', YT[csl, tsl], y16[s][:], [by16[s]], ())
        with k.scope():
            Wo = k.sb([128, 16, D], BF16, "Wo")
            g1b = k.sb([128, D], F32, "g1b")
            bWo, bg1 = P.bufs(2)
            for kc in range(16):
                k.dma('pool', Wo[:, kc, :], wout_in[kc * 128:(kc + 1) * 128, :], (), [bWo])
            bcast_row(g1b[:], modT[:, 32:48], 16, b_mod, bg1, "g1")
            yt = [k.sb([128, 16, 128], BF16, "yt%d" % i) for i in range(2)]
            xo = [k.sb([128, D], F32, "xo%d" % i) for i in range(2)]
            zt_ = [k.sb([128, 512], F32, "zt5_%d" % i) for i in range(2)]
            byt, bxo, bzt = P.bufs(2), P.bufs(2), P.bufs(2)
            YTv = YT.rearrange("(kc p) t -> p kc t", p=128)
            it = 0
            for t in range(NOWN // 128):
                s = t % 2
                k.dma('sp', yt[s][:], YTv[:, :, t * 128:(t + 1) * 128], (), [byt[s]])
                k.dma('sp', xo[s][:], xl[OWN0 + t * 128:OWN0 + (t + 1) * 128, :], (), [bxo[s]])
                for cb_ in range(4):
                    z2 = it % 2
                    bk = it % 4
                    it += 1
                    csl = slice(cb_ * 512, (cb_ + 1) * 512)
                    for kc in range(16):
                        k.mm(banks[bk][:, :], yt[s][:, kc, :], Wo[:, kc, csl], kc == 0, kc == 15, [byt[s], bWo], [bbank[bk]])
                    k.tt('dve', zt_[z2][:], banks[bk][:, :], g1b[:, csl], ALU.mult, [bbank[bk], bg1], [bzt[z2]])
                    k.tt('pool', xo[s][:, csl], xo[s][:, csl], zt_[z2][:], ALU.add, [bxo[s], bzt[z2]], [bxo[s]])
                k.dma('sp', X1[t * 128:(t + 1) * 128, :], xo[s][:], [bxo[s]], ())
        P.barrier()
        if stage == 5 and DEBUG_OUT:
            dbg_x1 = nc.dram_tensor("dbg_x1", [NOWN, D], F32, kind="ExternalOutput").ap()
            k.dma('sp', dbg_x1, X1, (), ())

    if stage == 6 and DEBUG_OUT:
        X2dbg = nc.dram_tensor("dbg_x2", [NOWN, D], F32, kind="ExternalOutput").ap()
    if stage >= 6:
        n2_in = din("norm2_w", [KC, 128])
        wq_in = din("peer_wq", [D, D])
        pk_in = [din("peer_keys1", [8, 128, 128]), din("peer_keys2", [8, 128, 128])]
        fnw6_in = din("final_norm_w", [1, D])
        H2T = k.dram([KC, 128, NOWN], BF16, "H2T")
        NEB = 64 if PEER_EB is None else PEER_EB
        with k.scope():
            A2 = k.sb([128, KC], F32, "A2")
            n2T = k.sb([128, KC], F32, "n2T")
            keysT = k.sb([128, 16, 128], BF16, "keysT")
            bA2, bn2, bg2, bfn, bkT = P.bufs(5)
            load_fm(n2T[:], n2_in, KC, bn2)
            k.stt(A2[:], modT[:, 64:80], 1.0, n2T[:], ALU.add, ALU.mult, [b_mod, bn2], [bA2])
            g2row = k.dram([1, D], F32, "row_g2")
            k.dma('sp', g2row[0].rearrange("(c p) -> p c", p=128), modT[:, 80:96], [b_mod], (), allow_slow_non_contiguous=True)
            with k.scope():
                kt_ = [k.sb([128, 128], F32, "kraw%d" % i) for i in range(2)]
                bkr = P.bufs(2)
                for hh2 in range(16):
                    s = hh2 % 2
                    k.dma('sp', kt_[s][:], pk_in[hh2 % 2][hh2 // 2], (), [bkr[s]])
                    k.tr(banks[s][:, 0:128], kt_[s][:], ident[:], [bkr[s], b_const], [bbank[s]])
                    k.cp('dve', keysT[:, hh2, :], banks[s][:, 0:128], [bbank[s]], [bkT])
            with k.scope():
                xt = [k.sb([128, D], F32, "x6t%d" % i) for i in range(2)]
                xs = [k.sb([128, D], BF16, "x6s%d" % i) for i in range(2)]
                st = [k.sb([128, 4], F32, "s6t%d" % i) for i in range(2)]
                ho = [k.sb([128, KC, 128], BF16, "h6o%d" % i) for i in range(2)]
                bxt, bxs, bst, bho = P.bufs(2), P.bufs(2), P.bufs(2), P.bufs(2)
                for t in range(NOWN // 128):
                    s = t % 2
                    k.dma('sp', xt[s][:], X1[t * 128:(t + 1) * 128, :], (), [bxt[s]])
                    k.act(xs[s][:], xt[s][:], AF.Square, [bxt[s]], [bxs[s], bst[s]], accum=st[s][:, 0:1])
                    k.ts('dve', st[s][:, 1:2], st[s][:, 0:1], 1.0 / D, 1e-6, ALU.mult, ALU.add, [bst[s]], [bst[s]])
                    k.act(st[s][:, 2:3], st[s][:, 1:2], AF.Sqrt, [bst[s]], [bst[s]])
                    k.recip(st[s][:, 3:4], st[s][:, 2:3], [bst[s]], [bst[s]])
                    k.ts('dve', xs[s][:], xt[s][:], st[s][:, 3:4], None, ALU.mult, None, [bxt[s], bst[s]], [bxs[s]])
                    for kc in range(KC):
                        pi = kc % 2
                        sl = slice((kc // 2 % 4) * 128, (kc // 2 % 4) * 128 + 128)
                        k.tr(pbf[pi][:, sl], xs[s][:, kc * 128:(kc + 1) * 128], identb[:], [bxs[s], b_const], [bpbf[pi]])
                        if kc % 2 == 0:
                            k.act(ho[s][:, kc, :], pbf[pi][:, sl], AF.Identity, [bpbf[pi], bA2, b_mod], [bho[s]],
                                  bias=modT[:, 48 + kc:49 + kc], scale=A2[:, kc:kc + 1])
                        else:
                            k.ts('dve', ho[s][:, kc, :], pbf[pi][:, sl], A2[:, kc:kc + 1], modT[:, 48 + kc:49 + kc],
                                 ALU.mult, ALU.add, [bpbf[pi], bA2, b_mod], [bho[s]])
                    k.dma('sp', H2T[:, :, t * 128:(t + 1) * 128].rearrange("c p t -> p c t"), ho[s][:], [bho[s]], ())
            P.barrier()
            wqv = wq_in.rearrange("(kc p) c -> p kc c", p=128)
            outv6 = out_d.rearrange("(t p) d -> t p d", p=128)
            for tg in range(4 if PEER_TG is None else PEER_TG):
                with k.scope():
                    h2g = k.sb([128, KC, 512], BF16, "h2g")
                    stile = k.sb([128, 4, 16, 128], F32, "stile")
                    Bt = k.sb([128, 4, 8, 128], F32, "Bt")
                    tau = k.sb([128, 4, 8], F32, "tau")
                    OUTacc = k.sb([128, 4, D], F32, "OUTacc")
                    bh2, bst_, bBt, btau, bOut = P.bufs(5)
                    k.dma('sp', h2g[:], H2T[:, :, tg * 512:(tg + 1) * 512].rearrange("c p t -> p c t"), (), [bh2])
                    k.memset('pool', OUTacc[:], 0.0, [bOut])
                    with k.scope():
                        wqb = [k.sb([128, KC, 128], BF16, "wqb%d" % i) for i in range(2)]
                        qh = [k.sb([128, 512], BF16, "qh%d" % i) for i in range(2)]
                        bwq, bqh = P.bufs(2), P.bufs(2)
                        for hh2 in range(16):
                            s = hh2 % 2
                            k.dma('pool', wqb[s][:], wqv[:, :, hh2 * 128:(hh2 + 1) * 128], (), [bwq[s]])
                            for kc in range(KC):
                                k.mm(banks[s][:, :], wqb[s][:, kc, :], h2g[:, kc, :], kc == 0, kc == KC - 1, [bwq[s], bh2], [bbank[s]])
                            k.cp('act', qh[s][:], banks[s][:, :], [bbank[s]], [bqh[s]])
                            for tl in range(4):
                                k.mm(banks[2 + s][:, tl * 128:(tl + 1) * 128], qh[s][:, tl * 128:(tl + 1) * 128], keysT[:, hh2, :], True, True,
                                     [bqh[s], bkT], [bbank[2 + s]])
                            k.cp('dve', stile[:, :, hh2, :], banks[2 + s][:, :].rearrange("p (t j) -> p t j", t=4), [bbank[2 + s]], [bst_])
                    with k.scope():
                        SC = []
                        for ci in range(2):
                            SC.append(dict(v16=k.sb([128, 2, 16], F32, "v16_%d" % ci), vv=k.sb([128, 2, 128], F32, "vv_%d" % ci),
                                           cand=k.sb([128, 16, 16], F32, "cand_%d" % ci), cv=k.sb([128, 256], F32, "cv_%d" % ci),
                                           c16=k.sb([128, 16], F32, "c16_%d" % ci), sm=k.sb([128, 8], F32, "sm_%d" % ci), b=P.buf()))

                        def p3_chain(tl, h, S_):
                            v16, vv, cand, cv, c16, sm, bs = S_["v16"], S_["vv"], S_["cand"], S_["cv"], S_["c16"], S_["sm"], S_["b"]
                            ops = []
                            for half in range(2):
                                src = stile[:, tl, 2 * h + half, :]
                                ops.append(lambda src=src, half=half: P.op('dve', lambda e: e.max(out=v16[:, half, 0:8], in_=src), [bst_], [bs]))
                            for half in range(2):
                                src = stile[:, tl, 2 * h + half, :]
                                ops.append(lambda src=src, half=half: P.op('dve', lambda e: e.match_replace(out=vv[:, half, :], in_to_replace=v16[:, half, 0:8], in_values=src, imm_value=-3.0e38), [bst_, bs], [bs]))
                            for half in range(2):
                                ops.append(lambda half=half: P.op('dve', lambda e: e.max(out=v16[:, half, 8:16], in_=vv[:, half, :]), [bs], [bs]))
                            ops.append(lambda: k.tt('dve', cand[:], v16[:, 1, :].unsqueeze(1).to_broadcast([128, 16, 16]),
                                                    v16[:, 0, :].unsqueeze(2).to_broadcast([128, 16, 16]), ALU.add, [bs], [bs]))
                            cf = cand[:].rearrange("p a b -> p (a b)")
                            ops.append(lambda: P.op('dve', lambda e: e.max(out=c16[:, 0:8], in_=cf), [bs], [bs]))
                            ops.append(lambda: P.op('dve', lambda e: e.match_replace(out=cv[:], in_to_replace=c16[:, 0:8], in_values=cf, imm_value=-3.0e38), [bs], [bs]))
                            ops.append(lambda: P.op('dve', lambda e: e.max(out=c16[:, 8:16], in_=cv[:]), [bs], [bs]))
                            ops.append(lambda: k.ts('dve', sm[:, 0:1], c16[:, 0:1], -1.0, None, ALU.mult, None, [bs], [bs]))
                            ops.append(lambda: k.act(cv[:, 0:16], c16[:], AF.Exp, [bs], [bs], bias=sm[:, 0:1], accum=sm[:, 1:2]))
                            ops.append(lambda: k.act(sm[:, 2:3], sm[:, 1:2], AF.Ln, [bs], [bs]))
                            ops.append(lambda: k.tt('dve', sm[:, 3:4], sm[:, 0:1], sm[:, 2:3], ALU.subtract, [bs], [bs]))
                            ops.append(lambda: k.ts('dve', Bt[:, tl, h, :], stile[:, tl, 2 * h, :], sm[:, 3:4], None, ALU.add, None, [bst_, bs], [bBt]))
                            ops.append(lambda: k.act(sm[:, 4:5], c16[:, 15:16], AF.Exp, [bs], [bs], bias=sm[:, 3:4]))
                            ops.append(lambda: k.ts('dve', tau[:, tl, h:h + 1], sm[:, 4:5], 0.99999, None, ALU.mult, None, [bs], [btau]))
                            return ops

                        pairs = [(tl, h) for tl in range(4) for h in range(8)]
                        for pi_ in range(0, len(pairs), 2):
                            ca = p3_chain(pairs[pi_][0], pairs[pi_][1], SC[0])
                            cb2 = p3_chain(pairs[pi_ + 1][0], pairs[pi_ + 1][1], SC[1])
                            for oa, ob_ in zip(ca, cb2):
                                oa()
                                ob_()
                    with k.scope():
                        ub = [k.sb([128, KC, 256], BF16, "ub%d" % i) for i in range(2)]
                        vb = [k.sb([128, 2, D], BF16, "vb%d" % i) for i in range(2)]
                        bub, bvb = P.bufs(2), P.bufs(2)
                        gx_ = [k.sb([128, 256], F32, "gx%d" % i) for i in range(2)]
                        gy_ = [k.sb([128, 256], F32, "gy%d" % i) for i in range(2)]
                        es_ = [k.sb([128, 8, 128], F32, "es%d" % i) for i in range(2)]
                        mk_ = [k.sb([128, 8, 128], F32, "mk%d" % i) for i in range(2)]
                        coef_ = [k.sb([128, 2, 128], F32, "coef%d" % i) for i in range(2)]
                        G16_ = [k.sb([128, 256], BF16, "G16_%d" % i) for i in range(2)]
                        GT_ = [[k.sb([128, 128], BF16, "GT%d_%d" % (p_, i)) for i in range(2)] for p_ in range(2)]
                        bgx_, bgy_, bmk_, bG16_ = P.bufs(2), P.bufs(2), P.bufs(2), P.bufs(2)
                        bes_ = [P.bufs(8) for p_ in range(2)]
                        bcoef_ = [P.bufs(2) for p_ in range(2)]
                        bOutS = [[P.buf() for c_ in range(4)] for t_ in range(4)]
                        bGT_ = [P.bufs(2) for p_ in range(2)]
                        pit = 0
                        def head(eb, tl, par, s):
                            gx, gy, coef, G16 = gx_[par], gy_[par], coef_[par], G16_[par]
                            bgx, bgy, bcoef, bG16 = bgx_[par], bgy_[par], bcoef_[par], bG16_[par]
                            for kc in range(KC):
                                k.mm(banks[par][:, 0:256], h2g[:, kc, tl * 128:(tl + 1) * 128], ub[s][:, kc, :], kc == 0, kc == KC - 1,
                                     [bh2, bub[s]], [bbank[par]])
                            k.cp('act', gx[:], banks[par][:, 0:256], [bbank[par]], [bgx])
                            k.tt('pool', gy[:], gx[:], gx[:], ALU.mult, [bgx], [bgy])
                            k.ts('pool', gy[:], gy[:], 0.044715, 1.0, ALU.mult, ALU.add, [bgy], [bgy])
                            k.tt('pool', gy[:], gy[:], gx[:], ALU.mult, [bgy, bgx], [bgy])
                            k.act(gy[:], gy[:], AF.Sigmoid, [bgy], [bgy], scale=1.5957691216057308)
                            k.tt('pool', gy[:], gy[:], gx[:], ALU.mult, [bgy, bgx], [bgy])
                            for il in range(2):
                                i_ = 2 * eb + il
                                for h in range(8):
                                    k.act(es_[il][:, h, :], stile[:, tl, 2 * h + 1, :], AF.Exp, [bst_, bBt], [bes_[il][h]], bias=Bt[:, tl, h, i_:i_ + 1])
                            taub = tau[:, tl, :].unsqueeze(2).to_broadcast([128, 8, 128])
                            for il in range(2):
                                k.tt('dve', mk_[il][:], es_[il][:], taub, ALU.is_ge, bes_[il] + [btau], [bmk_[il]])
                            for il in range(2):
                                k.tt('dve', mk_[il][:], mk_[il][:], es_[il][:], ALU.mult, [bmk_[il]] + bes_[il], [bmk_[il]])
                            for il in range(2):
                                k.tt('dve', mk_[il][:, 0:4, :], mk_[il][:, 0:4, :], mk_[il][:, 4:8, :], ALU.add, [bmk_[il]], [bmk_[il]])
                            for il in range(2):
                                k.tt('dve', mk_[il][:, 0:2, :], mk_[il][:, 0:2, :], mk_[il][:, 2:4, :], ALU.add, [bmk_[il]], [bmk_[il]])
                            for il in range(2):
                                k.tt('dve', coef[:, il, :], mk_[il][:, 0, :], mk_[il][:, 1, :], ALU.add, [bmk_[il]], [bcoef[il]])
                            k.tt('dve', G16[:], gy[:], coef[:].rearrange("p i j -> p (i j)"), ALU.mult, [bgy] + bcoef, [bG16])

                        def tail(eb, tl, par, s):
                            G16, GT, bG16, bGT = G16_[par], GT_[par], bG16_[par], bGT_[par]
                            for il in range(2):
                                k.tr(pbf[il][:, par * 128:(par + 1) * 128], G16[:, il * 128:(il + 1) * 128], identb[:], [bG16, b_const], [bpbf[il]])
                                k.cp('act', GT[il][:], pbf[il][:, par * 128:(par + 1) * 128], [bpbf[il]], [bGT[il]])
                            for cb_ in range(4):
                                bk = 2 + cb_
                                csl = slice(cb_ * 512, (cb_ + 1) * 512)
                                k.mm(banks[bk][:, :], GT[0][:], vb[s][:, 0, csl], True, False, [bGT[0], bvb[s]], [bbank[bk]])
                                k.mm(banks[bk][:, :], GT[1][:], vb[s][:, 1, csl], False, True, [bGT[1], bvb[s]], [bbank[bk]])
                                k.tt('dve', OUTacc[:, tl, csl], OUTacc[:, tl, csl], banks[bk][:, :], ALU.add, [bOut, bOutS[tl][cb_], bbank[bk]], [bOutS[tl][cb_]])

                        prev = None
                        pit = 0
                        for eb in range(NEB):
                            s = eb % 2
                            k.dma('sp', ub[s][:], UBd[eb], [bconvU[eb]], [bub[s]])
                            k.dma('sp', vb[s][:], VBd[eb], [bconvV[eb]], [bvb[s]])
                            for tl in range(4):
                                par = pit % 2
                                pit += 1
                                head(eb, tl, par, s)
                                if prev is not None:
                                    tail(*prev)
                                prev = (eb, tl, par, s)
                        tail(*prev)
                    with k.scope():
                        g2b = k.sb([128, D], F32, "g2b")
                        fnw6 = k.sb([128, D], F32, "fnw6")
                        k.dma('sp', g2b[:], g2row[0].partition_broadcast(128), (), [bg2])
                        k.dma('sp', fnw6[:], fnw6_in[0].partition_broadcast(128), (), [bfn])
                        x1t = [k.sb([128, D], F32, "x1t%d" % i) for i in range(2)]
                        fj6 = [k.sb([128, D], BF16, "fj6%d" % i) for i in range(2)]
                        fs6 = [k.sb([128, 4], F32, "fs6%d" % i) for i in range(2)]
                        bx1t, bfj6, bfs6 = P.bufs(2), P.bufs(2), P.bufs(2)
                        for tl in range(4):
                            s = tl % 2
                            t = tg * 4 + tl
                            k.dma('sp', x1t[s][:], X1[t * 128:(t + 1) * 128, :], (), [bx1t[s]])
                            k.tt('dve', OUTacc[:, tl, :], OUTacc[:, tl, :], g2b[:], ALU.mult, [bOut, bg2] + bOutS[tl], [bOut])
                            k.tt('dve', x1t[s][:], x1t[s][:], OUTacc[:, tl, :], ALU.add, [bx1t[s], bOut], [bx1t[s]])
                            if stage == 6 and DEBUG_OUT:
                                k.dma('sp', X2dbg[t * 128:(t + 1) * 128, :], x1t[s][:], [bx1t[s]], ())
                            k.act(fj6[s][:], x1t[s][:], AF.Square, [bx1t[s]], [bfj6[s], bfs6[s]], accum=fs6[s][:, 0:1])
                            k.ts('dve', fs6[s][:, 1:2], fs6[s][:, 0:1], 1.0 / D, 1e-6, ALU.mult, ALU.add, [bfs6[s]], [bfs6[s]])
                            k.act(fs6[s][:, 2:3], fs6[s][:, 1:2], AF.Sqrt, [bfs6[s]], [bfs6[s]])
                            k.recip(fs6[s][:, 3:4], fs6[s][:, 2:3], [bfs6[s]], [bfs6[s]])
                            k.stt(x1t[s][:], x1t[s][:], fs6[s][:, 3:4], fnw6[:], ALU.mult, ALU.mult, [bx1t[s], bfs6[s], bfn], [bx1t[s]])
                            k.dma('sp', outv6[t], x1t[s][:], [bx1t[s]], ())

    if stage < 6:
      pass
    fnw_in = din("final_norm_w", [1, D]) if stage < 6 else None
    fnw = k.sb([128, D], F32, "fnw")
    b_fnw = P.buf()
    if stage < 6:
        k.dma('sp', fnw[:], fnw_in[0].partition_broadcast(128), (), [b_fnw])
    X2v = xl.rearrange("(t p) d -> t p d", p=128)
    outv = out_d.rearrange("(t p) d -> t p d", p=128)
    ft = [k.sb([128, D], F32, "ft%d" % i) for i in range(2)]
    fj = [k.sb([128, D], BF16, "fj%d" % i) for i in range(2)]
    fs = [k.sb([128, 4], F32, "fs%d" % i) for i in range(2)]
    bft, bfj, bfs = P.bufs(2), P.bufs(2), P.bufs(2)
    for t in range(NOWN // 128 if stage < 6 else 0):
        s = t % 2
        k.dma('sp', ft[s][:], X2v[OWN0 // 128 + t], (), [bft[s]])
        k.act(fj[s][:], ft[s][:], AF.Square, [bft[s]], [bfj[s], bfs[s]], accum=fs[s][:, 0:1])
        k.ts('dve', fs[s][:, 1:2], fs[s][:, 0:1], 1.0 / D, 1e-6, ALU.mult, ALU.add, [bfs[s]], [bfs[s]])
        k.act(fs[s][:, 2:3], fs[s][:, 1:2], AF.Sqrt, [bfs[s]], [bfs[s]])
        k.recip(fs[s][:, 3:4], fs[s][:, 2:3], [bfs[s]], [bfs[s]])
        k.stt(ft[s][:], ft[s][:], fs[s][:, 3:4], fnw[:], ALU.mult, ALU.mult, [bft[s], bfs[s], b_fnw], [bft[s]])
        k.dma('sp', outv[t], ft[s][:], [bft[s]], ())

    with nc.Block() as block:
        P.emit(block)
    k.es.close()
    return nc


def _gconst():
    NEGM = -30000.0
    r = np.arange(128)[:, None]
    c = np.arange(128)[None, :]
    g = np.zeros((128, 5, 128), np.float32)
    g[:, 4, :] = (r // 32 == c // 32)
    g[:, 0, :] = (r <= c)
    g[:, 1, :] = np.where(r > c, 0.0, NEGM)
    g[:, 2, :] = np.where(c > r, 0.0, NEGM)
    g[:, 3, :] = np.where(c >= r, 0.0, NEGM)
    return g


GCONST = _gconst()


def _nsa_consts():
    NEGM = -30000.0
    c = {}
    n128 = np.zeros((128, 385), np.float32)
    for m in range(16):
        n128[m + 16, m] = -1.0
    for m in range(16, 32):
        n128[m - 16, m] = 1.0
    half = 16
    inv_freq = (500000.0 ** (-np.arange(half, dtype=np.float32) / half)).astype(np.float32)
    for d in range(32):
        n128[d, 128] = inv_freq[d % 16]
    r = np.arange(128)[:, None]
    q = np.arange(128)[None, :]
    n128[:, 129:257] = np.where(r <= q, 0.0, NEGM)
    n128[:, 257:385] = np.where(r > q, 0.0, NEGM)
    c["nsa_c128"] = n128
    u = np.arange(LOC)[None, :]
    c["maskB"] = np.where(16 * r + 31 <= u, 0.0, NEGM).astype(np.float32)
    c["eexp"] = (np.arange(LOC)[None, :] // 64 == np.arange(64)[:, None]).astype(np.float32)
    ovl = np.zeros((128, 2, 65), np.float32)
    for j in range(2):
        for nl in range(128):
            n = j * 128 + nl
            if n >= 255:
                continue
            for sblk in range(64):
                if 16 * n < 64 * sblk + 64 and 16 * n + 32 > 64 * sblk:
                    ovl[nl, j, sblk] = 1.0
            ovl[nl, j, 64] = 1.0
    c["ovl"] = ovl
    fvv = np.zeros((128, 2, 16, 64), np.float32)
    for qt in range(16):
        for qq in range(128):
            t = OWN0 + qt * 128 + qq
            cur = t // 64
            for sblk in range(64):
                fb = 0.0
                if sblk == cur:
                    fb = 2.0e9
                elif sblk == cur - 1:
                    fb = 3.0e9
                fvv[qq, 0, qt, sblk] = fb
                fvv[qq, 1, qt, sblk] = 3.0e38 if 64 * sblk <= t else -1.0e30
    c["fbigvis"] = fvv
    return c


NSA_CONSTS = _nsa_consts()


def _nsa_core(half):
    a = np.zeros((128, 129), np.float32)
    first = 0 if half == 1 else 32
    a[:, first] = 1.0e9
    a[:, 64:128] = 3.0e38
    if half == 0:
        a[:, 64:64 + 32] = -1.0e30
        a[:, 128] = -30000.0
    return a


def _pos_loc(inp, b, half):
    p = np.asarray(inp['positions'], np.int32)[b]
    out = np.zeros((1, LOC), np.int32)
    if half == 1:
        out[0] = p
    else:
        out[0, OWN0:] = p[:NOWN]
    return out


PEER_UT_CACHE = {}


def make_in_maps(inp):
    x = np.asarray(inp['x'], np.float32)
    c = np.asarray(inp['c'], np.float32)
    maps = []
    ident = np.eye(128, dtype=np.float32)
    for core in range(8):
        b, half = core // 2, core % 2
        xl = np.zeros((LOC, D), np.float32)
        if half == 0:
            xl[OWN0:] = x[b, :NOWN]
        else:
            xl[:] = x[b]
        m = {
            "xl": xl,
            "cb": np.ascontiguousarray(c[b].reshape(KC, 128)),
            "ada_w": np.ascontiguousarray(inp['ada_w'][0]),
            "ada_b": np.ascontiguousarray(np.asarray(inp['ada_b'][0]).reshape(96, 128)),
            "norm1_w": np.ascontiguousarray(np.asarray(inp['norm1_w'][0]).reshape(KC, 128)),
            "w_in": np.ascontiguousarray(inp['w_in'][0]),
            "ident": ident,
            "pvalid": np.full((128, 1), float(half), np.float32),
            "w_branch_gdn": np.ascontiguousarray(inp['w_branch_gdn'][0], np.float32),
            "w_branch_nsa": np.ascontiguousarray(inp['w_branch_nsa'][0], np.float32),
            "w_out": np.ascontiguousarray(inp['w_out'][0], np.float32),
            "norm2_w": np.ascontiguousarray(np.asarray(inp['norm2_w'][0], np.float32).reshape(KC, 128)),
            "peer_wq": np.ascontiguousarray(inp['peer_wq'][0], np.float32),
            "peer_keys1": np.ascontiguousarray(inp['peer_keys1'][0], np.float32),
            "peer_keys2": np.ascontiguousarray(inp['peer_keys2'][0], np.float32),
            "peer_uT": PEER_UT_CACHE.get(id(inp['peer_u'])) if id(inp['peer_u']) in PEER_UT_CACHE else PEER_UT_CACHE.setdefault(id(inp['peer_u']), np.ascontiguousarray(np.asarray(inp['peer_u'][0], np.float32).T)),
            "peer_v": np.ascontiguousarray(inp['peer_v'][0], np.float32),
            "gconst": GCONST,
            "pos": np.ascontiguousarray(_pos_loc(inp, b, half)),
            "nsa_core": _nsa_core(half),
            "cmp_pos_k": np.ascontiguousarray(inp['cmp_pos_k'][0], np.float32), "cmp_pos_v": np.ascontiguousarray(inp['cmp_pos_v'][0], np.float32),
            "cmp_w1_k": np.ascontiguousarray(inp['cmp_w1_k'][0], np.float32), "cmp_w1_v": np.ascontiguousarray(inp['cmp_w1_v'][0], np.float32),
            "cmp_w2_k": np.ascontiguousarray(inp['cmp_w2_k'][0], np.float32), "cmp_w2_v": np.ascontiguousarray(inp['cmp_w2_v'][0], np.float32),
            **NSA_CONSTS,
            "gdn_conv_w": np.ascontiguousarray(np.asarray(inp['gdn_conv_w'][0], np.float32).reshape(96, 128)),
            "gdn_A_log": np.ascontiguousarray(np.broadcast_to(np.asarray(inp['gdn_A_log'][0], np.float32)[None, :], (32, 8))),
            "gdn_dt_bias": np.ascontiguousarray(np.broadcast_to(np.asarray(inp['gdn_dt_bias'][0], np.float32)[None, :], (32, 8))),
            "gdn_norm_w": np.ascontiguousarray(np.asarray(inp['gdn_norm_w'][0], np.float32).reshape(1, 128)),
            "final_norm_w": np.ascontiguousarray(np.asarray(inp['final_norm_w'], np.float32).reshape(1, D)),
        }
        maps.append(m)
    return maps


def kernel(**inputs):
    nc = build_program(6)
    maps = make_in_maps(inputs)
    res = run_bass_kernel_spmd(nc, maps, core_ids=list(range(8)))
    out = np.zeros((4, SEQ, D), np.float32)
    for core in range(8):
        b, half = core // 2, core % 2
        out[b, half * NOWN:(half + 1) * NOWN] = res.results[core]["out"]
    return out
```

```python
import numpy as np
from contextlib import ExitStack
import concourse.bass as bass
import concourse.mybir as mybir
from concourse.bass_utils import run_bass_kernel_spmd

F32 = mybir.dt.float32
BF16 = mybir.dt.bfloat16
I32 = mybir.dt.int32
ALU = mybir.AluOpType
AF = mybir.ActivationFunctionType
AX = mybir.AxisListType

ENGS = ['pe', 'act', 'dve', 'pool', 'sp']

D = 2048
SEQ = 4096
LOC = 4096
OWN0 = 2048
NOWN = 2048
D_IN = 11840
KC = 16
DEBUG_OUT = False
GDN_ONE_HEAD = False
FROM_REF = False
GDN_SUB = 9
PF_ROWS = 80 * 128
GDN_WAVES = None
GDN_STEPS = 10 ** 9
NSA_GROUPS_RUN = None
NSA_QT_RUN = None
SKIP_GDN = False
SKIP_NSA = False
SKIP_P5 = False
REF_IN = set()
PEER_EB = None
PEER_TG = None


class Buf:
    __slots__ = ('name', 'lw', 'rd', 'excl')

    def __init__(self, name):
        self.name = name
        self.lw = None
        self.rd = {}
        self.excl = False


class Prog:
    def __init__(self, nc, n_dma_sems=40):
        self.nc = nc
        self.ops = {e: [] for e in ENGS}
        self.cnt = {e: 0 for e in ENGS}
        self.esem = {e: nc.alloc_semaphore(name="es_" + e) for e in ENGS}
        self.dsem = [nc.alloc_semaphore(name="ds_%d" % i) for i in range(n_dma_sems)]
        self.dval = [0] * n_dma_sems
        self.dnext = 0
        self.bsem = [nc.alloc_semaphore(name="bs_%d" % i) for i in range(8)]
        self.bval = [0] * 8
        self.bnext = 0
        self.waited = {e: {} for e in ENGS}
        self.pend = {e: [] for e in ENGS}
        self.nbuf = 0

    def barrier(self):
        snap = [(('e', o), self.cnt[o]) for o in ENGS if self.cnt[o] > 0]
        snap += [(('d', i), self.dval[i]) for i in range(len(self.dsem)) if self.dval[i] > 0]
        for e in ENGS:
            self.pend[e] = snap

    def _pending(self, eng, waits):
        w = self.waited[eng]
        for key, val in self.pend[eng]:
            if key == ('e', eng):
                continue
            if w.get(key, 0) < val:
                w[key] = val
                waits.append((self._sem(key), val))
        self.pend[eng] = []

    def buf(self, name=None):
        self.nbuf += 1
        return Buf(name or ("b%d" % self.nbuf))

    def bufs(self, n):
        return [self.buf() for _ in range(n)]

    def _sem(self, key):
        if key[0] == 'e':
            return self.esem[key[1]]
        return self.dsem[key[1]] if key[0] == 'd' else self.bsem[key[1]]

    def _deps(self, eng, reads, writes):
        need = {}

        def add(tok, raw):
            if tok is None:
                return
            key, val, teng = tok
            if teng == eng and eng == 'pe':
                return
            if need.get(key, 0) < val:
                need[key] = val

        for b in reads:
            add(b.lw, True)
            if b.excl:
                for t in b.rd.values():
                    if t[2] != eng:
                        add(t, True)
        for b in writes:
            add(b.lw, False)
            for t in b.rd.values():
                add(t, False)
        waits = []
        w = self.waited[eng]
        for key, val in need.items():
            if w.get(key, 0) < val:
                w[key] = val
                waits.append((self._sem(key), val))
        return waits

    def _mark(self, tok, reads, writes):
        for b in reads:
            b.rd[tok[0]] = tok
        for b in writes:
            b.lw = tok
            b.rd = {}

    def op(self, eng, fn, reads=(), writes=()):
        waits = self._deps(eng, reads, writes)
        self._pending(eng, waits)
        self.cnt[eng] += 1
        tok = (('e', eng), self.cnt[eng], eng)
        self.ops[eng].append((waits, fn, (self.esem[eng], 1)))
        self._mark(tok, reads, writes)
        return tok

    def dma(self, eng, out, in_, reads=(), writes=(), bg=False, **kw):
        if bg:
            sems, vals, idx, kk = self.bsem, self.bval, self.bnext, 'b'
            self.bnext = (self.bnext + 1) % len(self.bsem)
        else:
            sems, vals, idx, kk = self.dsem, self.dval, self.dnext, 'd'
            self.dnext = (self.dnext + 1) % len(self.dsem)
        waits = self._deps(eng, reads, writes)
        self._pending(eng, waits)
        key = (kk, idx)
        w = self.waited[eng]
        if w.get(key, 0) < vals[idx]:
            w[key] = vals[idx]
            waits.append((sems[idx], vals[idx]))
        vals[idx] += 16
        tok = (key, vals[idx], None)
        sem_ = sems[idx]
        self.ops[eng].append((waits, (lambda e: e.dma_start(out=out, in_=in_, **kw)), (sem_, 16)))
        self._mark(tok, reads, writes)
        return tok

    def emit(self, block):
        fin = [(self.dsem[i], self.dval[i]) for i in range(len(self.dsem)) if self.dval[i] > 0]
        fin += [(self.bsem[i], self.bval[i]) for i in range(len(self.bsem)) if self.bval[i] > 0]
        fin += [(self.esem[e], self.cnt[e]) for e in ENGS if e != 'sp' and self.cnt[e] > 0]

        def run(e, name):
            for waits, fn, inc in self.ops[name]:
                for s, v in waits:
                    e.wait_ge(s, v)
                ins = fn(e)
                ins.then_inc(inc[0], inc[1])
            if name == 'sp':
                for s, v in fin:
                    e.wait_ge(s, v)

        @block.tensor
        def _(e):
            run(e, 'pe')

        @block.scalar
        def _(e):
            run(e, 'act')

        @block.vector
        def _(e):
            run(e, 'dve')

        @block.gpsimd
        def _(e):
            run(e, 'pool')

        @block.sync
        def _(e):
            run(e, 'sp')


class K:
    def __init__(self, nc):
        self.nc = nc
        self.P = Prog(nc)
        self.es = ExitStack()
        self.n = 0

    def scope(self):
        kk = self

        class _S:
            def __enter__(s_):
                s_.old = kk.es
                kk.es = ExitStack()
                return s_

            def __exit__(s_, *a):
                kk.es.close()
                kk.es = s_.old
                kk.P.barrier()
                return False
        return _S()

    def sb(self, shape, dt=F32, name=None):
        self.n += 1
        return self.es.enter_context(self.nc.sbuf_tensor(("%s_%d" % (name, self.n)) if name else ("t%d" % self.n), list(shape), dt))

    def ps(self, shape, dt=F32, name=None):
        self.n += 1
        return self.es.enter_context(self.nc.psum_tensor(name or ("p%d" % self.n), list(shape), dt))

    def dram(self, shape, dt=F32, name=None, kind="Internal"):
        self.n += 1
        return self.nc.dram_tensor(name or ("d%d" % self.n), list(shape), dt, kind=kind).ap()

    def mm(self, out, lhsT, rhs, start, stop, r, w):
        return self.P.op('pe', lambda e: e.matmul(out, lhsT=lhsT, rhs=rhs, start=start, stop=stop), r, w)

    def tr(self, out, in_, ident, r, w):
        return self.P.op('pe', lambda e: e.transpose(out, in_, ident), r, w)

    def act(self, out, in_, func, r, w, bias=None, scale=None, accum=None, eng='act'):
        kw = {}
        if bias is not None:
            kw['bias'] = bias
        if scale is not None:
            kw['scale'] = scale
        if accum is not None:
            kw['accum_out'] = accum
        return self.P.op('act', lambda e: e.activation(out=out, in_=in_, func=func, **kw), r, w)

    def ts(self, eng, out, in0, s1, s2, op0, op1, r, w):
        if op1 is None:
            return self.P.op(eng, lambda e: e.tensor_scalar(out=out, in0=in0, scalar1=s1, scalar2=None, op0=op0), r, w)
        return self.P.op(eng, lambda e: e.tensor_scalar(out=out, in0=in0, scalar1=s1, scalar2=s2, op0=op0, op1=op1), r, w)

    def tt(self, eng, out, in0, in1, op, r, w):
        return self.P.op(eng, lambda e: e.tensor_tensor(out=out, in0=in0, in1=in1, op=op), r, w)

    def stt(self, out, in0, scalar, in1, op0, op1, r, w):
        return self.P.op('dve', lambda e: e.scalar_tensor_tensor(out=out, in0=in0, scalar=scalar, in1=in1, op0=op0, op1=op1), r, w)

    def cp(self, eng, out, in_, r, w):
        if eng == 'act':
            return self.P.op('act', lambda e: e.copy(out=out, in_=in_), r, w)
        return self.P.op(eng, lambda e: e.tensor_copy(out=out, in_=in_), r, w)

    def memset(self, eng, ap, val, w):
        return self.P.op(eng, lambda e: e.memset(ap, val), (), w)

    def recip(self, out, in_, r, w):
        return self.P.op('dve', lambda e: e.reciprocal(out=out, in_=in_), r, w)

    def dma(self, eng, out, in_, r, w, **kw):
        return self.P.dma(eng, out, in_, r, w, **kw)


def build_program(stage=99):
    nc = bass.Bass("TRN2", target_bir_lowering=False)
    k = K(nc)
    P = k.P

    def din(name, shape, dt=F32):
        return nc.dram_tensor(name, list(shape), dt, kind="ExternalInput").ap()

    xl = din("xl", [LOC, D])
    cb = din("cb", [KC, 128])
    ada_w = din("ada_w", [D, 6 * D]) if not FROM_REF else None
    ada_b = din("ada_b", [96, 128])
    norm1_w = din("norm1_w", [KC, 128])
    w_in = din("w_in", [D, D_IN]) if not FROM_REF else None
    ident_in = din("ident", [128, 128])
    pvalid_in = din("pvalid", [128, 1])
    out_d = nc.dram_tensor("out", [NOWN, D], F32, kind="ExternalOutput").ap()
    dbg_mod = nc.dram_tensor("dbg_mod", [128, 96], F32, kind="ExternalOutput").ap()
    def scratch(name, shape, dt=F32):
        if name in REF_IN:
            return din(name + "_in", shape, dt)
        return k.dram(shape, dt, name)

    if FROM_REF:
        PF = din("PF_in", [PF_ROWS, LOC])
        PT = din("PT_in", [LOC, 1600])
    else:
        PF = k.dram([80 * 128, LOC], F32, "PF")
        PT = k.dram([LOC, 1600], F32, "PT")
    OAT = scratch("OAT", [1024, NOWN], BF16)
    OBT = scratch("OBT", [2048, NOWN], BF16)
    YT = scratch("YT", [D, NOWN], BF16)
    X1 = scratch("X1", [NOWN, D], F32)

    ident = k.sb([128, 128], F32, "ident_sb")
    identb = k.sb([128, 128], BF16, "identb")
    pvalid = k.sb([128, 1], F32, "pvalid_sb")
    b_const = P.buf("const")
    k.dma('sp', ident[:], ident_in, (), [b_const])
    k.dma('sp', pvalid[:], pvalid_in, (), [b_const])
    k.cp('dve', identb[:], ident[:], [b_const], [b_const])

    banks = [k.ps([128, 512], F32, "bank%d" % i) for i in range(6)]
    bbank = [P.buf("bank%d" % i) for i in range(6)]
    for b_ in bbank:
        b_.excl = True
    pbf = [k.ps([128, 1024], BF16, "pbf%d" % i) for i in range(2)]
    bpbf = [P.buf("pbf%d" % i) for i in range(2)]
    for b_ in bpbf:
        b_.excl = True

    def load_fm(dst, src, n, bdst, bank=5):
        tmp = k.sb([96, 128], F32)
        bt = P.buf()
        k.dma('sp', tmp[0:n, :], src, (), [bt])
        k.tr(banks[bank][:, 0:n], tmp[0:n, :], ident[0:n, 0:n], [bt, b_const], [bbank[bank]])
        k.cp('dve', dst, banks[bank][:, 0:n], [bbank[bank]], [bdst])

    modT = k.sb([128, 96], F32, "modT")
    b_mod = P.buf("mod")
    A1 = k.sb([128, KC], F32, "A1")
    B1p = k.sb([128, KC], F32, "B1p")
    b_A1 = P.buf()
    if FROM_REF:
        modT_in = din("modT_in", [128, 96])
        k.dma('sp', modT[:], modT_in, (), [b_mod])
    with k.scope():
      if not FROM_REF:
        cT = k.sb([128, KC], F32, "cT")
        csT = k.sb([128, KC], F32, "csT")
        adabT = k.sb([128, 96], F32, "adabT")
        n1T = k.sb([128, KC], F32, "n1T")
        b_c, b_cs, b_adab, b_n1 = P.bufs(4)
        load_fm(cT[:], cb, KC, b_c)
        load_fm(adabT[:], ada_b, 96, b_adab)
        load_fm(n1T[:], norm1_w, KC, b_n1)
        k.act(csT[:], cT[:], AF.Silu, [b_c], [b_cs])
        NAW = 3
        awt = [k.sb([128, KC, 128], F32, "awt%d" % i) for i in range(NAW)]
        bawt = P.bufs(NAW)
        ada_v = ada_w.rearrange("(kc p) j -> p kc j", p=128)
        for m in range(96):
            s = m % NAW
            k.dma('sp', awt[s][:], ada_v[:, :, m * 128:(m + 1) * 128], (), [bawt[s]])
            for kc in range(KC):
                k.mm(banks[4][:, m:m + 1], awt[s][:, kc, :], csT[:, kc:kc + 1], kc == 0, kc == KC - 1,
                     [bawt[s], b_cs], [bbank[4]])
        k.tt('dve', modT[:], banks[4][:, 0:96], adabT[:], ALU.add, [bbank[4], b_adab], [b_mod])
        k.dma('sp', dbg_mod, modT[:], [b_mod], ())
        k.stt(A1[:], modT[:, 16:32], 1.0, n1T[:], ALU.add, ALU.mult, [b_mod, b_n1], [b_A1])
        k.ts('dve', B1p[:], modT[:, 0:16], pvalid[:, 0:1], None, ALU.mult, None, [b_mod, b_const], [b_A1])

    if stage >= 1 and not FROM_REF:
        hscope = k.scope()
        hscope.__enter__()
        hT = k.sb([128, KC, LOC], BF16, "hT")
        b_hT = [P.buf() for _ in range(LOC // 128)]
        with k.scope():
            xt = [k.sb([128, D], F32, "xt%d" % i) for i in range(2)]
            xs = [k.sb([128, D], BF16, "xs%d" % i) for i in range(2)]
            st = [k.sb([128, 4], F32, "st%d" % i) for i in range(2)]
            bxt, bxs, bst = P.bufs(2), P.bufs(2), P.bufs(2)
            xlv = xl.rearrange("(t p) d -> t p d", p=128)
            for t in range(LOC // 128):
                s = t % 2
                k.dma('sp', xt[s][:], xlv[t], (), [bxt[s]])
                k.act(xs[s][:], xt[s][:], AF.Square, [bxt[s]], [bxs[s], bst[s]], accum=st[s][:, 0:1])
                k.ts('dve', st[s][:, 1:2], st[s][:, 0:1], 1.0 / D, 1e-6, ALU.mult, ALU.add, [bst[s]], [bst[s]])
                k.act(st[s][:, 2:3], st[s][:, 1:2], AF.Sqrt, [bst[s]], [bst[s]])
                k.recip(st[s][:, 3:4], st[s][:, 2:3], [bst[s]], [bst[s]])
                k.ts('dve', xs[s][:], xt[s][:], st[s][:, 3:4], None, ALU.mult, None, [bxt[s], bst[s]], [bxs[s]])
                Bsel = B1p if t < OWN0 // 128 else modT
                for kc in range(KC):
                    pi = kc % 2
                    sl = slice((kc // 2 % 4) * 128, (kc // 2 % 4) * 128 + 128)
                    k.tr(pbf[pi][:, sl], xs[s][:, kc * 128:(kc + 1) * 128], identb[:], [bxs[s], b_const], [bpbf[pi]])
                    if kc % 2 == 0:
                        k.act(hT[:, kc, t * 128:(t + 1) * 128], pbf[pi][:, sl], AF.Identity, [bpbf[pi], b_A1, b_mod], [b_hT[t]],
                              bias=Bsel[:, kc:kc + 1], scale=A1[:, kc:kc + 1])
                    else:
                        k.ts('dve', hT[:, kc, t * 128:(t + 1) * 128], pbf[pi][:, sl], A1[:, kc:kc + 1], Bsel[:, kc:kc + 1],
                             ALU.mult, ALU.add, [bpbf[pi], b_A1, b_mod], [b_hT[t]])
        if stage == 1:
            dbg_h = nc.dram_tensor("dbg_h", [128, KC, LOC], BF16, kind="ExternalOutput").ap()
            k.dma('sp', dbg_h, hT[:], b_hT, ())

        if stage >= 2:
            FM = [(0, 3072, 0, 0), (4112, 2048, 3072, OWN0), (6160, 256, 5120, 0), (6416, 256, 5376, 0),
                  (6672, 256, 5632, 0), (7184, 256, 5888, 0), (7744, 2048, 6144, OWN0), (9792, 2048, 8192, OWN0)]
            TM = [(3072, 256, 0, OWN0), (3328, 256, 256, OWN0), (3584, 256, 512, OWN0), (3840, 256, 768, OWN0),
                  (4096, 16, 1024, 0), (6928, 256, 1040, 0), (7440, 256, 1296, 0), (7696, 48, 1552, OWN0)]
            with k.scope():
                wv = w_in.rearrange("(kc p) c -> p kc c", p=128)
                NW = 3
                wt = [k.sb([128, KC, 256], BF16, "wt%d" % i) for i in range(NW)]
                bwt = P.bufs(NW)
                ev = [k.sb([128, 512], F32, "ev%d" % i) for i in range(4)]
                bev = P.bufs(4)
                ei = 0
                wi = 0
                for (c0, ncols, r0, t0) in FM:
                    for cb_ in range(ncols // 256):
                        s = wi % NW
                        wi += 1
                        k.dma('pool', wt[s][:], wv[:, :, c0 + cb_ * 256:c0 + cb_ * 256 + 256], (), [bwt[s]])
                        for hf in range(2):
                            for tt in range(t0 // 512, LOC // 512):
                                e4 = ei % 4
                                ei += 1
                                for kc in range(KC):
                                    k.mm(banks[e4][:, :], wt[s][:, kc, hf * 128:(hf + 1) * 128],
                                         hT[:, kc, tt * 512:(tt + 1) * 512], kc == 0, kc == KC - 1,
                                         [bwt[s]] + b_hT[tt * 4:(tt + 1) * 4], [bbank[e4]])
                                k.cp('act' if e4 % 2 == 0 else 'dve', ev[e4][:], banks[e4][:, :], [bbank[e4]], [bev[e4]])
                                row = r0 + cb_ * 256 + hf * 128
                                k.dma('sp', PF[row:row + 128, tt * 512:(tt + 1) * 512], ev[e4][:], [bev[e4]], ())
                for (c0, ncols, p0, t0) in TM:
                    s = wi % NW
                    wi += 1
                    k.dma('pool', wt[s][:, :, 0:ncols], wv[:, :, c0:c0 + ncols], (), [bwt[s]])
                    for t in range(t0 // 128, LOC // 128):
                        e4 = ei % 4
                        ei += 1
                        for kc in range(KC):
                            k.mm(banks[e4][:, 0:ncols], hT[:, kc, t * 128:(t + 1) * 128], wt[s][:, kc, 0:ncols],
                                 kc == 0, kc == KC - 1, [bwt[s], b_hT[t]], [bbank[e4]])
                        k.cp('act' if e4 % 2 == 0 else 'dve', ev[e4][:, 0:ncols], banks[e4][:, 0:ncols], [bbank[e4]], [bev[e4]])
                        k.dma('sp', PT[t * 128:(t + 1) * 128, p0:p0 + ncols], ev[e4][:, 0:ncols], [bev[e4]], ())
        hscope.__exit__(None, None, None)
        if stage == 2 and DEBUG_OUT:
            dbg_pf = nc.dram_tensor("dbg_pf", [4, 128, LOC], F32, kind="ExternalOutput").ap()
            dbg_pt = nc.dram_tensor("dbg_pt", [LOC, 576], F32, kind="ExternalOutput").ap()
            for i, r in enumerate([0, 3072, 5120, 6144]):
                k.dma('sp', dbg_pf[i], PF[r:r + 128, :], (), ())
            k.dma('sp', dbg_pt, PT[:, 1024:1600], (), ())

    if stage >= 6:
        uT_in = din("peer_uT", [D, 16384])
        pv_in = din("peer_v", [16384, D])
        UBd = k.dram([64, 128, KC, 256], BF16, "UBd")
        VBd = k.dram([64, 128, 2, D], BF16, "VBd")
        bconvU = [P.buf() for _ in range(64)]
        bconvV = [P.buf() for _ in range(64)]
        uTv0 = uT_in.rearrange("(kc p) e -> p kc e", p=128)
        for eb in range(64):
            P.dma('pool', UBd[eb], uTv0[:, :, eb * 256:(eb + 1) * 256], (), [bconvU[eb]], bg=True)
            P.dma('pool', VBd[eb], pv_in[eb * 256:(eb + 1) * 256, :].rearrange("(i p) d -> p i d", p=128), (), [bconvV[eb]], bg=True)
    CONV_TOK = {}
    if stage >= 3 and not SKIP_GDN:
        gconst_in = din("gconst", [128, 5, 128])
        convw_in = din("gdn_conv_w", [96, 128])
        alog_in = din("gdn_A_log", [32, 8])
        dtb_in = din("gdn_dt_bias", [32, 8])
        gnw_in = din("gdn_norm_w", [1, 128])
        bq = [[bbank[bi]] * 4 for bi in range(6)]
        NCH = LOC // 128
        with k.scope():
            gcn = k.sb([128, 5, 128], F32, "gcn")
            ones = k.sb([128, 128], F32, "ones")
            cwT = k.sb([128, 96], F32, "cwT")
            nwb = k.sb([128, 128], F32, "nwb")
            b_gc = P.buf()
            b_cw = P.buf()
            k.dma('sp', gcn[:], gconst_in, (), [b_gc])
            k.memset('dve', ones[:], 1.0, [b_gc])
            k.dma('sp', nwb[:], gnw_in[0].partition_broadcast(128), (), [b_gc])
            load_fm(cwT[:], convw_in, 96, b_cw)
            TriU, msl, msu, miu, bmk = gcn[:, 0, :], gcn[:, 1, :], gcn[:, 2, :], gcn[:, 3, :], gcn[:, 4, :]
            ab = k.sb([128, NCH, 16], F32, "ab")
            dtb = k.sb([128, NCH, 8], F32, "dtb")
            alg = k.sb([128, NCH, 8], F32, "alg")
            vt = {nm: k.sb([128, NCH, 8], F32, "v_" + nm) for nm in
                  ["t1", "g", "beta", "lnb", "Gc", "nGc", "u", "gam", "bg", "kd", "gend", "Gl"]}
            b_v = P.buf()
            k.dma('sp', ab[:], PT[:, 1024:1040].rearrange("(n p) c -> p n c", p=128), (), [b_v])
            k.dma('sp', dtb[:], dtb_in.partition_broadcast(128), (), [b_v])
            k.dma('sp', alg[:], alog_in.partition_broadcast(128), (), [b_v])
            k.tt('dve', vt["t1"][:], ab[:, :, 0:8], dtb[:], ALU.add, [b_v], [b_v])
            k.act(vt["t1"][:], vt["t1"][:], AF.Exp, [b_v], [b_v])
            e_, ser, lnp = vt["t1"], vt["Gl"], vt["Gc"]
            k.ts('dve', ser[:], e_[:], -0.25, 1.0 / 3.0, ALU.mult, ALU.add, [b_v], [b_v])
            k.tt('dve', ser[:], ser[:], e_[:], ALU.mult, [b_v], [b_v])
            k.ts('dve', ser[:], ser[:], -0.5, None, ALU.add, None, [b_v], [b_v])
            k.tt('dve', ser[:], ser[:], e_[:], ALU.mult, [b_v], [b_v])
            k.ts('dve', ser[:], ser[:], 1.0, None, ALU.add, None, [b_v], [b_v])
            k.tt('dve', ser[:], ser[:], e_[:], ALU.mult, [b_v], [b_v])
            k.ts('dve', lnp[:], e_[:], 0.1, None, ALU.max, None, [b_v], [b_v])
            k.act(lnp[:], lnp[:], AF.Ln, [b_v], [b_v], bias=1.0)
            k.ts('dve', vt["u"][:], e_[:], 0.1, None, ALU.is_lt, None, [b_v], [b_v])
            k.tt('dve', ser[:], ser[:], lnp[:], ALU.subtract, [b_v], [b_v])
            k.tt('dve', ser[:], ser[:], vt["u"][:], ALU.mult, [b_v], [b_v])
            k.tt('dve', vt["t1"][:], lnp[:], ser[:], ALU.add, [b_v], [b_v])
            k.act(alg[:], alg[:], AF.Exp, [b_v], [b_v])
            k.stt(vt["g"][:], vt["t1"][:], -1.0, alg[:], ALU.mult, ALU.mult, [b_v], [b_v])
            k.act(vt["beta"][:], ab[:, :, 8:16], AF.Sigmoid, [b_v], [b_v])
            k.act(vt["lnb"][:], vt["beta"][:], AF.Ln, [b_v], [b_v])
            gflat = vt["g"][:].rearrange("p n h -> p (n h)")
            k.mm(banks[0][:, 0:256], TriU, gflat, True, True, [b_v, b_gc], [bq[0][0], bq[0][1]])
            k.mm(banks[0][:, 256:512], ones[:], gflat, True, True, [b_v, b_gc], [bq[0][2], bq[0][3]])
            fl = lambda nm: vt[nm][:].rearrange("p n h -> p (n h)")
            k.cp('dve', fl("Gc"), banks[0][:, 0:256], [bq[0][0], bq[0][1]], [b_v])
            k.cp('dve', fl("Gl"), banks[0][:, 256:512], [bq[0][2], bq[0][3]], [b_v])
            k.ts('dve', fl("nGc"), fl("Gc"), -1.0, None, ALU.mult, None, [b_v], [b_v])
            k.tt('dve', fl("u"), fl("Gc"), fl("lnb"), ALU.add, [b_v], [b_v])
            k.act(fl("gam"), fl("Gc"), AF.Exp, [b_v], [b_v])
            k.act(fl("bg"), fl("u"), AF.Exp, [b_v], [b_v])
            k.tt('dve', fl("kd"), fl("Gl"), fl("Gc"), ALU.subtract, [b_v], [b_v])
            k.act(fl("kd"), fl("kd"), AF.Exp, [b_v], [b_v])
            k.act(fl("gend"), fl("Gl"), AF.Exp, [b_v], [b_v])

            def col(nm, n, h):
                return vt[nm][:, n, h:h + 1]

            WV = 4
            for h in range(0 if GDN_SUB < 1 else (1 if GDN_ONE_HEAD else 8)):
                with k.scope():
                    QT = k.sb([128, LOC], F32, "QT")
                    KT = k.sb([128, LOC], F32, "KT")
                    VT = k.sb([128, LOC], F32, "VT")
                    xr = k.sb([128, LOC], F32, "xr")
                    bQ, bK, bV, bxr = P.bufs(4)
                    for (dst, bd, row0, ti) in [(QT, bQ, h * 128, h), (KT, bK, 1024 + h * 128, 8 + h), (VT, bV, 2048 + h * 128, 16 + h)]:
                        k.dma('sp', xr[:], PF[row0:row0 + 128, :], (), [bxr])
                        k.ts('dve', dst[:], xr[:], cwT[:, 3 * 24 + ti:3 * 24 + ti + 1], None, ALU.mult, None, [bxr, b_cw], [bd])
                        for sh in (1, 2, 3):
                            j = 3 - sh
                            k.stt(dst[:, sh:LOC], xr[:, 0:LOC - sh], cwT[:, j * 24 + ti:j * 24 + ti + 1], dst[:, sh:LOC],
                                  ALU.mult, ALU.add, [bxr, b_cw, bd], [bd])
                        k.act(dst[:], dst[:], AF.Silu, [bd], [bd])
                    for (dst, bd, lnc) in [(QT, bQ, float(np.log(128.0 ** -0.5))), (KT, bK, 0.0)]:
                        for tt in range(LOC // 512):
                            bk = tt % 4
                            sl = slice(tt * 512, (tt + 1) * 512)
                            k.tt('dve', xr[:, sl], dst[:, sl], dst[:, sl], ALU.mult, [bd], [bxr])
                            k.mm(banks[bk][:, :], ones[:], xr[:, sl], True, True, [bxr, b_gc], bq[bk])
                            k.act(xr[:, sl], banks[bk][:, :], AF.Ln, bq[bk], [bxr], bias=1e-6)
                            k.act(xr[:, sl], xr[:, sl], AF.Exp, [bxr], [bxr], scale=-0.5, bias=lnc)
                            k.tt('dve', dst[:, sl], dst[:, sl], xr[:, sl], ALU.mult, [bd, bxr], [bd])
                    zt = k.sb([128, NOWN // 128, 128], F32, "zt")
                    bz = P.buf()
                    k.dma('sp', zt[:], PT[OWN0:LOC, h * 128:(h + 1) * 128].rearrange("(n p) c -> p n c", p=128), (), [bz])
                    k.act(zt[:], zt[:], AF.Silu, [bz], [bz])
                    S = k.sb([128, 128], F32, "S")
                    bS = P.buf()
                    k.memset('dve', S[:], 0.0, [bS])
                    names = ["R", "Kd", "dg", "tE", "L", "LT", "QK", "Pa", "Pb", "WT", "U", "tmp", "O", "o16", "Z", "DT", "X", "Tt"]
                    T = [{nm: k.sb([128, 256 if nm in ("R", "X", "Tt") else 128], BF16 if nm == "o16" else F32, "w%d_%s" % (i, nm)) for nm in names}
                         for i in range(WV)]
                    Bf = [{nm: P.buf() for nm in names + ["st"]} for i in range(WV)]
                    stt_ = [k.sb([128, 4], F32, "w%d_st" % i) for i in range(WV)]
                    PTk = [[k.sb([128, 128], F32, "w%d_PT%d" % (i, l)) for l in range(4)] for i in range(WV)]
                    bPTk = [[P.buf() for l in range(4)] for i in range(WV)]
                    bctr = [0]

                    def nb():
                        bctr[0] = (bctr[0] + 1) % 6
                        return bctr[0]

                    for w0 in (GDN_WAVES if GDN_WAVES is not None else range(0, NCH if GDN_SUB >= 2 else 0, WV)):
                        chunks = list(range(w0, w0 + WV))
                        own = w0 >= OWN0 // 128
                        def pre_steps(i, n):
                            t, b = T[i], Bf[i]
                            cs = slice(n * 128, (n + 1) * 128)
                            q4 = slice(i * 128, (i + 1) * 128)
                            st = []
                            bkT1, bkT2, bkKK, bkKQ, bkB = 0, 1, 2, 3, 4
                            st.append(lambda: k.tr(banks[0][:, q4], KT[:, cs], ident[:], [bK, b_const], [bq[0][i]]))
                            st.append(lambda: k.act(t["R"][:, 0:128], banks[0][:, q4], AF.Identity, [bq[0][i], b_v], [b["R"]], scale=col("bg", n, h)))
                            st.append(lambda: k.ts('dve', t["Kd"][:], banks[0][:, q4], col("kd", n, h), None, ALU.mult, None, [bq[0][i], b_v], [b["Kd"]]))
                            st.append(lambda: k.tr(banks[1][:, q4], VT[:, cs], ident[:], [bV, b_const], [bq[1][i]]))
                            st.append(lambda: k.ts('dve', t["R"][:, 128:256], banks[1][:, q4], col("beta", n, h), None, ALU.mult, None, [bq[1][i], b_v], [b["R"]]))
                            st.append(lambda: k.mm(banks[2][:, q4], KT[:, cs], KT[:, cs], True, True, [bK], [bq[2][i]]))
                            st.append(lambda: k.ts('dve', t["dg"][:], ident[:], col("nGc", n, h), None, ALU.mult, None, [b_const, b_v], [b["dg"]]))
                            st.append(lambda: k.mm(banks[4][:, q4], ones[:], t["dg"][:], True, True, [b["dg"], b_gc], [bq[4][i]]))
                            st.append(lambda: k.stt(t["tE"][:], banks[4][:, q4], col("u", n, h), msl, ALU.add, ALU.add, [bq[4][i], b_v, b_gc], [b["tE"]]))
                            st.append(lambda: k.act(t["tE"][:], t["tE"][:], AF.Exp, [b["tE"]], [b["tE"]]))
                            st.append(lambda: k.tt('dve', t["L"][:], banks[2][:, q4], t["tE"][:], ALU.mult, [bq[2][i], b["tE"]], [b["L"]]))
                            st.append(lambda: k.ts('dve', t["dg"][:], ident[:], col("u", n, h), None, ALU.mult, None, [b_const, b_v], [b["dg"]]))
                            st.append(lambda: k.mm(banks[5][:, q4], ones[:], t["dg"][:], True, True, [b["dg"], b_gc], [bq[5][i]]))
                            st.append(lambda: k.stt(t["tE"][:], banks[5][:, q4], col("nGc", n, h), msu, ALU.add, ALU.add, [bq[5][i], b_v, b_gc], [b["tE"]]))
                            st.append(lambda: k.act(t["tE"][:], t["tE"][:], AF.Exp, [b["tE"]], [b["tE"]]))
                            st.append(lambda: k.tt('dve', t["LT"][:], banks[2][:, q4], t["tE"][:], ALU.mult, [bq[2][i], b["tE"]], [b["LT"]]))
                            if own:
                                st.append(lambda: k.mm(banks[3][:, q4], KT[:, cs], QT[:, cs], True, True, [bK, bQ], [bq[3][i]]))
                                st.append(lambda: k.ts('dve', t["dg"][:], ident[:], col("Gc", n, h), None, ALU.mult, None, [b_const, b_v], [b["dg"]]))
                                st.append(lambda: k.mm(banks[4][:, q4], ones[:], t["dg"][:], True, True, [b["dg"], b_gc], [bq[4][i]]))
                                st.append(lambda: k.stt(t["tE"][:], banks[4][:, q4], col("nGc", n, h), miu, ALU.add, ALU.add, [bq[4][i], b_v, b_gc], [b["tE"]]))
                                st.append(lambda: k.act(t["tE"][:], t["tE"][:], AF.Exp, [b["tE"]], [b["tE"]]))
                                st.append(lambda: k.tt('dve', t["QK"][:], banks[3][:, q4], t["tE"][:], ALU.mult, [bq[3][i], b["tE"]], [b["QK"]]))
                            st.append(lambda: k.tt('dve', t["tmp"][:], t["LT"][:], bmk, ALU.mult, [b["LT"], b_gc], [b["tmp"]]))
                            st.append(lambda: k.tt('dve', t["LT"][:], t["LT"][:], t["tmp"][:], ALU.subtract, [b["LT"], b["tmp"]], [b["LT"]]))
                            st.append(lambda: k.tt('dve', t["L"][:], t["L"][:], bmk, ALU.mult, [b["L"], b_gc], [b["L"]]))
                            cP, cbP, cPT, cbPT = t["L"], b["L"], t["tmp"], b["tmp"]
                            NLEV = 4
                            for lev in range(NLEV):
                                nP, nbP = (t["Pa"], b["Pa"]) if lev % 2 == 0 else (t["Pb"], b["Pb"])
                                bA = lev % 2
                                last = lev == NLEV - 1
                                if not last:
                                    st.append((lambda cP=cP, cbP=cbP, cPT=cPT, cbPT=cbPT, bA=bA: k.mm(banks[bA][:, q4], cPT[:], cP[:], True, True, [cbPT, cbP], [bq[bA][i]])))
                                    st.append((lambda nP=nP, nbP=nbP, bA=bA: k.cp('act', nP[:], banks[bA][:, q4], [bq[bA][i]], [nbP])))
                                st.append((lambda cP=cP, cbP=cbP, cPT=cPT, cbPT=cbPT, bA=bA: k.mm(banks[2 + bA][:, q4], cP[:], cPT[:], True, True, [cbPT, cbP], [bq[2 + bA][i]])))
                                st.append((lambda lev=lev, bA=bA: k.cp('dve', PTk[i][lev][:], banks[2 + bA][:, q4], [bq[2 + bA][i]], [bPTk[i][lev]])))
                                cP, cbP, cPT, cbPT = nP, nbP, PTk[i][lev], bPTk[i][lev]
                            st.append(lambda: k.cp('dve', t["Z"][:], ident[:], [b_const], [b["Z"]]))
                            for lev in [3, 2, 1, 0, -1]:
                                bA = 4 + (lev % 2)
                                if lev >= 0:
                                    st.append((lambda lev=lev, bA=bA: k.mm(banks[bA][:, q4], PTk[i][lev][:], t["Z"][:], True, True, [bPTk[i][lev], b["Z"]], [bq[bA][i]])))
                                    st.append((lambda bA=bA: k.tt('dve', t["Z"][:], t["Z"][:], banks[bA][:, q4], ALU.add, [bq[bA][i], b["Z"]], [b["Z"]])))
                                else:
                                    st.append((lambda bA=bA: k.mm(banks[bA][:, q4], t["tmp"][:], t["Z"][:], True, True, [b["tmp"], b["Z"]], [bq[bA][i]])))
                                    st.append((lambda bA=bA: k.tt('dve', t["Z"][:], t["Z"][:], banks[bA][:, q4], ALU.subtract, [bq[bA][i], b["Z"]], [b["Z"]])))
                            st.append(lambda: k.tr(banks[0][:, q4], t["Z"][:], ident[:], [b["Z"], b_const], [bq[0][i]]))
                            st.append(lambda: k.cp('act', t["DT"][:], banks[0][:, q4], [bq[0][i]], [b["DT"]]))
                            hq = slice((i % 2) * 256, (i % 2) * 256 + 256)
                            bkR = 4 + (i // 2)
                            bkR2 = 2 + (i // 2)
                            st.append(lambda: k.mm(banks[bkR][:, hq], t["DT"][:], t["R"][:], True, True, [b["DT"], b["R"]], [bq[bkR][0]]))
                            st.append(lambda: k.cp('dve', t["X"][:], banks[bkR][:, hq], [bq[bkR][0]], [b["X"]]))
                            for sweep in range(3):
                                st.append(lambda: k.mm(banks[bkR2][:, hq], t["LT"][:], t["X"][:], True, True, [b["LT"], b["X"]], [bq[bkR2][0]]))
                                st.append(lambda: k.tt('dve', t["Tt"][:], t["R"][:], banks[bkR2][:, hq], ALU.subtract, [b["R"], bq[bkR2][0]], [b["Tt"]]))
                                st.append(lambda: k.mm(banks[bkR][:, hq], t["DT"][:], t["Tt"][:], True, True, [b["DT"], b["Tt"]], [bq[bkR][0]]))
                                st.append(lambda: k.cp('dve', t["X"][:], banks[bkR][:, hq], [bq[bkR][0]], [b["X"]]))
                            st.append(lambda: k.cp('act', t["R"][:], t["X"][:], [b["X"]], [b["R"]]))
                            st.append(lambda: k.tr(banks[0][:, q4], t["R"][:, 0:128], ident[:], [b["R"], b_const], [bq[0][i]]))
                            st.append(lambda: k.cp('act', t["WT"][:], banks[0][:, q4], [bq[0][i]], [b["WT"]]))
                            return st

                        allst = [pre_steps(i, n) for i, n in enumerate(chunks)]
                        for si in range(min(GDN_STEPS, len(allst[0]))):
                            for i in range(WV):
                                allst[i][si]()
                        for i, n in enumerate(chunks if GDN_SUB >= 3 else []):
                            t, b = T[i], Bf[i]
                            cs = slice(n * 128, (n + 1) * 128)
                            q4 = slice(i * 128, (i + 1) * 128)
                            k.mm(banks[1][:, q4], t["WT"][:], S[:], True, True, [b["WT"], bS], [bq[1][i]])
                            k.tt('dve', t["U"][:], t["R"][:, 128:256], banks[1][:, q4], ALU.subtract, [b["R"], bq[1][i]], [b["U"]])
                            if own:
                                k.mm(banks[2][:, q4], QT[:, cs], S[:], True, True, [bQ, bS], [bq[2][i]])
                                k.mm(banks[3][:, q4], t["QK"][:], t["U"][:], True, True, [b["QK"], b["U"]], [bq[3][i]])
                                k.act(t["tmp"][:], banks[2][:, q4], AF.Identity, [bq[2][i], b_v], [b["tmp"]], scale=col("gam", n, h))
                                k.tt('dve', t["O"][:], t["tmp"][:], banks[3][:, q4], ALU.add, [b["tmp"], bq[3][i]], [b["O"]])
                            k.mm(banks[0][:, q4], t["Kd"][:], t["U"][:], True, True, [b["Kd"], b["U"]], [bq[0][i]])
                            k.stt(S[:], S[:], col("gend", n, h), banks[0][:, q4], ALU.mult, ALU.add, [bS, b_v, bq[0][i]], [bS])
                            if own:
                                no = n - OWN0 // 128
                                sti = stt_[i]
                                k.act(t["tmp"][:], t["O"][:], AF.Square, [b["O"]], [b["tmp"], b["st"]], accum=sti[:, 0:1])
                                k.act(sti[:, 1:2], sti[:, 0:1], AF.Ln, [b["st"]], [b["st"]], scale=1.0 / 128, bias=1e-6)
                                k.act(sti[:, 2:3], sti[:, 1:2], AF.Exp, [b["st"]], [b["st"]], scale=-0.5)
                                k.stt(t["O"][:], t["O"][:], sti[:, 2:3], nwb[:], ALU.mult, ALU.mult, [b["O"], b["st"], b_gc], [b["O"]])
                                k.tt('dve', t["O"][:], t["O"][:], zt[:, no, :], ALU.mult, [b["O"], bz], [b["O"]])
                                k.tr(banks[4][:, q4], t["O"][:], ident[:], [b["O"], b_const], [bq[4][i]])
                                k.cp('act', t["o16"][:], banks[4][:, q4], [bq[4][i]], [b["o16"]])
                                k.dma('sp', OAT[h * 128:(h + 1) * 128, no * 128:(no + 1) * 128], t["o16"][:], [b["o16"]], ())
        if stage == 3 and DEBUG_OUT and not SKIP_GDN:
            dbg_oa = nc.dram_tensor("dbg_oa", [1024, NOWN], BF16, kind="ExternalOutput").ap()
            k.dma('sp', dbg_oa, OAT, (), ())

    if stage >= 4 and not SKIP_NSA:
        pos_in = din("pos", [1, LOC], I32)
        nsc_in = din("nsa_c128", [128, 385])
        maskB_in = din("maskB", [128, LOC])
        eexp_in = din("eexp", [64, LOC])
        ovl_in = din("ovl", [128, 2, 65])
        fv_in = din("fbigvis", [128, 2, 16, 64])
        ncore_in = din("nsa_core", [128, 129])
        cpos_in = [din("cmp_pos_k", [32, 128]), din("cmp_pos_v", [32, 128])]
        cw1_in = [din("cmp_w1_k", [LOC, 256]), din("cmp_w1_v", [LOC, 256])]
        cw2_in = [din("cmp_w2_k", [256, 128]), din("cmp_w2_v", [256, 128])]
        NQT = NOWN // 128
        with k.scope():
            nsc = k.sb([128, 385], F32, "nsc")
            ncore = k.sb([128, 129], F32, "ncore")
            maskB = k.sb([128, LOC], BF16, "maskB")
            eexp = k.sb([64, LOC], BF16, "eexp")
            ovl = k.sb([128, 2, 65], BF16, "ovl")
            fv = k.sb([128, 2, 16, 64], F32, "fv")
            cmk = k.sb([128, 2, 128], BF16, "cmk")
            b_nc = P.buf()
            k.dma('sp', nsc[:], nsc_in, (), [b_nc])
            k.dma('sp', ncore[:], ncore_in, (), [b_nc])
            k.dma('sp', fv[:], fv_in, (), [b_nc])
            k.dma('pool', maskB[:], maskB_in, (), [b_nc])
            k.dma('pool', eexp[:], eexp_in, (), [b_nc])
            k.dma('pool', ovl[:], ovl_in, (), [b_nc])
            k.cp('dve', cmk[:, 0, :], nsc[:, 129:257], [b_nc], [b_nc])
            k.cp('dve', cmk[:, 1, :], nsc[:, 257:385], [b_nc], [b_nc])
            ropeP = nsc[:, 0:128]
            invf = nsc[:, 128:129]
            kvb = ncore[:, 128:129]
            cosT = k.sb([128, LOC], F32, "cosT")
            sinT = k.sb([128, LOC], F32, "sinT")
            cosq = k.sb([128, NOWN], F32, "cosq")
            sinq = k.sb([128, NOWN], F32, "sinq")
            b_rt = P.buf()
            with k.scope():
                posi = k.sb([128, LOC], I32, "posi")
                ang = k.sb([128, LOC], F32, "ang")
                kk_ = k.sb([128, LOC], I32, "kk")
                kf = k.sb([128, LOC], F32, "kf")
                b_a = P.buf()
                k.dma('sp', posi[:], pos_in[0].partition_broadcast(128), (), [b_a])
                k.cp('dve', ang[:], posi[:], [b_a], [b_a])
                k.ts('dve', ang[:], ang[:], invf, None, ALU.mult, None, [b_a, b_nc], [b_a])
                TWO_PI = float(2 * np.pi)
                for (dst, shift) in [(sinT, 0.0), (cosT, float(np.pi / 2))]:
                    k.ts('dve', kf[:], ang[:], shift, 1.0 / TWO_PI, ALU.add, ALU.mult, [b_a], [b_a])
                    k.cp('dve', kk_[:], kf[:], [b_a], [b_a])
                    k.cp('dve', kf[:], kk_[:], [b_a], [b_a])
                    k.stt(kf[:], kf[:], -TWO_PI, ang[:], ALU.mult, ALU.add, [b_a], [b_a])
                    k.ts('dve', kf[:], kf[:], shift, None, ALU.add, None, [b_a], [b_a])
                    k.ts('dve', dst[:], kf[:], float(np.pi), -TWO_PI, ALU.is_gt, ALU.mult, [b_a], [b_rt])
                    k.tt('dve', kf[:], kf[:], dst[:], ALU.add, [b_a, b_rt], [b_a])
                    k.ts('dve', dst[:], kf[:], -float(np.pi), TWO_PI, ALU.is_lt, ALU.mult, [b_a], [b_rt])
                    k.tt('dve', kf[:], kf[:], dst[:], ALU.add, [b_a, b_rt], [b_a])
                    k.act(dst[:], kf[:], AF.Sin, [b_a], [b_rt])
                k.ts('dve', cosq[:], cosT[:, OWN0:LOC], 128.0 ** -0.5, None, ALU.mult, None, [b_rt], [b_rt])
                k.ts('dve', sinq[:], sinT[:, OWN0:LOC], 128.0 ** -0.5, None, ALU.mult, None, [b_rt], [b_rt])

            def rope(dst_bf, X, bX, ct, st_, ntok, bdst, tmpa, tmpb, btmp):
                for tt in range(ntok // 512):
                    sl = slice(tt * 512, (tt + 1) * 512)
                    bk = tt % 3
                    k.mm(banks[bk][:, :], ropeP, X[:, sl], True, True, [bX, b_nc], [bbank[bk]])
                    k.tt('pool', tmpa[:, 0:512], X[:, sl], ct[:, sl], ALU.mult, [bX, b_rt], [btmp[0]])
                    k.tt('dve', tmpb[:, 0:512], banks[bk][:, :], st_[:, sl], ALU.mult, [bbank[bk], b_rt], [btmp[1]])
                    k.tt('dve', dst_bf[:, sl], tmpa[:, 0:512], tmpb[:, 0:512], ALU.add, [btmp[0], btmp[1]], [bdst])

            gts = k.sb([128, NQT, 48], F32, "gts")
            b_g = P.buf()
            k.dma('sp', gts[:], PT[OWN0:LOC, 1552:1600].rearrange("(n p) c -> p n c", p=128), (), [b_g])
            k.act(gts[:], gts[:], AF.Sigmoid, [b_g], [b_g])

            for g in range(2 if NSA_GROUPS_RUN is None else NSA_GROUPS_RUN):
                with k.scope():
                    KsT = k.sb([128, LOC], BF16, "KsT")
                    KwT = k.sb([128, LOC], BF16, "KwT")
                    Vs1 = k.sb([128, 32, 129], BF16, "Vs1")
                    Vw1 = k.sb([128, 32, 129], BF16, "Vw1")
                    KcmpT = k.sb([128, 256], BF16, "KcmpT")
                    RHSc = k.sb([128, 2, 193], BF16, "RHSc")
                    QTh = [k.sb([128, NOWN], BF16, "QTh%d" % i) for i in range(8)]
                    bKs, bKw, bVs, bVw, bKc, bRc = P.bufs(6)
                    bQh = P.bufs(8)
                    k.memset('dve', Vs1[:, :, 128:129], 1.0, [bVs])
                    k.memset('dve', Vw1[:, :, 128:129], 1.0, [bVw])
                    k.dma('pool', Vs1[:, :, 0:128], PT[:, 1040 + g * 128:1040 + (g + 1) * 128].rearrange("(n p) c -> p n c", p=128), (), [bVs])
                    k.dma('pool', Vw1[:, :, 0:128], PT[:, 1296 + g * 128:1296 + (g + 1) * 128].rearrange("(n p) c -> p n c", p=128), (), [bVw])
                    k.memset('dve', KcmpT[:], 0.0, [bKc])
                    k.cp('dve', RHSc[:, :, 0:65], ovl[:], [b_nc], [bRc])
                    with k.scope():
                        X = k.sb([128, LOC], F32, "ropeX")
                        tmpa = k.sb([128, 512], F32, "ropeA")
                        tmpb = k.sb([128, 512], F32, "ropeB")
                        KcT = k.sb([128, LOC], BF16, "KcT")
                        VcT = k.sb([128, LOC], BF16, "VcT")
                        bX, bKcT, bVcT = P.bufs(3)
                        btmp = P.bufs(2)
                        for (dstb, bd, row0) in [(KcT, bKcT, 5120 + g * 128), (KsT, bKs, 5632 + g * 128), (KwT, bKw, 5888 + g * 128)]:
                            k.dma('sp', X[:], PF[row0:row0 + 128, :], (), [bX])
                            rope(dstb, X, bX, cosT, sinT, LOC, bd, tmpa, tmpb, btmp)
                        k.dma('pool', VcT[:], PF[5376 + g * 128:5376 + (g + 1) * 128, :], (), [bVcT])
                        for hh in range(8):
                            row0 = 3072 + (g * 8 + hh) * 128
                            k.dma('sp', X[:, 0:NOWN], PF[row0:row0 + 128, OWN0:LOC], (), [bX])
                            rope(QTh[hh], X, bX, cosq, sinq, NOWN, bQh[hh], tmpa, tmpb, btmp)
                        w1 = k.sb([128, 32, 256], BF16, "cw1")
                        w2 = k.sb([128, 2, 128], BF16, "cw2")
                        cpT = k.sb([128, 32], F32, "cpT")
                        cpTb = k.sb([128, 32], BF16, "cpTb")
                        hidT = k.sb([128, 2, 256], BF16, "hidT")
                        hx = k.sb([128, 256], F32, "hx")
                        hy = k.sb([128, 256], F32, "hy")
                        hb = k.sb([128, 2], F32, "hb")
                        bw1, bw2, bcp, bhid, bhx, bhy, bhb = P.bufs(7)
                        for which, (srcT, bsrc) in enumerate([(KcT, bKcT), (VcT, bVcT)]):
                            k.dma('pool', w1[:], cw1_in[which].rearrange("(l d) h -> d l h", d=128), (), [bw1])
                            k.dma('pool', w2[:], cw2_in[which].rearrange("(t p) d -> p t d", p=128), (), [bw2])
                            load_fm(cpT[:], cpos_in[which], 32, bcp, bank=3)
                            k.cp('dve', cpTb[:], cpT[:], [bcp], [bcp])
                            k.memset('dve', hidT[:], 0.0, [bhid])
                            for ht in range(2):
                                for l in range(32):
                                    k.mm(banks[0][:, 0:255], w1[:, l, ht * 128:(ht + 1) * 128], srcT[:, l:l + 16 * 254 + 1:16],
                                         l == 0, l == 31, [bw1, bsrc], [bbank[0]])
                                for l in range(32):
                                    k.mm(banks[1][:, 0:1], w1[:, l, ht * 128:(ht + 1) * 128], cpTb[:, l:l + 1],
                                         l == 0, l == 31, [bw1, bcp], [bbank[1]])
                                k.cp('dve', hb[:, ht:ht + 1], banks[1][:, 0:1], [bbank[1]], [bhb])
                                k.act(hx[:, 0:255], banks[0][:, 0:255], AF.Identity, [bbank[0], bhb], [bhx], bias=hb[:, ht:ht + 1])
                                k.tt('dve', hy[:, 0:255], hx[:, 0:255], hx[:, 0:255], ALU.mult, [bhx], [bhy])
                                k.ts('dve', hy[:, 0:255], hy[:, 0:255], 0.044715, 1.0, ALU.mult, ALU.add, [bhy], [bhy])
                                k.tt('dve', hy[:, 0:255], hy[:, 0:255], hx[:, 0:255], ALU.mult, [bhy, bhx], [bhy])
                                k.act(hy[:, 0:255], hy[:, 0:255], AF.Sigmoid, [bhy], [bhy], scale=1.5957691216057308)
                                k.tt('dve', hidT[:, ht, 0:255], hy[:, 0:255], hx[:, 0:255], ALU.mult, [bhy, bhx], [bhid])
                            if which == 0:
                                for ht in range(2):
                                    k.mm(banks[2][:, 0:255], w2[:, ht, :], hidT[:, ht, 0:255], ht == 0, ht == 1, [bw2, bhid], [bbank[2]])
                                k.cp('dve', KcmpT[:, 0:255], banks[2][:, 0:255], [bbank[2]], [bKc])
                            else:
                                for j in range(2):
                                    nn = 128 if j == 0 else 127
                                    for ht in range(2):
                                        k.mm(banks[2][0:nn, 0:128], hidT[:, ht, j * 128:j * 128 + nn], w2[:, ht, :], ht == 0, ht == 1, [bw2, bhid], [bbank[2]])
                                    k.memset('dve', RHSc[:, j, 65:193], 0.0, [bRc])
                                    k.cp('dve', RHSc[0:nn, j, 65:193], banks[2][0:nn, 0:128], [bbank[2]], [bRc])
                    impacc = k.sb([128, 64], F32, "impacc")
                    v1 = k.sb([128, 64], F32, "selv1")
                    v2 = k.sb([128, 64], F32, "selv2")
                    m8 = k.sb([128, 16], F32, "m8")
                    selb = k.sb([128, 64], F32, "selb")
                    selbT = k.sb([64, 128], BF16, "selbT")
                    Oacc = k.sb([128, 8, 128], F32, "Oacc")
                    o16 = [k.sb([128, 128], BF16, "no16_%d" % i) for i in range(2)]
                    Et = [k.sb([128, 128], BF16, "Et%d" % i) for i in range(4)]
                    sc_ = k.sb([128, 8, 8], F32, "nsc_small")
                    bimp, bsel, bselT = P.bufs(3)
                    bOh = P.bufs(8)
                    bsch = P.bufs(8)
                    bo16 = P.bufs(2)
                    bEt = P.bufs(4)
                    ectr = [0]

                    def score_exp(lhsK, bK_, rhsQ, bQ_, extra, bias_ap):
                        e = ectr[0] % 4
                        bk = ectr[0] % 2
                        ectr[0] += 1
                        k.mm(banks[bk][:, 0:128], lhsK, rhsQ, True, len(extra) == 0, [bK_, bQ_], [bbank[bk]])
                        for xi, (l_, r_, bl_) in enumerate(extra):
                            k.mm(banks[bk][:, 0:128], l_, r_, False, xi == len(extra) - 1, bl_, [bbank[bk]])
                        if bias_ap is None:
                            k.act(Et[e][:], banks[bk][:, 0:128], AF.Exp, [bbank[bk]], [bEt[e]])
                        else:
                            k.act(Et[e][:], banks[bk][:, 0:128], AF.Exp, [bbank[bk], b_nc], [bEt[e]], bias=bias_ap)
                        return Et[e], bEt[e]

                    def tbank():
                        bk = ectr[0] % 2
                        ectr[0] += 1
                        return bk

                    for qt in range(NQT if NSA_QT_RUN is None else NSA_QT_RUN):
                        qsl = slice(qt * 128, (qt + 1) * 128)
                        for hh in range(8):
                            head = g * 8 + hh
                            sch, bsc = sc_[:, hh, :], bsch[hh]
                            cbk = 3 + (hh % 3)
                            ets = []
                            for j in range(2):
                                u0 = (OWN0 + 128 * qt) if j == 0 else 128 * qt
                                ets.append(score_exp(KcmpT[:, j * 128:(j + 1) * 128], bKc, QTh[hh][:, qsl], bQh[hh],
                                                     [(identb[:], maskB[:, u0:u0 + 128], [b_const, b_nc])], kvb if j == 0 else None))
                            for j in range(2):
                                k.mm(banks[cbk][:, 0:193], ets[j][0][:], RHSc[:, j, :], j == 0, j == 1, [ets[j][1], bRc], [bbank[cbk]])
                            k.ts('dve', sch[:, 0:1], banks[cbk][:, 64:65], 1e-30, None, ALU.max, None, [bbank[cbk]], [bsc])
                            k.recip(sch[:, 1:2], sch[:, 0:1], [bsc], [bsc])
                            if hh == 0:
                                k.ts('dve', impacc[:], banks[cbk][:, 0:64], sch[:, 1:2], None, ALU.mult, None, [bbank[cbk], bsc], [bimp])
                            else:
                                k.stt(impacc[:], banks[cbk][:, 0:64], sch[:, 1:2], impacc[:], ALU.mult, ALU.add, [bbank[cbk], bsc, bimp], [bimp])
                            k.ts('dve', sch[:, 2:3], gts[:, qt, head * 3:head * 3 + 1], sch[:, 1:2], None, ALU.mult, None, [b_g, bsc], [bsc])
                            k.ts('dve', Oacc[:, hh, :], banks[cbk][:, 65:193], sch[:, 2:3], None, ALU.mult, None, [bbank[cbk], bsc], [bOh[hh]])
                        k.tt('dve', v1[:], impacc[:], fv[:, 0, qt, :], ALU.max, [bimp, b_nc], [bsel])
                        k.tt('dve', v1[:], v1[:], ncore[:, 0:64], ALU.max, [bsel, b_nc], [bsel])
                        k.tt('dve', v1[:], v1[:], fv[:, 1, qt, :], ALU.min, [bsel, b_nc], [bsel])
                        k.tt('dve', v1[:], v1[:], ncore[:, 64:128], ALU.min, [bsel, b_nc], [bsel])
                        P.op('dve', lambda e: e.max(out=m8[:, 0:8], in_=v1[:]), [bsel], [bsel])
                        P.op('dve', lambda e: e.match_replace(out=v2[:], in_to_replace=m8[:, 0:8], in_values=v1[:], imm_value=-3.0e38), [bsel], [bsel])
                        P.op('dve', lambda e: e.max(out=m8[:, 8:16], in_=v2[:]), [bsel], [bsel])
                        k.ts('dve', v2[:], v1[:], m8[:, 15:16], None, ALU.is_ge, None, [bsel], [bsel])
                        k.ts('dve', selb[:], v1[:], -1.0e29, None, ALU.is_gt, None, [bsel], [bsel])
                        k.tt('dve', selb[:], selb[:], v2[:], ALU.mult, [bsel], [bsel])
                        k.ts('dve', selb[:], selb[:], -1.0, 30000.0, ALU.add, ALU.mult, [bsel], [bsel])
                        tb = tbank()
                        k.tr(banks[tb][0:64, 0:128], selb[:], ident[:], [bsel, b_const], [bbank[tb]])
                        k.cp('dve', selbT[:], banks[tb][0:64, 0:128], [bbank[tb]], [bselT])
                        for hh in range(8):
                            head = g * 8 + hh
                            sch, bsc, bO = sc_[:, hh, :], bsch[hh], bOh[hh]
                            sbk = 4 if hh % 2 == 0 else 2
                            wbk = 5 if hh % 2 == 0 else 3
                            kdiag = OWN0 // 128 + qt
                            tiles = []
                            for kt in range(0, kdiag + 1):
                                ex = [(eexp[:, kt * 128:(kt + 1) * 128], selbT[:], [b_nc, bselT])]
                                if kt == kdiag:
                                    ex.append((identb[:], cmk[:, 0, :], [b_const, b_nc]))
                                tiles.append((KsT[:, kt * 128:(kt + 1) * 128], bKs, ex, None, sbk, Vs1[:, kt, :], bVs, kt == 0, kt == kdiag))
                            for kt in range(kdiag - 4, kdiag + 1):
                                ex = []
                                if kt == kdiag - 4:
                                    ex.append((identb[:], cmk[:, 1, :], [b_const, b_nc]))
                                if kt == kdiag:
                                    ex.append((identb[:], cmk[:, 0, :], [b_const, b_nc]))
                                tiles.append((KwT[:, kt * 128:(kt + 1) * 128], bKw, ex, kvb if kt < OWN0 // 128 else None, wbk, Vw1[:, kt, :], bVw,
                                              kt == kdiag - 4, kt == kdiag))
                            pend = None
                            for (lk, blk_, ex, bias_, abk, vap, bv_, st_f, sp_f) in tiles:
                                et, bet = score_exp(lk, blk_, QTh[hh][:, qsl], bQh[hh], ex, bias_)
                                if pend is not None:
                                    pe_, pb_, pa_, pv_, pbv_, ps_, pp_ = pend
                                    k.mm(banks[pa_][:, 0:129], pe_[:], pv_, ps_, pp_, [pb_, pbv_], [bbank[pa_]])
                                pend = (et, bet, abk, vap, bv_, st_f, sp_f)
                            pe_, pb_, pa_, pv_, pbv_, ps_, pp_ = pend
                            k.mm(banks[pa_][:, 0:129], pe_[:], pv_, ps_, pp_, [pb_, pbv_], [bbank[pa_]])
                            k.ts('dve', sch[:, 3:4], banks[sbk][:, 128:129], 1e-30, None, ALU.max, None, [bbank[sbk]], [bsc])
                            k.recip(sch[:, 4:5], sch[:, 3:4], [bsc], [bsc])
                            k.ts('dve', sch[:, 4:5], sch[:, 4:5], gts[:, qt, head * 3 + 1:head * 3 + 2], None, ALU.mult, None, [b_g, bsc], [bsc])
                            k.stt(Oacc[:, hh, :], banks[sbk][:, 0:128], sch[:, 4:5], Oacc[:, hh, :], ALU.mult, ALU.add, [bbank[sbk], bsc, bO], [bO])
                            k.ts('dve', sch[:, 5:6], banks[wbk][:, 128:129], 1e-30, None, ALU.max, None, [bbank[wbk]], [bsc])
                            k.recip(sch[:, 6:7], sch[:, 5:6], [bsc], [bsc])
                            k.ts('dve', sch[:, 6:7], sch[:, 6:7], gts[:, qt, head * 3 + 2:head * 3 + 3], None, ALU.mult, None, [b_g, bsc], [bsc])
                            k.stt(Oacc[:, hh, :], banks[wbk][:, 0:128], sch[:, 6:7], Oacc[:, hh, :], ALU.mult, ALU.add, [bbank[wbk], bsc, bO], [bO])
                            oi = hh % 2
                            tb = tbank()
                            k.tr(banks[tb][:, 0:128], Oacc[:, hh, :], ident[:], [bO, b_const], [bbank[tb]])
                            k.cp('dve', o16[oi][:], banks[tb][:, 0:128], [bbank[tb]], [bo16[oi]])
                            k.dma('sp', OBT[head * 128:(head + 1) * 128, qsl], o16[oi][:], [bo16[oi]], ())
        if stage == 4 and DEBUG_OUT and not SKIP_NSA:
            dbg_ob = nc.dram_tensor("dbg_ob", [2048, NOWN], BF16, kind="ExternalOutput").ap()
            k.dma('sp', dbg_ob, OBT, (), ())

    def bcast_row(dst, src_fm, n, bsrc, bdst, name):
        rowd = k.dram([1, n * 128], F32, "row_" + name)
        t_ = P.dma('sp', rowd[0].rearrange("(c p) -> p c", p=128), src_fm, [bsrc], (), allow_slow_non_contiguous=True)
        brow = P.buf()
        brow.lw = t_
        k.dma('sp', dst, rowd[0].partition_broadcast(128), [brow], [bdst])

    if stage >= 5 and not SKIP_P5:
        wbg_in = din("w_branch_gdn", [1024, D])
        wbn_in = din("w_branch_nsa", [D, D])
        wout_in = din("w_out", [D, D])
        with k.scope():
            Wg = k.sb([128, 8, D], BF16, "Wg")
            Wn = k.sb([128, 16, D], BF16, "Wn")
            bWg, bWn = P.bufs(2)
            for kc in range(8):
                k.dma('pool', Wg[:, kc, :], wbg_in[kc * 128:(kc + 1) * 128, :], (), [bWg])
            for kc in range(16):
                k.dma('pool', Wn[:, kc, :], wbn_in[kc * 128:(kc + 1) * 128, :], (), [bWn])
            oat = [k.sb([128, 8, 512], BF16, "oat%d" % i) for i in range(2)]
            obt = [k.sb([128, 16, 512], BF16, "obt%d" % i) for i in range(2)]
            boat, bobt = P.bufs(2), P.bufs(2)
            ga = [k.sb([128, 512], F32, "ga%d" % i) for i in range(2)]
            gb = [k.sb([128, 512], F32, "gb%d" % i) for i in range(2)]
            y16 = [k.sb([128, 512], BF16, "y16_%d" % i) for i in range(2)]
            bga, bgb, by16 = P.bufs(2), P.bufs(2), P.bufs(2)
            OATv = OAT.rearrange("(kc p) t -> p kc t", p=128)
            OBTv = OBT.rearrange("(kc p) t -> p kc t", p=128)
            it = 0
            for tt in range(NOWN // 512):
                s2 = tt % 2
                tsl = slice(tt * 512, (tt + 1) * 512)
                k.dma('sp', oat[s2][:], OATv[:, :, tsl], (), [boat[s2]])
                k.dma('sp', obt[s2][:], OBTv[:, :, tsl], (), [bobt[s2]])
                for ct in range(16):
                    s = it % 2
                    it += 1
                    bA, bB = (0, 1) if s == 0 else (2, 3)
                    csl = slice(ct * 128, (ct + 1) * 128)
                    k.dma('sp', ga[s][:], PF[6144 + ct * 128:6144 + (ct + 1) * 128, OWN0 + tt * 512:OWN0 + (tt + 1) * 512], (), [bga[s]])
                    k.dma('sp', gb[s][:], PF[8192 + ct * 128:8192 + (ct + 1) * 128, OWN0 + tt * 512:OWN0 + (tt + 1) * 512], (), [bgb[s]])
                    k.act(ga[s][:], ga[s][:], AF.Sigmoid, [bga[s]], [bga[s]])
                    k.act(gb[s][:], gb[s][:], AF.Sigmoid, [bgb[s]], [bgb[s]])
                    for kc in range(8):
                        k.mm(banks[bA][:, :], Wg[:, kc, csl], oat[s2][:, kc, :], kc == 0, kc == 7, [bWg, boat[s2]], [bbank[bA]])
                    for kc in range(16):
                        k.mm(banks[bB][:, :], Wn[:, kc, csl], obt[s2][:, kc, :], kc == 0, kc == 15, [bWn, bobt[s2]], [bbank[bB]])
                    k.tt('dve', ga[s][:], ga[s][:], banks[bA][:, :], ALU.mult, [bga[s], bbank[bA]], [bga[s]])
                    k.tt('dve', gb[s][:], gb[s][:], banks[bB][:, :], ALU.mult, [bgb[s], bbank[bB]], [bgb[s]])
                    k.tt('pool', y16[s][:], ga[s][:], gb[s][:], ALU.add, [bga[s], bgb[s]], [by16[s]])
                    k.dma('sp', YT[csl, tsl], y16[s][:], [by16[s]], ())
        with k.scope():
            Wo = k.sb([128, 16, D], BF16, "Wo")
            g1b = k.sb([128, D], F32, "g1b")
            bWo, bg1 = P.bufs(2)
            for kc in range(16):
                k.dma('pool', Wo[:, kc, :], wout_in[kc * 128:(kc + 1) * 128, :], (), [bWo])
            bcast_row(g1b[:], modT[:, 32:48], 16, b_mod, bg1, "g1")
            yt = [k.sb([128, 16, 128], BF16, "yt%d" % i) for i in range(2)]
            xo = [k.sb([128, D], F32, "xo%d" % i) for i in range(2)]
            zt_ = [k.sb([128, 512], F32, "zt5_%d" % i) for i in range(2)]
            byt, bxo, bzt = P.bufs(2), P.bufs(2), P.bufs(2)
            YTv = YT.rearrange("(kc p) t -> p kc t", p=128)
            it = 0
            for t in range(NOWN // 128):
                s = t % 2
                k.dma('sp', yt[s][:], YTv[:, :, t * 128:(t + 1) * 128], (), [byt[s]])
                k.dma('sp', xo[s][:], xl[OWN0 + t * 128:OWN0 + (t + 1) * 128, :], (), [bxo[s]])
                for cb_ in range(4):
                    z2 = it % 2
                    bk = it % 4
                    it += 1
                    csl = slice(cb_ * 512, (cb_ + 1) * 512)
                    for kc in range(16):
                        k.mm(banks[bk][:, :], yt[s][:, kc, :], Wo[:, kc, csl], kc == 0, kc == 15, [byt[s], bWo], [bbank[bk]])
                    k.tt('dve', zt_[z2][:], banks[bk][:, :], g1b[:, csl], ALU.mult, [bbank[bk], bg1], [bzt[z2]])
                    k.tt('pool', xo[s][:, csl], xo[s][:, csl], zt_[z2][:], ALU.add, [bxo[s], bzt[z2]], [bxo[s]])
                k.dma('sp', X1[t * 128:(t + 1) * 128, :], xo[s][:], [bxo[s]], ())
        P.barrier()
        if stage == 5 and DEBUG_OUT:
            dbg_x1 = nc.dram_tensor("dbg_x1", [NOWN, D], F32, kind="ExternalOutput").ap()
            k.dma('sp', dbg_x1, X1, (), ())

    if stage == 6 and DEBUG_OUT:
        X2dbg = nc.dram_tensor("dbg_x2", [NOWN, D], F32, kind="ExternalOutput").ap()
    if stage >= 6:
        n2_in = din("norm2_w", [KC, 128])
        wq_in = din("peer_wq", [D, D])
        pk_in = [din("peer_keys1", [8, 128, 128]), din("peer_keys2", [8, 128, 128])]
        fnw6_in = din("final_norm_w", [1, D])
        H2T = k.dram([KC, 128, NOWN], BF16, "H2T")
        NEB = 64 if PEER_EB is None else PEER_EB
        with k.scope():
            A2 = k.sb([128, KC], F32, "A2")
            n2T = k.sb([128, KC], F32, "n2T")
            keysT = k.sb([128, 16, 128], BF16, "keysT")
            bA2, bn2, bg2, bfn, bkT = P.bufs(5)
            load_fm(n2T[:], n2_in, KC, bn2)
            k.stt(A2[:], modT[:, 64:80], 1.0, n2T[:], ALU.add, ALU.mult, [b_mod, bn2], [bA2])
            g2row = k.dram([1, D], F32, "row_g2")
            k.dma('sp', g2row[0].rearrange("(c p) -> p c", p=128), modT[:, 80:96], [b_mod], (), allow_slow_non_contiguous=True)
            with k.scope():
                kt_ = [k.sb([128, 128], F32, "kraw%d" % i) for i in range(2)]
                bkr = P.bufs(2)
                for hh2 in range(16):
                    s = hh2 % 2
                    k.dma('sp', kt_[s][:], pk_in[hh2 % 2][hh2 // 2], (), [bkr[s]])
                    k.tr(banks[s][:, 0:128], kt_[s][:], ident[:], [bkr[s], b_const], [bbank[s]])
                    k.cp('dve', keysT[:, hh2, :], banks[s][:, 0:128], [bbank[s]], [bkT])
            with k.scope():
                xt = [k.sb([128, D], F32, "x6t%d" % i) for i in range(2)]
                xs = [k.sb([128, D], BF16, "x6s%d" % i) for i in range(2)]
                st = [k.sb([128, 4], F32, "s6t%d" % i) for i in range(2)]
                ho = [k.sb([128, KC, 128], BF16, "h6o%d" % i) for i in range(2)]
                bxt, bxs, bst, bho = P.bufs(2), P.bufs(2), P.bufs(2), P.bufs(2)
                for t in range(NOWN // 128):
                    s = t % 2
                    k.dma('sp', xt[s][:], X1[t * 128:(t + 1) * 128, :], (), [bxt[s]])
                    k.act(xs[s][:], xt[s][:], AF.Square, [bxt[s]], [bxs[s], bst[s]], accum=st[s][:, 0:1])
                    k.ts('dve', st[s][:, 1:2], st[s][:, 0:1], 1.0 / D, 1e-6, ALU.mult, ALU.add, [bst[s]], [bst[s]])
                    k.act(st[s][:, 2:3], st[s][:, 1:2], AF.Sqrt, [bst[s]], [bst[s]])
                    k.recip(st[s][:, 3:4], st[s][:, 2:3], [bst[s]], [bst[s]])
                    k.ts('dve', xs[s][:], xt[s][:], st[s][:, 3:4], None, ALU.mult, None, [bxt[s], bst[s]], [bxs[s]])
                    for kc in range(KC):
                        pi = kc % 2
                        sl = slice((kc // 2 % 4) * 128, (kc // 2 % 4) * 128 + 128)
                        k.tr(pbf[pi][:, sl], xs[s][:, kc * 128:(kc + 1) * 128], identb[:], [bxs[s], b_const], [bpbf[pi]])
                        if kc % 2 == 0:
                            k.act(ho[s][:, kc, :], pbf[pi][:, sl], AF.Identity, [bpbf[pi], bA2, b_mod], [bho[s]],
                                  bias=modT[:, 48 + kc:49 + kc], scale=A2[:, kc:kc + 1])
                        else:
                            k.ts('dve', ho[s][:, kc, :], pbf[pi][:, sl], A2[:, kc:kc + 1], modT[:, 48 + kc:49 + kc],
                                 ALU.mult, ALU.add, [bpbf[pi], bA2, b_mod], [bho[s]])
                    k.dma('sp', H2T[:, :, t * 128:(t + 1) * 128].rearrange("c p t -> p c t"), ho[s][:], [bho[s]], ())
            P.barrier()
            wqv = wq_in.rearrange("(kc p) c -> p kc c", p=128)
            outv6 = out_d.rearrange("(t p) d -> t p d", p=128)
            for tg in range(4 if PEER_TG is None else PEER_TG):
                with k.scope():
                    h2g = k.sb([128, KC, 512], BF16, "h2g")
                    stile = k.sb([128, 4, 16, 128], F32, "stile")
                    Bt = k.sb([128, 4, 8, 128], F32, "Bt")
                    tau = k.sb([128, 4, 8], F32, "tau")
                    OUTacc = k.sb([128, 4, D], F32, "OUTacc")
                    bh2, bst_, bBt, btau, bOut = P.bufs(5)
                    k.dma('sp', h2g[:], H2T[:, :, tg * 512:(tg + 1) * 512].rearrange("c p t -> p c t"), (), [bh2])
                    k.memset('pool', OUTacc[:], 0.0, [bOut])
                    with k.scope():
                        wqb = [k.sb([128, KC, 128], BF16, "wqb%d" % i) for i in range(2)]
                        qh = [k.sb([128, 512], BF16, "qh%d" % i) for i in range(2)]
                        bwq, bqh = P.bufs(2), P.bufs(2)
                        for hh2 in range(16):
                            s = hh2 % 2
                            k.dma('pool', wqb[s][:], wqv[:, :, hh2 * 128:(hh2 + 1) * 128], (), [bwq[s]])
                            for kc in range(KC):
                                k.mm(banks[s][:, :], wqb[s][:, kc, :], h2g[:, kc, :], kc == 0, kc == KC - 1, [bwq[s], bh2], [bbank[s]])
                            k.cp('act', qh[s][:], banks[s][:, :], [bbank[s]], [bqh[s]])
                            for tl in range(4):
                                k.mm(banks[2 + s][:, tl * 128:(tl + 1) * 128], qh[s][:, tl * 128:(tl + 1) * 128], keysT[:, hh2, :], True, True,
                                     [bqh[s], bkT], [bbank[2 + s]])
                            k.cp('dve', stile[:, :, hh2, :], banks[2 + s][:, :].rearrange("p (t j) -> p t j", t=4), [bbank[2 + s]], [bst_])
                    with k.scope():
                        SC = []
                        for ci in range(2):
                            SC.append(dict(v16=k.sb([128, 2, 16], F32, "v16_%d" % ci), vv=k.sb([128, 2, 128], F32, "vv_%d" % ci),
                                           cand=k.sb([128, 16, 16], F32, "cand_%d" % ci), cv=k.sb([128, 256], F32, "cv_%d" % ci),
                                           c16=k.sb([128, 16], F32, "c16_%d" % ci), sm=k.sb([128, 8], F32, "sm_%d" % ci), b=P.buf()))

                        def p3_chain(tl, h, S_):
                            v16, vv, cand, cv, c16, sm, bs = S_["v16"], S_["vv"], S_["cand"], S_["cv"], S_["c16"], S_["sm"], S_["b"]
                            ops = []
                            for half in range(2):
                                src = stile[:, tl, 2 * h + half, :]
                                ops.append(lambda src=src, half=half: P.op('dve', lambda e: e.max(out=v16[:, half, 0:8], in_=src), [bst_], [bs]))
                            for half in range(2):
                                src = stile[:, tl, 2 * h + half, :]
                                ops.append(lambda src=src, half=half: P.op('dve', lambda e: e.match_replace(out=vv[:, half, :], in_to_replace=v16[:, half, 0:8], in_values=src, imm_value=-3.0e38), [bst_, bs], [bs]))
                            for half in range(2):
                                ops.append(lambda half=half: P.op('dve', lambda e: e.max(out=v16[:, half, 8:16], in_=vv[:, half, :]), [bs], [bs]))
                            ops.append(lambda: k.tt('dve', cand[:], v16[:, 1, :].unsqueeze(1).to_broadcast([128, 16, 16]),
                                                    v16[:, 0, :].unsqueeze(2).to_broadcast([128, 16, 16]), ALU.add, [bs], [bs]))
                            cf = cand[:].rearrange("p a b -> p (a b)")
                            ops.append(lambda: P.op('dve', lambda e: e.max(out=c16[:, 0:8], in_=cf), [bs], [bs]))
                            ops.append(lambda: P.op('dve', lambda e: e.match_replace(out=cv[:], in_to_replace=c16[:, 0:8], in_values=cf, imm_value=-3.0e38), [bs], [bs]))
                            ops.append(lambda: P.op('dve', lambda e: e.max(out=c16[:, 8:16], in_=cv[:]), [bs], [bs]))
                            ops.append(lambda: k.ts('dve', sm[:, 0:1], c16[:, 0:1], -1.0, None, ALU.mult, None, [bs], [bs]))
                            ops.append(lambda: k.act(cv[:, 0:16], c16[:], AF.Exp, [bs], [bs], bias=sm[:, 0:1], accum=sm[:, 1:2]))
                            ops.append(lambda: k.act(sm[:, 2:3], sm[:, 1:2], AF.Ln, [bs], [bs]))
                            ops.append(lambda: k.tt('dve', sm[:, 3:4], sm[:, 0:1], sm[:, 2:3], ALU.subtract, [bs], [bs]))
                            ops.append(lambda: k.ts('dve', Bt[:, tl, h, :], stile[:, tl, 2 * h, :], sm[:, 3:4], None, ALU.add, None, [bst_, bs], [bBt]))
                            ops.append(lambda: k.act(sm[:, 4:5], c16[:, 15:16], AF.Exp, [bs], [bs], bias=sm[:, 3:4]))
                            ops.append(lambda: k.ts('dve', tau[:, tl, h:h + 1], sm[:, 4:5], 0.99999, None, ALU.mult, None, [bs], [btau]))
                            return ops

                        pairs = [(tl, h) for tl in range(4) for h in range(8)]
                        for pi_ in range(0, len(pairs), 2):
                            ca = p3_chain(pairs[pi_][0], pairs[pi_][1], SC[0])
                            cb2 = p3_chain(pairs[pi_ + 1][0], pairs[pi_ + 1][1], SC[1])
                            for oa, ob_ in zip(ca, cb2):
                                oa()
                                ob_()
                    with k.scope():
                        ub = [k.sb([128, KC, 256], BF16, "ub%d" % i) for i in range(2)]
                        vb = [k.sb([128, 2, D], BF16, "vb%d" % i) for i in range(2)]
                        bub, bvb = P.bufs(2), P.bufs(2)
                        gx_ = [k.sb([128, 256], F32, "gx%d" % i) for i in range(2)]
                        gy_ = [k.sb([128, 256], F32, "gy%d" % i) for i in range(2)]
                        es_ = [k.sb([128, 8, 128], F32, "es%d" % i) for i in range(2)]
                        mk_ = [k.sb([128, 8, 128], F32, "mk%d" % i) for i in range(2)]
                        coef_ = [k.sb([128, 2, 128], F32, "coef%d" % i) for i in range(2)]
                        G16_ = [k.sb([128, 256], BF16, "G16_%d" % i) for i in range(2)]
                        GT_ = [[k.sb([128, 128], BF16, "GT%d_%d" % (p_, i)) for i in range(2)] for p_ in range(2)]
                        bgx_, bgy_, bmk_, bG16_ = P.bufs(2), P.bufs(2), P.bufs(2), P.bufs(2)
                        bes_ = [P.bufs(8) for p_ in range(2)]
                        bcoef_ = [P.bufs(2) for p_ in range(2)]
                        bOutS = [[P.buf() for c_ in range(4)] for t_ in range(4)]
                        bGT_ = [P.bufs(2) for p_ in range(2)]
                        pit = 0
                        def head(eb, tl, par, s):
                            gx, gy, coef, G16 = gx_[par], gy_[par], coef_[par], G16_[par]
                            bgx, bgy, bcoef, bG16 = bgx_[par], bgy_[par], bcoef_[par], bG16_[par]
                            for kc in range(KC):
                                k.mm(banks[par][:, 0:256], h2g[:, kc, tl * 128:(tl + 1) * 128], ub[s][:, kc, :], kc == 0, kc == KC - 1,
                                     [bh2, bub[s]], [bbank[par]])
                            k.cp('act', gx[:], banks[par][:, 0:256], [bbank[par]], [bgx])
                            k.tt('pool', gy[:], gx[:], gx[:], ALU.mult, [bgx], [bgy])
                            k.ts('pool', gy[:], gy[:], 0.044715, 1.0, ALU.mult, ALU.add, [bgy], [bgy])
                            k.tt('pool', gy[:], gy[:], gx[:], ALU.mult, [bgy, bgx], [bgy])
                            k.act(gy[:], gy[:], AF.Sigmoid, [bgy], [bgy], scale=1.5957691216057308)
                            k.tt('pool', gy[:], gy[:], gx[:], ALU.mult, [bgy, bgx], [bgy])
                            for il in range(2):
                                i_ = 2 * eb + il
                                for h in range(8):
                                    k.act(es_[il][:, h, :], stile[:, tl, 2 * h + 1, :], AF.Exp, [bst_, bBt], [bes_[il][h]], bias=Bt[:, tl, h, i_:i_ + 1])
                            taub = tau[:, tl, :].unsqueeze(2).to_broadcast([128, 8, 128])
                            for il in range(2):
                                k.tt('dve', mk_[il][:], es_[il][:], taub, ALU.is_ge, bes_[il] + [btau], [bmk_[il]])
                            for il in range(2):
                                k.tt('dve', mk_[il][:], mk_[il][:], es_[il][:], ALU.mult, [bmk_[il]] + bes_[il], [bmk_[il]])
                            for il in range(2):
                                k.tt('dve', mk_[il][:, 0:4, :], mk_[il][:, 0:4, :], mk_[il][:, 4:8, :], ALU.add, [bmk_[il]], [bmk_[il]])
                            for il in range(2):
                                k.tt('pool', mk_[il][:, 0:2, :], mk_[il][:, 0:2, :], mk_[il][:, 2:4, :], ALU.add, [bmk_[il]], [bmk_[il]])
                            for il in range(2):
                                k.tt('pool', coef[:, il, :], mk_[il][:, 0, :], mk_[il][:, 1, :], ALU.add, [bmk_[il]], [bcoef[il]])
                            k.tt('dve', G16[:], gy[:], coef[:].rearrange("p i j -> p (i j)"), ALU.mult, [bgy] + bcoef, [bG16])

                        def tail(eb, tl, par, s):
                            G16, GT, bG16, bGT = G16_[par], GT_[par], bG16_[par], bGT_[par]
                            for il in range(2):
                                k.tr(pbf[il][:, par * 128:(par + 1) * 128], G16[:, il * 128:(il + 1) * 128], identb[:], [bG16, b_const], [bpbf[il]])
                                k.cp('act', GT[il][:], pbf[il][:, par * 128:(par + 1) * 128], [bpbf[il]], [bGT[il]])
                            for cb_ in range(4):
                                bk = 2 + cb_
                                csl = slice(cb_ * 512, (cb_ + 1) * 512)
                                k.mm(banks[bk][:, :], GT[0][:], vb[s][:, 0, csl], True, False, [bGT[0], bvb[s]], [bbank[bk]])
                                k.mm(banks[bk][:, :], GT[1][:], vb[s][:, 1, csl], False, True, [bGT[1], bvb[s]], [bbank[bk]])
                                k.tt('dve', OUTacc[:, tl, csl], OUTacc[:, tl, csl], banks[bk][:, :], ALU.add, [bOut, bOutS[tl][cb_], bbank[bk]], [bOutS[tl][cb_]])

                        prev = None
                        pit = 0
                        for eb in range(NEB):
                            s = eb % 2
                            k.dma('sp', ub[s][:], UBd[eb], [bconvU[eb]], [bub[s]])
                            k.dma('sp', vb[s][:], VBd[eb], [bconvV[eb]], [bvb[s]])
                            for tl in range(4):
                                par = pit % 2
                                pit += 1
                                head(eb, tl, par, s)
                                if prev is not None:
                                    tail(*prev)
                                prev = (eb, tl, par, s)
                        tail(*prev)
                    with k.scope():
                        g2b = k.sb([128, D], F32, "g2b")
                        fnw6 = k.sb([128, D], F32, "fnw6")
                        k.dma('sp', g2b[:], g2row[0].partition_broadcast(128), (), [bg2])
                        k.dma('sp', fnw6[:], fnw6_in[0].partition_broadcast(128), (), [bfn])
                        x1t = [k.sb([128, D], F32, "x1t%d" % i) for i in range(2)]
                        fj6 = [k.sb([128, D], BF16, "fj6%d" % i) for i in range(2)]
                        fs6 = [k.sb([128, 4], F32, "fs6%d" % i) for i in range(2)]
                        bx1t, bfj6, bfs6 = P.bufs(2), P.bufs(2), P.bufs(2)
                        for tl in range(4):
                            s = tl % 2
                            t = tg * 4 + tl
                            k.dma('sp', x1t[s][:], X1[t * 128:(t + 1) * 128, :], (), [bx1t[s]])
                            k.tt('dve', OUTacc[:, tl, :], OUTacc[:, tl, :], g2b[:], ALU.mult, [bOut, bg2] + bOutS[tl], [bOut])
                            k.tt('dve', x1t[s][:], x1t[s][:], OUTacc[:, tl, :], ALU.add, [bx1t[s], bOut], [bx1t[s]])
                            if stage == 6 and DEBUG_OUT:
                                k.dma('sp', X2dbg[t * 128:(t + 1) * 128, :], x1t[s][:], [bx1t[s]], ())
                            k.act(fj6[s][:], x1t[s][:], AF.Square, [bx1t[s]], [bfj6[s], bfs6[s]], accum=fs6[s][:, 0:1])
                            k.ts('dve', fs6[s][:, 1:2], fs6[s][:, 0:1], 1.0 / D, 1e-6, ALU.mult, ALU.add, [bfs6[s]], [bfs6[s]])
                            k.act(fs6[s][:, 2:3], fs6[s][:, 1:2], AF.Sqrt, [bfs6[s]], [bfs6[s]])
                            k.recip(fs6[s][:, 3:4], fs6[s][:, 2:3], [bfs6[s]], [bfs6[s]])
                            k.stt(x1t[s][:], x1t[s][:], fs6[s][:, 3:4], fnw6[:], ALU.mult, ALU.mult, [bx1t[s], bfs6[s], bfn], [bx1t[s]])
                            k.dma('sp', outv6[t], x1t[s][:], [bx1t[s]], ())

    if stage < 6:
      pass
    fnw_in = din("final_norm_w", [1, D]) if stage < 6 else None
    fnw = k.sb([128, D], F32, "fnw")
    b_fnw = P.buf()
    if stage < 6:
        k.dma('sp', fnw[:], fnw_in[0].partition_broadcast(128), (), [b_fnw])
    X2v = xl.rearrange("(t p) d -> t p d", p=128)
    outv = out_d.rearrange("(t p) d -> t p d", p=128)
    ft = [k.sb([128, D], F32, "ft%d" % i) for i in range(2)]
    fj = [k.sb([128, D], BF16, "fj%d" % i) for i in range(2)]
    fs = [k.sb([128, 4], F32, "fs%d" % i) for i in range(2)]
    bft, bfj, bfs = P.bufs(2), P.bufs(2), P.bufs(2)
    for t in range(NOWN // 128 if stage < 6 else 0):
        s = t % 2
        k.dma('sp', ft[s][:], X2v[OWN0 // 128 + t], (), [bft[s]])
        k.act(fj[s][:], ft[s][:], AF.Square, [bft[s]], [bfj[s], bfs[s]], accum=fs[s][:, 0:1])
        k.ts('dve', fs[s][:, 1:2], fs[s][:, 0:1], 1.0 / D, 1e-6, ALU.mult, ALU.add, [bfs[s]], [bfs[s]])
        k.act(fs[s][:, 2:3], fs[s][:, 1:2], AF.Sqrt, [bfs[s]], [bfs[s]])
        k.recip(fs[s][:, 3:4], fs[s][:, 2:3], [bfs[s]], [bfs[s]])
        k.stt(ft[s][:], ft[s][:], fs[s][:, 3:4], fnw[:], ALU.mult, ALU.mult, [bft[s], bfs[s], b_fnw], [bft[s]])
        k.dma('sp', outv[t], ft[s][:], [bft[s]], ())

    with nc.Block() as block:
        P.emit(block)
    k.es.close()
    return nc


def _gconst():
    NEGM = -30000.0
    r = np.arange(128)[:, None]
    c = np.arange(128)[None, :]
    g = np.zeros((128, 5, 128), np.float32)
    g[:, 4, :] = (r // 32 == c // 32)
    g[:, 0, :] = (r <= c)
    g[:, 1, :] = np.where(r > c, 0.0, NEGM)
    g[:, 2, :] = np.where(c > r, 0.0, NEGM)
    g[:, 3, :] = np.where(c >= r, 0.0, NEGM)
    return g


GCONST = _gconst()


def _nsa_consts():
    NEGM = -30000.0
    c = {}
    n128 = np.zeros((128, 385), np.float32)
    for m in range(16):
        n128[m + 16, m] = -1.0
    for m in range(16, 32):
        n128[m - 16, m] = 1.0
    half = 16
    inv_freq = (500000.0 ** (-np.arange(half, dtype=np.float32) / half)).astype(np.float32)
    for d in range(32):
        n128[d, 128] = inv_freq[d % 16]
    r = np.arange(128)[:, None]
    q = np.arange(128)[None, :]
    n128[:, 129:257] = np.where(r <= q, 0.0, NEGM)
    n128[:, 257:385] = np.where(r > q, 0.0, NEGM)
    c["nsa_c128"] = n128
    u = np.arange(LOC)[None, :]
    c["maskB"] = np.where(16 * r + 31 <= u, 0.0, NEGM).astype(np.float32)
    c["eexp"] = (np.arange(LOC)[None, :] // 64 == np.arange(64)[:, None]).astype(np.float32)
    ovl = np.zeros((128, 2, 65), np.float32)
    for j in range(2):
        for nl in range(128):
            n = j * 128 + nl
            if n >= 255:
                continue
            for sblk in range(64):
                if 16 * n < 64 * sblk + 64 and 16 * n + 32 > 64 * sblk:
                    ovl[nl, j, sblk] = 1.0
            ovl[nl, j, 64] = 1.0
    c["ovl"] = ovl
    fvv = np.zeros((128, 2, 16, 64), np.float32)
    for qt in range(16):
        for qq in range(128):
            t = OWN0 + qt * 128 + qq
            cur = t // 64
            for sblk in range(64):
                fb = 0.0
                if sblk == cur:
                    fb = 2.0e9
                elif sblk == cur - 1:
                    fb = 3.0e9
                fvv[qq, 0, qt, sblk] = fb
                fvv[qq, 1, qt, sblk] = 3.0e38 if 64 * sblk <= t else -1.0e30
    c["fbigvis"] = fvv
    return c


NSA_CONSTS = _nsa_consts()


def _nsa_core(half):
    a = np.zeros((128, 129), np.float32)
    first = 0 if half == 1 else 32
    a[:, first] = 1.0e9
    a[:, 64:128] = 3.0e38
    if half == 0:
        a[:, 64:64 + 32] = -1.0e30
        a[:, 128] = -30000.0
    return a


def _pos_loc(inp, b, half):
    p = np.asarray(inp['positions'], np.int32)[b]
    out = np.zeros((1, LOC), np.int32)
    if half == 1:
        out[0] = p
    else:
        out[0, OWN0:] = p[:NOWN]
    return out


PEER_UT_CACHE = {}


def make_in_maps(inp):
    x = np.asarray(inp['x'], np.float32)
    c = np.asarray(inp['c'], np.float32)
    maps = []
    ident = np.eye(128, dtype=np.float32)
    for core in range(8):
        b, half = core // 2, core % 2
        xl = np.zeros((LOC, D), np.float32)
        if half == 0:
            xl[OWN0:] = x[b, :NOWN]
        else:
            xl[:] = x[b]
        m = {
            "xl": xl,
            "cb": np.ascontiguousarray(c[b].reshape(KC, 128)),
            "ada_w": np.ascontiguousarray(inp['ada_w'][0]),
            "ada_b": np.ascontiguousarray(np.asarray(inp['ada_b'][0]).reshape(96, 128)),
            "norm1_w": np.ascontiguousarray(np.asarray(inp['norm1_w'][0]).reshape(KC, 128)),
            "w_in": np.ascontiguousarray(inp['w_in'][0]),
            "ident": ident,
            "pvalid": np.full((128, 1), float(half), np.float32),
            "w_branch_gdn": np.ascontiguousarray(inp['w_branch_gdn'][0], np.float32),
            "w_branch_nsa": np.ascontiguousarray(inp['w_branch_nsa'][0], np.float32),
            "w_out": np.ascontiguousarray(inp['w_out'][0], np.float32),
            "norm2_w": np.ascontiguousarray(np.asarray(inp['norm2_w'][0], np.float32).reshape(KC, 128)),
            "peer_wq": np.ascontiguousarray(inp['peer_wq'][0], np.float32),
            "peer_keys1": np.ascontiguousarray(inp['peer_keys1'][0], np.float32),
            "peer_keys2": np.ascontiguousarray(inp['peer_keys2'][0], np.float32),
            "peer_uT": PEER_UT_CACHE.get(id(inp['peer_u'])) if id(inp['peer_u']) in PEER_UT_CACHE else PEER_UT_CACHE.setdefault(id(inp['peer_u']), np.ascontiguousarray(np.asarray(inp['peer_u'][0], np.float32).T)),
            "peer_v": np.ascontiguousarray(inp['peer_v'][0], np.float32),
            "gconst": GCONST,
            "pos": np.ascontiguousarray(_pos_loc(inp, b, half)),
            "nsa_core": _nsa_core(half),
            "cmp_pos_k": np.ascontiguousarray(inp['cmp_pos_k'][0], np.float32), "cmp_pos_v": np.ascontiguousarray(inp['cmp_pos_v'][0], np.float32),
            "cmp_w1_k": np.ascontiguousarray(inp['cmp_w1_k'][0], np.float32), "cmp_w1_v": np.ascontiguousarray(inp['cmp_w1_v'][0], np.float32),
            "cmp_w2_k": np.ascontiguousarray(inp['cmp_w2_k'][0], np.float32), "cmp_w2_v": np.ascontiguousarray(inp['cmp_w2_v'][0], np.float32),
            **NSA_CONSTS,
            "gdn_conv_w": np.ascontiguousarray(np.asarray(inp['gdn_conv_w'][0], np.float32).reshape(96, 128)),
            "gdn_A_log": np.ascontiguousarray(np.broadcast_to(np.asarray(inp['gdn_A_log'][0], np.float32)[None, :], (32, 8))),
            "gdn_dt_bias": np.ascontiguousarray(np.broadcast_to(np.asarray(inp['gdn_dt_bias'][0], np.float32)[None, :], (32, 8))),
            "gdn_norm_w": np.ascontiguousarray(np.asarray(inp['gdn_norm_w'][0], np.float32).reshape(1, 128)),
            "final_norm_w": np.ascontiguousarray(np.asarray(inp['final_norm_w'], np.float32).reshape(1, D)),
        }
        maps.append(m)
    return maps


def kernel(**inputs):
    nc = build_program(6)
    maps = make_in_maps(inputs)
    res = run_bass_kernel_spmd(nc, maps, core_ids=list(range(8)))
    out = np.zeros((4, SEQ, D), np.float32)
    for core in range(8):
        b, half = core // 2, core % 2
        out[b, half * NOWN:(half + 1) * NOWN] = res.results[core]["out"]
    return out
```

```python
import numpy as np
from contextlib import ExitStack
import concourse.bass as bass
import concourse.mybir as mybir
from concourse.bass_utils import run_bass_kernel_spmd

F32 = mybir.dt.float32
BF16 = mybir.dt.bfloat16
I32 = mybir.dt.int32
ALU = mybir.AluOpType
AF = mybir.ActivationFunctionType
AX = mybir.AxisListType

ENGS = ['pe', 'act', 'dve', 'pool', 'sp']

D = 2048
SEQ = 4096
LOC = 4096
OWN0 = 2048
NOWN = 2048
D_IN = 11840
KC = 16
DEBUG_OUT = False
GDN_ONE_HEAD = False
FROM_REF = False
GDN_SUB = 9
PF_ROWS = 80 * 128
GDN_WAVES = None
GDN_STEPS = 10 ** 9
NSA_GROUPS_RUN = None
NSA_QT_RUN = None
SKIP_GDN = False
SKIP_NSA = False
SKIP_P5 = False
REF_IN = set()
PEER_EB = None
PEER_TG = None


class Buf:
    __slots__ = ('name', 'lw', 'rd', 'excl')

    def __init__(self, name):
        self.name = name
        self.lw = None
        self.rd = {}
        self.excl = False


class Prog:
    def __init__(self, nc, n_dma_sems=40):
        self.nc = nc
        self.ops = {e: [] for e in ENGS}
        self.cnt = {e: 0 for e in ENGS}
        self.esem = {e: nc.alloc_semaphore(name="es_" + e) for e in ENGS}
        self.dsem = [nc.alloc_semaphore(name="ds_%d" % i) for i in range(n_dma_sems)]
        self.dval = [0] * n_dma_sems
        self.dnext = 0
        self.bsem = [nc.alloc_semaphore(name="bs_%d" % i) for i in range(8)]
        self.bval = [0] * 8
        self.bnext = 0
        self.waited = {e: {} for e in ENGS}
        self.pend = {e: [] for e in ENGS}
        self.nbuf = 0

    def barrier(self):
        snap = [(('e', o), self.cnt[o]) for o in ENGS if self.cnt[o] > 0]
        snap += [(('d', i), self.dval[i]) for i in range(len(self.dsem)) if self.dval[i] > 0]
        for e in ENGS:
            self.pend[e] = snap

    def _pending(self, eng, waits):
        w = self.waited[eng]
        for key, val in self.pend[eng]:
            if key == ('e', eng):
                continue
            if w.get(key, 0) < val:
                w[key] = val
                waits.append((self._sem(key), val))
        self.pend[eng] = []

    def buf(self, name=None):
        self.nbuf += 1
        return Buf(name or ("b%d" % self.nbuf))

    def bufs(self, n):
        return [self.buf() for _ in range(n)]

    def _sem(self, key):
        if key[0] == 'e':
            return self.esem[key[1]]
        return self.dsem[key[1]] if key[0] == 'd' else self.bsem[key[1]]

    def _deps(self, eng, reads, writes):
        need = {}

        def add(tok, raw):
            if tok is None:
                return
            key, val, teng = tok
            if teng == eng and eng == 'pe':
                return
            if need.get(key, 0) < val:
                need[key] = val

        for b in reads:
            add(b.lw, True)
            if b.excl:
                for t in b.rd.values():
                    if t[2] != eng:
                        add(t, True)
        for b in writes:
            add(b.lw, False)
            for t in b.rd.values():
                add(t, False)
        waits = []
        w = self.waited[eng]
        for key, val in need.items():
            if w.get(key, 0) < val:
                w[key] = val
                waits.append((self._sem(key), val))
        return waits

    def _mark(self, tok, reads, writes):
        for b in reads:
            b.rd[tok[0]] = tok
        for b in writes:
            b.lw = tok
            b.rd = {}

    def op(self, eng, fn, reads=(), writes=()):
        waits = self._deps(eng, reads, writes)
        self._pending(eng, waits)
        self.cnt[eng] += 1
        tok = (('e', eng), self.cnt[eng], eng)
        self.ops[eng].append((waits, fn, (self.esem[eng], 1)))
        self._mark(tok, reads, writes)
        return tok

    def dma(self, eng, out, in_, reads=(), writes=(), bg=False, **kw):
        if bg:
            sems, vals, idx, kk = self.bsem, self.bval, self.bnext, 'b'
            self.bnext = (self.bnext + 1) % len(self.bsem)
        else:
            sems, vals, idx, kk = self.dsem, self.dval, self.dnext, 'd'
            self.dnext = (self.dnext + 1) % len(self.dsem)
        waits = self._deps(eng, reads, writes)
        self._pending(eng, waits)
        key = (kk, idx)
        w = self.waited[eng]
        if w.get(key, 0) < vals[idx]:
            w[key] = vals[idx]
            waits.append((sems[idx], vals[idx]))
        vals[idx] += 16
        tok = (key, vals[idx], None)
        sem_ = sems[idx]
        self.ops[eng].append((waits, (lambda e: e.dma_start(out=out, in_=in_, **kw)), (sem_, 16)))
        self._mark(tok, reads, writes)
        return tok

    def emit(self, block):
        fin = [(self.dsem[i], self.dval[i]) for i in range(len(self.dsem)) if self.dval[i] > 0]
        fin += [(self.bsem[i], self.bval[i]) for i in range(len(self.bsem)) if self.bval[i] > 0]
        fin += [(self.esem[e], self.cnt[e]) for e in ENGS if e != 'sp' and self.cnt[e] > 0]

        def run(e, name):
            for waits, fn, inc in self.ops[name]:
                for s, v in waits:
                    e.wait_ge(s, v)
                ins = fn(e)
                ins.then_inc(inc[0], inc[1])
            if name == 'sp':
                for s, v in fin:
                    e.wait_ge(s, v)

        @block.tensor
        def _(e):
            run(e, 'pe')

        @block.scalar
        def _(e):
            run(e, 'act')

        @block.vector
        def _(e):
            run(e, 'dve')

        @block.gpsimd
        def _(e):
            run(e, 'pool')

        @block.sync
        def _(e):
            run(e, 'sp')


class K:
    def __init__(self, nc):
        self.nc = nc
        self.P = Prog(nc)
        self.es = ExitStack()
        self.n = 0

    def scope(self):
        kk = self

        class _S:
            def __enter__(s_):
                s_.old = kk.es
                kk.es = ExitStack()
                return s_

            def __exit__(s_, *a):
                kk.es.close()
                kk.es = s_.old
                kk.P.barrier()
                return False
        return _S()

    def sb(self, shape, dt=F32, name=None):
        self.n += 1
        return self.es.enter_context(self.nc.sbuf_tensor(("%s_%d" % (name, self.n)) if name else ("t%d" % self.n), list(shape), dt))

    def ps(self, shape, dt=F32, name=None):
        self.n += 1
        return self.es.enter_context(self.nc.psum_tensor(name or ("p%d" % self.n), list(shape), dt))

    def dram(self, shape, dt=F32, name=None, kind="Internal"):
        self.n += 1
        return self.nc.dram_tensor(name or ("d%d" % self.n), list(shape), dt, kind=kind).ap()

    def mm(self, out, lhsT, rhs, start, stop, r, w):
        return self.P.op('pe', lambda e: e.matmul(out, lhsT=lhsT, rhs=rhs, start=start, stop=stop), r, w)

    def tr(self, out, in_, ident, r, w):
        return self.P.op('pe', lambda e: e.transpose(out, in_, ident), r, w)

    def act(self, out, in_, func, r, w, bias=None, scale=None, accum=None, eng='act'):
        kw = {}
        if bias is not None:
            kw['bias'] = bias
        if scale is not None:
            kw['scale'] = scale
        if accum is not None:
            kw['accum_out'] = accum
        return self.P.op('act', lambda e: e.activation(out=out, in_=in_, func=func, **kw), r, w)

    def ts(self, eng, out, in0, s1, s2, op0, op1, r, w):
        if op1 is None:
            return self.P.op(eng, lambda e: e.tensor_scalar(out=out, in0=in0, scalar1=s1, scalar2=None, op0=op0), r, w)
        return self.P.op(eng, lambda e: e.tensor_scalar(out=out, in0=in0, scalar1=s1, scalar2=s2, op0=op0, op1=op1), r, w)

    def tt(self, eng, out, in0, in1, op, r, w):
        return self.P.op(eng, lambda e: e.tensor_tensor(out=out, in0=in0, in1=in1, op=op), r, w)

    def stt(self, out, in0, scalar, in1, op0, op1, r, w):
        return self.P.op('dve', lambda e: e.scalar_tensor_tensor(out=out, in0=in0, scalar=scalar, in1=in1, op0=op0, op1=op1), r, w)

    def cp(self, eng, out, in_, r, w):
        if eng == 'act':
            return self.P.op('act', lambda e: e.copy(out=out, in_=in_), r, w)
        return self.P.op(eng, lambda e: e.tensor_copy(out=out, in_=in_), r, w)

    def memset(self, eng, ap, val, w):
        return self.P.op(eng, lambda e: e.memset(ap, val), (), w)

    def recip(self, out, in_, r, w):
        return self.P.op('dve', lambda e: e.reciprocal(out=out, in_=in_), r, w)

    def dma(self, eng, out, in_, r, w, **kw):
        return self.P.dma(eng, out, in_, r, w, **kw)


def build_program(stage=99):
    nc = bass.Bass("TRN2", target_bir_lowering=False)
    k = K(nc)
    P = k.P

    def din(name, shape, dt=F32):
        return nc.dram_tensor(name, list(shape), dt, kind="ExternalInput").ap()

    xl = din("xl", [LOC, D])
    cb = din("cb", [KC, 128])
    ada_w = din("ada_w", [D, 6 * D]) if not FROM_REF else None
    ada_b = din("ada_b", [96, 128])
    norm1_w = din("norm1_w", [KC, 128])
    w_in = din("w_in", [D, D_IN]) if not FROM_REF else None
    ident_in = din("ident", [128, 128])
    pvalid_in = din("pvalid", [128, 1])
    out_d = nc.dram_tensor("out", [NOWN, D], F32, kind="ExternalOutput").ap()
    dbg_mod = nc.dram_tensor("dbg_mod", [128, 96], F32, kind="ExternalOutput").ap()
    def scratch(name, shape, dt=F32):
        if name in REF_IN:
            return din(name + "_in", shape, dt)
        return k.dram(shape, dt, name)

    if FROM_REF:
        PF = din("PF_in", [PF_ROWS, LOC])
        PT = din("PT_in", [LOC, 1600])
    else:
        PF = k.dram([80 * 128, LOC], F32, "PF")
        PT = k.dram([LOC, 1600], F32, "PT")
    OAT = scratch("OAT", [1024, NOWN], BF16)
    OBT = scratch("OBT", [2048, NOWN], BF16)
    YT = scratch("YT", [D, NOWN], BF16)
    X1 = scratch("X1", [NOWN, D], F32)

    ident = k.sb([128, 128], F32, "ident_sb")
    identb = k.sb([128, 128], BF16, "identb")
    pvalid = k.sb([128, 1], F32, "pvalid_sb")
    b_const = P.buf("const")
    k.dma('sp', ident[:], ident_in, (), [b_const])
    k.dma('sp', pvalid[:], pvalid_in, (), [b_const])
    k.cp('dve', identb[:], ident[:], [b_const], [b_const])

    banks = [k.ps([128, 512], F32, "bank%d" % i) for i in range(6)]
    bbank = [P.buf("bank%d" % i) for i in range(6)]
    for b_ in bbank:
        b_.excl = True
    pbf = [k.ps([128, 1024], BF16, "pbf%d" % i) for i in range(2)]
    bpbf = [P.buf("pbf%d" % i) for i in range(2)]
    for b_ in bpbf:
        b_.excl = True

    def load_fm(dst, src, n, bdst, bank=5):
        tmp = k.sb([96, 128], F32)
        bt = P.buf()
        k.dma('sp', tmp[0:n, :], src, (), [bt])
        k.tr(banks[bank][:, 0:n], tmp[0:n, :], ident[0:n, 0:n], [bt, b_const], [bbank[bank]])
        k.cp('dve', dst, banks[bank][:, 0:n], [bbank[bank]], [bdst])

    modT = k.sb([128, 96], F32, "modT")
    b_mod = P.buf("mod")
    A1 = k.sb([128, KC], F32, "A1")
    B1p = k.sb([128, KC], F32, "B1p")
    b_A1 = P.buf()
    if FROM_REF:
        modT_in = din("modT_in", [128, 96])
        k.dma('sp', modT[:], modT_in, (), [b_mod])
    with k.scope():
      if not FROM_REF:
        cT = k.sb([128, KC], F32, "cT")
        csT = k.sb([128, KC], F32, "csT")
        adabT = k.sb([128, 96], F32, "adabT")
        n1T = k.sb([128, KC], F32, "n1T")
        b_c, b_cs, b_adab, b_n1 = P.bufs(4)
        load_fm(cT[:], cb, KC, b_c)
        load_fm(adabT[:], ada_b, 96, b_adab)
        load_fm(n1T[:], norm1_w, KC, b_n1)
        k.act(csT[:], cT[:], AF.Silu, [b_c], [b_cs])
        NAW = 3
        awt = [k.sb([128, KC, 128], F32, "awt%d" % i) for i in range(NAW)]
        bawt = P.bufs(NAW)
        ada_v = ada_w.rearrange("(kc p) j -> p kc j", p=128)
        for m in range(96):
            s = m % NAW
            k.dma('sp', awt[s][:], ada_v[:, :, m * 128:(m + 1) * 128], (), [bawt[s]])
            for kc in range(KC):
                k.mm(banks[4][:, m:m + 1], awt[s][:, kc, :], csT[:, kc:kc + 1], kc == 0, kc == KC - 1,
                     [bawt[s], b_cs], [bbank[4]])
        k.tt('dve', modT[:], banks[4][:, 0:96], adabT[:], ALU.add, [bbank[4], b_adab], [b_mod])
        k.dma('sp', dbg_mod, modT[:], [b_mod], ())
        k.stt(A1[:], modT[:, 16:32], 1.0, n1T[:], ALU.add, ALU.mult, [b_mod, b_n1], [b_A1])
        k.ts('dve', B1p[:], modT[:, 0:16], pvalid[:, 0:1], None, ALU.mult, None, [b_mod, b_const], [b_A1])

    if stage >= 1 and not FROM_REF:
        hscope = k.scope()
        hscope.__enter__()
        hT = k.sb([128, KC, LOC], BF16, "hT")
        b_hT = [P.buf() for _ in range(LOC // 128)]
        with k.scope():
            xt = [k.sb([128, D], F32, "xt%d" % i) for i in range(2)]
            xs = [k.sb([128, D], BF16, "xs%d" % i) for i in range(2)]
            st = [k.sb([128, 4], F32, "st%d" % i) for i in range(2)]
            bxt, bxs, bst = P.bufs(2), P.bufs(2), P.bufs(2)
            xlv = xl.rearrange("(t p) d -> t p d", p=128)
            for t in range(LOC // 128):
                s = t % 2
                k.dma('sp', xt[s][:], xlv[t], (), [bxt[s]])
                k.act(xs[s][:], xt[s][:], AF.Square, [bxt[s]], [bxs[s], bst[s]], accum=st[s][:, 0:1])
                k.ts('dve', st[s][:, 1:2], st[s][:, 0:1], 1.0 / D, 1e-6, ALU.mult, ALU.add, [bst[s]], [bst[s]])
                k.act(st[s][:, 2:3], st[s][:, 1:2], AF.Sqrt, [bst[s]], [bst[s]])
                k.recip(st[s][:, 3:4], st[s][:, 2:3], [bst[s]], [bst[s]])
                k.ts('dve', xs[s][:], xt[s][:], st[s][:, 3:4], None, ALU.mult, None, [bxt[s], bst[s]], [bxs[s]])
                Bsel = B1p if t < OWN0 // 128 else modT
                for kc in range(KC):
                    pi = kc % 2
                    sl = slice((kc // 2 % 4) * 128, (kc // 2 % 4) * 128 + 128)
                    k.tr(pbf[pi][:, sl], xs[s][:, kc * 128:(kc + 1) * 128], identb[:], [bxs[s], b_const], [bpbf[pi]])
                    if kc % 2 == 0:
                        k.act(hT[:, kc, t * 128:(t + 1) * 128], pbf[pi][:, sl], AF.Identity, [bpbf[pi], b_A1, b_mod], [b_hT[t]],
                              bias=Bsel[:, kc:kc + 1], scale=A1[:, kc:kc + 1])
                    else:
                        k.ts('dve', hT[:, kc, t * 128:(t + 1) * 128], pbf[pi][:, sl], A1[:, kc:kc + 1], Bsel[:, kc:kc + 1],
                             ALU.mult, ALU.add, [bpbf[pi], b_A1, b_mod], [b_hT[t]])
        if stage == 1:
            dbg_h = nc.dram_tensor("dbg_h", [128, KC, LOC], BF16, kind="ExternalOutput").ap()
            k.dma('sp', dbg_h, hT[:], b_hT, ())

        if stage >= 2:
            FM = [(0, 3072, 0, 0), (4112, 2048, 3072, OWN0), (6160, 256, 5120, 0), (6416, 256, 5376, 0),
                  (6672, 256, 5632, 0), (7184, 256, 5888, 0), (7744, 2048, 6144, OWN0), (9792, 2048, 8192, OWN0)]
            TM = [(3072, 256, 0, OWN0), (3328, 256, 256, OWN0), (3584, 256, 512, OWN0), (3840, 256, 768, OWN0),
                  (4096, 16, 1024, 0), (6928, 256, 1040, 0), (7440, 256, 1296, 0), (7696, 48, 1552, OWN0)]
            with k.scope():
                wv = w_in.rearrange("(kc p) c -> p kc c", p=128)
                NW = 3
                wt = [k.sb([128, KC, 256], BF16, "wt%d" % i) for i in range(NW)]
                bwt = P.bufs(NW)
                ev = [k.sb([128, 512], F32, "ev%d" % i) for i in range(4)]
                bev = P.bufs(4)
                ei = 0
                wi = 0
                for (c0, ncols, r0, t0) in FM:
                    for cb_ in range(ncols // 256):
                        s = wi % NW
                        wi += 1
                        k.dma('pool', wt[s][:], wv[:, :, c0 + cb_ * 256:c0 + cb_ * 256 + 256], (), [bwt[s]])
                        for hf in range(2):
                            for tt in range(t0 // 512, LOC // 512):
                                e4 = ei % 4
                                ei += 1
                                for kc in range(KC):
                                    k.mm(banks[e4][:, :], wt[s][:, kc, hf * 128:(hf + 1) * 128],
                                         hT[:, kc, tt * 512:(tt + 1) * 512], kc == 0, kc == KC - 1,
                                         [bwt[s]] + b_hT[tt * 4:(tt + 1) * 4], [bbank[e4]])
                                k.cp('act' if e4 % 2 == 0 else 'dve', ev[e4][:], banks[e4][:, :], [bbank[e4]], [bev[e4]])
                                row = r0 + cb_ * 256 + hf * 128
                                k.dma('sp', PF[row:row + 128, tt * 512:(tt + 1) * 512], ev[e4][:], [bev[e4]], ())
                for (c0, ncols, p0, t0) in TM:
                    s = wi % NW
                    wi += 1
                    k.dma('pool', wt[s][:, :, 0:ncols], wv[:, :, c0:c0 + ncols], (), [bwt[s]])
                    for t in range(t0 // 128, LOC // 128):
                        e4 = ei % 4
                        ei += 1
                        for kc in range(KC):
                            k.mm(banks[e4][:, 0:ncols], hT[:, kc, t * 128:(t + 1) * 128], wt[s][:, kc, 0:ncols],
                                 kc == 0, kc == KC - 1, [bwt[s], b_hT[t]], [bbank[e4]])
                        k.cp('act' if e4 % 2 == 0 else 'dve', ev[e4][:, 0:ncols], banks[e4][:, 0:ncols], [bbank[e4]], [bev[e4]])
                        k.dma('sp', PT[t * 128:(t + 1) * 128, p0:p0 + ncols], ev[e4][:, 0:ncols], [bev[e4]], ())
        hscope.__exit__(None, None, None)
        if stage == 2 and DEBUG_OUT:
            dbg_pf = nc.dram_tensor("dbg_pf", [4, 128, LOC], F32, kind="ExternalOutput").ap()
            dbg_pt = nc.dram_tensor("dbg_pt", [LOC, 576], F32, kind="ExternalOutput").ap()
            for i, r in enumerate([0, 3072, 5120, 6144]):
                k.dma('sp', dbg_pf[i], PF[r:r + 128, :], (), ())
            k.dma('sp', dbg_pt, PT[:, 1024:1600], (), ())

    if stage >= 6:
        uT_in = din("peer_uT", [D, 16384])
        pv_in = din("peer_v", [16384, D])
        UBd = k.dram([64, 128, KC, 256], BF16, "UBd")
        VBd = k.dram([64, 128, 2, D], BF16, "VBd")
        bconvU = [P.buf() for _ in range(64)]
        bconvV = [P.buf() for _ in range(64)]
        uTv0 = uT_in.rearrange("(kc p) e -> p kc e", p=128)
        for eb in range(64):
            P.dma('pool', UBd[eb], uTv0[:, :, eb * 256:(eb + 1) * 256], (), [bconvU[eb]], bg=True)
            P.dma('pool', VBd[eb], pv_in[eb * 256:(eb + 1) * 256, :].rearrange("(i p) d -> p i d", p=128), (), [bconvV[eb]], bg=True)
    CONV_TOK = {}
    if stage >= 3 and not SKIP_GDN:
        gconst_in = din("gconst", [128, 5, 128])
        convw_in = din("gdn_conv_w", [96, 128])
        alog_in = din("gdn_A_log", [32, 8])
        dtb_in = din("gdn_dt_bias", [32, 8])
        gnw_in = din("gdn_norm_w", [1, 128])
        bq = [[bbank[bi]] * 4 for bi in range(6)]
        NCH = LOC // 128
        with k.scope():
            gcn = k.sb([128, 5, 128], F32, "gcn")
            ones = k.sb([128, 128], F32, "ones")
            cwT = k.sb([128, 96], F32, "cwT")
            nwb = k.sb([128, 128], F32, "nwb")
            b_gc = P.buf()
            b_cw = P.buf()
            k.dma('sp', gcn[:], gconst_in, (), [b_gc])
            k.memset('dve', ones[:], 1.0, [b_gc])
            k.dma('sp', nwb[:], gnw_in[0].partition_broadcast(128), (), [b_gc])
            load_fm(cwT[:], convw_in, 96, b_cw)
            TriU, msl, msu, miu, bmk = gcn[:, 0, :], gcn[:, 1, :], gcn[:, 2, :], gcn[:, 3, :], gcn[:, 4, :]
            ab = k.sb([128, NCH, 16], F32, "ab")
            dtb = k.sb([128, NCH, 8], F32, "dtb")
            alg = k.sb([128, NCH, 8], F32, "alg")
            vt = {nm: k.sb([128, NCH, 8], F32, "v_" + nm) for nm in
                  ["t1", "g", "beta", "lnb", "Gc", "nGc", "u", "gam", "bg", "kd", "gend", "Gl"]}
            b_v = P.buf()
            k.dma('sp', ab[:], PT[:, 1024:1040].rearrange("(n p) c -> p n c", p=128), (), [b_v])
            k.dma('sp', dtb[:], dtb_in.partition_broadcast(128), (), [b_v])
            k.dma('sp', alg[:], alog_in.partition_broadcast(128), (), [b_v])
            k.tt('dve', vt["t1"][:], ab[:, :, 0:8], dtb[:], ALU.add, [b_v], [b_v])
            k.act(vt["t1"][:], vt["t1"][:], AF.Exp, [b_v], [b_v])
            e_, ser, lnp = vt["t1"], vt["Gl"], vt["Gc"]
            k.ts('dve', ser[:], e_[:], -0.25, 1.0 / 3.0, ALU.mult, ALU.add, [b_v], [b_v])
            k.tt('dve', ser[:], ser[:], e_[:], ALU.mult, [b_v], [b_v])
            k.ts('dve', ser[:], ser[:], -0.5, None, ALU.add, None, [b_v], [b_v])
            k.tt('dve', ser[:], ser[:], e_[:], ALU.mult, [b_v], [b_v])
            k.ts('dve', ser[:], ser[:], 1.0, None, ALU.add, None, [b_v], [b_v])
            k.tt('dve', ser[:], ser[:], e_[:], ALU.mult, [b_v], [b_v])
            k.ts('dve', lnp[:], e_[:], 0.1, None, ALU.max, None, [b_v], [b_v])
            k.act(lnp[:], lnp[:], AF.Ln, [b_v], [b_v], bias=1.0)
            k.ts('dve', vt["u"][:], e_[:], 0.1, None, ALU.is_lt, None, [b_v], [b_v])
            k.tt('dve', ser[:], ser[:], lnp[:], ALU.subtract, [b_v], [b_v])
            k.tt('dve', ser[:], ser[:], vt["u"][:], ALU.mult, [b_v], [b_v])
            k.tt('dve', vt["t1"][:], lnp[:], ser[:], ALU.add, [b_v], [b_v])
            k.act(alg[:], alg[:], AF.Exp, [b_v], [b_v])
            k.stt(vt["g"][:], vt["t1"][:], -1.0, alg[:], ALU.mult, ALU.mult, [b_v], [b_v])
            k.act(vt["beta"][:], ab[:, :, 8:16], AF.Sigmoid, [b_v], [b_v])
            k.act(vt["lnb"][:], vt["beta"][:], AF.Ln, [b_v], [b_v])
            gflat = vt["g"][:].rearrange("p n h -> p (n h)")
            k.mm(banks[0][:, 0:256], TriU, gflat, True, True, [b_v, b_gc], [bq[0][0], bq[0][1]])
            k.mm(banks[0][:, 256:512], ones[:], gflat, True, True, [b_v, b_gc], [bq[0][2], bq[0][3]])
            fl = lambda nm: vt[nm][:].rearrange("p n h -> p (n h)")
            k.cp('dve', fl("Gc"), banks[0][:, 0:256], [bq[0][0], bq[0][1]], [b_v])
            k.cp('dve', fl("Gl"), banks[0][:, 256:512], [bq[0][2], bq[0][3]], [b_v])
            k.ts('dve', fl("nGc"), fl("Gc"), -1.0, None, ALU.mult, None, [b_v], [b_v])
            k.tt('dve', fl("u"), fl("Gc"), fl("lnb"), ALU.add, [b_v], [b_v])
            k.act(fl("gam"), fl("Gc"), AF.Exp, [b_v], [b_v])
            k.act(fl("bg"), fl("u"), AF.Exp, [b_v], [b_v])
            k.tt('dve', fl("kd"), fl("Gl"), fl("Gc"), ALU.subtract, [b_v], [b_v])
            k.act(fl("kd"), fl("kd"), AF.Exp, [b_v], [b_v])
            k.act(fl("gend"), fl("Gl"), AF.Exp, [b_v], [b_v])

            def col(nm, n, h):
                return vt[nm][:, n, h:h + 1]

            WV = 4
            for h in range(0 if GDN_SUB < 1 else (1 if GDN_ONE_HEAD else 8)):
                with k.scope():
                    QT = k.sb([128, LOC], F32, "QT")
                    KT = k.sb([128, LOC], F32, "KT")
                    VT = k.sb([128, LOC], F32, "VT")
                    xr = k.sb([128, LOC], F32, "xr")
                    bQ, bK, bV, bxr = P.bufs(4)
                    for (dst, bd, row0, ti) in [(QT, bQ, h * 128, h), (KT, bK, 1024 + h * 128, 8 + h), (VT, bV, 2048 + h * 128, 16 + h)]:
                        k.dma('sp', xr[:], PF[row0:row0 + 128, :], (), [bxr])
                        k.ts('dve', dst[:], xr[:], cwT[:, 3 * 24 + ti:3 * 24 + ti + 1], None, ALU.mult, None, [bxr, b_cw], [bd])
                        for sh in (1, 2, 3):
                            j = 3 - sh
                            k.stt(dst[:, sh:LOC], xr[:, 0:LOC - sh], cwT[:, j * 24 + ti:j * 24 + ti + 1], dst[:, sh:LOC],
                                  ALU.mult, ALU.add, [bxr, b_cw, bd], [bd])
                        k.act(dst[:], dst[:], AF.Silu, [bd], [bd])
                    for (dst, bd, lnc) in [(QT, bQ, float(np.log(128.0 ** -0.5))), (KT, bK, 0.0)]:
                        for tt in range(LOC // 512):
                            bk = tt % 4
                            sl = slice(tt * 512, (tt + 1) * 512)
                            k.tt('dve', xr[:, sl], dst[:, sl], dst[:, sl], ALU.mult, [bd], [bxr])
                            k.mm(banks[bk][:, :], ones[:], xr[:, sl], True, True, [bxr, b_gc], bq[bk])
                            k.act(xr[:, sl], banks[bk][:, :], AF.Ln, bq[bk], [bxr], bias=1e-6)
                            k.act(xr[:, sl], xr[:, sl], AF.Exp, [bxr], [bxr], scale=-0.5, bias=lnc)
                            k.tt('dve', dst[:, sl], dst[:, sl], xr[:, sl], ALU.mult, [bd, bxr], [bd])
                    zt = k.sb([128, NOWN // 128, 128], F32, "zt")
                    bz = P.buf()
                    k.dma('sp', zt[:], PT[OWN0:LOC, h * 128:(h + 1) * 128].rearrange("(n p) c -> p n c", p=128), (), [bz])
                    k.act(zt[:], zt[:], AF.Silu, [bz], [bz])
                    S = k.sb([128, 128], F32, "S")
                    bS = P.buf()
                    k.memset('dve', S[:], 0.0, [bS])
                    names = ["R", "Kd", "dg", "tE", "L", "LT", "QK", "Pa", "Pb", "WT", "U", "tmp", "O", "o16", "Z", "DT", "X", "Tt"]
                    T = [{nm: k.sb([128, 256 if nm in ("R", "X", "Tt") else 128], BF16 if nm == "o16" else F32, "w%d_%s" % (i, nm)) for nm in names}
                         for i in range(WV)]
                    Bf = [{nm: P.buf() for nm in names + ["st"]} for i in range(WV)]
                    stt_ = [k.sb([128, 4], F32, "w%d_st" % i) for i in range(WV)]
                    PTk = [[k.sb([128, 128], F32, "w%d_PT%d" % (i, l)) for l in range(4)] for i in range(WV)]
                    bPTk = [[P.buf() for l in range(4)] for i in range(WV)]
                    bctr = [0]

                    def nb():
                        bctr[0] = (bctr[0] + 1) % 6
                        return bctr[0]

                    for w0 in (GDN_WAVES if GDN_WAVES is not None else range(0, NCH if GDN_SUB >= 2 else 0, WV)):
                        chunks = list(range(w0, w0 + WV))
                        own = w0 >= OWN0 // 128
                        def pre_steps(i, n):
                            t, b = T[i], Bf[i]
                            cs = slice(n * 128, (n + 1) * 128)
                            q4 = slice(i * 128, (i + 1) * 128)
                            st = []
                            bkT1, bkT2, bkKK, bkKQ, bkB = 0, 1, 2, 3, 4
                            st.append(lambda: k.tr(banks[0][:, q4], KT[:, cs], ident[:], [bK, b_const], [bq[0][i]]))
                            st.append(lambda: k.act(t["R"][:, 0:128], banks[0][:, q4], AF.Identity, [bq[0][i], b_v], [b["R"]], scale=col("bg", n, h)))
                            st.append(lambda: k.ts('dve', t["Kd"][:], banks[0][:, q4], col("kd", n, h), None, ALU.mult, None, [bq[0][i], b_v], [b["Kd"]]))
                            st.append(lambda: k.tr(banks[1][:, q4], VT[:, cs], ident[:], [bV, b_const], [bq[1][i]]))
                            st.append(lambda: k.ts('dve', t["R"][:, 128:256], banks[1][:, q4], col("beta", n, h), None, ALU.mult, None, [bq[1][i], b_v], [b["R"]]))
                            st.append(lambda: k.mm(banks[2][:, q4], KT[:, cs], KT[:, cs], True, True, [bK], [bq[2][i]]))
                            st.append(lambda: k.ts('dve', t["dg"][:], ident[:], col("nGc", n, h), None, ALU.mult, None, [b_const, b_v], [b["dg"]]))
                            st.append(lambda: k.mm(banks[4][:, q4], ones[:], t["dg"][:], True, True, [b["dg"], b_gc], [bq[4][i]]))
                            st.append(lambda: k.stt(t["tE"][:], banks[4][:, q4], col("u", n, h), msl, ALU.add, ALU.add, [bq[4][i], b_v, b_gc], [b["tE"]]))
                            st.append(lambda: k.act(t["tE"][:], t["tE"][:], AF.Exp, [b["tE"]], [b["tE"]]))
                            st.append(lambda: k.tt('dve', t["L"][:], banks[2][:, q4], t["tE"][:], ALU.mult, [bq[2][i], b["tE"]], [b["L"]]))
                            st.append(lambda: k.ts('dve', t["dg"][:], ident[:], col("u", n, h), None, ALU.mult, None, [b_const, b_v], [b["dg"]]))
                            st.append(lambda: k.mm(banks[5][:, q4], ones[:], t["dg"][:], True, True, [b["dg"], b_gc], [bq[5][i]]))
                            st.append(lambda: k.stt(t["tE"][:], banks[5][:, q4], col("nGc", n, h), msu, ALU.add, ALU.add, [bq[5][i], b_v, b_gc], [b["tE"]]))
                            st.append(lambda: k.act(t["tE"][:], t["tE"][:], AF.Exp, [b["tE"]], [b["tE"]]))
                            st.append(lambda: k.tt('dve', t["LT"][:], banks[2][:, q4], t["tE"][:], ALU.mult, [bq[2][i], b["tE"]], [b["LT"]]))
                            if own:
                                st.append(lambda: k.mm(banks[3][:, q4], KT[:, cs], QT[:, cs], True, True, [bK, bQ], [bq[3][i]]))
                                st.append(lambda: k.ts('dve', t["dg"][:], ident[:], col("Gc", n, h), None, ALU.mult, None, [b_const, b_v], [b["dg"]]))
                                st.append(lambda: k.mm(banks[4][:, q4], ones[:], t["dg"][:], True, True, [b["dg"], b_gc], [bq[4][i]]))
                                st.append(lambda: k.stt(t["tE"][:], banks[4][:, q4], col("nGc", n, h), miu, ALU.add, ALU.add, [bq[4][i], b_v, b_gc], [b["tE"]]))
                                st.append(lambda: k.act(t["tE"][:], t["tE"][:], AF.Exp, [b["tE"]], [b["tE"]]))
                                st.append(lambda: k.tt('dve', t["QK"][:], banks[3][:, q4], t["tE"][:], ALU.mult, [bq[3][i], b["tE"]], [b["QK"]]))
                            st.append(lambda: k.tt('dve', t["tmp"][:], t["LT"][:], bmk, ALU.mult, [b["LT"], b_gc], [b["tmp"]]))
                            st.append(lambda: k.tt('dve', t["LT"][:], t["LT"][:], t["tmp"][:], ALU.subtract, [b["LT"], b["tmp"]], [b["LT"]]))
                            st.append(lambda: k.tt('dve', t["L"][:], t["L"][:], bmk, ALU.mult, [b["L"], b_gc], [b["L"]]))
                            cP, cbP, cPT, cbPT = t["L"], b["L"], t["tmp"], b["tmp"]
                            NLEV = 4
                            for lev in range(NLEV):
                                nP, nbP = (t["Pa"], b["Pa"]) if lev % 2 == 0 else (t["Pb"], b["Pb"])
                                bA = lev % 2
                                last = lev == NLEV - 1
                                if not last:
                                    st.append((lambda cP=cP, cbP=cbP, cPT=cPT, cbPT=cbPT, bA=bA: k.mm(banks[bA][:, q4], cPT[:], cP[:], True, True, [cbPT, cbP], [bq[bA][i]])))
                                    st.append((lambda nP=nP, nbP=nbP, bA=bA: k.cp('act', nP[:], banks[bA][:, q4], [bq[bA][i]], [nbP])))
                                st.append((lambda cP=cP, cbP=cbP, cPT=cPT, cbPT=cbPT, bA=bA: k.mm(banks[2 + bA][:, q4], cP[:], cPT[:], True, True, [cbPT, cbP], [bq[2 + bA][i]])))
                                st.append((lambda lev=lev, bA=bA: k.cp('dve', PTk[i][lev][:], banks[2 + bA][:, q4], [bq[2 + bA][i]], [bPTk[i][lev]])))
                                cP, cbP, cPT, cbPT = nP, nbP, PTk[i][lev], bPTk[i][lev]
                            st.append(lambda: k.cp('dve', t["Z"][:], ident[:], [b_const], [b["Z"]]))
                            for lev in [3, 2, 1, 0, -1]:
                                bA = 4 + (lev % 2)
                                if lev >= 0:
                                    st.append((lambda lev=lev, bA=bA: k.mm(banks[bA][:, q4], PTk[i][lev][:], t["Z"][:], True, True, [bPTk[i][lev], b["Z"]], [bq[bA][i]])))
                                    st.append((lambda bA=bA: k.tt('dve', t["Z"][:], t["Z"][:], banks[bA][:, q4], ALU.add, [bq[bA][i], b["Z"]], [b["Z"]])))
                                else:
                                    st.append((lambda bA=bA: k.mm(banks[bA][:, q4], t["tmp"][:], t["Z"][:], True, True, [b["tmp"], b["Z"]], [bq[bA][i]])))
                                    st.append((lambda bA=bA: k.tt('dve', t["Z"][:], t["Z"][:], banks[bA][:, q4], ALU.subtract, [bq[bA][i], b["Z"]], [b["Z"]])))
                            st.append(lambda: k.tr(banks[0][:, q4], t["Z"][:], ident[:], [b["Z"], b_const], [bq[0][i]]))
                            st.append(lambda: k.cp('act', t["DT"][:], banks[0][:, q4], [bq[0][i]], [b["DT"]]))
                            hq = slice((i % 2) * 256, (i % 2) * 256 + 256)
                            bkR = 4 + (i // 2)
                            bkR2 = 2 + (i // 2)
                            st.append(lambda: k.mm(banks[bkR][:, hq], t["DT"][:], t["R"][:], True, True, [b["DT"], b["R"]], [bq[bkR][0]]))
                            st.append(lambda: k.cp('dve', t["X"][:], banks[bkR][:, hq], [bq[bkR][0]], [b["X"]]))
                            for sweep in range(3):
                                st.append(lambda: k.mm(banks[bkR2][:, hq], t["LT"][:], t["X"][:], True, True, [b["LT"], b["X"]], [bq[bkR2][0]]))
                                st.append(lambda: k.tt('dve', t["Tt"][:], t["R"][:], banks[bkR2][:, hq], ALU.subtract, [b["R"], bq[bkR2][0]], [b["Tt"]]))
                                st.append(lambda: k.mm(banks[bkR][:, hq], t["DT"][:], t["Tt"][:], True, True, [b["DT"], b["Tt"]], [bq[bkR][0]]))
                                st.append(lambda: k.cp('dve', t["X"][:], banks[bkR][:, hq], [bq[bkR][0]], [b["X"]]))
                            st.append(lambda: k.cp('act', t["R"][:], t["X"][:], [b["X"]], [b["R"]]))
                            st.append(lambda: k.tr(banks[0][:, q4], t["R"][:, 0:128], ident[:], [b["R"], b_const], [bq[0][i]]))
                            st.append(lambda: k.cp('act', t["WT"][:], banks[0][:, q4], [bq[0][i]], [b["WT"]]))
                            return st

                        allst = [pre_steps(i, n) for i, n in enumerate(chunks)]
                        for si in range(min(GDN_STEPS, len(allst[0]))):
                            for i in range(WV):
                                allst[i][si]()
                        for i, n in enumerate(chunks if GDN_SUB >= 3 else []):
                            t, b = T[i], Bf[i]
                            cs = slice(n * 128, (n + 1) * 128)
                            q4 = slice(i * 128, (i + 1) * 128)
                            k.mm(banks[1][:, q4], t["WT"][:], S[:], True, True, [b["WT"], bS], [bq[1][i]])
                            k.tt('dve', t["U"][:], t["R"][:, 128:256], banks[1][:, q4], ALU.subtract, [b["R"], bq[1][i]], [b["U"]])
                            if own:
                                k.mm(banks[2][:, q4], QT[:, cs], S[:], True, True, [bQ, bS], [bq[2][i]])
                                k.mm(banks[3][:, q4], t["QK"][:], t["U"][:], True, True, [b["QK"], b["U"]], [bq[3][i]])
                                k.act(t["tmp"][:], banks[2][:, q4], AF.Identity, [bq[2][i], b_v], [b["tmp"]], scale=col("gam", n, h))
                                k.tt('dve', t["O"][:], t["tmp"][:], banks[3][:, q4], ALU.add, [b["tmp"], bq[3][i]], [b["O"]])
                            k.mm(banks[0][:, q4], t["Kd"][:], t["U"][:], True, True, [b["Kd"], b["U"]], [bq[0][i]])
                            k.stt(S[:], S[:], col("gend", n, h), banks[0][:, q4], ALU.mult, ALU.add, [bS, b_v, bq[0][i]], [bS])
                            if own:
                                no = n - OWN0 // 128
                                sti = stt_[i]
                                k.act(t["tmp"][:], t["O"][:], AF.Square, [b["O"]], [b["tmp"], b["st"]], accum=sti[:, 0:1])
                                k.act(sti[:, 1:2], sti[:, 0:1], AF.Ln, [b["st"]], [b["st"]], scale=1.0 / 128, bias=1e-6)
                                k.act(sti[:, 2:3], sti[:, 1:2], AF.Exp, [b["st"]], [b["st"]], scale=-0.5)
                                k.stt(t["O"][:], t["O"][:], sti[:, 2:3], nwb[:], ALU.mult, ALU.mult, [b["O"], b["st"], b_gc], [b["O"]])
                                k.tt('dve', t["O"][:], t["O"][:], zt[:, no, :], ALU.mult, [b["O"], bz], [b["O"]])
                                k.tr(banks[4][:, q4], t["O"][:], ident[:], [b["O"], b_const], [bq[4][i]])
                                k.cp('act', t["o16"][:], banks[4][:, q4], [bq[4][i]], [b["o16"]])
                                k.dma('sp', OAT[h * 128:(h + 1) * 128, no * 128:(no + 1) * 128], t["o16"][:], [b["o16"]], ())
        if stage == 3 and DEBUG_OUT and not SKIP_GDN:
            dbg_oa = nc.dram_tensor("dbg_oa", [1024, NOWN], BF16, kind="ExternalOutput").ap()
            k.dma('sp', dbg_oa, OAT, (), ())

    if stage >= 4 and not SKIP_NSA:
        pos_in = din("pos", [1, LOC], I32)
        nsc_in = din("nsa_c128", [128, 385])
        maskB_in = din("maskB", [128, LOC])
        eexp_in = din("eexp", [64, LOC])
        ovl_in = din("ovl", [128, 2, 65])
        fv_in = din("fbigvis", [128, 2, 16, 64])
        ncore_in = din("nsa_core", [128, 129])
        cpos_in = [din("cmp_pos_k", [32, 128]), din("cmp_pos_v", [32, 128])]
        cw1_in = [din("cmp_w1_k", [LOC, 256]), din("cmp_w1_v", [LOC, 256])]
        cw2_in = [din("cmp_w2_k", [256, 128]), din("cmp_w2_v", [256, 128])]
        NQT = NOWN // 128
        with k.scope():
            nsc = k.sb([128, 385], F32, "nsc")
            ncore = k.sb([128, 129], F32, "ncore")
            maskB = k.sb([128, LOC], BF16, "maskB")
            eexp = k.sb([64, LOC], BF16, "eexp")
            ovl = k.sb([128, 2, 65], BF16, "ovl")
            fv = k.sb([128, 2, 16, 64], F32, "fv")
            cmk = k.sb([128, 2, 128], BF16, "cmk")
            b_nc = P.buf()
            k.dma('sp', nsc[:], nsc_in, (), [b_nc])
            k.dma('sp', ncore[:], ncore_in, (), [b_nc])
            k.dma('sp', fv[:], fv_in, (), [b_nc])
            k.dma('pool', maskB[:], maskB_in, (), [b_nc])
            k.dma('pool', eexp[:], eexp_in, (), [b_nc])
            k.dma('pool', ovl[:], ovl_in, (), [b_nc])
            k.cp('dve', cmk[:, 0, :], nsc[:, 129:257], [b_nc], [b_nc])
            k.cp('dve', cmk[:, 1, :], nsc[:, 257:385], [b_nc], [b_nc])
            ropeP = nsc[:, 0:128]
            invf = nsc[:, 128:129]
            kvb = ncore[:, 128:129]
            cosT = k.sb([128, LOC], F32, "cosT")
            sinT = k.sb([128, LOC], F32, "sinT")
            cosq = k.sb([128, NOWN], F32, "cosq")
            sinq = k.sb([128, NOWN], F32, "sinq")
            b_rt = P.buf()
            with k.scope():
                posi = k.sb([128, LOC], I32, "posi")
                ang = k.sb([128, LOC], F32, "ang")
                kk_ = k.sb([128, LOC], I32, "kk")
                kf = k.sb([128, LOC], F32, "kf")
                b_a = P.buf()
                k.dma('sp', posi[:], pos_in[0].partition_broadcast(128), (), [b_a])
                k.cp('dve', ang[:], posi[:], [b_a], [b_a])
                k.ts('dve', ang[:], ang[:], invf, None, ALU.mult, None, [b_a, b_nc], [b_a])
                TWO_PI = float(2 * np.pi)
                for (dst, shift) in [(sinT, 0.0), (cosT, float(np.pi / 2))]:
                    k.ts('dve', kf[:], ang[:], shift, 1.0 / TWO_PI, ALU.add, ALU.mult, [b_a], [b_a])
                    k.cp('dve', kk_[:], kf[:], [b_a], [b_a])
                    k.cp('dve', kf[:], kk_[:], [b_a], [b_a])
                    k.stt(kf[:], kf[:], -TWO_PI, ang[:], ALU.mult, ALU.add, [b_a], [b_a])
                    k.ts('dve', kf[:], kf[:], shift, None, ALU.add, None, [b_a], [b_a])
                    k.ts('dve', dst[:], kf[:], float(np.pi), -TWO_PI, ALU.is_gt, ALU.mult, [b_a], [b_rt])
                    k.tt('dve', kf[:], kf[:], dst[:], ALU.add, [b_a, b_rt], [b_a])
                    k.ts('dve', dst[:], kf[:], -float(np.pi), TWO_PI, ALU.is_lt, ALU.mult, [b_a], [b_rt])
                    k.tt('dve', kf[:], kf[:], dst[:], ALU.add, [b_a, b_rt], [b_a])
                    k.act(dst[:], kf[:], AF.Sin, [b_a], [b_rt])
                k.ts('dve', cosq[:], cosT[:, OWN0:LOC], 128.0 ** -0.5, None, ALU.mult, None, [b_rt], [b_rt])
                k.ts('dve', sinq[:], sinT[:, OWN0:LOC], 128.0 ** -0.5, None, ALU.mult, None, [b_rt], [b_rt])

            def rope(dst_bf, X, bX, ct, st_, ntok, bdst, tmpa, tmpb, btmp):
                for tt in range(ntok // 512):
                    sl = slice(tt * 512, (tt + 1) * 512)
                    bk = tt % 3
                    k.mm(banks[bk][:, :], ropeP, X[:, sl], True, True, [bX, b_nc], [bbank[bk]])
                    k.tt('pool', tmpa[:, 0:512], X[:, sl], ct[:, sl], ALU.mult, [bX, b_rt], [btmp[0]])
                    k.tt('dve', tmpb[:, 0:512], banks[bk][:, :], st_[:, sl], ALU.mult, [bbank[bk], b_rt], [btmp[1]])
                    k.tt('dve', dst_bf[:, sl], tmpa[:, 0:512], tmpb[:, 0:512], ALU.add, [btmp[0], btmp[1]], [bdst])

            gts = k.sb([128, NQT, 48], F32, "gts")
            b_g = P.buf()
            k.dma('sp', gts[:], PT[OWN0:LOC, 1552:1600].rearrange("(n p) c -> p n c", p=128), (), [b_g])
            k.act(gts[:], gts[:], AF.Sigmoid, [b_g], [b_g])

            for g in range(2 if NSA_GROUPS_RUN is None else NSA_GROUPS_RUN):
                with k.scope():
                    KsT = k.sb([128, LOC], BF16, "KsT")
                    KwT = k.sb([128, LOC], BF16, "KwT")
                    Vs1 = k.sb([128, 32, 129], BF16, "Vs1")
                    Vw1 = k.sb([128, 32, 129], BF16, "Vw1")
                    KcmpT = k.sb([128, 256], BF16, "KcmpT")
                    RHSc = k.sb([128, 2, 193], BF16, "RHSc")
                    QTh = [k.sb([128, NOWN], BF16, "QTh%d" % i) for i in range(8)]
                    bKs, bKw, bVs, bVw, bKc, bRc = P.bufs(6)
                    bQh = P.bufs(8)
                    k.memset('dve', Vs1[:, :, 128:129], 1.0, [bVs])
                    k.memset('dve', Vw1[:, :, 128:129], 1.0, [bVw])
                    k.dma('pool', Vs1[:, :, 0:128], PT[:, 1040 + g * 128:1040 + (g + 1) * 128].rearrange("(n p) c -> p n c", p=128), (), [bVs])
                    k.dma('pool', Vw1[:, :, 0:128], PT[:, 1296 + g * 128:1296 + (g + 1) * 128].rearrange("(n p) c -> p n c", p=128), (), [bVw])
                    k.memset('dve', KcmpT[:], 0.0, [bKc])
                    k.cp('dve', RHSc[:, :, 0:65], ovl[:], [b_nc], [bRc])
                    with k.scope():
                        X = k.sb([128, LOC], F32, "ropeX")
                        tmpa = k.sb([128, 512], F32, "ropeA")
                        tmpb = k.sb([128, 512], F32, "ropeB")
                        KcT = k.sb([128, LOC], BF16, "KcT")
                        VcT = k.sb([128, LOC], BF16, "VcT")
                        bX, bKcT, bVcT = P.bufs(3)
                        btmp = P.bufs(2)
                        for (dstb, bd, row0) in [(KcT, bKcT, 5120 + g * 128), (KsT, bKs, 5632 + g * 128), (KwT, bKw, 5888 + g * 128)]:
                            k.dma('sp', X[:], PF[row0:row0 + 128, :], (), [bX])
                            rope(dstb, X, bX, cosT, sinT, LOC, bd, tmpa, tmpb, btmp)
                        k.dma('pool', VcT[:], PF[5376 + g * 128:5376 + (g + 1) * 128, :], (), [bVcT])
                        for hh in range(8):
                            row0 = 3072 + (g * 8 + hh) * 128
                            k.dma('sp', X[:, 0:NOWN], PF[row0:row0 + 128, OWN0:LOC], (), [bX])
                            rope(QTh[hh], X, bX, cosq, sinq, NOWN, bQh[hh], tmpa, tmpb, btmp)
                        w1 = k.sb([128, 32, 256], BF16, "cw1")
                        w2 = k.sb([128, 2, 128], BF16, "cw2")
                        cpT = k.sb([128, 32], F32, "cpT")
                        cpTb = k.sb([128, 32], BF16, "cpTb")
                        hidT = k.sb([128, 2, 256], BF16, "hidT")
                        hx = k.sb([128, 256], F32, "hx")
                        hy = k.sb([128, 256], F32, "hy")
                        hb = k.sb([128, 2], F32, "hb")
                        bw1, bw2, bcp, bhid, bhx, bhy, bhb = P.bufs(7)
                        for which, (srcT, bsrc) in enumerate([(KcT, bKcT), (VcT, bVcT)]):
                            k.dma('pool', w1[:], cw1_in[which].rearrange("(l d) h -> d l h", d=128), (), [bw1])
                            k.dma('pool', w2[:], cw2_in[which].rearrange("(t p) d -> p t d", p=128), (), [bw2])
                            load_fm(cpT[:], cpos_in[which], 32, bcp, bank=3)
                            k.cp('dve', cpTb[:], cpT[:], [bcp], [bcp])
                            k.memset('dve', hidT[:], 0.0, [bhid])
                            for ht in range(2):
                                for l in range(32):
                                    k.mm(banks[0][:, 0:255], w1[:, l, ht * 128:(ht + 1) * 128], srcT[:, l:l + 16 * 254 + 1:16],
                                         l == 0, l == 31, [bw1, bsrc], [bbank[0]])
                                for l in range(32):
                                    k.mm(banks[1][:, 0:1], w1[:, l, ht * 128:(ht + 1) * 128], cpTb[:, l:l + 1],
                                         l == 0, l == 31, [bw1, bcp], [bbank[1]])
                                k.cp('dve', hb[:, ht:ht + 1], banks[1][:, 0:1], [bbank[1]], [bhb])
                                k.act(hx[:, 0:255], banks[0][:, 0:255], AF.Identity, [bbank[0], bhb], [bhx], bias=hb[:, ht:ht + 1])
                                k.tt('dve', hy[:, 0:255], hx[:, 0:255], hx[:, 0:255], ALU.mult, [bhx], [bhy])
                                k.ts('dve', hy[:, 0:255], hy[:, 0:255], 0.044715, 1.0, ALU.mult, ALU.add, [bhy], [bhy])
                                k.tt('dve', hy[:, 0:255], hy[:, 0:255], hx[:, 0:255], ALU.mult, [bhy, bhx], [bhy])
                                k.act(hy[:, 0:255], hy[:, 0:255], AF.Sigmoid, [bhy], [bhy], scale=1.5957691216057308)
                                k.tt('dve', hidT[:, ht, 0:255], hy[:, 0:255], hx[:, 0:255], ALU.mult, [bhy, bhx], [bhid])
                            if which == 0:
                                for ht in range(2):
                                    k.mm(banks[2][:, 0:255], w2[:, ht, :], hidT[:, ht, 0:255], ht == 0, ht == 1, [bw2, bhid], [bbank[2]])
                                k.cp('dve', KcmpT[:, 0:255], banks[2][:, 0:255], [bbank[2]], [bKc])
                            else:
                                for j in range(2):
                                    nn = 128 if j == 0 else 127
                                    for ht in range(2):
                                        k.mm(banks[2][0:nn, 0:128], hidT[:, ht, j * 128:j * 128 + nn], w2[:, ht, :], ht == 0, ht == 1, [bw2, bhid], [bbank[2]])
                                    k.memset('dve', RHSc[:, j, 65:193], 0.0, [bRc])
                                    k.cp('dve', RHSc[0:nn, j, 65:193], banks[2][0:nn, 0:128], [bbank[2]], [bRc])
                    impacc = k.sb([128, 64], F32, "impacc")
                    v1 = k.sb([128, 64], F32, "selv1")
                    v2 = k.sb([128, 64], F32, "selv2")
                    m8 = k.sb([128, 16], F32, "m8")
                    selb = k.sb([128, 64], F32, "selb")
                    selbT = k.sb([64, 128], BF16, "selbT")
                    Oacc = k.sb([128, 8, 128], F32, "Oacc")
                    o16 = [k.sb([128, 128], BF16, "no16_%d" % i) for i in range(2)]
                    Et = [k.sb([128, 128], BF16, "Et%d" % i) for i in range(4)]
                    sc_ = k.sb([128, 8, 8], F32, "nsc_small")
                    bimp, bsel, bselT = P.bufs(3)
                    bOh = P.bufs(8)
                    bsch = P.bufs(8)
                    bo16 = P.bufs(2)
                    bEt = P.bufs(4)
                    ectr = [0]

                    def score_exp(lhsK, bK_, rhsQ, bQ_, extra, bias_ap):
                        e = ectr[0] % 4
                        bk = ectr[0] % 2
                        ectr[0] += 1
                        k.mm(banks[bk][:, 0:128], lhsK, rhsQ, True, len(extra) == 0, [bK_, bQ_], [bbank[bk]])
                        for xi, (l_, r_, bl_) in enumerate(extra):
                            k.mm(banks[bk][:, 0:128], l_, r_, False, xi == len(extra) - 1, bl_, [bbank[bk]])
                        if bias_ap is None:
                            k.act(Et[e][:], banks[bk][:, 0:128], AF.Exp, [bbank[bk]], [bEt[e]])
                        else:
                            k.act(Et[e][:], banks[bk][:, 0:128], AF.Exp, [bbank[bk], b_nc], [bEt[e]], bias=bias_ap)
                        return Et[e], bEt[e]

                    def tbank():
                        bk = ectr[0] % 2
                        ectr[0] += 1
                        return bk

                    for qt in range(NQT if NSA_QT_RUN is None else NSA_QT_RUN):
                        qsl = slice(qt * 128, (qt + 1) * 128)
                        for hh in range(8):
                            head = g * 8 + hh
                            sch, bsc = sc_[:, hh, :], bsch[hh]
                            cbk = 3 + (hh % 3)
                            ets = []
                            for j in range(2):
                                u0 = (OWN0 + 128 * qt) if j == 0 else 128 * qt
                                ets.append(score_exp(KcmpT[:, j * 128:(j + 1) * 128], bKc, QTh[hh][:, qsl], bQh[hh],
                                                     [(identb[:], maskB[:, u0:u0 + 128], [b_const, b_nc])], kvb if j == 0 else None))
                            for j in range(2):
                                k.mm(banks[cbk][:, 0:193], ets[j][0][:], RHSc[:, j, :], j == 0, j == 1, [ets[j][1], bRc], [bbank[cbk]])
                            k.ts('dve', sch[:, 0:1], banks[cbk][:, 64:65], 1e-30, None, ALU.max, None, [bbank[cbk]], [bsc])
                            k.recip(sch[:, 1:2], sch[:, 0:1], [bsc], [bsc])
                            if hh == 0:
                                k.ts('dve', impacc[:], banks[cbk][:, 0:64], sch[:, 1:2], None, ALU.mult, None, [bbank[cbk], bsc], [bimp])
                            else:
                                k.stt(impacc[:], banks[cbk][:, 0:64], sch[:, 1:2], impacc[:], ALU.mult, ALU.add, [bbank[cbk], bsc, bimp], [bimp])
                            k.ts('dve', sch[:, 2:3], gts[:, qt, head * 3:head * 3 + 1], sch[:, 1:2], None, ALU.mult, None, [b_g, bsc], [bsc])
                            k.ts('dve', Oacc[:, hh, :], banks[cbk][:, 65:193], sch[:, 2:3], None, ALU.mult, None, [bbank[cbk], bsc], [bOh[hh]])
                        k.tt('dve', v1[:], impacc[:], fv[:, 0, qt, :], ALU.max, [bimp, b_nc], [bsel])
                        k.tt('dve', v1[:], v1[:], ncore[:, 0:64], ALU.max, [bsel, b_nc], [bsel])
                        k.tt('dve', v1[:], v1[:], fv[:, 1, qt, :], ALU.min, [bsel, b_nc], [bsel])
                        k.tt('dve', v1[:], v1[:], ncore[:, 64:128], ALU.min, [bsel, b_nc], [bsel])
                        P.op('dve', lambda e: e.max(out=m8[:, 0:8], in_=v1[:]), [bsel], [bsel])
                        P.op('dve', lambda e: e.match_replace(out=v2[:], in_to_replace=m8[:, 0:8], in_values=v1[:], imm_value=-3.0e38), [bsel], [bsel])
                        P.op('dve', lambda e: e.max(out=m8[:, 8:16], in_=v2[:]), [bsel], [bsel])
                        k.ts('dve', v2[:], v1[:], m8[:, 15:16], None, ALU.is_ge, None, [bsel], [bsel])
                        k.ts('dve', selb[:], v1[:], -1.0e29, None, ALU.is_gt, None, [bsel], [bsel])
                        k.tt('dve', selb[:], selb[:], v2[:], ALU.mult, [bsel], [bsel])
                        k.ts('dve', selb[:], selb[:], -1.0, 30000.0, ALU.add, ALU.mult, [bsel], [bsel])
                        tb = tbank()
                        k.tr(banks[tb][0:64, 0:128], selb[:], ident[:], [bsel, b_const], [bbank[tb]])
                        k.cp('dve', selbT[:], banks[tb][0:64, 0:128], [bbank[tb]], [bselT])
                        for hh in range(8):
                            head = g * 8 + hh
                            sch, bsc, bO = sc_[:, hh, :], bsch[hh], bOh[hh]
                            sbk = 4 if hh % 2 == 0 else 2
                            wbk = 5 if hh % 2 == 0 else 3
                            kdiag = OWN0 // 128 + qt
                            tiles = []
                            for kt in range(0, kdiag + 1):
                                ex = [(eexp[:, kt * 128:(kt + 1) * 128], selbT[:], [b_nc, bselT])]
                                if kt == kdiag:
                                    ex.append((identb[:], cmk[:, 0, :], [b_const, b_nc]))
                                tiles.append((KsT[:, kt * 128:(kt + 1) * 128], bKs, ex, None, sbk, Vs1[:, kt, :], bVs, kt == 0, kt == kdiag))
                            for kt in range(kdiag - 4, kdiag + 1):
                                ex = []
                                if kt == kdiag - 4:
                                    ex.append((identb[:], cmk[:, 1, :], [b_const, b_nc]))
                                if kt == kdiag:
                                    ex.append((identb[:], cmk[:, 0, :], [b_const, b_nc]))
                                tiles.append((KwT[:, kt * 128:(kt + 1) * 128], bKw, ex, kvb if kt < OWN0 // 128 else None, wbk, Vw1[:, kt, :], bVw,
                                              kt == kdiag - 4, kt == kdiag))
                            pend = None
                            for (lk, blk_, ex, bias_, abk, vap, bv_, st_f, sp_f) in tiles:
                                et, bet = score_exp(lk, blk_, QTh[hh][:, qsl], bQh[hh], ex, bias_)
                                if pend is not None:
                                    pe_, pb_, pa_, pv_, pbv_, ps_, pp_ = pend
                                    k.mm(banks[pa_][:, 0:129], pe_[:], pv_, ps_, pp_, [pb_, pbv_], [bbank[pa_]])
                                pend = (et, bet, abk, vap, bv_, st_f, sp_f)
                            pe_, pb_, pa_, pv_, pbv_, ps_, pp_ = pend
                            k.mm(banks[pa_][:, 0:129], pe_[:], pv_, ps_, pp_, [pb_, pbv_], [bbank[pa_]])
                            k.ts('dve', sch[:, 3:4], banks[sbk][:, 128:129], 1e-30, None, ALU.max, None, [bbank[sbk]], [bsc])
                            k.recip(sch[:, 4:5], sch[:, 3:4], [bsc], [bsc])
                            k.ts('dve', sch[:, 4:5], sch[:, 4:5], gts[:, qt, head * 3 + 1:head * 3 + 2], None, ALU.mult, None, [b_g, bsc], [bsc])
                            k.stt(Oacc[:, hh, :], banks[sbk][:, 0:128], sch[:, 4:5], Oacc[:, hh, :], ALU.mult, ALU.add, [bbank[sbk], bsc, bO], [bO])
                            k.ts('dve', sch[:, 5:6], banks[wbk][:, 128:129], 1e-30, None, ALU.max, None, [bbank[wbk]], [bsc])
                            k.recip(sch[:, 6:7], sch[:, 5:6], [bsc], [bsc])
                            k.ts('dve', sch[:, 6:7], sch[:, 6:7], gts[:, qt, head * 3 + 2:head * 3 + 3], None, ALU.mult, None, [b_g, bsc], [bsc])
                            k.stt(Oacc[:, hh, :], banks[wbk][:, 0:128], sch[:, 6:7], Oacc[:, hh, :], ALU.mult, ALU.add, [bbank[wbk], bsc, bO], [bO])
                            oi = hh % 2
                            tb = tbank()
                            k.tr(banks[tb][:, 0:128], Oacc[:, hh, :], ident[:], [bO, b_const], [bbank[tb]])
                            k.cp('dve', o16[oi][:], banks[tb][:, 0:128], [bbank[tb]], [bo16[oi]])
                            k.dma('sp', OBT[head * 128:(head + 1) * 128, qsl], o16[oi][:], [bo16[oi]], ())
        if stage == 4 and DEBUG_OUT and not SKIP_NSA:
            dbg_ob = nc.dram_tensor("dbg_ob", [2048, NOWN], BF16, kind="ExternalOutput").ap()
            k.dma('sp', dbg_ob, OBT, (), ())

    def bcast_row(dst, src_fm, n, bsrc, bdst, name):
        rowd = k.dram([1, n * 128], F32, "row_" + name)
        t_ = P.dma('sp', rowd[0].rearrange("(c p) -> p c", p=128), src_fm, [bsrc], (), allow_slow_non_contiguous=True)
        brow = P.buf()
        brow.lw = t_
        k.dma('sp', dst, rowd[0].partition_broadcast(128), [brow], [bdst])

    if stage >= 5 and not SKIP_P5:
        wbg_in = din("w_branch_gdn", [1024, D])
        wbn_in = din("w_branch_nsa", [D, D])
        wout_in = din("w_out", [D, D])
        with k.scope():
            Wg = k.sb([128, 8, D], BF16, "Wg")
            Wn = k.sb([128, 16, D], BF16, "Wn")
            bWg, bWn = P.bufs(2)
            for kc in range(8):
                k.dma('pool', Wg[:, kc, :], wbg_in[kc * 128:(kc + 1) * 128, :], (), [bWg])
            for kc in range(16):
                k.dma('pool', Wn[:, kc, :], wbn_in[kc * 128:(kc + 1) * 128, :], (), [bWn])
            oat = [k.sb([128, 8, 512], BF16, "oat%d" % i) for i in range(2)]
            obt = [k.sb([128, 16, 512], BF16, "obt%d" % i) for i in range(2)]
            boat, bobt = P.bufs(2), P.bufs(2)
            ga = [k.sb([128, 512], F32, "ga%d" % i) for i in range(2)]
            gb = [k.sb([128, 512], F32, "gb%d" % i) for i in range(2)]
            y16 = [k.sb([128, 512], BF16, "y16_%d" % i) for i in range(2)]
            bga, bgb, by16 = P.bufs(2), P.bufs(2), P.bufs(2)
            OATv = OAT.rearrange("(kc p) t -> p kc t", p=128)
            OBTv = OBT.rearrange("(kc p) t -> p kc t", p=128)
            it = 0
            for tt in range(NOWN // 512):
                s2 = tt % 2
                tsl = slice(tt * 512, (tt + 1) * 512)
                k.dma('sp', oat[s2][:], OATv[:, :, tsl], (), [boat[s2]])
                k.dma('sp', obt[s2][:], OBTv[:, :, tsl], (), [bobt[s2]])
                for ct in range(16):
                    s = it % 2
                    it += 1
                    bA, bB = (0, 1) if s == 0 else (2, 3)
                    csl = slice(ct * 128, (ct + 1) * 128)
                    k.dma('sp', ga[s][:], PF[6144 + ct * 128:6144 + (ct + 1) * 128, OWN0 + tt * 512:OWN0 + (tt + 1) * 512], (), [bga[s]])
                    k.dma('sp', gb[s][:], PF[8192 + ct * 128:8192 + (ct + 1) * 128, OWN0 + tt * 512:OWN0 + (tt + 1) * 512], (), [bgb[s]])
                    k.act(ga[s][:], ga[s][:], AF.Sigmoid, [bga[s]], [bga[s]])
                    k.act(gb[s][:], gb[s][:], AF.Sigmoid, [bgb[s]], [bgb[s]])
                    for kc in range(8):
                        k.mm(banks[bA][:, :], Wg[:, kc, csl], oat[s2][:, kc, :], kc == 0, kc == 7, [bWg, boat[s2]], [bbank[bA]])
                    for kc in range(16):
                        k.mm(banks[bB][:, :], Wn[:, kc, csl], obt[s2][:, kc, :], kc == 0, kc == 15, [bWn, bobt[s2]], [bbank[bB]])
                    k.tt('dve', ga[s][:], ga[s][:], banks[bA][:, :], ALU.mult, [bga[s], bbank[bA]], [bga[s]])
                    k.tt('dve', gb[s][:], gb[s][:], banks[bB][:, :], ALU.mult, [bgb[s], bbank[bB]], [bgb[s]])
                    k.tt('pool', y16[s][:], ga[s][:], gb[s][:], ALU.add, [bga[s], bgb[s]], [by16[s]])
                    k.dma('sp', YT[csl, tsl], y16[s][:], [by16[s]], ())
        with k.scope():
            Wo = k.sb([128, 16, D], BF16, "Wo")
            g1b = k.sb([128, D], F32, "g1b")
            bWo, bg1 = P.bufs(2)
            for kc in range(16):
                k.dma('pool', Wo[:, kc, :], wout_in[kc * 128:(kc + 1) * 128, :], (), [bWo])
            bcast_row(g1b[:], modT[:, 32:48], 16, b_mod, bg1, "g1")
            yt = [k.sb([128, 16, 128], BF16, "yt%d" % i) for i in range(2)]
            xo = [k.sb([128, D], F32, "xo%d" % i) for i in range(2)]
            zt_ = [k.sb([128, 512], F32, "zt5_%d" % i) for i in range(2)]
            byt, bxo, bzt = P.bufs(2), P.bufs(2), P.bufs(2)
            YTv = YT.rearrange("(kc p) t -> p kc t", p=128)
            it = 0
            for t in range(NOWN // 128):
                s = t % 2
                k.dma('sp', yt[s][:], YTv[:, :, t * 128:(t + 1) * 128], (), [byt[s]])
                k.dma('sp', xo[s][:], xl[OWN0 + t * 128:OWN0 + (t + 1) * 128, :], (), [bxo[s]])
                for cb_ in range(4):
                    z2 = it % 2
                    bk = it % 4
                    it += 1
                    csl = slice(cb_ * 512, (cb_ + 1) * 512)
                    for kc in range(16):
                        k.mm(banks[bk][:, :], yt[s][:, kc, :], Wo[:, kc, csl], kc == 0, kc == 15, [byt[s], bWo], [bbank[bk]])
                    k.tt('dve', zt_[z2][:], banks[bk][:, :], g1b[:, csl], ALU.mult, [bbank[bk], bg1], [bzt[z2]])
                    k.tt('pool', xo[s][:, csl], xo[s][:, csl], zt_[z2][:], ALU.add, [bxo[s], bzt[z2]], [bxo[s]])
                k.dma('sp', X1[t * 128:(t + 1) * 128, :], xo[s][:], [bxo[s]], ())
        P.barrier()
        if stage == 5 and DEBUG_OUT:
            dbg_x1 = nc.dram_tensor("dbg_x1", [NOWN, D], F32, kind="ExternalOutput").ap()
            k.dma('sp', dbg_x1, X1, (), ())

    if stage == 6 and DEBUG_OUT:
        X2dbg = nc.dram_tensor("dbg_x2", [NOWN, D], F32, kind="ExternalOutput").ap()
    if stage >= 6:
        n2_in = din("norm2_w", [KC, 128])
        wq_in = din("peer_wq", [D, D])
        pk_in = [din("peer_keys1", [8, 128, 128]), din("peer_keys2", [8, 128, 128])]
        fnw6_in = din("final_norm_w", [1, D])
        H2T = k.dram([KC, 128, NOWN], BF16, "H2T")
        NEB = 64 if PEER_EB is None else PEER_EB
        with k.scope():
            A2 = k.sb([128, KC], F32, "A2")
            n2T = k.sb([128, KC], F32, "n2T")
            keysT = k.sb([128, 16, 128], BF16, "keysT")
            bA2, bn2, bg2, bfn, bkT = P.bufs(5)
            load_fm(n2T[:], n2_in, KC, bn2)
            k.stt(A2[:], modT[:, 64:80], 1.0, n2T[:], ALU.add, ALU.mult, [b_mod, bn2], [bA2])
            g2row = k.dram([1, D], F32, "row_g2")
            k.dma('sp', g2row[0].rearrange("(c p) -> p c", p=128), modT[:, 80:96], [b_mod], (), allow_slow_non_contiguous=True)
            with k.scope():
                kt_ = [k.sb([128, 128], F32, "kraw%d" % i) for i in range(2)]
                bkr = P.bufs(2)
                for hh2 in range(16):
                    s = hh2 % 2
                    k.dma('sp', kt_[s][:], pk_in[hh2 % 2][hh2 // 2], (), [bkr[s]])
                    k.tr(banks[s][:, 0:128], kt_[s][:], ident[:], [bkr[s], b_const], [bbank[s]])
                    k.cp('dve', keysT[:, hh2, :], banks[s][:, 0:128], [bbank[s]], [bkT])
            with k.scope():
                xt = [k.sb([128, D], F32, "x6t%d" % i) for i in range(2)]
                xs = [k.sb([128, D], BF16, "x6s%d" % i) for i in range(2)]
                st = [k.sb([128, 4], F32, "s6t%d" % i) for i in range(2)]
                ho = [k.sb([128, KC, 128], BF16, "h6o%d" % i) for i in range(2)]
                bxt, bxs, bst, bho = P.bufs(2), P.bufs(2), P.bufs(2), P.bufs(2)
                for t in range(NOWN // 128):
                    s = t % 2
                    k.dma('sp', xt[s][:], X1[t * 128:(t + 1) * 128, :], (), [bxt[s]])
                    k.act(xs[s][:], xt[s][:], AF.Square, [bxt[s]], [bxs[s], bst[s]], accum=st[s][:, 0:1])
                    k.ts('dve', st[s][:, 1:2], st[s][:, 0:1], 1.0 / D, 1e-6, ALU.mult, ALU.add, [bst[s]], [bst[s]])
                    k.act(st[s][:, 2:3], st[s][:, 1:2], AF.Sqrt, [bst[s]], [bst[s]])
                    k.recip(st[s][:, 3:4], st[s][:, 2:3], [bst[s]], [bst[s]])
                    k.ts('dve', xs[s][:], xt[s][:], st[s][:, 3:4], None, ALU.mult, None, [bxt[s], bst[s]], [bxs[s]])
                    for kc in range(KC):
                        pi = kc % 2
                        sl = slice((kc // 2 % 4) * 128, (kc // 2 % 4) * 128 + 128)
                        k.tr(pbf[pi][:, sl], xs[s][:, kc * 128:(kc + 1) * 128], identb[:], [bxs[s], b_const], [bpbf[pi]])
                        if kc % 2 == 0:
                            k.act(ho[s][:, kc, :], pbf[pi][:, sl], AF.Identity, [bpbf[pi], bA2, b_mod], [bho[s]],
                                  bias=modT[:, 48 + kc:49 + kc], scale=A2[:, kc:kc + 1])
                        else:
                            k.ts('dve', ho[s][:, kc, :], pbf[pi][:, sl], A2[:, kc:kc + 1], modT[:, 48 + kc:49 + kc],
                                 ALU.mult, ALU.add, [bpbf[pi], bA2, b_mod], [bho[s]])
                    k.dma('sp', H2T[:, :, t * 128:(t + 1) * 128].rearrange("c p t -> p c t"), ho[s][:], [bho[s]], ())
            P.barrier()
            wqv = wq_in.rearrange("(kc p) c -> p kc c", p=128)
            outv6 = out_d.rearrange("(t p) d -> t p d", p=128)
            for tg in range(4 if PEER_TG is None else PEER_TG):
                with k.scope():
                    h2g = k.sb([128, KC, 512], BF16, "h2g")
                    stile = k.sb([128, 4, 16, 128], F32, "stile")
                    Bt = k.sb([128, 4, 8, 128], F32, "Bt")
                    tau = k.sb([128, 4, 8], F32, "tau")
                    OUTacc = k.sb([128, 4, D], F32, "OUTacc")
                    bh2, bst_, bBt, btau, bOut = P.bufs(5)
                    k.dma('sp', h2g[:], H2T[:, :, tg * 512:(tg + 1) * 512].rearrange("c p t -> p c t"), (), [bh2])
                    k.memset('pool', OUTacc[:], 0.0, [bOut])
                    with k.scope():
                        wqb = [k.sb([128, KC, 128], BF16, "wqb%d" % i) for i in range(2)]
                        qh = [k.sb([128, 512], BF16, "qh%d" % i) for i in range(2)]
                        bwq, bqh = P.bufs(2), P.bufs(2)
                        for hh2 in range(16):
                            s = hh2 % 2
                            k.dma('pool', wqb[s][:], wqv[:, :, hh2 * 128:(hh2 + 1) * 128], (), [bwq[s]])
                            for kc in range(KC):
                                k.mm(banks[s][:, :], wqb[s][:, kc, :], h2g[:, kc, :], kc == 0, kc == KC - 1, [bwq[s], bh2], [bbank[s]])
                            k.cp('act', qh[s][:], banks[s][:, :], [bbank[s]], [bqh[s]])
                            for tl in range(4):
                                k.mm(banks[2 + s][:, tl * 128:(tl + 1) * 128], qh[s][:, tl * 128:(tl + 1) * 128], keysT[:, hh2, :], True, True,
                                     [bqh[s], bkT], [bbank[2 + s]])
                            k.cp('dve', stile[:, :, hh2, :], banks[2 + s][:, :].rearrange("p (t j) -> p t j", t=4), [bbank[2 + s]], [bst_])
                    with k.scope():
                        SC = []
                        for ci in range(2):
                            SC.append(dict(v16=k.sb([128, 2, 16], F32, "v16_%d" % ci), vv=k.sb([128, 2, 128], F32, "vv_%d" % ci),
                                           cand=k.sb([128, 16, 16], F32, "cand_%d" % ci), cv=k.sb([128, 256], F32, "cv_%d" % ci),
                                           c16=k.sb([128, 16], F32, "c16_%d" % ci), sm=k.sb([128, 8], F32, "sm_%d" % ci), b=P.buf()))

                        def p3_chain(tl, h, S_):
                            v16, vv, cand, cv, c16, sm, bs = S_["v16"], S_["vv"], S_["cand"], S_["cv"], S_["c16"], S_["sm"], S_["b"]
                            ops = []
                            for half in range(2):
                                src = stile[:, tl, 2 * h + half, :]
                                ops.append(lambda src=src, half=half: P.op('dve', lambda e: e.max(out=v16[:, half, 0:8], in_=src), [bst_], [bs]))
                            for half in range(2):
                                src = stile[:, tl, 2 * h + half, :]
                                ops.append(lambda src=src, half=half: P.op('dve', lambda e: e.match_replace(out=vv[:, half, :], in_to_replace=v16[:, half, 0:8], in_values=src, imm_value=-3.0e38), [bst_, bs], [bs]))
                            for half in range(2):
                                ops.append(lambda half=half: P.op('dve', lambda e: e.max(out=v16[:, half, 8:16], in_=vv[:, half, :]), [bs], [bs]))
                            ops.append(lambda: k.tt('dve', cand[:], v16[:, 1, :].unsqueeze(1).to_broadcast([128, 16, 16]),
                                                    v16[:, 0, :].unsqueeze(2).to_broadcast([128, 16, 16]), ALU.add, [bs], [bs]))
                            cf = cand[:].rearrange("p a b -> p (a b)")
                            ops.append(lambda: P.op('dve', lambda e: e.max(out=c16[:, 0:8], in_=cf), [bs], [bs]))
                            ops.append(lambda: P.op('dve', lambda e: e.match_replace(out=cv[:], in_to_replace=c16[:, 0:8], in_values=cf, imm_value=-3.0e38), [bs], [bs]))
                            ops.append(lambda: P.op('dve', lambda e: e.max(out=c16[:, 8:16], in_=cv[:]), [bs], [bs]))
                            ops.append(lambda: k.ts('dve', sm[:, 0:1], c16[:, 0:1], -1.0, None, ALU.mult, None, [bs], [bs]))
                            ops.append(lambda: k.act(cv[:, 0:16], c16[:], AF.Exp, [bs], [bs], bias=sm[:, 0:1], accum=sm[:, 1:2]))
                            ops.append(lambda: k.act(sm[:, 2:3], sm[:, 1:2], AF.Ln, [bs], [bs]))
                            ops.append(lambda: k.tt('dve', sm[:, 3:4], sm[:, 0:1], sm[:, 2:3], ALU.subtract, [bs], [bs]))
                            ops.append(lambda: k.ts('dve', Bt[:, tl, h, :], stile[:, tl, 2 * h, :], sm[:, 3:4], None, ALU.add, None, [bst_, bs], [bBt]))
                            ops.append(lambda: k.act(sm[:, 4:5], c16[:, 15:16], AF.Exp, [bs], [bs], bias=sm[:, 3:4]))
                            ops.append(lambda: k.ts('dve', tau[:, tl, h:h + 1], sm[:, 4:5], 0.99999, None, ALU.mult, None, [bs], [btau]))
                            return ops

                        pairs = [(tl, h) for tl in range(4) for h in range(8)]
                        for pi_ in range(0, len(pairs), 2):
                            ca = p3_chain(pairs[pi_][0], pairs[pi_][1], SC[0])
                            cb2 = p3_chain(pairs[pi_ + 1][0], pairs[pi_ + 1][1], SC[1])
                            for oa, ob_ in zip(ca, cb2):
                                oa()
                                ob_()
                    with k.scope():
                        ub = [k.sb([128, KC, 256], BF16, "ub%d" % i) for i in range(2)]
                        vb = [k.sb([128, 2, D], BF16, "vb%d" % i) for i in range(2)]
                        bub, bvb = P.bufs(2), P.bufs(2)
                        gx_ = [k.sb([128, 256], F32, "gx%d" % i) for i in range(2)]
                        gy_ = [k.sb([128, 256], F32, "gy%d" % i) for i in range(2)]
                        es_ = [k.sb([128, 8, 128], F32, "es%d" % i) for i in range(2)]
                        mk_ = [k.sb([128, 8, 128], F32, "mk%d" % i) for i in range(2)]
                        coef_ = [k.sb([128, 2, 128], F32, "coef%d" % i) for i in range(2)]
                        G16_ = [k.sb([128, 256], BF16, "G16_%d" % i) for i in range(2)]
                        GT_ = [[k.sb([128, 128], BF16, "GT%d_%d" % (p_, i)) for i in range(2)] for p_ in range(2)]
                        bgx_, bgy_, bmk_, bG16_ = P.bufs(2), P.bufs(2), P.bufs(2), P.bufs(2)
                        bes_ = [P.bufs(8) for p_ in range(2)]
                        bcoef_ = [P.bufs(2) for p_ in range(2)]
                        bOutS = [[P.buf() for c_ in range(4)] for t_ in range(4)]
                        bGT_ = [P.bufs(2) for p_ in range(2)]
                        pit = 0
                        def head(eb, tl, par, s):
                            gx, gy, coef, G16 = gx_[par], gy_[par], coef_[par], G16_[par]
                            bgx, bgy, bcoef, bG16 = bgx_[par], bgy_[par], bcoef_[par], bG16_[par]
                            for kc in range(KC):
                                k.mm(banks[par][:, 0:256], h2g[:, kc, tl * 128:(tl + 1) * 128], ub[s][:, kc, :], kc == 0, kc == KC - 1,
                                     [bh2, bub[s]], [bbank[par]])
                            k.cp('act', gx[:], banks[par][:, 0:256], [bbank[par]], [bgx])
                            k.tt('pool', gy[:], gx[:], gx[:], ALU.mult, [bgx], [bgy])
                            k.ts('pool', gy[:], gy[:], 0.044715, 1.0, ALU.mult, ALU.add, [bgy], [bgy])
                            k.tt('pool', gy[:], gy[:], gx[:], ALU.mult, [bgy, bgx], [bgy])
                            for il in range(2):
                                i_ = 2 * eb + il
                                for h in range(8):
                                    k.act(es_[il][:, h, :], stile[:, tl, 2 * h + 1, :], AF.Exp, [bst_, bBt], [bes_[il][h]], bias=Bt[:, tl, h, i_:i_ + 1])
                            k.act(gy[:], gy[:], AF.Sigmoid, [bgy], [bgy], scale=1.5957691216057308)
                            k.tt('pool', gy[:], gy[:], gx[:], ALU.mult, [bgy, bgx], [bgy])
                            taub = tau[:, tl, :].unsqueeze(2).to_broadcast([128, 8, 128])
                            for il in range(2):
                                k.tt('dve', mk_[il][:], es_[il][:], taub, ALU.is_ge, bes_[il] + [btau], [bmk_[il]])
                            for il in range(2):
                                k.tt('dve', mk_[il][:], mk_[il][:], es_[il][:], ALU.mult, [bmk_[il]] + bes_[il], [bmk_[il]])
                            for il in range(2):
                                k.tt('dve', mk_[il][:, 0:4, :], mk_[il][:, 0:4, :], mk_[il][:, 4:8, :], ALU.add, [bmk_[il]], [bmk_[il]])
                            for il in range(2):
                                k.tt('dve', mk_[il][:, 0:2, :], mk_[il][:, 0:2, :], mk_[il][:, 2:4, :], ALU.add, [bmk_[il]], [bmk_[il]])
                            for il in range(2):
                                k.tt('dve', coef[:, il, :], mk_[il][:, 0, :], mk_[il][:, 1, :], ALU.add, [bmk_[il]], [bcoef[il]])
                            k.tt('dve', G16[:], gy[:], coef[:].rearrange("p i j -> p (i j)"), ALU.mult, [bgy] + bcoef, [bG16])

                        def tail(eb, tl, par, s):
                            G16, GT, bG16, bGT = G16_[par], GT_[par], bG16_[par], bGT_[par]
                            for il in range(2):
                                k.tr(pbf[il][:, par * 128:(par + 1) * 128], G16[:, il * 128:(il + 1) * 128], identb[:], [bG16, b_const], [bpbf[il]])
                                k.cp('act', GT[il][:], pbf[il][:, par * 128:(par + 1) * 128], [bpbf[il]], [bGT[il]])
                            for cb_ in range(4):
                                bk = 2 + cb_
                                csl = slice(cb_ * 512, (cb_ + 1) * 512)
                                k.mm(banks[bk][:, :], GT[0][:], vb[s][:, 0, csl], True, False, [bGT[0], bvb[s]], [bbank[bk]])
                                k.mm(banks[bk][:, :], GT[1][:], vb[s][:, 1, csl], False, True, [bGT[1], bvb[s]], [bbank[bk]])
                                k.tt('dve', OUTacc[:, tl, csl], OUTacc[:, tl, csl], banks[bk][:, :], ALU.add, [bOut, bOutS[tl][cb_], bbank[bk]], [bOutS[tl][cb_]])

                        prev = None
                        pit = 0
                        for eb in range(NEB):
                            s = eb % 2
                            k.dma('sp', ub[s][:], UBd[eb], [bconvU[eb]], [bub[s]])
                            k.dma('sp', vb[s][:], VBd[eb], [bconvV[eb]], [bvb[s]])
                            for tl in range(4):
                                par = pit % 2
                                pit += 1
                                head(eb, tl, par, s)
                                if prev is not None:
                                    tail(*prev)
                                prev = (eb, tl, par, s)
                        tail(*prev)
                    with k.scope():
                        g2b = k.sb([128, D], F32, "g2b")
                        fnw6 = k.sb([128, D], F32, "fnw6")
                        k.dma('sp', g2b[:], g2row[0].partition_broadcast(128), (), [bg2])
                        k.dma('sp', fnw6[:], fnw6_in[0].partition_broadcast(128), (), [bfn])
                        x1t = [k.sb([128, D], F32, "x1t%d" % i) for i in range(2)]
                        fj6 = [k.sb([128, D], BF16, "fj6%d" % i) for i in range(2)]
                        fs6 = [k.sb([128, 4], F32, "fs6%d" % i) for i in range(2)]
                        bx1t, bfj6, bfs6 = P.bufs(2), P.bufs(2), P.bufs(2)
                        for tl in range(4):
                            s = tl % 2
                            t = tg * 4 + tl
                            k.dma('sp', x1t[s][:], X1[t * 128:(t + 1) * 128, :], (), [bx1t[s]])
                            k.tt('dve', OUTacc[:, tl, :], OUTacc[:, tl, :], g2b[:], ALU.mult, [bOut, bg2] + bOutS[tl], [bOut])
                            k.tt('dve', x1t[s][:], x1t[s][:], OUTacc[:, tl, :], ALU.add, [bx1t[s], bOut], [bx1t[s]])
                            if stage == 6 and DEBUG_OUT:
                                k.dma('sp', X2dbg[t * 128:(t + 1) * 128, :], x1t[s][:], [bx1t[s]], ())
                            k.act(fj6[s][:], x1t[s][:], AF.Square, [bx1t[s]], [bfj6[s], bfs6[s]], accum=fs6[s][:, 0:1])
                            k.ts('dve', fs6[s][:, 1:2], fs6[s][:, 0:1], 1.0 / D, 1e-6, ALU.mult, ALU.add, [bfs6[s]], [bfs6[s]])
                            k.act(fs6[s][:, 2:3], fs6[s][:, 1:2], AF.Sqrt, [bfs6[s]], [bfs6[s]])
                            k.recip(fs6[s][:, 3:4], fs6[s][:, 2:3], [bfs6[s]], [bfs6[s]])
                            k.stt(x1t[s][:], x1t[s][:], fs6[s][:, 3:4], fnw6[:], ALU.mult, ALU.mult, [bx1t[s], bfs6[s], bfn], [bx1t[s]])
                            k.dma('sp', outv6[t], x1t[s][:], [bx1t[s]], ())

    if stage < 6:
      pass
    fnw_in = din("final_norm_w", [1, D]) if stage < 6 else None
    fnw = k.sb([128, D], F32, "fnw")
    b_fnw = P.buf()
    if stage < 6:
        k.dma('sp', fnw[:], fnw_in[0].partition_broadcast(128), (), [b_fnw])
    X2v = xl.rearrange("(t p) d -> t p d", p=128)
    outv = out_d.rearrange("(t p) d -> t p d", p=128)
    ft = [k.sb([128, D], F32, "ft%d" % i) for i in range(2)]
    fj = [k.sb([128, D], BF16, "fj%d" % i) for i in range(2)]
    fs = [k.sb([128, 4], F32, "fs%d" % i) for i in range(2)]
    bft, bfj, bfs = P.bufs(2), P.bufs(2), P.bufs(2)
    for t in range(NOWN // 128 if stage < 6 else 0):
        s = t % 2
        k.dma('sp', ft[s][:], X2v[OWN0 // 128 + t], (), [bft[s]])
        k.act(fj[s][:], ft[s][:], AF.Square, [bft[s]], [bfj[s], bfs[s]], accum=fs[s][:, 0:1])
        k.ts('dve', fs[s][:, 1:2], fs[s][:, 0:1], 1.0 / D, 1e-6, ALU.mult, ALU.add, [bfs[s]], [bfs[s]])
        k.act(fs[s][:, 2:3], fs[s][:, 1:2], AF.Sqrt, [bfs[s]], [bfs[s]])
        k.recip(fs[s][:, 3:4], fs[s][:, 2:3], [bfs[s]], [bfs[s]])
        k.stt(ft[s][:], ft[s][:], fs[s][:, 3:4], fnw[:], ALU.mult, ALU.mult, [bft[s], bfs[s], b_fnw], [bft[s]])
        k.dma('sp', outv[t], ft[s][:], [bft[s]], ())

    with nc.Block() as block:
        P.emit(block)
    k.es.close()
    return nc


def _gconst():
    NEGM = -30000.0
    r = np.arange(128)[:, None]
    c = np.arange(128)[None, :]
    g = np.zeros((128, 5, 128), np.float32)
    g[:, 4, :] = (r // 32 == c // 32)
    g[:, 0, :] = (r <= c)
    g[:, 1, :] = np.where(r > c, 0.0, NEGM)
    g[:, 2, :] = np.where(c > r, 0.0, NEGM)
    g[:, 3, :] = np.where(c >= r, 0.0, NEGM)
    return g


GCONST = _gconst()


def _nsa_consts():
    NEGM = -30000.0
    c = {}
    n128 = np.zeros((128, 385), np.float32)
    for m in range(16):
        n128[m + 16, m] = -1.0
    for m in range(16, 32):
        n128[m - 16, m] = 1.0
    half = 16
    inv_freq = (500000.0 ** (-np.arange(half, dtype=np.float32) / half)).astype(np.float32)
    for d in range(32):
        n128[d, 128] = inv_freq[d % 16]
    r = np.arange(128)[:, None]
    q = np.arange(128)[None, :]
    n128[:, 129:257] = np.where(r <= q, 0.0, NEGM)
    n128[:, 257:385] = np.where(r > q, 0.0, NEGM)
    c["nsa_c128"] = n128
    u = np.arange(LOC)[None, :]
    c["maskB"] = np.where(16 * r + 31 <= u, 0.0, NEGM).astype(np.float32)
    c["eexp"] = (np.arange(LOC)[None, :] // 64 == np.arange(64)[:, None]).astype(np.float32)
    ovl = np.zeros((128, 2, 65), np.float32)
    for j in range(2):
        for nl in range(128):
            n = j * 128 + nl
            if n >= 255:
                continue
            for sblk in range(64):
                if 16 * n < 64 * sblk + 64 and 16 * n + 32 > 64 * sblk:
                    ovl[nl, j, sblk] = 1.0
            ovl[nl, j, 64] = 1.0
    c["ovl"] = ovl
    fvv = np.zeros((128, 2, 16, 64), np.float32)
    for qt in range(16):
        for qq in range(128):
            t = OWN0 + qt * 128 + qq
            cur = t // 64
            for sblk in range(64):
                fb = 0.0
                if sblk == cur:
                    fb = 2.0e9
                elif sblk == cur - 1:
                    fb = 3.0e9
                fvv[qq, 0, qt, sblk] = fb
                fvv[qq, 1, qt, sblk] = 3.0e38 if 64 * sblk <= t else -1.0e30
    c["fbigvis"] = fvv
    return c


NSA_CONSTS = _nsa_consts()


def _nsa_core(half):
    a = np.zeros((128, 129), np.float32)
    first = 0 if half == 1 else 32
    a[:, first] = 1.0e9
    a[:, 64:128] = 3.0e38
    if half == 0:
        a[:, 64:64 + 32] = -1.0e30
        a[:, 128] = -30000.0
    return a


def _pos_loc(inp, b, half):
    p = np.asarray(inp['positions'], np.int32)[b]
    out = np.zeros((1, LOC), np.int32)
    if half == 1:
        out[0] = p
    else:
        out[0, OWN0:] = p[:NOWN]
    return out


PEER_UT_CACHE = {}


def make_in_maps(inp):
    x = np.asarray(inp['x'], np.float32)
    c = np.asarray(inp['c'], np.float32)
    maps = []
    ident = np.eye(128, dtype=np.float32)
    for core in range(8):
        b, half = core // 2, core % 2
        xl = np.zeros((LOC, D), np.float32)
        if half == 0:
            xl[OWN0:] = x[b, :NOWN]
        else:
            xl[:] = x[b]
        m = {
            "xl": xl,
            "cb": np.ascontiguousarray(c[b].reshape(KC, 128)),
            "ada_w": np.ascontiguousarray(inp['ada_w'][0]),
            "ada_b": np.ascontiguousarray(np.asarray(inp['ada_b'][0]).reshape(96, 128)),
            "norm1_w": np.ascontiguousarray(np.asarray(inp['norm1_w'][0]).reshape(KC, 128)),
            "w_in": np.ascontiguousarray(inp['w_in'][0]),
            "ident": ident,
            "pvalid": np.full((128, 1), float(half), np.float32),
            "w_branch_gdn": np.ascontiguousarray(inp['w_branch_gdn'][0], np.float32),
            "w_branch_nsa": np.ascontiguousarray(inp['w_branch_nsa'][0], np.float32),
            "w_out": np.ascontiguousarray(inp['w_out'][0], np.float32),
            "norm2_w": np.ascontiguousarray(np.asarray(inp['norm2_w'][0], np.float32).reshape(KC, 128)),
            "peer_wq": np.ascontiguousarray(inp['peer_wq'][0], np.float32),
            "peer_keys1": np.ascontiguousarray(inp['peer_keys1'][0], np.float32),
            "peer_keys2": np.ascontiguousarray(inp['peer_keys2'][0], np.float32),
            "peer_uT": PEER_UT_CACHE.get(id(inp['peer_u'])) if id(inp['peer_u']) in PEER_UT_CACHE else PEER_UT_CACHE.setdefault(id(inp['peer_u']), np.ascontiguousarray(np.asarray(inp['peer_u'][0], np.float32).T)),
            "peer_v": np.ascontiguousarray(inp['peer_v'][0], np.float32),
            "gconst": GCONST,
            "pos": np.ascontiguousarray(_pos_loc(inp, b, half)),
            "nsa_core": _nsa_core(half),
            "cmp_pos_k": np.ascontiguousarray(inp['cmp_pos_k'][0], np.float32), "cmp_pos_v": np.ascontiguousarray(inp['cmp_pos_v'][0], np.float32),
            "cmp_w1_k": np.ascontiguousarray(inp['cmp_w1_k'][0], np.float32), "cmp_w1_v": np.ascontiguousarray(inp['cmp_w1_v'][0], np.float32),
            "cmp_w2_k": np.ascontiguousarray(inp['cmp_w2_k'][0], np.float32), "cmp_w2_v": np.ascontiguousarray(inp['cmp_w2_v'][0], np.float32),
            **NSA_CONSTS,
            "gdn_conv_w": np.ascontiguousarray(np.asarray(inp['gdn_conv_w'][0], np.float32).reshape(96, 128)),
            "gdn_A_log": np.ascontiguousarray(np.broadcast_to(np.asarray(inp['gdn_A_log'][0], np.float32)[None, :], (32, 8))),
            "gdn_dt_bias": np.ascontiguousarray(np.broadcast_to(np.asarray(inp['gdn_dt_bias'][0], np.float32)[None, :], (32, 8))),
            "gdn_norm_w": np.ascontiguousarray(np.asarray(inp['gdn_norm_w'][0], np.float32).reshape(1, 128)),
            "final_norm_w": np.ascontiguousarray(np.asarray(inp['final_norm_w'], np.float32).reshape(1, D)),
        }
        maps.append(m)
    return maps


def kernel(**inputs):
    nc = build_program(6)
    maps = make_in_maps(inputs)
    res = run_bass_kernel_spmd(nc, maps, core_ids=list(range(8)))
    out = np.zeros((4, SEQ, D), np.float32)
    for core in range(8):
        b, half = core // 2, core % 2
        out[b, half * NOWN:(half + 1) * NOWN] = res.results[core]["out"]
    return out
```
